# Optimizing a Trainium2 kernel written in Bass

```python
import math
import jax, jax.numpy as jnp
from jax import lax
import numpy as np

D_MODEL = 2048
BATCH = 8
SEQ = 4096
DEPTH = 4

MEM_LEN = 256
D_BRANCH = 1024
N_BRANCH = 4
S5_GROUP = 16
S5_GROUPS = D_BRANCH // S5_GROUP
S5_STATE = 64
ML_HEADS = 4
ML_HEAD_DIM = D_BRANCH // ML_HEADS
ML_CHUNK = 128
ML_CONV = 4
DA_HEADS = 8
DA_HEAD_DIM = 64
DA_V_DIM = 2 * DA_HEAD_DIM
DA_BLOCK = 128
XA_HEADS = 4
XA_HEAD_DIM = D_BRANCH // XA_HEADS
ROPE_THETA = 10000.0
EPS = 1e-6

IN_SPLITS = (D_BRANCH, D_BRANCH,
             D_BRANCH, D_BRANCH, D_BRANCH, D_BRANCH, D_BRANCH,
             ML_HEADS, ML_HEADS,
             D_BRANCH, D_BRANCH, D_BRANCH, D_BRANCH,
             D_BRANCH, D_BRANCH,
             N_BRANCH * D_MODEL)
D_IN = sum(IN_SPLITS)

kernel_name = 'hybrid_s5_mlstm_diffattn_memxattn'


def split_cols(y, sizes):
    idx = np.cumsum(sizes)[:-1].tolist()
    return jnp.split(y, idx, axis=-1)


def rms_norm(x, g):
    xf = x.astype(jnp.float32)
    y = xf * lax.rsqrt(jnp.mean(xf * xf, axis=-1, keepdims=True) + EPS)
    return (y * g.astype(jnp.float32)).astype(x.dtype)


def rope_tables(seq, dim):
    inv = 1.0 / (ROPE_THETA ** (jnp.arange(0, dim, 2, dtype=jnp.float32) / dim))
    ang = jnp.arange(seq, dtype=jnp.float32)[:, None] * inv[None, :]
    return jnp.cos(ang), jnp.sin(ang)


def apply_rope(x, cos, sin):
    x1, x2 = jnp.split(x.astype(jnp.float32), 2, axis=-1)
    shape = (1, cos.shape[0]) + (1,) * (x.ndim - 3) + (cos.shape[1],)
    c = cos.reshape(shape)
    s = sin.reshape(shape)
    return jnp.concatenate([x1 * c - x2 * s, x2 * c + x1 * s], axis=-1).astype(x.dtype)


def s5_mixer(u, lam_re, lam_im, log_dt, b_re, b_im, c_re, c_im, d_skip, w_glu, b_glu):
    bsz, seq, _ = u.shape
    f32 = jnp.float32
    uf = u.astype(f32)
    lam = lax.complex(jnp.minimum(lam_re.astype(f32), -1e-4), lam_im.astype(f32))
    dt = jnp.exp(log_dt.astype(f32))[:, None]
    lam_bar = jnp.exp(lam * dt)
    b = lax.complex(b_re.astype(f32), b_im.astype(f32))
    b_bar = ((lam_bar - 1.0) / lam)[..., None] * b
    ug = uf.reshape(bsz, seq, S5_GROUPS, S5_GROUP).astype(jnp.complex64)
    bu = jnp.einsum('gpc,blgc->blgp', b_bar, ug)
    a = jnp.broadcast_to(lam_bar, (1, seq) + lam_bar.shape)

    def combine(e1, e2):
        a1, x1 = e1
        a2, x2 = e2
        return a1 * a2, a2 * x1 + x2

    _, states = lax.associative_scan(combine, (a, bu), axis=1)
    c = lax.complex(c_re.astype(f32), c_im.astype(f32))
    y = jnp.real(jnp.einsum('gcp,blgp->blgc', c, states)).reshape(bsz, seq, D_BRANCH)
    y = y + d_skip.astype(f32) * uf
    y = jax.nn.gelu(y).astype(u.dtype)
    return y * jax.nn.sigmoid(y @ w_glu + b_glu)


def mlstm_mixer(q, k, v, o_pre, i_pre, f_pre, conv_w, conv_b, b_i, b_f, norm_g):
    bsz, seq, _ = q.shape
    f32 = jnp.float32
    qk = jnp.concatenate([q, k], axis=-1)
    qk = lax.conv_general_dilated(qk, conv_w[:, None, :], window_strides=(1,),
                                  padding=((ML_CONV - 1, 0),),
                                  dimension_numbers=('NWC', 'WIO', 'NWC'),
                                  feature_group_count=2 * D_BRANCH) + conv_b
    q, k = jnp.split(jax.nn.silu(qk), 2, axis=-1)
    nc = seq // ML_CHUNK

    def heads(t):
        return t.astype(f32).reshape(bsz, nc, ML_CHUNK, ML_HEADS, ML_HEAD_DIM).transpose(1, 0, 3, 2, 4)

    def gates(t):
        return t.reshape(bsz, nc, ML_CHUNK, ML_HEADS).transpose(1, 0, 3, 2)

    qh = heads(q)
    kh = heads(k) * (ML_HEAD_DIM ** -0.5)
    vh = heads(v)
    ig = gates(i_pre.astype(f32) + b_i.astype(f32))
    lf = gates(jax.nn.log_sigmoid(f_pre.astype(f32) + b_f.astype(f32)))
    causal = jnp.tril(jnp.ones((ML_CHUNK, ML_CHUNK), dtype=bool))

    def step(carry, inp):
        cmat, nvec, m = carry
        qc, kc, vc, ic, fc = inp
        acum = jnp.cumsum(fc, axis=-1)
        gtot = acum[..., -1]
        dmat = jnp.where(causal, acum[..., :, None] - acum[..., None, :] + ic[..., None, :], -jnp.inf)
        inter = acum + m[..., None]
        m_row = jnp.maximum(jnp.max(dmat, axis=-1), inter)
        s = jnp.einsum('bhqd,bhkd->bhqk', qc, kc) * jnp.exp(dmat - m_row[..., None])
        sc = jnp.exp(inter - m_row)
        num = jnp.einsum('bhqk,bhkd->bhqd', s, vc) + sc[..., None] * jnp.einsum('bhqd,bhde->bhqe', qc, cmat)
        den = jnp.sum(s, axis=-1) + sc * jnp.einsum('bhqd,bhd->bhq', qc, nvec)
        h = num / jnp.maximum(jnp.abs(den), jnp.exp(-m_row))[..., None]
        kw_log = gtot[..., None] - acum + ic
        m_new = jnp.maximum(gtot + m, jnp.max(kw_log, axis=-1))
        kw = jnp.exp(kw_log - m_new[..., None])
        decay = jnp.exp(gtot + m - m_new)
        cmat = decay[..., None, None] * cmat + jnp.einsum('bhkd,bhke->bhde', kc * kw[..., None], vc)
        nvec = decay[..., None] * nvec + jnp.einsum('bhk,bhkd->bhd', kw, kc)
        return (cmat, nvec, m_new), h

    init = (jnp.zeros((bsz, ML_HEADS, ML_HEAD_DIM, ML_HEAD_DIM), f32),
            jnp.zeros((bsz, ML_HEADS, ML_HEAD_DIM), f32),
            jnp.zeros((bsz, ML_HEADS), f32))
    _, hs = lax.scan(step, init, (qh, kh, vh, ig, lf))
    hs = hs.transpose(1, 0, 3, 2, 4).reshape(bsz, seq, ML_HEADS, ML_HEAD_DIM)
    hs = rms_norm(hs, norm_g.reshape(ML_HEADS, ML_HEAD_DIM)).reshape(bsz, seq, D_BRANCH)
    return (jax.nn.sigmoid(o_pre.astype(f32)) * hs).astype(q.dtype)


def diff_attention(q, k, v, cos, sin, lq1, lk1, lq2, lk2, subln_g, lambda_init):
    bsz, seq, _ = q.shape
    f32 = jnp.float32
    q = apply_rope(q.reshape(bsz, seq, DA_HEADS, 2, DA_HEAD_DIM), cos, sin)
    k = apply_rope(k.reshape(bsz, seq, DA_HEADS, 2, DA_HEAD_DIM), cos, sin)
    v = v.reshape(bsz, seq, DA_HEADS, DA_V_DIM)
    lam = (jnp.exp(jnp.sum(lq1.astype(f32) * lk1.astype(f32)))
           - jnp.exp(jnp.sum(lq2.astype(f32) * lk2.astype(f32))) + lambda_init)
    scale = DA_HEAD_DIM ** -0.5
    kpos = jnp.arange(seq)

    def block(j):
        start = j * DA_BLOCK
        qb = lax.dynamic_slice_in_dim(q, start, DA_BLOCK, axis=1)
        s = jnp.einsum('bqhcd,bkhcd->bhcqk', qb, k).astype(f32) * scale
        qpos = start + jnp.arange(DA_BLOCK)
        s = jnp.where(kpos[None, :] <= qpos[:, None], s, -jnp.inf)
        p = jax.nn.softmax(s, axis=-1)
        attn = (p[:, :, 0] - lam * p[:, :, 1]).astype(v.dtype)
        return jnp.einsum('bhqk,bkhd->bqhd', attn, v)

    o = lax.map(block, jnp.arange(seq // DA_BLOCK))
    o = o.transpose(1, 0, 2, 3, 4).reshape(bsz, seq, DA_HEADS, DA_V_DIM)
    o = rms_norm(o, subln_g) * (1.0 - lambda_init)
    return o.reshape(bsz, seq, D_BRANCH)


def memory_attention(q, mem_n, w_kv):
    bsz, seq, _ = q.shape
    kmem, vmem = jnp.split(mem_n @ w_kv, 2, axis=-1)
    q = q.reshape(bsz, seq, XA_HEADS, XA_HEAD_DIM)
    kmem = kmem.reshape(bsz, -1, XA_HEADS, XA_HEAD_DIM)
    vmem = vmem.reshape(bsz, -1, XA_HEADS, XA_HEAD_DIM)
    s = jnp.einsum('blhd,bmhd->bhlm', q, kmem).astype(jnp.float32) * (XA_HEAD_DIM ** -0.5)
    p = jax.nn.softmax(s, axis=-1).astype(vmem.dtype)
    return jnp.einsum('bhlm,bmhd->blhd', p, vmem).reshape(bsz, seq, D_BRANCH)


def setup_inputs(seed: int = 0) -> dict:
    key = jax.random.key(seed)
    k = jax.random.split(key, 32)
    f32 = jnp.float32

    def nrm(i, shape, scale):
        return scale * jax.random.normal(k[i], shape, f32)

    def gain(i, shape):
        return 1.0 + nrm(i, shape, 0.02)

    G, P, C = S5_GROUPS, S5_STATE, S5_GROUP
    lam_im = jnp.broadcast_to(jnp.pi * jnp.arange(P, dtype=f32), (DEPTH, G, P)) + nrm(5, (DEPTH, G, P), 0.01)
    return {
        'x': nrm(0, (BATCH, SEQ, D_MODEL), 1.0),
        'mem': nrm(1, (BATCH, MEM_LEN, D_MODEL), 1.0),
        'g_pre': gain(2, (DEPTH, D_MODEL)),
        'w_in': nrm(3, (DEPTH, D_MODEL, D_IN), D_MODEL ** -0.5),
        's5_lam_re': -0.5 + nrm(4, (DEPTH, G, P), 0.01),
        's5_lam_im': lam_im,
        's5_log_dt': jax.random.uniform(k[6], (DEPTH, G), f32, minval=math.log(1e-3), maxval=math.log(1e-1)),
        's5_b_re': nrm(7, (DEPTH, G, P, C), (2 * C) ** -0.5),
        's5_b_im': nrm(8, (DEPTH, G, P, C), (2 * C) ** -0.5),
        's5_c_re': nrm(9, (DEPTH, G, C, P), (2 * P) ** -0.5),
        's5_c_im': nrm(10, (DEPTH, G, C, P), (2 * P) ** -0.5),
        's5_d': nrm(11, (DEPTH, D_BRANCH), 1.0),
        's5_w_glu': nrm(12, (DEPTH, D_BRANCH, D_BRANCH), D_BRANCH ** -0.5),
        's5_b_glu': nrm(13, (DEPTH, D_BRANCH), 0.01),
        'ml_conv_w': nrm(14, (DEPTH, ML_CONV, 2 * D_BRANCH), ML_CONV ** -0.5),
        'ml_conv_b': nrm(15, (DEPTH, 2 * D_BRANCH), 0.01),
        'ml_b_i': nrm(16, (DEPTH, ML_HEADS), 0.1),
        'ml_b_f': jnp.linspace(3.0, 6.0, ML_HEADS, dtype=f32)[None, :] + nrm(17, (DEPTH, ML_HEADS), 0.01),
        'ml_norm_g': gain(18, (DEPTH, D_BRANCH)),
        'da_lq1': nrm(19, (DEPTH, DA_HEAD_DIM), 0.1),
        'da_lk1': nrm(20, (DEPTH, DA_HEAD_DIM), 0.1),
        'da_lq2': nrm(21, (DEPTH, DA_HEAD_DIM), 0.1),
        'da_lk2': nrm(22, (DEPTH, DA_HEAD_DIM), 0.1),
        'da_subln_g': gain(23, (DEPTH, DA_V_DIM)),
        'g_mem': gain(24, (DEPTH, D_MODEL)),
        'xa_w_kv': nrm(25, (DEPTH, D_MODEL, 2 * D_BRANCH), D_MODEL ** -0.5),
        'w_branch': nrm(26, (DEPTH, N_BRANCH, D_BRANCH, D_MODEL), D_BRANCH ** -0.5),
        'w_out': nrm(27, (DEPTH, D_MODEL, D_MODEL), D_MODEL ** -0.5),
        'g_post': gain(28, (DEPTH, D_MODEL)),
    }


def reference(x, mem, g_pre, w_in, s5_lam_re, s5_lam_im, s5_log_dt, s5_b_re, s5_b_im, s5_c_re, s5_c_im,
              s5_d, s5_w_glu, s5_b_glu, ml_conv_w, ml_conv_b, ml_b_i, ml_b_f, ml_norm_g,
              da_lq1, da_lk1, da_lq2, da_lk2, da_subln_g, g_mem, xa_w_kv, w_branch, w_out, g_post):
    bsz, seq, _ = x.shape
    cos, sin = rope_tables(seq, DA_HEAD_DIM)
    for l in range(DEPTH):
        lambda_init = 0.8 - 0.6 * math.exp(-0.3 * l)
        h = rms_norm(x, g_pre[l])
        (s5_u, s5_z, ml_q, ml_k, ml_v, ml_o, ml_z, ml_i, ml_f,
         da_q, da_k, da_v, da_z, xa_q, xa_z, gate_pre) = split_cols(h @ w_in[l], IN_SPLITS)
        y_s5 = s5_mixer(s5_u, s5_lam_re[l], s5_lam_im[l], s5_log_dt[l], s5_b_re[l], s5_b_im[l],
                        s5_c_re[l], s5_c_im[l], s5_d[l], s5_w_glu[l], s5_b_glu[l]) * jax.nn.silu(s5_z)
        y_ml = mlstm_mixer(ml_q, ml_k, ml_v, ml_o, ml_i, ml_f, ml_conv_w[l], ml_conv_b[l],
                           ml_b_i[l], ml_b_f[l], ml_norm_g[l]) * jax.nn.silu(ml_z)
        y_da = diff_attention(da_q, da_k, da_v, cos, sin, da_lq1[l], da_lk1[l], da_lq2[l], da_lk2[l],
                              da_subln_g[l], lambda_init) * jax.nn.silu(da_z)
        y_xa = memory_attention(xa_q, rms_norm(mem, g_mem[l]), xa_w_kv[l]) * jax.nn.silu(xa_z)
        gates = jax.nn.sigmoid(gate_pre.reshape(bsz, seq, N_BRANCH, D_MODEL))
        branches = (y_s5, y_ml, y_da, y_xa)
        merged = gates[:, :, 0] * (branches[0] @ w_branch[l, 0])
        for b in range(1, N_BRANCH):
            merged = merged + gates[:, :, b] * (branches[b] @ w_branch[l, b])
        x = x + rms_norm(merged @ w_out[l], g_post[l])
    return x
```

```python
import math
from contextlib import ExitStack

import numpy as np
import ml_dtypes
import concourse.bass as bass
import concourse.mybir as mybir
from concourse.bass_utils import run_bass_kernel_spmd

F32 = mybir.dt.float32
BF16 = mybir.dt.bfloat16
AF = mybir.ActivationFunctionType
ALU = mybir.AluOpType
AX = mybir.AxisListType

D = 2048
DB = 1024
MEM = 256
NG = 64
D_IN = 21512
EPS = 1e-6
SAME_ENGINE_SYNC = True

C_S5U, C_S5Z = 0, 1024
C_MLQ, C_MLK, C_MLV, C_MLO, C_MLZ = 2048, 3072, 4096, 5120, 6144
C_MLIF = 7168
C_DAQ, C_DAK, C_DAV, C_DAZ = 7176, 8200, 9224, 10248
C_XAQ, C_XAZ = 11272, 12296
C_GATE = 13320


class Obj:
    __slots__ = ("lw", "rd", "name")

    def __init__(self, name=""):
        self.lw = None
        self.rd = {}
        self.name = name


class Tile:
    def __init__(self, h, name):
        self.h = h
        self.o = Obj(name)

    def __getitem__(self, k):
        return self.h[k]


class Ring:
    def __init__(self, tiles):
        self.tiles = tiles
        self.i = 0

    def next(self):
        t = self.tiles[self.i]
        self.i = (self.i + 1) % len(self.tiles)
        return t


class DT:
    def __init__(self, nc, name, shape, dtype, kind="Internal"):
        self.h = nc.dram_tensor(name, list(shape), dtype, kind=kind)
        self.ap = self.h.ap()
        self.objs = {}
        self.name = name

    def o(self, key=0):
        if key not in self.objs:
            self.objs[key] = Obj(f"{self.name}:{key}")
        return self.objs[key]


class Sched:
    def __init__(self, nc, stack, n_sp=40, n_pool=8, n_act=4):
        self.nc = nc
        self.eng = {"pe": nc.tensor, "act": nc.scalar, "dve": nc.vector, "pool": nc.gpsimd, "sp": nc.sync}
        self.semobj = {}
        for e in ("pe", "act", "dve", "pool"):
            self.semobj[e] = stack.enter_context(nc.semaphore("s_" + e))
        self.tick = {e: 0 for e in ("pe", "act", "dve", "pool")}
        self.waited = {e: {} for e in self.eng}
        self.dq = {"sp": n_sp, "pool": n_pool, "act": n_act}
        self.dnext = {q: 0 for q in self.dq}
        self.dcnt = {}
        for q, n in self.dq.items():
            for i in range(n):
                self.semobj[(q, i)] = stack.enter_context(nc.semaphore(f"d_{q}{i}"))
                self.dcnt[(q, i)] = 0
        self.n_inst = 0
        self.released = {}

    def _deps(self, reads, writes):
        deps = []
        for t in reads:
            o = t.o if isinstance(t, Tile) else t
            if o.lw is not None:
                deps.append(o.lw)
        for t in writes:
            o = t.o if isinstance(t, Tile) else t
            if o.lw is not None:
                deps.append(o.lw)
            deps.extend(o.rd.items())
        return deps

    def _wait(self, e, deps):
        w = self.waited[e]
        need = {}
        for sk, val in deps:
            if w.get(sk, 0) < val and need.get(sk, 0) < val:
                need[sk] = val
        for sk, val in need.items():
            w[sk] = val
            self.eng[e].wait_ge(self.semobj[sk], val)
            self.n_inst += 1

    def _mark(self, tok, reads, writes):
        for t in writes:
            o = t.o if isinstance(t, Tile) else t
            o.lw = tok
            o.rd = {}
        for t in reads:
            o = t.o if isinstance(t, Tile) else t
            if o.rd.get(tok[0], 0) < tok[1]:
                o.rd[tok[0]] = tok[1]

    def op(self, e, fn, reads=(), writes=()):
        deps = self._deps(reads, writes)
        if e == "pe" or not SAME_ENGINE_SYNC:
            deps = [d for d in deps if d[0] != e]
        self._wait(e, deps)
        self.tick[e] += 1
        fn(self.eng[e]).then_inc(self.semobj[e], 1)
        self.n_inst += 1
        self._mark((e, self.tick[e]), reads, writes)

    def mm(self, items, reads=(), writes=(), skip=False):
        deps = [d for d in self._deps(reads, writes) if d[0] != "pe"]
        self._wait("pe", deps)
        n = len(items)
        for i, (out, lhsT, rhs, st, sp) in enumerate(items):
            if skip:
                ins = self.nc.tensor.matmul(out, lhsT, rhs, start=st, stop=sp, skip_group_check=True)
            else:
                ins = self.nc.tensor.matmul(out, lhsT, rhs, start=st, stop=sp)
            self.n_inst += 1
            if i == n - 1:
                self.tick["pe"] += 1
                ins.then_inc(self.semobj["pe"], 1)
        self._mark(("pe", self.tick["pe"]), reads, writes)

    def transpose(self, out, in_, ident, reads=(), writes=()):
        self.op("pe", lambda be: be.transpose(out, in_, ident), reads=reads, writes=writes)

    def dma(self, q, out, in_, reads=(), writes=(), slow=False):
        i = self.dnext[q]
        self.dnext[q] = (i + 1) % self.dq[q]
        sk = (q, i)
        prev = self.dcnt[sk]
        deps = self._deps(reads, writes)
        if q in self.tick and not SAME_ENGINE_SYNC:
            deps = [d for d in deps if d[0] != q]
        if prev > 0:
            deps.append((sk, prev))
        self._wait(q, deps)
        self.dcnt[sk] = prev + 16
        if slow:
            self.eng[q].dma_start(out=out, in_=in_, allow_slow_non_contiguous=True).then_inc(self.semobj[sk], 16)
        else:
            self.eng[q].dma_start(out=out, in_=in_).then_inc(self.semobj[sk], 16)
        self.n_inst += 1
        self._mark((sk, prev + 16), reads, writes)

    def final_wait(self, e, objs):
        deps = []
        for o in objs:
            if o.lw is not None:
                deps.append(o.lw)
        self._wait(e, deps)


class Scope(ExitStack):
    def __init__(self, S):
        super().__init__()
        self.S = S
        self.tiles = []

    def __exit__(self, *a):
        rel = self.S.released
        for t in self.tiles:
            o = t.o
            if o.lw is not None and rel.get(o.lw[0], 0) < o.lw[1]:
                rel[o.lw[0]] = o.lw[1]
            for k, v in o.rd.items():
                if rel.get(k, 0) < v:
                    rel[k] = v
        return super().__exit__(*a)


_UID = [0]


def _uname(name):
    _UID[0] += 1
    return f"{name}_{_UID[0]}"


def _new_tile(S, stack, h, name):
    t = Tile(h, name)
    t.o.rd = dict(S.released)
    stack.tiles.append(t)
    return t


def sb(S, stack, name, shape, dtype):
    return _new_tile(S, stack, stack.enter_context(S.nc.sbuf_tensor(_uname(name), list(shape), dtype)), name)


def ps(S, stack, name, shape, dtype=F32):
    return _new_tile(S, stack, stack.enter_context(S.nc.psum_tensor(_uname(name), list(shape), dtype)), name)


def sb_ring(S, stack, name, shape, dtype, n):
    return Ring([sb(S, stack, f"{name}{i}", shape, dtype) for i in range(n)])


def ps_ring(S, stack, name, shape, dtype, n):
    return Ring([ps(S, stack, f"{name}{i}", shape, dtype) for i in range(n)])


class Ctx:
    pass


def declare(nc, L, depth, debug):
    c = Ctx()
    c.L, c.depth = L, depth
    kin = "ExternalInput"
    c.x = DT(nc, "x", [L, D], F32, kin)
    c.mem = DT(nc, "mem", [MEM, D], F32, kin)
    shapes = dict(
        g_pre=[depth, D], w_in=[depth, D, D_IN], s5_lam_re=[depth, NG, 64], s5_lam_im=[depth, NG, 64],
        s5_log_dt=[depth, NG], s5_b_re=[depth, NG, 64, 16], s5_b_im=[depth, NG, 64, 16],
        s5_c_re=[depth, NG, 16, 64], s5_c_im=[depth, NG, 16, 64], s5_d=[depth, DB],
        s5_w_glu=[depth, DB, DB], s5_b_glu=[depth, DB], ml_conv_w=[depth, 4, 2 * DB], ml_conv_b=[depth, 2 * DB],
        ml_b_i=[depth, 4], ml_b_f=[depth, 4], ml_norm_g=[depth, DB], da_lq1=[depth, 64], da_lk1=[depth, 64],
        da_lq2=[depth, 64], da_lk2=[depth, 64], da_subln_g=[depth, 128], g_mem=[depth, D],
        xa_w_kv=[depth, D, 2 * DB], w_branch=[depth, 4, DB, D], w_out=[depth, D, D], g_post=[depth, D])
    c.w = {k: DT(nc, k, v, F32, kin) for k, v in shapes.items()}
    c.ident = DT(nc, "c_ident", [128, 128], BF16, kin)
    c.identf = DT(nc, "c_identf", [128, 128], F32, kin)
    c.trow = DT(nc, "c_trow", [1, L], F32, kin)
    c.ropec = DT(nc, "c_ropec", [128, L], BF16, kin)
    c.ropes = DT(nc, "c_ropes", [128, L], BF16, kin)
    c.maskkq = DT(nc, "c_maskkq", [128, 128], BF16, kin)
    c.maskg = DT(nc, "c_maskg", [128, 8], F32, kin)
    c.mask2 = DT(nc, "c_mask2", [128, 8, 128], F32, kin)
    c.out = DT(nc, "out", [L, D], F32, "ExternalOutput")
    sk = "ExternalOutput" if debug else "Internal"
    c.dbg = debug
    c.xs = [DT(nc, f"xs{i}", [L, D], F32, sk) for i in range(2)]
    c.s5uT = DT(nc, "s5uT", [DB, L], BF16, sk)
    c.s5zT = DT(nc, "s5zT", [DB, L], BF16, sk)
    c.mlqT = DT(nc, "mlqT", [DB, L], BF16, sk)
    c.mlkT = DT(nc, "mlkT", [DB, L], BF16, sk)
    c.mlv = DT(nc, "mlv", [L, DB], BF16, sk)
    c.mlo = DT(nc, "mlo", [L, DB], BF16, sk)
    c.mlz = DT(nc, "mlz", [L, DB], BF16, sk)
    c.mlif = DT(nc, "mlif", [8, L], F32, sk)
    c.daqT = DT(nc, "daqT", [DB, L], BF16, sk)
    c.dakT = DT(nc, "dakT", [DB, L], BF16, sk)
    c.dav = DT(nc, "dav", [L, DB], BF16, sk)
    c.daz = DT(nc, "daz", [L, DB], BF16, sk)
    c.xaqT = DT(nc, "xaqT", [DB, L], BF16, sk)
    c.xaz = DT(nc, "xaz", [L, DB], BF16, sk)
    c.gT = DT(nc, "gT", [4 * D, L], BF16, sk)
    c.yT = [DT(nc, f"yT{b}", [DB, L], BF16, sk) for b in range(4)]
    c.mT = DT(nc, "mT", [D, L], BF16, sk)
    c.mlg = DT(nc, "mlg", [3, 4, L + 1], F32, "Internal")
    c.s5yT = DT(nc, "s5yT", [DB, L], BF16, sk)
    return c


def bcast_rows(ap_row, nparts):
    return ap_row.partition_broadcast(nparts)


def phase_norm_T(S, c, src, g_row_ap, hT, L, rows=None, tag="a"):
    nc = S.nc
    rows = L if rows is None else rows
    with Scope(S) as st:
        gb = sb(S, st, tag + "_gb", [128, D], F32)
        S.dma("sp", gb[:], bcast_rows(g_row_ap, 128), writes=[gb])
        idt = sb(S, st, tag + "_id", [128, 128], BF16)
        S.dma("sp", idt[:], c.ident.ap[:, :], writes=[idt])
        xr = sb_ring(S, st, tag + "_x", [128, D], F32, 2)
        jr = sb_ring(S, st, tag + "_j", [128, D], F32, 1)
        hr = sb_ring(S, st, tag + "_h", [128, D], BF16, 2)
        ssr = sb_ring(S, st, tag + "_ss", [128, 4], F32, 2)
        pr = ps_ring(S, st, tag + "_p", [128, 512], BF16, 4)
        for t in range(rows // 128):
            xt = xr.next()
            S.dma("sp", xt[:], src.ap[t * 128:(t + 1) * 128, :], reads=[src.o(t)], writes=[xt])
            jk = jr.next()
            ss = ssr.next()
            S.op("act", lambda be: be.activation(out=jk[:], in_=xt[:], func=AF.Square, accum_out=ss[:, 0:1]),
                 reads=[xt], writes=[jk, ss])
            S.op("dve", lambda be: be.tensor_scalar(ss[:, 1:2], ss[:, 0:1], 1.0 / D, EPS, op0=ALU.mult, op1=ALU.add),
                 reads=[ss], writes=[ss])
            S.op("act", lambda be: be.activation(out=ss[:, 2:3], in_=ss[:, 1:2], func=AF.Sqrt), reads=[ss], writes=[ss])
            S.op("dve", lambda be: be.reciprocal(ss[:, 3:4], ss[:, 2:3]), reads=[ss], writes=[ss])
            ht = hr.next()
            S.op("dve", lambda be: be.scalar_tensor_tensor(out=ht[:], in0=xt[:], scalar=ss[:, 3:4], in1=gb[:],
                                                           op0=ALU.mult, op1=ALU.mult),
                 reads=[xt, ss, gb], writes=[ht])
            for q in range(4):
                pt = pr.next()
                for j in range(4):
                    kc = q * 4 + j
                    S.transpose(pt[:, j * 128:(j + 1) * 128], ht[:, kc * 128:(kc + 1) * 128], idt[:],
                                reads=[ht, idt], writes=[pt])
                eng = "act" if q % 2 else "dve"
                dst = hT[:, q * 4:(q + 1) * 4, t * 128:(t + 1) * 128]
                srcp = pt[:].rearrange("p (j n) -> p j n", j=4)
                if eng == "act":
                    S.op("act", lambda be: be.copy(out=dst, in_=srcp), reads=[pt], writes=[hT])
                else:
                    S.op("dve", lambda be: be.tensor_copy(dst, srcp), reads=[pt], writes=[hT])


def phase_inproj(S, c, l, hT, L):
    nc = S.nc
    w_in = c.w["w_in"].ap
    NT, NC = L // 128, L // 512
    with Scope(S) as st:
        wr = sb_ring(S, st, "ip_w", [128, 16, 512], BF16, 2)
        wsw = sb(S, st, "ip_wsw", [128, 16, 512], BF16)
        pr = ps_ring(S, st, "ip_p", [128, 512], F32, 6)
        sbf = sb_ring(S, st, "ip_sb", [128, 512], BF16, 4)
        sf32 = sb_ring(S, st, "ip_sf", [128, 512], F32, 2)
        pre = sb_ring(S, st, "ip_pre", [128, 3 + 512], F32, 2)
        acc = sb_ring(S, st, "ip_acc", [128, 512], F32, 2)
        rcr = sb_ring(S, st, "ip_rc", [128, 512], BF16, 2)
        rsr = sb_ring(S, st, "ip_rs", [128, 512], BF16, 2)
        cw = sb(S, st, "ip_cw", [128, 4, 16], F32)
        cb = sb(S, st, "ip_cb", [128, 16], F32)
        for j in range(4):
            S.dma("sp", cw[:, j, :], c.w["ml_conv_w"].ap[l, j].rearrange("(b p) -> p b", p=128), writes=[cw], slow=True)
        S.dma("sp", cb[:], c.w["ml_conv_b"].ap[l].rearrange("(b p) -> p b", p=128), writes=[cb], slow=True)

        def load_w(c0, n):
            wc = wr.next()
            S.dma("pool", wc[:, :, :n], w_in[l, :, c0:c0 + n].rearrange("(k p) n -> p k n", p=128), writes=[wc])
            return wc

        def tok_major(c0, dst, func):
            for cc in range(0, DB, 512):
                wc = load_w(c0 + cc, 512)
                for t in range(NT):
                    p = pr.next()
                    S.mm([(p[:], hT[:, kc, t * 128:(t + 1) * 128], wc[:, kc, :], kc == 0, kc == 15) for kc in range(16)],
                         reads=[hT, wc], writes=[p])
                    s = sbf.next()
                    if func is None:
                        S.op("dve", lambda be: be.tensor_copy(s[:], p[:]), reads=[p], writes=[s])
                    else:
                        S.op("act", lambda be: be.activation(out=s[:], in_=p[:], func=func), reads=[p], writes=[s])
                    S.dma("sp", dst.ap[t * 128:(t + 1) * 128, cc:cc + 512], s[:], reads=[s], writes=[dst.o(t)])

        def feat_major(c0, ncols, dst, row0, kind, cbase=0):
            for cc in range(0, ncols, 512):
                n = min(512, ncols - cc)
                wc = load_w(c0 + cc, n)
                if kind == "rope":
                    srcv = w_in[l, :, c0 + cc:c0 + cc + n].rearrange("(k p) (b h d) -> p k b h d", p=128, h=2, d=32)
                    dstv = wsw[:, :, :n].rearrange("p k (b h d) -> p k b h d", h=2, d=32)
                    for kc in range(16):
                        S.dma("pool", dstv[:, kc, :, 0, :], srcv[:, kc, :, 1, :], writes=[wsw])
                        S.dma("pool", dstv[:, kc, :, 1, :], srcv[:, kc, :, 0, :], writes=[wsw])
                for j in range(n // 128):
                    blk = (cc // 128) + j
                    prev = None
                    for tc in range(NC):
                        tsl = slice(tc * 512, (tc + 1) * 512)
                        p = pr.next()
                        S.mm([(p[:], wc[:, kc, j * 128:(j + 1) * 128], hT[:, kc, tsl], kc == 0, kc == 15)
                              for kc in range(16)], reads=[hT, wc], writes=[p])
                        s = sbf.next()
                        if kind == "copy":
                            S.op("dve", lambda be: be.tensor_copy(s[:], p[:]), reads=[p], writes=[s])
                        elif kind in ("silu", "sigmoid"):
                            f = AF.Silu if kind == "silu" else AF.Sigmoid
                            S.op("act", lambda be: be.activation(out=s[:], in_=p[:], func=f), reads=[p], writes=[s])
                        elif kind in ("conv", "convk"):
                            cblk = cbase + blk
                            pt = pre.next()
                            if prev is None:
                                S.op("pool", lambda be: be.memset(pt[:, 0:3], 0.0), writes=[pt])
                            else:
                                pv = prev
                                S.op("pool", lambda be: be.tensor_copy(pt[:, 0:3], pv[:, 512:515]), reads=[pv], writes=[pt])
                            S.op("act", lambda be: be.copy(out=pt[:, 3:515], in_=p[:]), reads=[p], writes=[pt])
                            a = acc.next()
                            S.op("dve", lambda be: be.tensor_scalar(a[:], pt[:, 3:515], cw[:, 3, cblk:cblk + 1], cb[:, cblk:cblk + 1],
                                                                   op0=ALU.mult, op1=ALU.add), reads=[pt, cw, cb], writes=[a])
                            for tap in range(3):
                                S.op("dve", lambda be: be.scalar_tensor_tensor(out=a[:], in0=pt[:, tap:tap + 512],
                                                                               scalar=cw[:, tap, cblk:cblk + 1], in1=a[:],
                                                                               op0=ALU.mult, op1=ALU.add),
                                     reads=[pt, cw, a], writes=[a])
                            if kind == "conv":
                                S.op("act", lambda be: be.activation(out=s[:], in_=a[:], func=AF.Silu), reads=[a], writes=[s])
                            else:
                                a2 = sf32.next()
                                S.op("act", lambda be: be.activation(out=a2[:], in_=a[:], func=AF.Silu), reads=[a], writes=[a2])
                                S.op("pool", lambda be: be.tensor_scalar(s[:], a2[:], 0.0625, None, op0=ALU.mult),
                                     reads=[a2], writes=[s])
                            prev = pt
                        elif kind == "rope":
                            p2 = pr.next()
                            S.mm([(p2[:], wsw[:, kc, j * 128:(j + 1) * 128], hT[:, kc, tsl], kc == 0, kc == 15)
                                  for kc in range(16)], reads=[hT, wsw], writes=[p2])
                            a = acc.next()
                            a2 = sf32.next()
                            rc, rs = rcr.next(), rsr.next()
                            S.dma("sp", rc[:], c.ropec.ap[:, tsl], writes=[rc])
                            S.dma("sp", rs[:], c.ropes.ap[:, tsl], writes=[rs])
                            S.op("dve", lambda be: be.tensor_tensor(a[:], p[:], rc[:], op=ALU.mult), reads=[p, rc], writes=[a])
                            S.op("dve", lambda be: be.tensor_tensor(a2[:], p2[:], rs[:], op=ALU.mult), reads=[p2, rs], writes=[a2])
                            S.op("pool", lambda be: be.tensor_tensor(s[:], a[:], a2[:], op=ALU.add), reads=[a, a2], writes=[s])
                        r0 = row0 + blk * 128
                        S.dma("sp", dst.ap[r0:r0 + 128, tsl], s[:], reads=[s], writes=[dst.o((r0 // 128, tc))])

        feat_major(C_S5U, DB, c.s5uT, 0, "copy")
        feat_major(C_S5Z, DB, c.s5zT, 0, "silu")
        feat_major(C_MLQ, DB, c.mlqT, 0, "conv", cbase=0)
        feat_major(C_MLK, DB, c.mlkT, 0, "convk", cbase=8)
        tok_major(C_MLV, c.mlv, None)
        tok_major(C_MLO, c.mlo, AF.Sigmoid)
        tok_major(C_MLZ, c.mlz, AF.Silu)
        wc = load_w(C_MLIF, 8)
        for tc in range(NC):
            tsl = slice(tc * 512, (tc + 1) * 512)
            p = pr.next()
            S.mm([(p[0:8, :], wc[:, kc, 0:8], hT[:, kc, tsl], kc == 0, kc == 15) for kc in range(16)],
                 reads=[hT, wc], writes=[p])
            a = acc.next()
            S.op("dve", lambda be: be.tensor_copy(a[0:8, :], p[0:8, :]), reads=[p], writes=[a])
            S.dma("sp", c.mlif.ap[:, tsl], a[0:8, :], reads=[a], writes=[c.mlif.o(tc)])
        feat_major(C_DAQ, DB, c.daqT, 0, "rope")
        feat_major(C_DAK, DB, c.dakT, 0, "rope")
        tok_major(C_DAV, c.dav, None)
        tok_major(C_DAZ, c.daz, AF.Silu)
        feat_major(C_XAQ, DB, c.xaqT, 0, "copy")
        tok_major(C_XAZ, c.xaz, AF.Silu)
        feat_major(C_GATE, 4 * D, c.gT, 0, "sigmoid")


def emit_yT(S, ytoks, dst, row0, nblk, tsl, key_tc, idt, ptr, ysbr):
    ysb = ysbr.next()
    for blk in range(nblk):
        pt = ptr.next()
        for qt in range(4):
            S.transpose(pt[:, qt * 128:(qt + 1) * 128], ytoks[qt][:, blk * 128:(blk + 1) * 128], idt[:],
                        reads=[ytoks[qt], idt], writes=[pt])
        if blk % 2:
            S.op("act", lambda be: be.copy(out=ysb[:, blk, :], in_=pt[:]), reads=[pt], writes=[ysb])
        else:
            S.op("dve", lambda be: be.tensor_copy(ysb[:, blk, :], pt[:]), reads=[pt], writes=[ysb])
    S.dma("sp", dst.ap[row0:row0 + nblk * 128, tsl].rearrange("(k p) n -> p k n", p=128), ysb[:, 0:nblk, :],
          reads=[ysb], writes=[dst.o((r, key_tc)) for r in range(row0 // 128, row0 // 128 + nblk)])


def phase_merge(S, c, l, L):
    NC = L // 512
    wb = c.w["w_branch"].ap
    with Scope(S) as st:
        wq = sb(S, st, "mg_w", [128, 32, 512], BF16)
        yr = sb_ring(S, st, "mg_y", [128, 32, 512], BF16, 2)
        gr = sb_ring(S, st, "mg_g", [128, 4, 512], BF16, 2)
        tr = sb_ring(S, st, "mg_t", [128, 4, 512], F32, 2)
        ar = sb_ring(S, st, "mg_a", [128, 2, 512], F32, 2)
        orr = sb_ring(S, st, "mg_o", [128, 512], BF16, 2)
        pr = ps_ring(S, st, "mg_p", [128, 512], F32, 8)
        gview = c.gT.ap.rearrange("(b r) n -> r b n", b=4)
        for dq in range(4):
            for b in range(4):
                S.dma("pool", wq[:, b * 8:(b + 1) * 8, :],
                      wb[l, b, :, dq * 512:(dq + 1) * 512].rearrange("(k p) n -> p k n", p=128), writes=[wq])
            for tc in range(NC):
                tsl = slice(tc * 512, (tc + 1) * 512)
                yt = yr.next()
                for b in range(4):
                    S.dma("sp", yt[:, b * 8:(b + 1) * 8, :], c.yT[b].ap[:, tsl].rearrange("(k p) n -> p k n", p=128),
                          reads=[c.yT[b].o((r, tc)) for r in range(8)], writes=[yt])
                for j in range(4):
                    r0 = dq * 512 + j * 128
                    gt = gr.next()
                    S.dma("sp", gt[:], gview[r0:r0 + 128, :, tsl],
                          reads=[c.gT.o(((b * D + r0) // 128, tc)) for b in range(4)], writes=[gt])
                    tt = tr.next()
                    for b in range(4):
                        p = pr.next()
                        S.mm([(p[:], wq[:, b * 8 + kc, j * 128:(j + 1) * 128], yt[:, b * 8 + kc, :], kc == 0, kc == 7)
                              for kc in range(8)], reads=[wq, yt], writes=[p])
                        S.op("dve", lambda be: be.tensor_tensor(tt[:, b, :], p[:], gt[:, b, :], op=ALU.mult),
                             reads=[p, gt], writes=[tt])
                    a = ar.next()
                    S.op("pool", lambda be: be.tensor_tensor(a[:], tt[:, 0:2, :], tt[:, 2:4, :], op=ALU.add), reads=[tt], writes=[a])
                    o = orr.next()
                    S.op("pool", lambda be: be.tensor_tensor(o[:], a[:, 0, :], a[:, 1, :], op=ALU.add), reads=[a], writes=[o])
                    S.dma("sp", c.mT.ap[r0:r0 + 128, tsl], o[:], reads=[o], writes=[c.mT.o((r0 // 128, tc))])


def phase_out(S, c, l, L, xin, xout):
    NT = L // 128
    with Scope(S) as st:
        wo = sb(S, st, "po_w", [128, 16, D], BF16)
        for q in range(4):
            S.dma("pool", wo[:, :, q * 512:(q + 1) * 512],
                  c.w["w_out"].ap[l, :, q * 512:(q + 1) * 512].rearrange("(k p) n -> p k n", p=128), writes=[wo])
        gp = sb(S, st, "po_g", [128, D], F32)
        S.dma("sp", gp[:], c.w["g_post"].ap[l:l + 1, :].partition_broadcast(128), writes=[gp])
        mr = sb_ring(S, st, "po_m", [128, 16, 128], BF16, 2)
        xr = sb_ring(S, st, "po_x", [128, D], F32, 2)
        orr = sb_ring(S, st, "po_o", [128, D], F32, 2)
        jr = sb_ring(S, st, "po_j", [128, 512], F32, 2)
        ssr = sb_ring(S, st, "po_ss", [128, 8], F32, 2)
        pr = ps_ring(S, st, "po_p", [128, 512], F32, 8)
        for t in range(NT):
            mt = mr.next()
            S.dma("sp", mt[:], c.mT.ap[:, t * 128:(t + 1) * 128].rearrange("(k p) n -> p k n", p=128),
                  reads=[c.mT.o((r, t // 4)) for r in range(16)], writes=[mt])
            xt = xr.next()
            S.dma("sp", xt[:], xin.ap[t * 128:(t + 1) * 128, :], reads=[xin.o(t)], writes=[xt])
            ss = ssr.next()
            pp = []
            for n in range(4):
                p = pr.next()
                pp.append(p)
                S.mm([(p[:], mt[:, kc, :], wo[:, kc, n * 512:(n + 1) * 512], kc == 0, kc == 15) for kc in range(16)],
                     reads=[mt, wo], writes=[p])
                jk = jr.next()
                S.op("act", lambda be: be.activation(out=jk[:], in_=p[:], func=AF.Square, accum_out=ss[:, n:n + 1]),
                     reads=[p], writes=[jk, ss])
            S.op("dve", lambda be: be.reduce_sum(out=ss[:, 4:5], in_=ss[:, 0:4], axis=AX.X), reads=[ss], writes=[ss])
            S.op("dve", lambda be: be.tensor_scalar(ss[:, 5:6], ss[:, 4:5], 1.0 / D, EPS, op0=ALU.mult, op1=ALU.add),
                 reads=[ss], writes=[ss])
            S.op("act", lambda be: be.activation(out=ss[:, 6:7], in_=ss[:, 5:6], func=AF.Sqrt), reads=[ss], writes=[ss])
            S.op("dve", lambda be: be.reciprocal(ss[:, 7:8], ss[:, 6:7]), reads=[ss], writes=[ss])
            ot = orr.next()
            for n in range(4):
                nsl = slice(n * 512, (n + 1) * 512)
                p = pp[n]
                S.op("dve", lambda be: be.scalar_tensor_tensor(out=ot[:, nsl], in0=p[:], scalar=ss[:, 7:8], in1=gp[:, nsl],
                                                               op0=ALU.mult, op1=ALU.mult), reads=[p, ss, gp], writes=[ot])
            S.op("pool", lambda be: be.tensor_tensor(ot[:], ot[:], xt[:], op=ALU.add), reads=[ot, xt], writes=[ot])
            S.dma("sp", xout.ap[t * 128:(t + 1) * 128, :], ot[:], reads=[ot], writes=[xout.o(t)])


def phase_xa(S, c, l, L):
    NC = L // 512
    wkv = c.w["xa_w_kv"].ap
    with Scope(S) as st:
        idt = sb(S, st, "xa_id", [128, 128], BF16)
        S.dma("sp", idt[:], c.ident.ap[:, :], writes=[idt])
        kT = sb(S, st, "xa_kT", [128, 8, MEM], BF16)
        vaug = sb(S, st, "xa_v", [128, 2, 4, 257], BF16)
        S.op("pool", lambda be: be.memset(vaug[:, :, :, 256:257], 1.0), writes=[vaug])
        with Scope(S) as st2:
            memT = sb(S, st2, "xa_memT", [128, 16, MEM], BF16)
            phase_norm_T(S, c, c.mem, c.w["g_mem"].ap[l:l + 1, :], memT, MEM, rows=MEM, tag="xn")
            wr = sb_ring(S, st2, "xa_w", [128, 16, 512], BF16, 2)
            pr = ps_ring(S, st2, "xa_pp", [128, 512], F32, 2)
            for q in range(4):
                wc = wr.next()
                S.dma("pool", wc[:], wkv[l, :, q * 512:(q + 1) * 512].rearrange("(k p) n -> p k n", p=128), writes=[wc])
                if q < 2:
                    for j in range(4):
                        p = pr.next()
                        S.mm([(p[:, 0:MEM], wc[:, kc, j * 128:(j + 1) * 128], memT[:, kc, :], kc == 0, kc == 15)
                              for kc in range(16)], reads=[wc, memT], writes=[p])
                        S.op("dve", lambda be: be.tensor_copy(kT[:, q * 4 + j, :], p[:, 0:MEM]), reads=[p], writes=[kT])
                else:
                    for mt in range(2):
                        p = pr.next()
                        S.mm([(p[:], memT[:, kc, mt * 128:(mt + 1) * 128], wc[:, kc, :], kc == 0, kc == 15)
                              for kc in range(16)], reads=[wc, memT], writes=[p])
                        h0 = (q - 2) * 2
                        S.op("dve", lambda be: be.tensor_copy(vaug[:, mt, h0:h0 + 2, 0:256],
                                                              p[:].rearrange("p (h d) -> p h d", h=2)),
                             reads=[p], writes=[vaug])
        qr = sb_ring(S, st, "xa_q", [128, 8, 512], BF16, 2)
        zr = sb_ring(S, st, "xa_z", [128, 4, DB], BF16, 2)
        ptr_ = sb_ring(S, st, "xa_pT", [128, 512], BF16, 4)
        yts = [sb_ring(S, st, f"xa_y{i}", [128, DB], BF16, 2) for i in range(4)]
        rr = sb_ring(S, st, "xa_r", [128, 2], F32, 4)
        ysbr = sb_ring(S, st, "xa_ysb", [128, 8, 512], BF16, 2)
        psr = ps_ring(S, st, "xa_ps", [128, 512], F32, 3)
        por = ps_ring(S, st, "xa_po", [128, 257], F32, 3)
        ptr2 = ps_ring(S, st, "xa_pt", [128, 512], BF16, 2)
        for tc in range(NC):
            tsl = slice(tc * 512, (tc + 1) * 512)
            qt_ = qr.next()
            S.dma("sp", qt_[:], c.xaqT.ap[:, tsl].rearrange("(k p) n -> p k n", p=128),
                  reads=[c.xaqT.o((r, tc)) for r in range(8)], writes=[qt_])
            zt = zr.next()
            S.dma("sp", zt[:], c.xaz.ap[tsl, :].rearrange("(t p) d -> p t d", p=128),
                  reads=[c.xaz.o(tc * 4 + i) for i in range(4)], writes=[zt])
            ytoks = [yts[i].next() for i in range(4)]
            for h in range(4):
                pTs = []
                for mt in range(2):
                    p = psr.next()
                    S.mm([(p[:], kT[:, h * 2 + db, mt * 128:(mt + 1) * 128], qt_[:, h * 2 + db, :], db == 0, db == 1)
                          for db in range(2)], reads=[kT, qt_], writes=[p])
                    pT = ptr_.next()
                    S.op("act", lambda be: be.activation(out=pT[:], in_=p[:], func=AF.Exp, scale=1.0 / 16.0),
                         reads=[p], writes=[pT])
                    pTs.append(pT)
                for qi in range(4):
                    po = por.next()
                    S.mm([(po[:], pTs[mt][:, qi * 128:(qi + 1) * 128], vaug[:, mt, h, :], mt == 0, mt == 1)
                          for mt in range(2)], reads=pTs + [vaug], writes=[po])
                    r = rr.next()
                    S.op("dve", lambda be: be.reciprocal(r[:, 0:1], po[:, 256:257]), reads=[po], writes=[r])
                    yk = ytoks[qi]
                    S.op("dve", lambda be: be.scalar_tensor_tensor(out=yk[:, h * 256:(h + 1) * 256], in0=po[:, 0:256],
                                                                   scalar=r[:, 0:1], in1=zt[:, qi, h * 256:(h + 1) * 256],
                                                                   op0=ALU.mult, op1=ALU.mult),
                         reads=[po, r, zt], writes=[yk])
            emit_yT(S, ytoks, c.yT[3], 0, 8, tsl, tc, idt, ptr2, ysbr)


def phase_da(S, c, l, L):
    NC, NT = L // 512, L // 128
    lam_init = 0.8 - 0.6 * math.exp(-0.3 * l)
    w = c.w
    with Scope(S) as st:
        idt = sb(S, st, "da_id", [128, 128], BF16)
        S.dma("sp", idt[:], c.ident.ap[:, :], writes=[idt])
        mk = sb(S, st, "da_mk", [128, 128], BF16)
        S.dma("sp", mk[:], c.maskkq.ap[:, :], writes=[mk])
        lt = sb(S, st, "da_lt", [128, 4, 64], F32)
        for i, nm in enumerate(("da_lq1", "da_lk1", "da_lq2", "da_lk2")):
            S.dma("sp", lt[:, i, :], w[nm].ap[l:l + 1, :].partition_broadcast(128), writes=[lt])
        lp = sb(S, st, "da_lp", [128, 2, 64], F32)
        lv = sb(S, st, "da_lv", [128, 8], F32)
        S.op("dve", lambda be: be.tensor_tensor(lp[:, 0, :], lt[:, 0, :], lt[:, 1, :], op=ALU.mult), reads=[lt], writes=[lp])
        S.op("dve", lambda be: be.tensor_tensor(lp[:, 1, :], lt[:, 2, :], lt[:, 3, :], op=ALU.mult), reads=[lt, lp], writes=[lp])
        S.op("dve", lambda be: be.reduce_sum(out=lv[:, 0:2], in_=lp[:], axis=AX.X), reads=[lp], writes=[lv])
        S.op("act", lambda be: be.activation(out=lv[:, 2:4], in_=lv[:, 0:2], func=AF.Exp), reads=[lv], writes=[lv])
        S.op("dve", lambda be: be.tensor_tensor(lv[:, 4:5], lv[:, 3:4], lv[:, 2:3], op=ALU.subtract), reads=[lv], writes=[lv])
        S.op("dve", lambda be: be.tensor_scalar(lv[:, 5:6], lv[:, 4:5], -lam_init, None, op0=ALU.add), reads=[lv], writes=[lv])
        sg = sb(S, st, "da_sg", [128, 128], F32)
        S.dma("sp", sg[:], w["da_subln_g"].ap[l:l + 1, :].partition_broadcast(128), writes=[sg])
        S.op("dve", lambda be: be.tensor_scalar(sg[:], sg[:], 1.0 - lam_init, None, op0=ALU.mult), reads=[sg], writes=[sg])

        kr = sb_ring(S, st, "da_k", [128, L], BF16, 2)
        vr = sb_ring(S, st, "da_v", [128, NT, 129], BF16, 2)
        qr = sb_ring(S, st, "da_q", [128, 512], BF16, 2)
        zr = sb_ring(S, st, "da_z", [128, 4, 128], BF16, 2)
        pTr = sb_ring(S, st, "da_pT", [128, 512], BF16, 4)
        yts = [sb_ring(S, st, f"da_y{i}", [128, 128], BF16, 2) for i in range(4)]
        ar = sb_ring(S, st, "da_a", [128, 128], F32, 2)
        dr = sb_ring(S, st, "da_d", [128, 128], F32, 2)
        jr = sb_ring(S, st, "da_j", [128, 128], F32, 2)
        rr = sb_ring(S, st, "da_r", [128, 8], F32, 4)
        ysbr = sb_ring(S, st, "da_ysb", [128, 1, 512], BF16, 2)
        psr = ps_ring(S, st, "da_ps", [128, 512], F32, 3)
        accs = [ps(S, st, f"da_acc{i}", [128, 3, 129], F32) for i in range(3)]
        ptr2 = ps_ring(S, st, "da_pt", [128, 512], BF16, 1)

        def acc(comp, qt):
            i = comp * 4 + qt
            return accs[i // 3], i % 3

        for h in range(8):
            kt_ = kr.next()
            S.dma("sp", kt_[:], c.dakT.ap[h * 128:(h + 1) * 128, :], reads=[c.dakT.o((h, tc)) for tc in range(NC)], writes=[kt_])
            vt = vr.next()
            S.dma("sp", vt[:, :, 0:128], c.dav.ap[:, h * 128:(h + 1) * 128].rearrange("(t p) d -> p t d", p=128),
                  reads=[c.dav.o(t) for t in range(NT)], writes=[vt])
            S.op("pool", lambda be: be.memset(vt[:, :, 128:129], 1.0), writes=[vt])
            for tc in range(NC):
                tsl = slice(tc * 512, (tc + 1) * 512)
                qt_ = qr.next()
                S.dma("sp", qt_[:], c.daqT.ap[h * 128:(h + 1) * 128, tsl], reads=[c.daqT.o((h, tc))], writes=[qt_])
                zt = zr.next()
                S.dma("sp", zt[:], c.daz.ap[tsl, h * 128:(h + 1) * 128].rearrange("(t p) d -> p t d", p=128),
                      reads=[c.daz.o(tc * 4 + i) for i in range(4)], writes=[zt])
                nk = 4 * tc + 4
                for a_t in accs:
                    S.op("dve", lambda be: be.memset(a_t[:], 0.0), writes=[a_t])
                for kt in range(nk):
                    dq = kt - 4 * tc
                    q0 = max(dq, 0) * 128
                    for comp in range(2):
                        csl = slice(comp * 64, (comp + 1) * 64)
                        p = psr.next()
                        S.mm([(p[:, q0:512], kt_[csl, kt * 128:(kt + 1) * 128], qt_[csl, q0:512], True, True)],
                             reads=[kt_, qt_], writes=[p])
                        pT = pTr.next()
                        S.op("act", lambda be: be.activation(out=pT[:, q0:512], in_=p[:, q0:512], func=AF.Exp, scale=0.125),
                             reads=[p], writes=[pT])
                        if dq >= 0:
                            S.op("pool", lambda be: be.tensor_tensor(pT[:, q0:q0 + 128], pT[:, q0:q0 + 128], mk[:], op=ALU.mult),
                                 reads=[pT, mk], writes=[pT])
                        for qi in range(max(dq, 0), 4):
                            a_t, a_i = acc(comp, qi)
                            S.mm([(a_t[:, a_i, :], pT[:, qi * 128:(qi + 1) * 128], vt[:, kt, :], False, False)],
                                 reads=[pT, vt], writes=[a_t], skip=True)
                ytoks = []
                for qi in range(4):
                    a0, i0 = acc(0, qi)
                    a1, i1 = acc(1, qi)
                    r = rr.next()
                    S.op("dve", lambda be: be.reciprocal(r[:, 0:1], a0[:, i0, 128:129]), reads=[a0], writes=[r])
                    S.op("dve", lambda be: be.reciprocal(r[:, 1:2], a1[:, i1, 128:129]), reads=[a1, r], writes=[r])
                    S.op("dve", lambda be: be.tensor_tensor(r[:, 2:3], r[:, 1:2], lv[:, 5:6], op=ALU.mult), reads=[r, lv], writes=[r])
                    a = ar.next()
                    S.op("act", lambda be: be.activation(out=a[:], in_=a0[:, i0, 0:128], func=AF.Copy, scale=r[:, 0:1]),
                         reads=[a0, r], writes=[a])
                    d = dr.next()
                    S.op("dve", lambda be: be.scalar_tensor_tensor(out=d[:], in0=a1[:, i1, 0:128], scalar=r[:, 2:3], in1=a[:],
                                                                   op0=ALU.mult, op1=ALU.add), reads=[a1, r, a], writes=[d])
                    jk = jr.next()
                    S.op("act", lambda be: be.activation(out=jk[:], in_=d[:], func=AF.Square, accum_out=r[:, 3:4]),
                         reads=[d, r], writes=[jk, r])
                    S.op("dve", lambda be: be.tensor_scalar(r[:, 4:5], r[:, 3:4], 1.0 / 128.0, EPS, op0=ALU.mult, op1=ALU.add),
                         reads=[r], writes=[r])
                    S.op("act", lambda be: be.activation(out=r[:, 5:6], in_=r[:, 4:5], func=AF.Sqrt), reads=[r], writes=[r])
                    S.op("dve", lambda be: be.reciprocal(r[:, 6:7], r[:, 5:6]), reads=[r], writes=[r])
                    S.op("dve", lambda be: be.scalar_tensor_tensor(out=d[:], in0=d[:], scalar=r[:, 6:7], in1=sg[:],
                                                                   op0=ALU.mult, op1=ALU.mult), reads=[d, r, sg], writes=[d])
                    yk = yts[qi].next()
                    S.op("pool", lambda be: be.tensor_tensor(yk[:], d[:], zt[:, qi, :], op=ALU.mult), reads=[d, zt], writes=[yk])
                    ytoks.append(yk)
                emit_yT(S, ytoks, c.yT[2], h * 128, 1, tsl, tc, idt, ptr2, ysbr)


def phase_ml(S, c, l, L):
    NCH = L // 128
    w = c.w
    mlg = c.mlg
    with Scope(S) as st:
        Bcol = sb(S, st, "ml_Bcol", [128, NCH, 4], F32)
        Ecol = sb(S, st, "ml_Ecol", [128, NCH, 4], F32)
        mu = sb(S, st, "ml_mu", [128, 4, NCH + 1], F32)
        negmu = sb(S, st, "ml_nmu", [128, 4, NCH + 1], F32)
        dec = sb(S, st, "ml_dec", [128, 4, NCH], F32)
        with Scope(S) as g:
            ig = sb(S, g, "mlg_i", [4, L], F32)
            fg = sb(S, g, "mlg_f", [4, L], F32)
            Ft = sb(S, g, "mlg_F", [4, L], F32)
            Gt = sb(S, g, "mlg_G", [4, L], F32)
            ones = sb(S, g, "mlg_1", [4, L], F32)
            bb = sb(S, g, "mlg_b", [4, 4], F32)
            S.dma("sp", ig[:], c.mlif.ap[0:4, :], reads=[c.mlif.o(tc) for tc in range(L // 512)], writes=[ig])
            S.dma("sp", fg[:], c.mlif.ap[4:8, :], reads=[c.mlif.o(tc) for tc in range(L // 512)], writes=[fg])
            S.dma("sp", bb[:, 0:1], w["ml_b_i"].ap[l].rearrange("(h o) -> h o", o=1), writes=[bb], slow=True)
            S.dma("sp", bb[:, 1:2], w["ml_b_f"].ap[l].rearrange("(h o) -> h o", o=1), writes=[bb], slow=True)
            S.op("dve", lambda be: be.tensor_scalar(bb[:, 2:3], bb[:, 1:2], -1.0, None, op0=ALU.mult), reads=[bb], writes=[bb])
            S.op("pool", lambda be: be.memset(ones[:], 1.0), writes=[ones])
            S.op("pool", lambda be: be.memset(bb[:, 3:4], 0.0), reads=[bb], writes=[bb])
            S.op("act", lambda be: be.activation(out=fg[:], in_=fg[:], func=AF.Exp, scale=-1.0, bias=bb[:, 2:3]),
                 reads=[fg, bb], writes=[fg])
            S.op("act", lambda be: be.activation(out=fg[:], in_=fg[:], func=AF.Ln, bias=1.0), reads=[fg], writes=[fg])
            S.op("dve", lambda be: be.tensor_scalar(fg[:], fg[:], -1.0, None, op0=ALU.mult), reads=[fg], writes=[fg])
            S.op("dve", lambda be: be.tensor_tensor_scan(out=Ft[:], data0=ones[:], data1=fg[:], initial=0.0,
                                                         op0=ALU.mult, op1=ALU.add), reads=[ones, fg], writes=[Ft])
            S.op("dve", lambda be: be.scalar_tensor_tensor(out=ig[:], in0=ig[:], scalar=bb[:, 0:1], in1=Ft[:],
                                                           op0=ALU.add, op1=ALU.subtract), reads=[ig, bb, Ft], writes=[ig])
            S.op("dve", lambda be: be.tensor_tensor_scan(out=Gt[:], data0=ones[:], data1=ig[:], initial=0.0,
                                                         op0=ALU.mult, op1=ALU.max), reads=[ones, ig], writes=[Gt])
            S.op("dve", lambda be: be.tensor_tensor(fg[:], Ft[:], Gt[:], op=ALU.add), reads=[Ft, Gt, fg], writes=[fg])
            S.op("act", lambda be: be.activation(out=fg[:], in_=fg[:], func=AF.Exp, scale=-1.0), reads=[fg], writes=[fg])
            S.dma("sp", mlg.ap[0, :, 0:L], ig[:], reads=[ig], writes=[mlg.o(0)])
            S.dma("sp", mlg.ap[1, :, 0:L], fg[:], reads=[fg], writes=[mlg.o(1)])
            S.dma("sp", mlg.ap[2, :, 1:L + 1], Gt[:], reads=[Gt], writes=[mlg.o(2)])
            S.dma("sp", mlg.ap[2, :, 0:1], bb[:, 3:4], reads=[bb], writes=[mlg.o(3)], slow=True)
            for h in range(4):
                S.dma("sp", Bcol[:, :, h], mlg.ap[0, h, 0:L].rearrange("(c t) -> t c", t=128), reads=[mlg.o(0)], writes=[Bcol], slow=True)
                S.dma("sp", Ecol[:, :, h], mlg.ap[1, h, 0:L].rearrange("(c t) -> t c", t=128), reads=[mlg.o(1)], writes=[Ecol], slow=True)
                S.dma("sp", mu[:, h, :], mlg.ap[2, h:h + 1, 0:L + 1:128].partition_broadcast(128),
                      reads=[mlg.o(2), mlg.o(3)], writes=[mu], slow=True)
        S.op("dve", lambda be: be.tensor_scalar(negmu[:], mu[:], -1.0, None, op0=ALU.mult), reads=[mu], writes=[negmu])
        S.op("dve", lambda be: be.tensor_tensor(dec[:], mu[:, :, 0:NCH], mu[:, :, 1:NCH + 1], op=ALU.subtract), reads=[mu], writes=[dec])
        S.op("act", lambda be: be.activation(out=dec[:], in_=dec[:], func=AF.Exp), reads=[dec], writes=[dec])

        idt = sb(S, st, "ml_id", [128, 128], BF16)
        S.dma("sp", idt[:], c.ident.ap[:, :], writes=[idt])
        mk = sb(S, st, "ml_mk", [128, 128], BF16)
        S.dma("sp", mk[:], c.maskkq.ap[:, :], writes=[mk])
        ng = sb(S, st, "ml_ng", [128, DB], F32)
        S.dma("sp", ng[:], w["ml_norm_g"].ap[l:l + 1, :].partition_broadcast(128), writes=[ng])
        qT = sb(S, st, "ml_qT", [128, 2, L], BF16)
        kT = sb(S, st, "ml_kT", [128, 2, L], BF16)
        va = sb(S, st, "ml_va", [128, NCH, 257], BF16)
        ot = sb(S, st, "ml_o", [128, NCH, 256], BF16)
        zt = sb(S, st, "ml_z", [128, NCH, 256], BF16)
        C32 = sb(S, st, "ml_C32", [128, 2, 257], F32)
        Cbf = sb(S, st, "ml_Cbf", [128, 2, 257], BF16)
        gbr = sb_ring(S, st, "ml_gb", [128, 128], F32, 3)
        ptr_ = sb_ring(S, st, "ml_pt", [128, 128], F32, 2)
        ptmr = sb_ring(S, st, "ml_ptm", [128, 128], F32, 2)
        str_ = sb_ring(S, st, "ml_st", [128, 128], BF16, 2)
        scr = sb_ring(S, st, "ml_sc", [128, 128], F32, 2)
        qsr = sb_ring(S, st, "ml_qs", [128, 2, 128], BF16, 2)
        rr = sb_ring(S, st, "ml_r", [128, 8], F32, 3)
        hr = sb_ring(S, st, "ml_h", [128, 256], F32, 2)
        jr = sb_ring(S, st, "ml_j", [128, 256], F32, 1)
        yr = sb_ring(S, st, "ml_y", [128, 256], BF16, 2)
        ysr = sb_ring(S, st, "ml_ys", [128, 2, 128], BF16, 2)
        kwr = sb_ring(S, st, "ml_kw", [128, 2], F32, 2)
        kkr = sb_ring(S, st, "ml_kk", [128, 256], BF16, 2)
        psS = ps_ring(S, st, "ml_pS", [128, 128], F32, 2)
        psN = ps_ring(S, st, "ml_pN", [128, 257], F32, 2)
        psT = ps_ring(S, st, "ml_pT", [128, 2, 128], BF16, 2)
        psC = [ps(S, st, f"ml_pC{i}", [128, 257], F32) for i in range(2)]
        for h in range(4):
            hs = slice(h * 256, (h + 1) * 256)
            S.dma("sp", qT[:], c.mlqT.ap[hs, :].rearrange("(k p) n -> p k n", p=128),
                  reads=[c.mlqT.o((2 * h + k, tc)) for k in range(2) for tc in range(L // 512)], writes=[qT])
            S.dma("sp", kT[:], c.mlkT.ap[hs, :].rearrange("(k p) n -> p k n", p=128),
                  reads=[c.mlkT.o((2 * h + k, tc)) for k in range(2) for tc in range(L // 512)], writes=[kT])
            S.dma("sp", va[:, :, 0:256], c.mlv.ap[:, hs].rearrange("(c p) d -> p c d", p=128),
                  reads=[c.mlv.o(t) for t in range(NCH)], writes=[va])
            S.op("pool", lambda be: be.memset(va[:, :, 256:257], 1.0), writes=[va])
            S.dma("sp", ot[:], c.mlo.ap[:, hs].rearrange("(c p) d -> p c d", p=128), reads=[c.mlo.o(t) for t in range(NCH)], writes=[ot])
            S.dma("sp", zt[:], c.mlz.ap[:, hs].rearrange("(c p) d -> p c d", p=128), reads=[c.mlz.o(t) for t in range(NCH)], writes=[zt])
            for ch in range(NCH):
                csl = slice(ch * 128, (ch + 1) * 128)
                gb = gbr.next()
                S.dma("sp", gb[:], mlg.ap[2, h:h + 1, 1 + ch * 128:1 + (ch + 1) * 128].partition_broadcast(128),
                      reads=[mlg.o(2)], writes=[gb])
                pS = psS.next()
                S.mm([(pS[:], kT[:, db, csl], qT[:, db, csl], db == 0, db == 1) for db in range(2)], reads=[kT, qT], writes=[pS])
                pt = ptr_.next()
                S.op("act", lambda be: be.activation(out=pt[:], in_=gb[:], func=AF.Exp, scale=-1.0, bias=Bcol[:, ch, h:h + 1]),
                     reads=[gb, Bcol], writes=[pt])
                ptm = ptmr.next()
                S.op("pool", lambda be: be.tensor_tensor(ptm[:], pt[:], mk[:], op=ALU.mult), reads=[pt, mk], writes=[ptm])
                stt = str_.next()
                S.op("dve", lambda be: be.tensor_tensor(stt[:], pS[:], ptm[:], op=ALU.mult), reads=[pS, ptm], writes=[stt])
                items = [(None, stt[:], va[:, ch, :])]
                rds = [stt, va]
                if ch > 0:
                    sc = scr.next()
                    S.op("act", lambda be: be.activation(out=sc[:], in_=gb[:], func=AF.Exp, scale=-1.0, bias=mu[:, h, ch:ch + 1]),
                         reads=[gb, mu], writes=[sc])
                    qs = qsr.next()
                    for db in range(2):
                        S.op("pool", lambda be: be.tensor_tensor(qs[:, db, :], qT[:, db, csl], sc[:], op=ALU.mult),
                             reads=[qT, sc], writes=[qs])
                    items += [(None, qs[:, 0, :], Cbf[:, 0, :]), (None, qs[:, 1, :], Cbf[:, 1, :])]
                    rds += [qs, Cbf]
                pN = psN.next()
                n = len(items)
                S.mm([(pN[:], a, b, i == 0, i == n - 1) for i, (_, a, b) in enumerate(items)], reads=rds, writes=[pN])
                r = rr.next()
                S.op("act", lambda be: be.activation(out=r[:, 6:7], in_=pN[:, 256:257], func=AF.Abs), reads=[pN], writes=[r])
                S.op("dve", lambda be: be.tensor_tensor(r[:, 0:1], r[:, 6:7], Ecol[:, ch, h:h + 1], op=ALU.max),
                     reads=[r, Ecol], writes=[r])
                S.op("dve", lambda be: be.reciprocal(r[:, 1:2], r[:, 0:1]), reads=[r], writes=[r])
                hh = hr.next()
                S.op("act", lambda be: be.activation(out=hh[:], in_=pN[:, 0:256], func=AF.Copy, scale=r[:, 1:2]),
                     reads=[pN, r], writes=[hh])
                jk = jr.next()
                S.op("act", lambda be: be.activation(out=jk[:], in_=hh[:], func=AF.Square, accum_out=r[:, 2:3]),
                     reads=[hh, r], writes=[jk, r])
                S.op("dve", lambda be: be.tensor_scalar(r[:, 3:4], r[:, 2:3], 1.0 / 256.0, EPS, op0=ALU.mult, op1=ALU.add),
                     reads=[r], writes=[r])
                S.op("act", lambda be: be.activation(out=r[:, 4:5], in_=r[:, 3:4], func=AF.Sqrt), reads=[r], writes=[r])
                S.op("dve", lambda be: be.reciprocal(r[:, 5:6], r[:, 4:5]), reads=[r], writes=[r])
                S.op("dve", lambda be: be.scalar_tensor_tensor(out=hh[:], in0=hh[:], scalar=r[:, 5:6], in1=ng[:, hs],
                                                               op0=ALU.mult, op1=ALU.mult), reads=[hh, r, ng], writes=[hh])
                S.op("pool", lambda be: be.tensor_tensor(hh[:], hh[:], ot[:, ch, :], op=ALU.mult), reads=[hh, ot], writes=[hh])
                yk = yr.next()
                S.op("pool", lambda be: be.tensor_tensor(yk[:], hh[:], zt[:, ch, :], op=ALU.mult), reads=[hh, zt], writes=[yk])
                pT = psT.next()
                for db in range(2):
                    S.transpose(pT[:, db, :], yk[:, db * 128:(db + 1) * 128], idt[:], reads=[yk, idt], writes=[pT])
                ys = ysr.next()
                S.op("act", lambda be: be.copy(out=ys[:], in_=pT[:]), reads=[pT], writes=[ys])
                S.dma("sp", c.yT[1].ap[hs, csl].rearrange("(k p) n -> p k n", p=128), ys[:], reads=[ys],
                      writes=[c.yT[1].o((2 * h + k, ch // 4)) for k in range(2)])
                if ch == NCH - 1:
                    continue
                kw = kwr.next()
                S.op("act", lambda be: be.activation(out=kw[:, 0:1], in_=Bcol[:, ch, h:h + 1], func=AF.Exp,
                                                     bias=negmu[:, h, ch + 1:ch + 2]), reads=[Bcol, negmu], writes=[kw])
                pK = psT.next()
                for db in range(2):
                    S.transpose(pK[:, db, :], kT[:, db, csl], idt[:], reads=[kT, idt], writes=[pK])
                kk = kkr.next()
                S.op("dve", lambda be: be.tensor_scalar(kk[:], pK[:].rearrange("p a b -> p (a b)"), kw[:, 0:1], None, op0=ALU.mult),
                     reads=[pK, kw], writes=[kk])
                for db in range(2):
                    S.mm([(psC[db][:], kk[:, db * 128:(db + 1) * 128], va[:, ch, :], True, True)], reads=[kk, va], writes=[psC[db]])
                    if ch == 0:
                        S.op("dve", lambda be: be.tensor_copy(C32[:, db, :], psC[db][:]), reads=[psC[db]], writes=[C32])
                    else:
                        S.op("dve", lambda be: be.scalar_tensor_tensor(out=C32[:, db, :], in0=C32[:, db, :], scalar=dec[:, h, ch:ch + 1],
                                                                       in1=psC[db][:], op0=ALU.mult, op1=ALU.add),
                             reads=[C32, dec, psC[db]], writes=[C32])
                S.op("act", lambda be: be.copy(out=Cbf[:], in_=C32[:]), reads=[C32], writes=[Cbf])


TWO_PI = 2.0 * math.pi


def _sincos(S, out_t, ang_src, th, off, kt):
    S.op("dve", lambda be: be.tensor_scalar(out_t[0], ang_src[0], th, off, op0=ALU.mult, op1=ALU.add),
         reads=ang_src[1], writes=[out_t[1]])
    S.op("dve", lambda be: be.tensor_copy(kt[0], out_t[0]), reads=[out_t[1]], writes=[kt[1]])
    S.op("pool", lambda be: be.tensor_tensor(out_t[0], out_t[0], kt[0], op=ALU.subtract), reads=[out_t[1], kt[1]], writes=[out_t[1]])
    S.op("act", lambda be: be.activation(out=out_t[0], in_=out_t[0], func=AF.Sin, scale=TWO_PI * (1.0 - 1e-6)),
         reads=[out_t[1]], writes=[out_t[1]])


def phase_s5(S, c, l, L):
    w = c.w
    SEG = min(L, 1024)
    NSEG = L // SEG
    NCS = SEG // 512
    OFF_S = 0.0
    OFF_C = 0.25
    with Scope(S) as st:
        BBpad = sb(S, st, "s5_BB", [128, NG, 128], BF16)
        BBsw = sb(S, st, "s5_BBs", [128, NG, 128], BF16)
        CCpad = sb(S, st, "s5_CC", [128, NG, 128], BF16)
        r2 = sb(S, st, "s5_r2", [128, NG], F32)
        th = sb(S, st, "s5_th", [128, NG], F32)
        offs = sb(S, st, "s5_offs", [128, 2], F32)
        dsk = sb(S, st, "s5_dsk", [128, 8], F32)
        bgl = sb(S, st, "s5_bg", [128, 8], F32)
        S.dma("sp", dsk[:], w["s5_d"].ap[l].rearrange("(b p) -> p b", p=128), writes=[dsk], slow=True)
        S.dma("sp", bgl[:], w["s5_b_glu"].ap[l].rearrange("(b p) -> p b", p=128), writes=[bgl], slow=True)
        S.op("pool", lambda be: be.memset(offs[0:64, 0:1], OFF_S), writes=[offs])
        S.op("pool", lambda be: be.memset(offs[64:128, 0:1], OFF_S + 0.5), reads=[offs], writes=[offs])
        S.op("pool", lambda be: be.memset(offs[:, 1:2], OFF_C), reads=[offs], writes=[offs])
        with Scope(S) as pp:
            def t3(name):
                return sb(S, pp, name, [128, 8, 64], F32)
            lre, lim, dt, er, cs, sn, wr, wi, t1, t2, Br, Bi, Bbr, Bbi = [t3(f"s5p{i}") for i in range(14)]
            mg = sb(S, pp, "s5_mg", [128, 8], F32)
            dt8 = sb(S, pp, "s5_dt8", [128, 8], F32)
            m2 = sb(S, pp, "s5_m2", [128, 8, 128], F32)
            S.dma("sp", mg[:], c.maskg.ap[:, :], writes=[mg])
            S.dma("sp", m2[:], c.mask2.ap[:, :, :], writes=[m2])
            hre, him, hdt = w["s5_lam_re"].h, w["s5_lam_im"].h, w["s5_log_dt"].h
            for g8 in range(8):
                ps_ = slice(g8 * 16, (g8 + 1) * 16)
                S.dma("sp", lre[ps_, :, :], bass.AP(tensor=hre, offset=l * 4096 + g8 * 64, ap=[[0, 16], [512, 8], [1, 64]]), writes=[lre], slow=True)
                S.dma("sp", lim[ps_, :, :], bass.AP(tensor=him, offset=l * 4096 + g8 * 64, ap=[[0, 16], [512, 8], [1, 64]]), writes=[lim], slow=True)
                S.dma("sp", dt8[ps_, :], bass.AP(tensor=hdt, offset=l * 64 + g8, ap=[[0, 16], [8, 8]]), writes=[dt8], slow=True)
                for blk in range(8):
                    S.dma("sp", Br[ps_, blk, :], w["s5_b_re"].ap[l, blk * 8 + g8].rearrange("p c -> c p"), writes=[Br], slow=True)
                    S.dma("sp", Bi[ps_, blk, :], w["s5_b_im"].ap[l, blk * 8 + g8].rearrange("p c -> c p"), writes=[Bi], slow=True)

            def V(e, fn, rd, wr_):
                S.op(e, fn, reads=rd, writes=wr_)
            V("dve", lambda be: be.tensor_scalar(lre[:], lre[:], -1e-4, None, op0=ALU.min), [lre], [lre])
            V("act", lambda be: be.activation(out=dt8[:], in_=dt8[:], func=AF.Exp), [dt8], [dt8])
            V("dve", lambda be: be.tensor_copy(dt[:], dt8[:].unsqueeze(2).to_broadcast([128, 8, 64])), [dt8], [dt])
            V("dve", lambda be: be.tensor_tensor(t1[:], lre[:], dt[:], op=ALU.mult), [lre, dt], [t1])
            V("act", lambda be: be.activation(out=er[:], in_=t1[:], func=AF.Exp), [t1], [er])
            V("dve", lambda be: be.tensor_tensor(t2[:], lim[:], dt[:], op=ALU.mult), [lim, dt], [t2])
            kA = sb(S, pp, "s5_kA", [128, 8, 64], mybir.dt.int32)
            _sincos(S, (cs[:], cs), (t2[:], [t2]), 1.0 / TWO_PI, OFF_C, (kA[:], kA))
            _sincos(S, (sn[:], sn), (t2[:], [t2]), 1.0 / TWO_PI, OFF_S, (kA[:], kA))
            V("dve", lambda be: be.tensor_tensor(cs[:], cs[:], er[:], op=ALU.mult), [cs, er], [cs])
            V("dve", lambda be: be.tensor_tensor(sn[:], sn[:], er[:], op=ALU.mult), [sn, er], [sn])
            V("dve", lambda be: be.tensor_scalar(cs[:], cs[:], -1.0, None, op0=ALU.add), [cs], [cs])
            V("dve", lambda be: be.tensor_tensor(t1[:], lre[:], lre[:], op=ALU.mult), [lre], [t1])
            V("dve", lambda be: be.tensor_tensor(t2[:], lim[:], lim[:], op=ALU.mult), [lim], [t2])
            V("dve", lambda be: be.tensor_tensor(t1[:], t1[:], t2[:], op=ALU.add), [t1, t2], [t1])
            V("dve", lambda be: be.reciprocal(t1[:], t1[:]), [t1], [t1])
            V("dve", lambda be: be.tensor_tensor(wr[:], cs[:], lre[:], op=ALU.mult), [cs, lre], [wr])
            V("dve", lambda be: be.tensor_tensor(t2[:], sn[:], lim[:], op=ALU.mult), [sn, lim], [t2])
            V("dve", lambda be: be.tensor_tensor(wr[:], wr[:], t2[:], op=ALU.add), [wr, t2], [wr])
            V("dve", lambda be: be.tensor_tensor(wr[:], wr[:], t1[:], op=ALU.mult), [wr, t1], [wr])
            V("dve", lambda be: be.tensor_tensor(wi[:], sn[:], lre[:], op=ALU.mult), [sn, lre], [wi])
            V("dve", lambda be: be.tensor_tensor(t2[:], cs[:], lim[:], op=ALU.mult), [cs, lim], [t2])
            V("dve", lambda be: be.tensor_tensor(wi[:], wi[:], t2[:], op=ALU.subtract), [wi, t2], [wi])
            V("dve", lambda be: be.tensor_tensor(wi[:], wi[:], t1[:], op=ALU.mult), [wi, t1], [wi])
            V("dve", lambda be: be.tensor_tensor(Bbr[:], wr[:], Br[:], op=ALU.mult), [wr, Br], [Bbr])
            V("dve", lambda be: be.tensor_tensor(t2[:], wi[:], Bi[:], op=ALU.mult), [wi, Bi], [t2])
            V("dve", lambda be: be.tensor_tensor(Bbr[:], Bbr[:], t2[:], op=ALU.subtract), [Bbr, t2], [Bbr])
            V("dve", lambda be: be.tensor_tensor(Bbi[:], wr[:], Bi[:], op=ALU.mult), [wr, Bi], [Bbi])
            V("dve", lambda be: be.tensor_tensor(t2[:], wi[:], Br[:], op=ALU.mult), [wi, Br], [t2])
            V("dve", lambda be: be.tensor_tensor(Bbi[:], Bbi[:], t2[:], op=ALU.add), [Bbi, t2], [Bbi])
            mgb = mg[:].unsqueeze(1).unsqueeze(3).to_broadcast([128, 8, 8, 64])
            for dst, lo, hi in ((BBpad, Bbr, Bbi), (BBsw, Bbi, Bbr)):
                for half, src in ((0, lo), (1, hi)):
                    dv = dst[:, :, half * 64:(half + 1) * 64].rearrange("p (blk g) q -> p blk g q", g=8)
                    sv = src[:].unsqueeze(2).to_broadcast([128, 8, 8, 64])
                    V("dve", lambda be: be.tensor_tensor(dv, sv, mgb, op=ALU.mult), [src, mg], [dst])
            lb = sb(S, pp, "s5_lb", [128, NG], F32)
            S.dma("sp", lb[0:64, :], w["s5_lam_re"].ap[l].rearrange("g p -> p g"), writes=[lb], slow=True)
            S.dma("sp", lb[64:128, :], w["s5_lam_re"].ap[l].rearrange("g p -> p g"), writes=[lb], slow=True)
            S.dma("sp", th[0:64, :], w["s5_lam_im"].ap[l].rearrange("g p -> p g"), writes=[th], slow=True)
            S.dma("sp", th[64:128, :], w["s5_lam_im"].ap[l].rearrange("g p -> p g"), writes=[th], slow=True)
            dtb = sb(S, pp, "s5_dtb", [128, NG], F32)
            S.dma("sp", dtb[:], w["s5_log_dt"].ap[l:l + 1, :].partition_broadcast(128), writes=[dtb])
            V("act", lambda be: be.activation(out=dtb[:], in_=dtb[:], func=AF.Exp), [dtb], [dtb])
            V("dve", lambda be: be.tensor_scalar(lb[:], lb[:], -1e-4, None, op0=ALU.min), [lb], [lb])
            V("dve", lambda be: be.tensor_tensor(lb[:], lb[:], dtb[:], op=ALU.mult), [lb, dtb], [lb])
            V("act", lambda be: be.activation(out=r2[:], in_=lb[:], func=AF.Exp), [lb], [r2])
            V("dve", lambda be: be.tensor_tensor(th[:], th[:], dtb[:], op=ALU.mult), [th, dtb], [th])
            kB = sb(S, pp, "s5_kB", [128, NG], mybir.dt.int32)
            V("dve", lambda be: be.tensor_scalar(th[:], th[:], 1.0 / TWO_PI, None, op0=ALU.mult), [th], [th])
            V("dve", lambda be: be.tensor_copy(kB[:], th[:]), [th], [kB])
            V("dve", lambda be: be.tensor_tensor(th[:], th[:], kB[:], op=ALU.subtract), [th, kB], [th])
            Cc = sb(S, pp, "s5_Cc", [128, 8, 128], F32)
            S.dma("sp", Cc[:, :, 0:64], w["s5_c_re"].ap[l].rearrange("(blk g8) co p -> (g8 co) blk p", g8=8), writes=[Cc])
            S.dma("sp", Cc[:, :, 64:128], w["s5_c_im"].ap[l].rearrange("(blk g8) co p -> (g8 co) blk p", g8=8), writes=[Cc])
            V("dve", lambda be: be.tensor_scalar(Cc[:, :, 64:128], Cc[:, :, 64:128], -1.0, None, op0=ALU.mult), [Cc], [Cc])
            idf = sb(S, pp, "s5_idf", [128, 128], F32)
            S.dma("sp", idf[:], c.identf.ap[:, :], writes=[idf])
            pcr = ps_ring(S, pp, "s5_pc", [128, 128], F32, 2)
            for blk in range(8):
                pc = pcr.next()
                S.transpose(pc[:], Cc[:, blk, :], idf[:], reads=[Cc, idf], writes=[pc])
                V("dve", lambda be: be.tensor_tensor(CCpad[:, blk * 8:(blk + 1) * 8, :], pc[:].unsqueeze(1).to_broadcast([128, 8, 128]),
                                                     m2[:], op=ALU.mult), [pc, m2], [CCpad])

        with Scope(S) as ms:
            trs = []
            for sg in range(NSEG):
                tr_ = sb(S, ms, f"s5_tr{sg}", [128, SEG], F32)
                S.dma("sp", tr_[:], c.trow.ap[0:1, sg * SEG:(sg + 1) * SEG].partition_broadcast(128), writes=[tr_])
                trs.append(tr_)

            def rg(name, dt_=F32):
                return sb_ring(S, ms, name, [128, SEG], dt_, 2)
            COSr, SINr, BUr, BSr, Vr, VSr, T2r, T3r = [rg(f"s5m{i}") for i in range(8)]
            Sgr = rg("s5_sg", BF16)
            kir = rg("s5_ki", mybir.dt.int32)
            ur = sb_ring(S, ms, "s5_u", [128, L], BF16, 2)
            car = sb(S, ms, "s5_car", [128, 8, 2], F32)
            yvr = sb_ring(S, ms, "s5_yv", [128, 512], F32, 2)
            tgr = sb_ring(S, ms, "s5_tg", [128, 512], F32, 2)
            ygr = sb_ring(S, ms, "s5_yg", [128, 512], BF16, 2)
            pbu = ps_ring(S, ms, "s5_pb", [128, 512], F32, 2)
            pbs = ps_ring(S, ms, "s5_pbs", [128, 512], F32, 2)
            pyr = ps_ring(S, ms, "s5_py", [128, 512], F32, 4)
            for blk in range(8):
                ut = ur.next()
                S.dma("sp", ut[:], c.s5uT.ap[blk * 128:(blk + 1) * 128, :], reads=[c.s5uT.o((blk, tc)) for tc in range(L // 512)], writes=[ut])
                for sg in range(NSEG):
                    pys = [pyr.next() for _ in range(NCS)]
                    for g8 in range(8):
                        g = blk * 8 + g8
                        COS, SIN, BU, BS, Vt, VS, T2, T3, Sg = [r_.next() for r_ in (COSr, SINr, BUr, BSr, Vr, VSr, T2r, T3r, Sgr)]
                        kt_ = kir.next()
                        _sincos(S, (COS[:], COS), (trs[sg][:], [trs[sg], th, offs]), th[:, g:g + 1], offs[:, 1:2], (kt_[:], kt_))
                        kt_ = kir.next()
                        _sincos(S, (SIN[:], SIN), (trs[sg][:], [trs[sg], th, offs]), th[:, g:g + 1], offs[:, 0:1], (kt_[:], kt_))
                        for cs_ in range(NCS):
                            fsl = slice(cs_ * 512, (cs_ + 1) * 512)
                            tsl = slice(sg * SEG + cs_ * 512, sg * SEG + (cs_ + 1) * 512)
                            p1, p2 = pbu.next(), pbs.next()
                            S.mm([(p1[:], BBpad[:, g, :], ut[:, tsl], True, True)], reads=[BBpad, ut], writes=[p1])
                            S.mm([(p2[:], BBsw[:, g, :], ut[:, tsl], True, True)], reads=[BBsw, ut], writes=[p2])
                            S.op("act", lambda be: be.copy(out=BU[:, fsl], in_=p1[:]), reads=[p1], writes=[BU])
                            S.op("act", lambda be: be.copy(out=BS[:, fsl], in_=p2[:]), reads=[p2], writes=[BS])
                        S.op("dve", lambda be: be.tensor_tensor(Vt[:], COS[:], BU[:], op=ALU.mult), reads=[COS, BU], writes=[Vt])
                        S.op("pool", lambda be: be.tensor_tensor(T2[:], SIN[:], BS[:], op=ALU.mult), reads=[SIN, BS], writes=[T2])
                        S.op("dve", lambda be: be.tensor_tensor(VS[:], COS[:], BS[:], op=ALU.mult), reads=[COS, BS], writes=[VS])
                        S.op("pool", lambda be: be.tensor_tensor(T3[:], SIN[:], BU[:], op=ALU.mult), reads=[SIN, BU], writes=[T3])
                        S.op("dve", lambda be: be.tensor_tensor(Vt[:], Vt[:], T2[:], op=ALU.add), reads=[Vt, T2], writes=[Vt])
                        S.op("pool", lambda be: be.tensor_tensor(VS[:], VS[:], T3[:], op=ALU.subtract), reads=[VS, T3], writes=[VS])
                        dec_ = r2[:, g:g + 1].to_broadcast([128, SEG])
                        i0 = 0.0 if sg == 0 else car[:, g8, 0:1]
                        i1 = 0.0 if sg == 0 else car[:, g8, 1:2]
                        S.op("dve", lambda be: be.tensor_tensor_scan(out=BU[:], data0=dec_, data1=Vt[:], initial=i0, op0=ALU.mult, op1=ALU.add),
                             reads=[r2, Vt, car], writes=[BU])
                        S.op("dve", lambda be: be.tensor_tensor_scan(out=BS[:], data0=dec_, data1=VS[:], initial=i1, op0=ALU.mult, op1=ALU.add),
                             reads=[r2, VS, car], writes=[BS])
                        if sg < NSEG - 1:
                            S.op("pool", lambda be: be.tensor_copy(car[:, g8, 0:1], BU[:, SEG - 1:SEG]), reads=[BU, car], writes=[car])
                            S.op("pool", lambda be: be.tensor_copy(car[:, g8, 1:2], BS[:, SEG - 1:SEG]), reads=[BS, car], writes=[car])
                        S.op("dve", lambda be: be.tensor_tensor(Vt[:], COS[:], BU[:], op=ALU.mult), reads=[COS, BU], writes=[Vt])
                        S.op("pool", lambda be: be.tensor_tensor(VS[:], SIN[:], BS[:], op=ALU.mult), reads=[SIN, BS], writes=[VS])
                        S.op("dve", lambda be: be.tensor_tensor(Sg[:], Vt[:], VS[:], op=ALU.subtract), reads=[Vt, VS], writes=[Sg])
                        for cs_ in range(NCS):
                            fsl = slice(cs_ * 512, (cs_ + 1) * 512)
                            S.mm([(pys[cs_][:], CCpad[:, g, :], Sg[:, fsl], g8 == 0, g8 == 7)], reads=[CCpad, Sg], writes=[pys[cs_]])
                    for cs_ in range(NCS):
                        tsl = slice(sg * SEG + cs_ * 512, sg * SEG + (cs_ + 1) * 512)
                        py = pys[cs_]
                        yv, tg, yg = yvr.next(), tgr.next(), ygr.next()
                        S.op("dve", lambda be: be.scalar_tensor_tensor(out=yv[:], in0=ut[:, tsl], scalar=dsk[:, blk:blk + 1], in1=py[:],
                                                                       op0=ALU.mult, op1=ALU.add), reads=[ut, dsk, py], writes=[yv])
                        S.op("pool", lambda be: be.tensor_tensor(tg[:], yv[:], yv[:], op=ALU.mult), reads=[yv], writes=[tg])
                        S.op("pool", lambda be: be.tensor_scalar(tg[:], tg[:], 0.044715, 1.0, op0=ALU.mult, op1=ALU.add), reads=[tg], writes=[tg])
                        S.op("pool", lambda be: be.tensor_tensor(tg[:], tg[:], yv[:], op=ALU.mult), reads=[tg, yv], writes=[tg])
                        S.op("act", lambda be: be.activation(out=tg[:], in_=tg[:], func=AF.Sigmoid, scale=2.0 * math.sqrt(2.0 / math.pi)),
                             reads=[tg], writes=[tg])
                        S.op("dve", lambda be: be.tensor_tensor(yg[:], yv[:], tg[:], op=ALU.mult), reads=[yv, tg], writes=[yg])
                        S.dma("sp", c.s5yT.ap[blk * 128:(blk + 1) * 128, tsl], yg[:], reads=[yg], writes=[c.s5yT.o((blk, tsl.start // 512))])

        with Scope(S) as gs:
            wg = sb(S, gs, "s5_wg", [128, 8, DB], BF16)
            for q in range(2):
                S.dma("pool", wg[:, :, q * 512:(q + 1) * 512],
                      w["s5_w_glu"].ap[l, :, q * 512:(q + 1) * 512].rearrange("(k p) n -> p k n", p=128), writes=[wg])
            ygr2 = sb_ring(S, gs, "s5_y2", [128, 8, 512], BF16, 2)
            zr2 = sb_ring(S, gs, "s5_z2", [128, 8, 512], BF16, 2)
            sgr = sb_ring(S, gs, "s5_sgm", [128, 512], F32, 2)
            outr = sb_ring(S, gs, "s5_o2", [128, 8, 512], BF16, 2)
            pgr = ps_ring(S, gs, "s5_pg", [128, 512], F32, 4)
            for tc in range(L // 512):
                tsl = slice(tc * 512, (tc + 1) * 512)
                yt, zt, ob = ygr2.next(), zr2.next(), outr.next()
                S.dma("sp", yt[:], c.s5yT.ap[:, tsl].rearrange("(k p) n -> p k n", p=128), reads=[c.s5yT.o((k, tc)) for k in range(8)], writes=[yt])
                S.dma("sp", zt[:], c.s5zT.ap[:, tsl].rearrange("(k p) n -> p k n", p=128), reads=[c.s5zT.o((k, tc)) for k in range(8)], writes=[zt])
                for j in range(8):
                    pg = pgr.next()
                    S.mm([(pg[:], wg[:, kc, j * 128:(j + 1) * 128], yt[:, kc, :], kc == 0, kc == 7) for kc in range(8)],
                         reads=[wg, yt], writes=[pg])
                    sg_ = sgr.next()
                    S.op("act", lambda be: be.activation(out=sg_[:], in_=pg[:], func=AF.Sigmoid, bias=bgl[:, j:j + 1]),
                         reads=[pg, bgl], writes=[sg_])
                    S.op("dve", lambda be: be.tensor_tensor(sg_[:], sg_[:], yt[:, j, :], op=ALU.mult), reads=[sg_, yt], writes=[sg_])
                    S.op("pool", lambda be: be.tensor_tensor(ob[:, j, :], sg_[:], zt[:, j, :], op=ALU.mult), reads=[sg_, zt], writes=[ob])
                S.dma("sp", c.yT[0].ap[:, tsl].rearrange("(k p) n -> p k n", p=128), ob[:], reads=[ob],
                      writes=[c.yT[0].o((k, tc)) for k in range(8)])


def build(L=4096, depth=4, debug=False, upto="all"):
    nc = bass.Bass("TRN2", target_bir_lowering=False)
    c = declare(nc, L, depth, debug)
    with ExitStack() as top:
        S = Sched(nc, top)
        for l in range(depth):
            xin = c.x if l == 0 else c.xs[(l - 1) % 2]
            xout = c.out if l == depth - 1 else c.xs[l % 2]
            with Scope(S) as st:
                hT = sb(S, st, "hT", [128, 16, L], BF16)
                phase_norm_T(S, c, xin, c.w["g_pre"].ap[l:l + 1, :], hT, L)
                if "inproj" in upto or upto == "all":
                    phase_inproj(S, c, l, hT, L)
            if "s5" in upto or upto == "all":
                phase_s5(S, c, l, L)
            if "ml" in upto or upto == "all":
                phase_ml(S, c, l, L)
            if "da" in upto or upto == "all":
                phase_da(S, c, l, L)
            if "xa" in upto or upto == "all":
                phase_xa(S, c, l, L)
            if "merge" in upto or upto == "all":
                phase_merge(S, c, l, L)
                phase_out(S, c, l, L, xin, xout)
        outs = [o for o in c.out.objs.values()]
        if debug:
            for d in [c.s5uT, c.s5zT, c.mlqT, c.mlkT, c.mlv, c.mlo, c.mlz, c.mlif, c.daqT, c.dakT, c.dav, c.daz,
                      c.xaqT, c.xaz, c.gT, c.mT] + c.yT + c.xs:
                outs += list(d.objs.values())
        S.final_wait("sp", outs)
        print("instructions:", S.n_inst)
    return nc, c


def make_consts(L):
    bf = ml_dtypes.bfloat16
    inv = 1.0 / (10000.0 ** (np.arange(0, 64, 2, dtype=np.float32) / 64.0))
    ang = np.arange(L, dtype=np.float32)[:, None] * inv[None, :]
    cos, sin = np.cos(ang).T, np.sin(ang).T
    c64 = np.concatenate([cos, cos], 0)
    s64 = np.concatenate([-sin, sin], 0)
    kq = (np.arange(128)[None, :] >= np.arange(128)[:, None]).astype(np.float32)
    return {
        "c_ident": np.eye(128, dtype=np.float32).astype(bf),
        "c_identf": np.eye(128, dtype=np.float32),
        "c_trow": np.arange(L, dtype=np.float32)[None, :].copy(),
        "c_ropec": np.concatenate([c64, c64], 0).astype(bf),
        "c_ropes": np.concatenate([s64, s64], 0).astype(bf),
        "c_maskkq": kq.astype(bf),
        "c_maskg": (np.arange(128)[:, None] // 16 == np.arange(8)[None, :]).astype(np.float32),
        "c_mask2": np.broadcast_to((np.arange(8)[:, None] == (np.arange(128)[None, :] // 16)).astype(np.float32)[None], (128, 8, 128)).copy(),
    }


SEQ_FULL = 4096
DEPTH_FULL = 4


def kernel(**inputs):
    L, depth = SEQ_FULL, DEPTH_FULL
    nc, _ = build(L=L, depth=depth, debug=False)
    consts = make_consts(L)
    shared = {k: np.ascontiguousarray(np.asarray(v, dtype=np.float32)) for k, v in inputs.items() if k not in ("x", "mem")}
    x = np.asarray(inputs["x"], dtype=np.float32)
    mem = np.asarray(inputs["mem"], dtype=np.float32)
    in_maps = []
    for b in range(x.shape[0]):
        m = dict(shared)
        m["x"] = np.ascontiguousarray(x[b])
        m["mem"] = np.ascontiguousarray(mem[b])
        m.update(consts)
        in_maps.append(m)
    res = run_bass_kernel_spmd(nc, in_maps, core_ids=list(range(len(in_maps))))
    return np.stack([np.asarray(r["out"], dtype=np.float32) for r in res.results], axis=0)
```

```python
import math
from contextlib import ExitStack

import numpy as np
import ml_dtypes
import concourse.bass as bass
import concourse.mybir as mybir
from concourse.bass_utils import run_bass_kernel_spmd

F32 = mybir.dt.float32
BF16 = mybir.dt.bfloat16
AF = mybir.ActivationFunctionType
ALU = mybir.AluOpType
AX = mybir.AxisListType

D = 2048
DB = 1024
MEM = 256
NG = 64
D_IN = 21512
EPS = 1e-6
import os
SAME_ENGINE_SYNC = os.environ.get("MK_SES", "1") == "1"

C_S5U, C_S5Z = 0, 1024
C_MLQ, C_MLK, C_MLV, C_MLO, C_MLZ = 2048, 3072, 4096, 5120, 6144
C_MLIF = 7168
C_DAQ, C_DAK, C_DAV, C_DAZ = 7176, 8200, 9224, 10248
C_XAQ, C_XAZ = 11272, 12296
C_GATE = 13320


class Obj:
    __slots__ = ("lw", "rd", "name")

    def __init__(self, name=""):
        self.lw = None
        self.rd = {}
        self.name = name


class Tile:
    def __init__(self, h, name):
        self.h = h
        self.o = Obj(name)

    def __getitem__(self, k):
        return self.h[k]


class Ring:
    def __init__(self, tiles):
        self.tiles = tiles
        self.i = 0

    def next(self):
        t = self.tiles[self.i]
        self.i = (self.i + 1) % len(self.tiles)
        return t


class DT:
    def __init__(self, nc, name, shape, dtype, kind="Internal"):
        self.h = nc.dram_tensor(name, list(shape), dtype, kind=kind)
        self.ap = self.h.ap()
        self.objs = {}
        self.name = name

    def o(self, key=0):
        if key not in self.objs:
            self.objs[key] = Obj(f"{self.name}:{key}")
        return self.objs[key]


class Sched:
    def __init__(self, nc, stack, n_sp=40, n_pool=8, n_act=4):
        self.nc = nc
        self.eng = {"pe": nc.tensor, "act": nc.scalar, "dve": nc.vector, "pool": nc.gpsimd, "sp": nc.sync}
        self.semobj = {}
        for e in ("pe", "act", "dve", "pool"):
            self.semobj[e] = stack.enter_context(nc.semaphore("s_" + e))
        self.tick = {e: 0 for e in ("pe", "act", "dve", "pool")}
        self.waited = {e: {} for e in self.eng}
        self.dq = {"sp": n_sp, "pool": n_pool, "act": n_act}
        self.dnext = {q: 0 for q in self.dq}
        self.dcnt = {}
        for q, n in self.dq.items():
            for i in range(n):
                self.semobj[(q, i)] = stack.enter_context(nc.semaphore(f"d_{q}{i}"))
                self.dcnt[(q, i)] = 0
        self.n_inst = 0
        self.released = {}

    def _deps(self, reads, writes):
        deps = []
        for t in reads:
            o = t.o if isinstance(t, Tile) else t
            if o.lw is not None:
                deps.append(o.lw)
        for t in writes:
            o = t.o if isinstance(t, Tile) else t
            if o.lw is not None:
                deps.append(o.lw)
            deps.extend(o.rd.items())
        return deps

    def _wait(self, e, deps):
        w = self.waited[e]
        need = {}
        for sk, val in deps:
            if w.get(sk, 0) < val and need.get(sk, 0) < val:
                need[sk] = val
        for sk, val in need.items():
            w[sk] = val
            self.eng[e].wait_ge(self.semobj[sk], val)
            self.n_inst += 1

    def _mark(self, tok, reads, writes):
        for t in writes:
            o = t.o if isinstance(t, Tile) else t
            o.lw = tok
            o.rd = {}
        for t in reads:
            o = t.o if isinstance(t, Tile) else t
            if o.rd.get(tok[0], 0) < tok[1]:
                o.rd[tok[0]] = tok[1]

    def op(self, e, fn, reads=(), writes=()):
        deps = self._deps(reads, writes)
        if e == "pe" or not SAME_ENGINE_SYNC:
            deps = [d for d in deps if d[0] != e]
        self._wait(e, deps)
        self.tick[e] += 1
        fn(self.eng[e]).then_inc(self.semobj[e], 1)
        self.n_inst += 1
        self._mark((e, self.tick[e]), reads, writes)

    def mm(self, items, reads=(), writes=(), skip=False):
        deps = [d for d in self._deps(reads, writes) if d[0] != "pe"]
        self._wait("pe", deps)
        n = len(items)
        for i, (out, lhsT, rhs, st, sp) in enumerate(items):
            if skip:
                ins = self.nc.tensor.matmul(out, lhsT, rhs, start=st, stop=sp, skip_group_check=True)
            else:
                ins = self.nc.tensor.matmul(out, lhsT, rhs, start=st, stop=sp)
            self.n_inst += 1
            if i == n - 1:
                self.tick["pe"] += 1
                ins.then_inc(self.semobj["pe"], 1)
        self._mark(("pe", self.tick["pe"]), reads, writes)

    def transpose(self, out, in_, ident, reads=(), writes=()):
        self.op("pe", lambda be: be.transpose(out, in_, ident), reads=reads, writes=writes)

    def dma(self, q, out, in_, reads=(), writes=(), slow=False):
        i = self.dnext[q]
        self.dnext[q] = (i + 1) % self.dq[q]
        sk = (q, i)
        prev = self.dcnt[sk]
        deps = self._deps(reads, writes)
        if q in self.tick and not SAME_ENGINE_SYNC:
            deps = [d for d in deps if d[0] != q]
        if prev > 0:
            deps.append((sk, prev))
        self._wait(q, deps)
        self.dcnt[sk] = prev + 16
        if slow:
            self.eng[q].dma_start(out=out, in_=in_, allow_slow_non_contiguous=True).then_inc(self.semobj[sk], 16)
        else:
            self.eng[q].dma_start(out=out, in_=in_).then_inc(self.semobj[sk], 16)
        self.n_inst += 1
        self._mark((sk, prev + 16), reads, writes)

    def final_wait(self, e, objs):
        deps = []
        for o in objs:
            if o.lw is not None:
                deps.append(o.lw)
        self._wait(e, deps)


class Scope(ExitStack):
    def __init__(self, S):
        super().__init__()
        self.S = S
        self.tiles = []

    def __exit__(self, *a):
        rel = self.S.released
        for t in self.tiles:
            o = t.o
            if o.lw is not None and rel.get(o.lw[0], 0) < o.lw[1]:
                rel[o.lw[0]] = o.lw[1]
            for k, v in o.rd.items():
                if rel.get(k, 0) < v:
                    rel[k] = v
        return super().__exit__(*a)


_UID = [0]


def _uname(name):
    _UID[0] += 1
    return f"{name}_{_UID[0]}"


def _new_tile(S, stack, h, name):
    t = Tile(h, name)
    t.o.rd = dict(S.released)
    stack.tiles.append(t)
    return t


def sb(S, stack, name, shape, dtype):
    return _new_tile(S, stack, stack.enter_context(S.nc.sbuf_tensor(_uname(name), list(shape), dtype)), name)


def ps(S, stack, name, shape, dtype=F32):
    return _new_tile(S, stack, stack.enter_context(S.nc.psum_tensor(_uname(name), list(shape), dtype)), name)


def sb_ring(S, stack, name, shape, dtype, n):
    return Ring([sb(S, stack, f"{name}{i}", shape, dtype) for i in range(n)])


def ps_ring(S, stack, name, shape, dtype, n):
    return Ring([ps(S, stack, f"{name}{i}", shape, dtype) for i in range(n)])


class Ctx:
    pass


def declare(nc, L, depth, debug):
    c = Ctx()
    c.L, c.depth = L, depth
    kin = "ExternalInput"
    c.x = DT(nc, "x", [L, D], F32, kin)
    c.mem = DT(nc, "mem", [MEM, D], F32, kin)
    shapes = dict(
        g_pre=[depth, D], w_in=[depth, D, D_IN], s5_lam_re=[depth, NG, 64], s5_lam_im=[depth, NG, 64],
        s5_log_dt=[depth, NG], s5_b_re=[depth, NG, 64, 16], s5_b_im=[depth, NG, 64, 16],
        s5_c_re=[depth, NG, 16, 64], s5_c_im=[depth, NG, 16, 64], s5_d=[depth, DB],
        s5_w_glu=[depth, DB, DB], s5_b_glu=[depth, DB], ml_conv_w=[depth, 4, 2 * DB], ml_conv_b=[depth, 2 * DB],
        ml_b_i=[depth, 4], ml_b_f=[depth, 4], ml_norm_g=[depth, DB], da_lq1=[depth, 64], da_lk1=[depth, 64],
        da_lq2=[depth, 64], da_lk2=[depth, 64], da_subln_g=[depth, 128], g_mem=[depth, D],
        xa_w_kv=[depth, D, 2 * DB], w_branch=[depth, 4, DB, D], w_out=[depth, D, D], g_post=[depth, D])
    c.w = {k: DT(nc, k, v, F32, kin) for k, v in shapes.items()}
    c.ident = DT(nc, "c_ident", [128, 128], BF16, kin)
    c.identf = DT(nc, "c_identf", [128, 128], F32, kin)
    c.trow = DT(nc, "c_trow", [1, L], F32, kin)
    c.ropec = DT(nc, "c_ropec", [128, L], BF16, kin)
    c.ropes = DT(nc, "c_ropes", [128, L], BF16, kin)
    c.maskkq = DT(nc, "c_maskkq", [128, 128], BF16, kin)
    c.maskg = DT(nc, "c_maskg", [128, 8], F32, kin)
    c.mask2 = DT(nc, "c_mask2", [128, 8, 128], F32, kin)
    c.out = DT(nc, "out", [L, D], F32, "ExternalOutput")
    sk = "ExternalOutput" if debug else "Internal"
    c.dbg = debug
    c.xs = [DT(nc, f"xs{i}", [L, D], F32, sk) for i in range(2)]
    c.s5uT = DT(nc, "s5uT", [DB, L], BF16, sk)
    c.s5zT = DT(nc, "s5zT", [DB, L], BF16, sk)
    c.mlqT = DT(nc, "mlqT", [DB, L], BF16, sk)
    c.mlkT = DT(nc, "mlkT", [DB, L], BF16, sk)
    c.mlv = DT(nc, "mlv", [L, DB], BF16, sk)
    c.mlo = DT(nc, "mlo", [L, DB], BF16, sk)
    c.mlz = DT(nc, "mlz", [L, DB], BF16, sk)
    c.mlif = DT(nc, "mlif", [8, L], F32, sk)
    c.daqT = DT(nc, "daqT", [DB, L], BF16, sk)
    c.dakT = DT(nc, "dakT", [DB, L], BF16, sk)
    c.dav = DT(nc, "dav", [L, DB], BF16, sk)
    c.daz = DT(nc, "daz", [L, DB], BF16, sk)
    c.xaqT = DT(nc, "xaqT", [DB, L], BF16, sk)
    c.xaz = DT(nc, "xaz", [L, DB], BF16, sk)
    c.gT = DT(nc, "gT", [4 * D, L], BF16, sk)
    c.yT = [DT(nc, f"yT{b}", [DB, L], BF16, sk) for b in range(4)]
    c.mT = DT(nc, "mT", [D, L], BF16, sk)
    c.mlg = DT(nc, "mlg", [3, 4, L + 1], F32, "Internal")
    c.s5yT = DT(nc, "s5yT", [DB, L], BF16, sk)
    return c


def bcast_rows(ap_row, nparts):
    return ap_row.partition_broadcast(nparts)


def phase_norm_T(S, c, src, g_row_ap, hT, L, rows=None, tag="a"):
    nc = S.nc
    rows = L if rows is None else rows
    with Scope(S) as st:
        gb = sb(S, st, tag + "_gb", [128, D], F32)
        S.dma("sp", gb[:], bcast_rows(g_row_ap, 128), writes=[gb])
        idt = sb(S, st, tag + "_id", [128, 128], BF16)
        S.dma("sp", idt[:], c.ident.ap[:, :], writes=[idt])
        xr = sb_ring(S, st, tag + "_x", [128, D], F32, 2)
        jr = sb_ring(S, st, tag + "_j", [128, D], F32, 1)
        hr = sb_ring(S, st, tag + "_h", [128, D], BF16, 2)
        ssr = sb_ring(S, st, tag + "_ss", [128, 4], F32, 2)
        pr = ps_ring(S, st, tag + "_p", [128, 512], BF16, 4)
        for t in range(rows // 128):
            xt = xr.next()
            S.dma("sp", xt[:], src.ap[t * 128:(t + 1) * 128, :], reads=[src.o(t)], writes=[xt])
            jk = jr.next()
            ss = ssr.next()
            S.op("act", lambda be: be.activation(out=jk[:], in_=xt[:], func=AF.Square, accum_out=ss[:, 0:1]),
                 reads=[xt], writes=[jk, ss])
            S.op("dve", lambda be: be.tensor_scalar(ss[:, 1:2], ss[:, 0:1], 1.0 / D, EPS, op0=ALU.mult, op1=ALU.add),
                 reads=[ss], writes=[ss])
            S.op("act", lambda be: be.activation(out=ss[:, 2:3], in_=ss[:, 1:2], func=AF.Sqrt), reads=[ss], writes=[ss])
            S.op("dve", lambda be: be.reciprocal(ss[:, 3:4], ss[:, 2:3]), reads=[ss], writes=[ss])
            ht = hr.next()
            S.op("dve", lambda be: be.scalar_tensor_tensor(out=ht[:], in0=xt[:], scalar=ss[:, 3:4], in1=gb[:],
                                                           op0=ALU.mult, op1=ALU.mult),
                 reads=[xt, ss, gb], writes=[ht])
            for q in range(4):
                pt = pr.next()
                for j in range(4):
                    kc = q * 4 + j
                    S.transpose(pt[:, j * 128:(j + 1) * 128], ht[:, kc * 128:(kc + 1) * 128], idt[:],
                                reads=[ht, idt], writes=[pt])
                eng = "act" if q % 2 else "dve"
                dst = hT[:, q * 4:(q + 1) * 4, t * 128:(t + 1) * 128]
                srcp = pt[:].rearrange("p (j n) -> p j n", j=4)
                if eng == "act":
                    S.op("act", lambda be: be.copy(out=dst, in_=srcp), reads=[pt], writes=[hT])
                else:
                    S.op("dve", lambda be: be.tensor_copy(dst, srcp), reads=[pt], writes=[hT])


def phase_inproj(S, c, l, hT, L):
    nc = S.nc
    w_in = c.w["w_in"].ap
    NT, NC = L // 128, L // 512
    with Scope(S) as st:
        wr = sb_ring(S, st, "ip_w", [128, 16, 512], BF16, 2)
        wsw = sb(S, st, "ip_wsw", [128, 16, 512], BF16)
        pr = ps_ring(S, st, "ip_p", [128, 512], F32, 6)
        sbf = sb_ring(S, st, "ip_sb", [128, 512], BF16, 4)
        sf32 = sb_ring(S, st, "ip_sf", [128, 512], F32, 2)
        pre = sb_ring(S, st, "ip_pre", [128, 3 + 512], F32, 2)
        acc = sb_ring(S, st, "ip_acc", [128, 512], F32, 2)
        rcr = sb_ring(S, st, "ip_rc", [128, 512], BF16, 2)
        rsr = sb_ring(S, st, "ip_rs", [128, 512], BF16, 2)
        cw = sb(S, st, "ip_cw", [128, 4, 16], F32)
        cb = sb(S, st, "ip_cb", [128, 16], F32)
        for j in range(4):
            S.dma("sp", cw[:, j, :], c.w["ml_conv_w"].ap[l, j].rearrange("(b p) -> p b", p=128), writes=[cw], slow=True)
        S.dma("sp", cb[:], c.w["ml_conv_b"].ap[l].rearrange("(b p) -> p b", p=128), writes=[cb], slow=True)

        def load_w(c0, n):
            wc = wr.next()
            S.dma("pool", wc[:, :, :n], w_in[l, :, c0:c0 + n].rearrange("(k p) n -> p k n", p=128), writes=[wc])
            return wc

        def tok_major(c0, dst, func):
            for cc in range(0, DB, 512):
                wc = load_w(c0 + cc, 512)
                for t in range(NT):
                    p = pr.next()
                    S.mm([(p[:], hT[:, kc, t * 128:(t + 1) * 128], wc[:, kc, :], kc == 0, kc == 15) for kc in range(16)],
                         reads=[hT, wc], writes=[p])
                    s = sbf.next()
                    if func is None:
                        S.op("dve", lambda be: be.tensor_copy(s[:], p[:]), reads=[p], writes=[s])
                    else:
                        S.op("act", lambda be: be.activation(out=s[:], in_=p[:], func=func), reads=[p], writes=[s])
                    S.dma("sp", dst.ap[t * 128:(t + 1) * 128, cc:cc + 512], s[:], reads=[s], writes=[dst.o(t)])

        def feat_major(c0, ncols, dst, row0, kind, cbase=0):
            for cc in range(0, ncols, 512):
                n = min(512, ncols - cc)
                wc = load_w(c0 + cc, n)
                if kind == "rope":
                    srcv = wc[:, :, :n].rearrange("p k (b h d) -> p k b h d", h=2, d=32)
                    dstv = wsw[:, :, :n].rearrange("p k (b h d) -> p k b h d", h=2, d=32)
                    for kc in range(16):
                        S.op("act" if kc % 2 else "dve",
                             (lambda be: be.copy(out=dstv[:, kc, :, 0, :], in_=srcv[:, kc, :, 1, :])) if kc % 2 else
                             (lambda be: be.tensor_copy(dstv[:, kc, :, 0, :], srcv[:, kc, :, 1, :])), reads=[wc], writes=[wsw])
                        S.op("act" if kc % 2 else "dve",
                             (lambda be: be.copy(out=dstv[:, kc, :, 1, :], in_=srcv[:, kc, :, 0, :])) if kc % 2 else
                             (lambda be: be.tensor_copy(dstv[:, kc, :, 1, :], srcv[:, kc, :, 0, :])), reads=[wc], writes=[wsw])
                for j in range(n // 128):
                    blk = (cc // 128) + j
                    prev = None
                    for tc in range(NC):
                        tsl = slice(tc * 512, (tc + 1) * 512)
                        p = pr.next()
                        S.mm([(p[:], wc[:, kc, j * 128:(j + 1) * 128], hT[:, kc, tsl], kc == 0, kc == 15)
                              for kc in range(16)], reads=[hT, wc], writes=[p])
                        s = sbf.next()
                        if kind == "copy":
                            S.op("dve", lambda be: be.tensor_copy(s[:], p[:]), reads=[p], writes=[s])
                        elif kind in ("silu", "sigmoid"):
                            f = AF.Silu if kind == "silu" else AF.Sigmoid
                            S.op("act", lambda be: be.activation(out=s[:], in_=p[:], func=f), reads=[p], writes=[s])
                        elif kind in ("conv", "convk"):
                            cblk = cbase + blk
                            pt = pre.next()
                            if prev is None:
                                S.op("pool", lambda be: be.memset(pt[:, 0:3], 0.0), writes=[pt])
                            else:
                                pv = prev
                                S.op("pool", lambda be: be.tensor_copy(pt[:, 0:3], pv[:, 512:515]), reads=[pv], writes=[pt])
                            S.op("act", lambda be: be.copy(out=pt[:, 3:515], in_=p[:]), reads=[p], writes=[pt])
                            a = acc.next()
                            S.op("dve", lambda be: be.tensor_scalar(a[:], pt[:, 3:515], cw[:, 3, cblk:cblk + 1], cb[:, cblk:cblk + 1],
                                                                   op0=ALU.mult, op1=ALU.add), reads=[pt, cw, cb], writes=[a])
                            for tap in range(3):
                                S.op("dve", lambda be: be.scalar_tensor_tensor(out=a[:], in0=pt[:, tap:tap + 512],
                                                                               scalar=cw[:, tap, cblk:cblk + 1], in1=a[:],
                                                                               op0=ALU.mult, op1=ALU.add),
                                     reads=[pt, cw, a], writes=[a])
                            if kind == "conv":
                                S.op("act", lambda be: be.activation(out=s[:], in_=a[:], func=AF.Silu), reads=[a], writes=[s])
                            else:
                                a2 = sf32.next()
                                S.op("act", lambda be: be.activation(out=a2[:], in_=a[:], func=AF.Silu), reads=[a], writes=[a2])
                                S.op("pool", lambda be: be.tensor_scalar(s[:], a2[:], 0.0625, None, op0=ALU.mult),
                                     reads=[a2], writes=[s])
                            prev = pt
                        elif kind == "rope":
                            p2 = pr.next()
                            S.mm([(p2[:], wsw[:, kc, j * 128:(j + 1) * 128], hT[:, kc, tsl], kc == 0, kc == 15)
                                  for kc in range(16)], reads=[hT, wsw], writes=[p2])
                            a = acc.next()
                            a2 = sf32.next()
                            rc, rs = rcr.next(), rsr.next()
                            S.dma("sp", rc[:], c.ropec.ap[:, tsl], writes=[rc])
                            S.dma("sp", rs[:], c.ropes.ap[:, tsl], writes=[rs])
                            S.op("dve", lambda be: be.tensor_tensor(a[:], p[:], rc[:], op=ALU.mult), reads=[p, rc], writes=[a])
                            S.op("dve", lambda be: be.tensor_tensor(a2[:], p2[:], rs[:], op=ALU.mult), reads=[p2, rs], writes=[a2])
                            S.op("pool", lambda be: be.tensor_tensor(s[:], a[:], a2[:], op=ALU.add), reads=[a, a2], writes=[s])
                        r0 = row0 + blk * 128
                        S.dma("sp", dst.ap[r0:r0 + 128, tsl], s[:], reads=[s], writes=[dst.o((r0 // 128, tc))])

        feat_major(C_S5U, DB, c.s5uT, 0, "copy")
        feat_major(C_S5Z, DB, c.s5zT, 0, "silu")
        feat_major(C_MLQ, DB, c.mlqT, 0, "conv", cbase=0)
        feat_major(C_MLK, DB, c.mlkT, 0, "convk", cbase=8)
        tok_major(C_MLV, c.mlv, None)
        tok_major(C_MLO, c.mlo, AF.Sigmoid)
        tok_major(C_MLZ, c.mlz, AF.Silu)
        wc = load_w(C_MLIF, 8)
        for tc in range(NC):
            tsl = slice(tc * 512, (tc + 1) * 512)
            p = pr.next()
            S.mm([(p[0:8, :], wc[:, kc, 0:8], hT[:, kc, tsl], kc == 0, kc == 15) for kc in range(16)],
                 reads=[hT, wc], writes=[p])
            a = acc.next()
            S.op("dve", lambda be: be.tensor_copy(a[0:8, :], p[0:8, :]), reads=[p], writes=[a])
            S.dma("sp", c.mlif.ap[:, tsl], a[0:8, :], reads=[a], writes=[c.mlif.o(tc)])
        feat_major(C_DAQ, DB, c.daqT, 0, "rope")
        feat_major(C_DAK, DB, c.dakT, 0, "rope")
        tok_major(C_DAV, c.dav, None)
        tok_major(C_DAZ, c.daz, AF.Silu)
        feat_major(C_XAQ, DB, c.xaqT, 0, "copy")
        tok_major(C_XAZ, c.xaz, AF.Silu)
        feat_major(C_GATE, 4 * D, c.gT, 0, "sigmoid")


def emit_yT(S, ytoks, dst, row0, nblk, tsl, key_tc, idt, ptr, ysbr):
    ysb = ysbr.next()
    for blk in range(nblk):
        pt = ptr.next()
        for qt in range(4):
            S.transpose(pt[:, qt * 128:(qt + 1) * 128], ytoks[qt][:, blk * 128:(blk + 1) * 128], idt[:],
                        reads=[ytoks[qt], idt], writes=[pt])
        if blk % 2:
            S.op("act", lambda be: be.copy(out=ysb[:, blk, :], in_=pt[:]), reads=[pt], writes=[ysb])
        else:
            S.op("dve", lambda be: be.tensor_copy(ysb[:, blk, :], pt[:]), reads=[pt], writes=[ysb])
    S.dma("sp", dst.ap[row0:row0 + nblk * 128, tsl].rearrange("(k p) n -> p k n", p=128), ysb[:, 0:nblk, :],
          reads=[ysb], writes=[dst.o((r, key_tc)) for r in range(row0 // 128, row0 // 128 + nblk)])


def phase_merge(S, c, l, L):
    NC = L // 512
    wb = c.w["w_branch"].ap
    with Scope(S) as st:
        wq = sb(S, st, "mg_w", [128, 32, 512], BF16)
        yr = sb_ring(S, st, "mg_y", [128, 32, 512], BF16, 2)
        gr = sb_ring(S, st, "mg_g", [128, 4, 512], BF16, 2)
        tr = sb_ring(S, st, "mg_t", [128, 4, 512], F32, 2)
        ar = sb_ring(S, st, "mg_a", [128, 2, 512], F32, 2)
        orr = sb_ring(S, st, "mg_o", [128, 512], BF16, 2)
        pr = ps_ring(S, st, "mg_p", [128, 512], F32, 8)
        gview = c.gT.ap.rearrange("(b r) n -> r b n", b=4)
        for dq in range(4):
            for b in range(4):
                S.dma("pool", wq[:, b * 8:(b + 1) * 8, :],
                      wb[l, b, :, dq * 512:(dq + 1) * 512].rearrange("(k p) n -> p k n", p=128), writes=[wq])
            for tc in range(NC):
                tsl = slice(tc * 512, (tc + 1) * 512)
                yt = yr.next()
                for b in range(4):
                    S.dma("sp", yt[:, b * 8:(b + 1) * 8, :], c.yT[b].ap[:, tsl].rearrange("(k p) n -> p k n", p=128),
                          reads=[c.yT[b].o((r, tc)) for r in range(8)], writes=[yt])
                for j in range(4):
                    r0 = dq * 512 + j * 128
                    gt = gr.next()
                    S.dma("sp", gt[:], gview[r0:r0 + 128, :, tsl],
                          reads=[c.gT.o(((b * D + r0) // 128, tc)) for b in range(4)], writes=[gt])
                    tt = tr.next()
                    for b in range(4):
                        p = pr.next()
                        S.mm([(p[:], wq[:, b * 8 + kc, j * 128:(j + 1) * 128], yt[:, b * 8 + kc, :], kc == 0, kc == 7)
                              for kc in range(8)], reads=[wq, yt], writes=[p])
                        S.op("dve", lambda be: be.tensor_tensor(tt[:, b, :], p[:], gt[:, b, :], op=ALU.mult),
                             reads=[p, gt], writes=[tt])
                    a = ar.next()
                    S.op("pool", lambda be: be.tensor_tensor(a[:], tt[:, 0:2, :], tt[:, 2:4, :], op=ALU.add), reads=[tt], writes=[a])
                    o = orr.next()
                    S.op("pool", lambda be: be.tensor_tensor(o[:], a[:, 0, :], a[:, 1, :], op=ALU.add), reads=[a], writes=[o])
                    S.dma("sp", c.mT.ap[r0:r0 + 128, tsl], o[:], reads=[o], writes=[c.mT.o((r0 // 128, tc))])


def phase_out(S, c, l, L, xin, xout):
    NT = L // 128
    with Scope(S) as st:
        wo = sb(S, st, "po_w", [128, 16, D], BF16)
        for q in range(4):
            S.dma("pool", wo[:, :, q * 512:(q + 1) * 512],
                  c.w["w_out"].ap[l, :, q * 512:(q + 1) * 512].rearrange("(k p) n -> p k n", p=128), writes=[wo])
        gp = sb(S, st, "po_g", [128, D], F32)
        S.dma("sp", gp[:], c.w["g_post"].ap[l:l + 1, :].partition_broadcast(128), writes=[gp])
        mr = sb_ring(S, st, "po_m", [128, 16, 128], BF16, 2)
        xr = sb_ring(S, st, "po_x", [128, D], F32, 2)
        orr = sb_ring(S, st, "po_o", [128, D], F32, 2)
        jr = sb_ring(S, st, "po_j", [128, 512], F32, 2)
        ssr = sb_ring(S, st, "po_ss", [128, 8], F32, 2)
        pr = ps_ring(S, st, "po_p", [128, 512], F32, 8)
        for t in range(NT):
            mt = mr.next()
            S.dma("sp", mt[:], c.mT.ap[:, t * 128:(t + 1) * 128].rearrange("(k p) n -> p k n", p=128),
                  reads=[c.mT.o((r, t // 4)) for r in range(16)], writes=[mt])
            xt = xr.next()
            S.dma("sp", xt[:], xin.ap[t * 128:(t + 1) * 128, :], reads=[xin.o(t)], writes=[xt])
            ss = ssr.next()
            pp = []
            for n in range(4):
                p = pr.next()
                pp.append(p)
                S.mm([(p[:], mt[:, kc, :], wo[:, kc, n * 512:(n + 1) * 512], kc == 0, kc == 15) for kc in range(16)],
                     reads=[mt, wo], writes=[p])
                jk = jr.next()
                S.op("act", lambda be: be.activation(out=jk[:], in_=p[:], func=AF.Square, accum_out=ss[:, n:n + 1]),
                     reads=[p], writes=[jk, ss])
            S.op("dve", lambda be: be.reduce_sum(out=ss[:, 4:5], in_=ss[:, 0:4], axis=AX.X), reads=[ss], writes=[ss])
            S.op("dve", lambda be: be.tensor_scalar(ss[:, 5:6], ss[:, 4:5], 1.0 / D, EPS, op0=ALU.mult, op1=ALU.add),
                 reads=[ss], writes=[ss])
            S.op("act", lambda be: be.activation(out=ss[:, 6:7], in_=ss[:, 5:6], func=AF.Sqrt), reads=[ss], writes=[ss])
            S.op("dve", lambda be: be.reciprocal(ss[:, 7:8], ss[:, 6:7]), reads=[ss], writes=[ss])
            ot = orr.next()
            for n in range(4):
                nsl = slice(n * 512, (n + 1) * 512)
                p = pp[n]
                S.op("dve", lambda be: be.scalar_tensor_tensor(out=ot[:, nsl], in0=p[:], scalar=ss[:, 7:8], in1=gp[:, nsl],
                                                               op0=ALU.mult, op1=ALU.mult), reads=[p, ss, gp], writes=[ot])
            S.op("pool", lambda be: be.tensor_tensor(ot[:], ot[:], xt[:], op=ALU.add), reads=[ot, xt], writes=[ot])
            S.dma("sp", xout.ap[t * 128:(t + 1) * 128, :], ot[:], reads=[ot], writes=[xout.o(t)])


def phase_xa(S, c, l, L):
    NC = L // 512
    wkv = c.w["xa_w_kv"].ap
    with Scope(S) as st:
        idt = sb(S, st, "xa_id", [128, 128], BF16)
        S.dma("sp", idt[:], c.ident.ap[:, :], writes=[idt])
        kT = sb(S, st, "xa_kT", [128, 8, MEM], BF16)
        vaug = sb(S, st, "xa_v", [128, 2, 4, 257], BF16)
        S.op("pool", lambda be: be.memset(vaug[:, :, :, 256:257], 1.0), writes=[vaug])
        with Scope(S) as st2:
            memT = sb(S, st2, "xa_memT", [128, 16, MEM], BF16)
            phase_norm_T(S, c, c.mem, c.w["g_mem"].ap[l:l + 1, :], memT, MEM, rows=MEM, tag="xn")
            wr = sb_ring(S, st2, "xa_w", [128, 16, 512], BF16, 2)
            pr = ps_ring(S, st2, "xa_pp", [128, 512], F32, 2)
            for q in range(4):
                wc = wr.next()
                S.dma("pool", wc[:], wkv[l, :, q * 512:(q + 1) * 512].rearrange("(k p) n -> p k n", p=128), writes=[wc])
                if q < 2:
                    for j in range(4):
                        p = pr.next()
                        S.mm([(p[:, 0:MEM], wc[:, kc, j * 128:(j + 1) * 128], memT[:, kc, :], kc == 0, kc == 15)
                              for kc in range(16)], reads=[wc, memT], writes=[p])
                        S.op("dve", lambda be: be.tensor_copy(kT[:, q * 4 + j, :], p[:, 0:MEM]), reads=[p], writes=[kT])
                else:
                    for mt in range(2):
                        p = pr.next()
                        S.mm([(p[:], memT[:, kc, mt * 128:(mt + 1) * 128], wc[:, kc, :], kc == 0, kc == 15)
                              for kc in range(16)], reads=[wc, memT], writes=[p])
                        h0 = (q - 2) * 2
                        S.op("dve", lambda be: be.tensor_copy(vaug[:, mt, h0:h0 + 2, 0:256],
                                                              p[:].rearrange("p (h d) -> p h d", h=2)),
                             reads=[p], writes=[vaug])
        qr = sb_ring(S, st, "xa_q", [128, 8, 512], BF16, 2)
        zr = sb_ring(S, st, "xa_z", [128, 4, DB], BF16, 2)
        ptr_ = sb_ring(S, st, "xa_pT", [128, 512], BF16, 4)
        yts = [sb_ring(S, st, f"xa_y{i}", [128, DB], BF16, 2) for i in range(4)]
        rr = sb_ring(S, st, "xa_r", [128, 2], F32, 4)
        ysbr = sb_ring(S, st, "xa_ysb", [128, 8, 512], BF16, 2)
        psr = ps_ring(S, st, "xa_ps", [128, 512], F32, 3)
        por = ps_ring(S, st, "xa_po", [128, 257], F32, 3)
        ptr2 = ps_ring(S, st, "xa_pt", [128, 512], BF16, 2)
        for tc in range(NC):
            tsl = slice(tc * 512, (tc + 1) * 512)
            qt_ = qr.next()
            S.dma("sp", qt_[:], c.xaqT.ap[:, tsl].rearrange("(k p) n -> p k n", p=128),
                  reads=[c.xaqT.o((r, tc)) for r in range(8)], writes=[qt_])
            zt = zr.next()
            S.dma("sp", zt[:], c.xaz.ap[tsl, :].rearrange("(t p) d -> p t d", p=128),
                  reads=[c.xaz.o(tc * 4 + i) for i in range(4)], writes=[zt])
            ytoks = [yts[i].next() for i in range(4)]
            for h in range(4):
                pTs = []
                for mt in range(2):
                    p = psr.next()
                    S.mm([(p[:], kT[:, h * 2 + db, mt * 128:(mt + 1) * 128], qt_[:, h * 2 + db, :], db == 0, db == 1)
                          for db in range(2)], reads=[kT, qt_], writes=[p])
                    pT = ptr_.next()
                    S.op("act", lambda be: be.activation(out=pT[:], in_=p[:], func=AF.Exp, scale=1.0 / 16.0),
                         reads=[p], writes=[pT])
                    pTs.append(pT)
                for qi in range(4):
                    po = por.next()
                    S.mm([(po[:], pTs[mt][:, qi * 128:(qi + 1) * 128], vaug[:, mt, h, :], mt == 0, mt == 1)
                          for mt in range(2)], reads=pTs + [vaug], writes=[po])
                    r = rr.next()
                    S.op("dve", lambda be: be.reciprocal(r[:, 0:1], po[:, 256:257]), reads=[po], writes=[r])
                    yk = ytoks[qi]
                    S.op("dve", lambda be: be.scalar_tensor_tensor(out=yk[:, h * 256:(h + 1) * 256], in0=po[:, 0:256],
                                                                   scalar=r[:, 0:1], in1=zt[:, qi, h * 256:(h + 1) * 256],
                                                                   op0=ALU.mult, op1=ALU.mult),
                         reads=[po, r, zt], writes=[yk])
            emit_yT(S, ytoks, c.yT[3], 0, 8, tsl, tc, idt, ptr2, ysbr)


def phase_da(S, c, l, L):
    NC, NT = L // 512, L // 128
    lam_init = 0.8 - 0.6 * math.exp(-0.3 * l)
    w = c.w
    with Scope(S) as st:
        idt = sb(S, st, "da_id", [128, 128], BF16)
        S.dma("sp", idt[:], c.ident.ap[:, :], writes=[idt])
        mk = sb(S, st, "da_mk", [128, 128], BF16)
        S.dma("sp", mk[:], c.maskkq.ap[:, :], writes=[mk])
        lt = sb(S, st, "da_lt", [128, 4, 64], F32)
        for i, nm in enumerate(("da_lq1", "da_lk1", "da_lq2", "da_lk2")):
            S.dma("sp", lt[:, i, :], w[nm].ap[l:l + 1, :].partition_broadcast(128), writes=[lt])
        lp = sb(S, st, "da_lp", [128, 2, 64], F32)
        lv = sb(S, st, "da_lv", [128, 8], F32)
        S.op("dve", lambda be: be.tensor_tensor(lp[:, 0, :], lt[:, 0, :], lt[:, 1, :], op=ALU.mult), reads=[lt], writes=[lp])
        S.op("dve", lambda be: be.tensor_tensor(lp[:, 1, :], lt[:, 2, :], lt[:, 3, :], op=ALU.mult), reads=[lt, lp], writes=[lp])
        S.op("dve", lambda be: be.reduce_sum(out=lv[:, 0:2], in_=lp[:], axis=AX.X), reads=[lp], writes=[lv])
        S.op("act", lambda be: be.activation(out=lv[:, 2:4], in_=lv[:, 0:2], func=AF.Exp), reads=[lv], writes=[lv])
        S.op("dve", lambda be: be.tensor_tensor(lv[:, 4:5], lv[:, 3:4], lv[:, 2:3], op=ALU.subtract), reads=[lv], writes=[lv])
        S.op("dve", lambda be: be.tensor_scalar(lv[:, 5:6], lv[:, 4:5], -lam_init, None, op0=ALU.add), reads=[lv], writes=[lv])
        sg = sb(S, st, "da_sg", [128, 128], F32)
        S.dma("sp", sg[:], w["da_subln_g"].ap[l:l + 1, :].partition_broadcast(128), writes=[sg])
        S.op("dve", lambda be: be.tensor_scalar(sg[:], sg[:], 1.0 - lam_init, None, op0=ALU.mult), reads=[sg], writes=[sg])

        kr = sb_ring(S, st, "da_k", [128, L], BF16, 2)
        vr = sb_ring(S, st, "da_v", [128, NT, 129], BF16, 2)
        qr = sb_ring(S, st, "da_q", [128, 512], BF16, 2)
        zr = sb_ring(S, st, "da_z", [128, 4, 128], BF16, 2)
        pTr = sb_ring(S, st, "da_pT", [128, 512], BF16, 4)
        yts = [sb_ring(S, st, f"da_y{i}", [128, 128], BF16, 2) for i in range(4)]
        ar = sb_ring(S, st, "da_a", [128, 128], F32, 2)
        dr = sb_ring(S, st, "da_d", [128, 128], F32, 2)
        jr = sb_ring(S, st, "da_j", [128, 128], F32, 2)
        rr = sb_ring(S, st, "da_r", [128, 8], F32, 4)
        ysbr = sb_ring(S, st, "da_ysb", [128, 1, 512], BF16, 2)
        psr = ps_ring(S, st, "da_ps", [128, 512], F32, 3)
        accs = [ps(S, st, f"da_acc{i}", [128, 3, 129], F32) for i in range(3)]
        ptr2 = ps_ring(S, st, "da_pt", [128, 512], BF16, 1)

        def acc(comp, qt):
            i = comp * 4 + qt
            return accs[i // 3], i % 3

        for h in range(8):
            kt_ = kr.next()
            S.dma("sp", kt_[:], c.dakT.ap[h * 128:(h + 1) * 128, :], reads=[c.dakT.o((h, tc)) for tc in range(NC)], writes=[kt_])
            vt = vr.next()
            S.dma("sp", vt[:, :, 0:128], c.dav.ap[:, h * 128:(h + 1) * 128].rearrange("(t p) d -> p t d", p=128),
                  reads=[c.dav.o(t) for t in range(NT)], writes=[vt])
            S.op("pool", lambda be: be.memset(vt[:, :, 128:129], 1.0), writes=[vt])
            for tc in range(NC):
                tsl = slice(tc * 512, (tc + 1) * 512)
                qt_ = qr.next()
                S.dma("sp", qt_[:], c.daqT.ap[h * 128:(h + 1) * 128, tsl], reads=[c.daqT.o((h, tc))], writes=[qt_])
                zt = zr.next()
                S.dma("sp", zt[:], c.daz.ap[tsl, h * 128:(h + 1) * 128].rearrange("(t p) d -> p t d", p=128),
                      reads=[c.daz.o(tc * 4 + i) for i in range(4)], writes=[zt])
                nk = 4 * tc + 4
                for a_t in accs:
                    S.op("dve", lambda be: be.memset(a_t[:], 0.0), writes=[a_t])
                steps = [(kt, comp) for kt in range(nk) for comp in range(2)]

                def emit_st(i):
                    kt, comp = steps[i]
                    dq = kt - 4 * tc
                    q0 = max(dq, 0) * 128
                    csl = slice(comp * 64, (comp + 1) * 64)
                    p = psr.next()
                    S.mm([(p[:, q0:512], kt_[csl, kt * 128:(kt + 1) * 128], qt_[csl, q0:512], True, True)],
                         reads=[kt_, qt_], writes=[p])
                    pT = pTr.next()
                    S.op("act", lambda be: be.activation(out=pT[:, q0:512], in_=p[:, q0:512], func=AF.Exp, scale=0.125),
                         reads=[p], writes=[pT])
                    if dq >= 0:
                        S.op("dve", lambda be: be.tensor_tensor(pT[:, q0:q0 + 128], pT[:, q0:q0 + 128], mk[:], op=ALU.mult),
                             reads=[pT, mk], writes=[pT])
                    return pT

                LOOK = 2
                pts = {}
                for i in range(min(LOOK, len(steps))):
                    pts[i] = emit_st(i)
                for i in range(len(steps)):
                    if i + LOOK < len(steps):
                        pts[i + LOOK] = emit_st(i + LOOK)
                    kt, comp = steps[i]
                    dq = kt - 4 * tc
                    pT = pts.pop(i)
                    for qi in range(max(dq, 0), 4):
                        a_t, a_i = acc(comp, qi)
                        S.mm([(a_t[:, a_i, :], pT[:, qi * 128:(qi + 1) * 128], vt[:, kt, :], False, False)],
                             reads=[pT, vt], writes=[a_t], skip=True)
                ytoks = []
                for qi in range(4):
                    a0, i0 = acc(0, qi)
                    a1, i1 = acc(1, qi)
                    r = rr.next()
                    S.op("dve", lambda be: be.reciprocal(r[:, 0:1], a0[:, i0, 128:129]), reads=[a0], writes=[r])
                    S.op("dve", lambda be: be.reciprocal(r[:, 1:2], a1[:, i1, 128:129]), reads=[a1, r], writes=[r])
                    S.op("dve", lambda be: be.tensor_tensor(r[:, 2:3], r[:, 1:2], lv[:, 5:6], op=ALU.mult), reads=[r, lv], writes=[r])
                    a = ar.next()
                    S.op("act", lambda be: be.activation(out=a[:], in_=a0[:, i0, 0:128], func=AF.Copy, scale=r[:, 0:1]),
                         reads=[a0, r], writes=[a])
                    d = dr.next()
                    S.op("dve", lambda be: be.scalar_tensor_tensor(out=d[:], in0=a1[:, i1, 0:128], scalar=r[:, 2:3], in1=a[:],
                                                                   op0=ALU.mult, op1=ALU.add), reads=[a1, r, a], writes=[d])
                    jk = jr.next()
                    S.op("act", lambda be: be.activation(out=jk[:], in_=d[:], func=AF.Square, accum_out=r[:, 3:4]),
                         reads=[d, r], writes=[jk, r])
                    S.op("dve", lambda be: be.tensor_scalar(r[:, 4:5], r[:, 3:4], 1.0 / 128.0, EPS, op0=ALU.mult, op1=ALU.add),
                         reads=[r], writes=[r])
                    S.op("act", lambda be: be.activation(out=r[:, 5:6], in_=r[:, 4:5], func=AF.Sqrt), reads=[r], writes=[r])
                    S.op("dve", lambda be: be.reciprocal(r[:, 6:7], r[:, 5:6]), reads=[r], writes=[r])
                    S.op("dve", lambda be: be.scalar_tensor_tensor(out=d[:], in0=d[:], scalar=r[:, 6:7], in1=sg[:],
                                                                   op0=ALU.mult, op1=ALU.mult), reads=[d, r, sg], writes=[d])
                    yk = yts[qi].next()
                    S.op("pool", lambda be: be.tensor_tensor(yk[:], d[:], zt[:, qi, :], op=ALU.mult), reads=[d, zt], writes=[yk])
                    ytoks.append(yk)
                emit_yT(S, ytoks, c.yT[2], h * 128, 1, tsl, tc, idt, ptr2, ysbr)


def phase_ml(S, c, l, L):
    NCH = L // 128
    w = c.w
    mlg = c.mlg
    with Scope(S) as st:
        Bcol = sb(S, st, "ml_Bcol", [128, NCH, 4], F32)
        Ecol = sb(S, st, "ml_Ecol", [128, NCH, 4], F32)
        mu = sb(S, st, "ml_mu", [128, 4, NCH + 1], F32)
        negmu = sb(S, st, "ml_nmu", [128, 4, NCH + 1], F32)
        dec = sb(S, st, "ml_dec", [128, 4, NCH], F32)
        with Scope(S) as g:
            ig = sb(S, g, "mlg_i", [4, L], F32)
            fg = sb(S, g, "mlg_f", [4, L], F32)
            Ft = sb(S, g, "mlg_F", [4, L], F32)
            Gt = sb(S, g, "mlg_G", [4, L], F32)
            ones = sb(S, g, "mlg_1", [4, L], F32)
            bb = sb(S, g, "mlg_b", [4, 4], F32)
            S.dma("sp", ig[:], c.mlif.ap[0:4, :], reads=[c.mlif.o(tc) for tc in range(L // 512)], writes=[ig])
            S.dma("sp", fg[:], c.mlif.ap[4:8, :], reads=[c.mlif.o(tc) for tc in range(L // 512)], writes=[fg])
            S.dma("sp", bb[:, 0:1], w["ml_b_i"].ap[l].rearrange("(h o) -> h o", o=1), writes=[bb], slow=True)
            S.dma("sp", bb[:, 1:2], w["ml_b_f"].ap[l].rearrange("(h o) -> h o", o=1), writes=[bb], slow=True)
            S.op("dve", lambda be: be.tensor_scalar(bb[:, 2:3], bb[:, 1:2], -1.0, None, op0=ALU.mult), reads=[bb], writes=[bb])
            S.op("pool", lambda be: be.memset(ones[:], 1.0), writes=[ones])
            S.op("pool", lambda be: be.memset(bb[:, 3:4], 0.0), reads=[bb], writes=[bb])
            S.op("act", lambda be: be.activation(out=fg[:], in_=fg[:], func=AF.Exp, scale=-1.0, bias=bb[:, 2:3]),
                 reads=[fg, bb], writes=[fg])
            S.op("act", lambda be: be.activation(out=fg[:], in_=fg[:], func=AF.Ln, bias=1.0), reads=[fg], writes=[fg])
            S.op("dve", lambda be: be.tensor_scalar(fg[:], fg[:], -1.0, None, op0=ALU.mult), reads=[fg], writes=[fg])
            S.op("dve", lambda be: be.tensor_tensor_scan(out=Ft[:], data0=ones[:], data1=fg[:], initial=0.0,
                                                         op0=ALU.mult, op1=ALU.add), reads=[ones, fg], writes=[Ft])
            S.op("dve", lambda be: be.scalar_tensor_tensor(out=ig[:], in0=ig[:], scalar=bb[:, 0:1], in1=Ft[:],
                                                           op0=ALU.add, op1=ALU.subtract), reads=[ig, bb, Ft], writes=[ig])
            S.op("dve", lambda be: be.tensor_tensor_scan(out=Gt[:], data0=ones[:], data1=ig[:], initial=0.0,
                                                         op0=ALU.mult, op1=ALU.max), reads=[ones, ig], writes=[Gt])
            S.op("dve", lambda be: be.tensor_tensor(fg[:], Ft[:], Gt[:], op=ALU.add), reads=[Ft, Gt, fg], writes=[fg])
            S.op("act", lambda be: be.activation(out=fg[:], in_=fg[:], func=AF.Exp, scale=-1.0), reads=[fg], writes=[fg])
            S.dma("sp", mlg.ap[0, :, 0:L], ig[:], reads=[ig], writes=[mlg.o(0)])
            S.dma("sp", mlg.ap[1, :, 0:L], fg[:], reads=[fg], writes=[mlg.o(1)])
            S.dma("sp", mlg.ap[2, :, 1:L + 1], Gt[:], reads=[Gt], writes=[mlg.o(2)])
            S.dma("sp", mlg.ap[2, :, 0:1], bb[:, 3:4], reads=[bb], writes=[mlg.o(3)], slow=True)
            for h in range(4):
                S.dma("sp", Bcol[:, :, h], mlg.ap[0, h, 0:L].rearrange("(c t) -> t c", t=128), reads=[mlg.o(0)], writes=[Bcol], slow=True)
                S.dma("sp", Ecol[:, :, h], mlg.ap[1, h, 0:L].rearrange("(c t) -> t c", t=128), reads=[mlg.o(1)], writes=[Ecol], slow=True)
                S.dma("sp", mu[:, h, :], mlg.ap[2, h:h + 1, 0:L + 1:128].partition_broadcast(128),
                      reads=[mlg.o(2), mlg.o(3)], writes=[mu], slow=True)
        S.op("dve", lambda be: be.tensor_scalar(negmu[:], mu[:], -1.0, None, op0=ALU.mult), reads=[mu], writes=[negmu])
        S.op("dve", lambda be: be.tensor_tensor(dec[:], mu[:, :, 0:NCH], mu[:, :, 1:NCH + 1], op=ALU.subtract), reads=[mu], writes=[dec])
        S.op("act", lambda be: be.activation(out=dec[:], in_=dec[:], func=AF.Exp), reads=[dec], writes=[dec])

        idt = sb(S, st, "ml_id", [128, 128], BF16)
        S.dma("sp", idt[:], c.ident.ap[:, :], writes=[idt])
        mk = sb(S, st, "ml_mk", [128, 128], BF16)
        S.dma("sp", mk[:], c.maskkq.ap[:, :], writes=[mk])
        ng = sb(S, st, "ml_ng", [128, DB], F32)
        S.dma("sp", ng[:], w["ml_norm_g"].ap[l:l + 1, :].partition_broadcast(128), writes=[ng])
        qT = sb(S, st, "ml_qT", [128, 2, L], BF16)
        kT = sb(S, st, "ml_kT", [128, 2, L], BF16)
        va = sb(S, st, "ml_va", [128, NCH, 257], BF16)
        ot = sb(S, st, "ml_o", [128, NCH, 256], BF16)
        zt = sb(S, st, "ml_z", [128, NCH, 256], BF16)
        C32 = sb(S, st, "ml_C32", [128, 2, 257], F32)
        Cbf = sb(S, st, "ml_Cbf", [128, 2, 257], BF16)
        gbr = sb_ring(S, st, "ml_gb", [128, 128], F32, 3)
        ptr_ = sb_ring(S, st, "ml_pt", [128, 128], F32, 2)
        ptmr = sb_ring(S, st, "ml_ptm", [128, 128], F32, 2)
        str_ = sb_ring(S, st, "ml_st", [128, 128], BF16, 2)
        scr = sb_ring(S, st, "ml_sc", [128, 128], F32, 2)
        qsr = sb_ring(S, st, "ml_qs", [128, 2, 128], BF16, 2)
        rr = sb_ring(S, st, "ml_r", [128, 8], F32, 3)
        hr = sb_ring(S, st, "ml_h", [128, 256], F32, 2)
        jr = sb_ring(S, st, "ml_j", [128, 256], F32, 1)
        yr = sb_ring(S, st, "ml_y", [128, 256], BF16, 2)
        ysr = sb_ring(S, st, "ml_ys", [128, 2, 128], BF16, 2)
        kwr = sb_ring(S, st, "ml_kw", [128, 2], F32, 2)
        kkr = sb_ring(S, st, "ml_kk", [128, 256], BF16, 2)
        psS = ps_ring(S, st, "ml_pS", [128, 128], F32, 2)
        psN = ps_ring(S, st, "ml_pN", [128, 257], F32, 2)
        psT = ps_ring(S, st, "ml_pT", [128, 2, 128], BF16, 2)
        psC = [ps(S, st, f"ml_pC{i}", [128, 257], F32) for i in range(2)]
        for h in range(4):
            hs = slice(h * 256, (h + 1) * 256)
            S.dma("sp", qT[:], c.mlqT.ap[hs, :].rearrange("(k p) n -> p k n", p=128),
                  reads=[c.mlqT.o((2 * h + k, tc)) for k in range(2) for tc in range(L // 512)], writes=[qT])
            S.dma("sp", kT[:], c.mlkT.ap[hs, :].rearrange("(k p) n -> p k n", p=128),
                  reads=[c.mlkT.o((2 * h + k, tc)) for k in range(2) for tc in range(L // 512)], writes=[kT])
            S.dma("sp", va[:, :, 0:256], c.mlv.ap[:, hs].rearrange("(c p) d -> p c d", p=128),
                  reads=[c.mlv.o(t) for t in range(NCH)], writes=[va])
            S.op("pool", lambda be: be.memset(va[:, :, 256:257], 1.0), writes=[va])
            S.dma("sp", ot[:], c.mlo.ap[:, hs].rearrange("(c p) d -> p c d", p=128), reads=[c.mlo.o(t) for t in range(NCH)], writes=[ot])
            S.dma("sp", zt[:], c.mlz.ap[:, hs].rearrange("(c p) d -> p c d", p=128), reads=[c.mlz.o(t) for t in range(NCH)], writes=[zt])
            for ch in range(NCH):
                csl = slice(ch * 128, (ch + 1) * 128)
                gb = gbr.next()
                S.dma("sp", gb[:], mlg.ap[2, h:h + 1, 1 + ch * 128:1 + (ch + 1) * 128].partition_broadcast(128),
                      reads=[mlg.o(2)], writes=[gb])
                pS = psS.next()
                S.mm([(pS[:], kT[:, db, csl], qT[:, db, csl], db == 0, db == 1) for db in range(2)], reads=[kT, qT], writes=[pS])
                pt = ptr_.next()
                S.op("act", lambda be: be.activation(out=pt[:], in_=gb[:], func=AF.Exp, scale=-1.0, bias=Bcol[:, ch, h:h + 1]),
                     reads=[gb, Bcol], writes=[pt])
                ptm = ptmr.next()
                S.op("pool", lambda be: be.tensor_tensor(ptm[:], pt[:], mk[:], op=ALU.mult), reads=[pt, mk], writes=[ptm])
                stt = str_.next()
                S.op("dve", lambda be: be.tensor_tensor(stt[:], pS[:], ptm[:], op=ALU.mult), reads=[pS, ptm], writes=[stt])
                items = [(None, stt[:], va[:, ch, :])]
                rds = [stt, va]
                if ch > 0:
                    sc = scr.next()
                    S.op("act", lambda be: be.activation(out=sc[:], in_=gb[:], func=AF.Exp, scale=-1.0, bias=mu[:, h, ch:ch + 1]),
                         reads=[gb, mu], writes=[sc])
                    qs = qsr.next()
                    for db in range(2):
                        S.op("pool", lambda be: be.tensor_tensor(qs[:, db, :], qT[:, db, csl], sc[:], op=ALU.mult),
                             reads=[qT, sc], writes=[qs])
                    items += [(None, qs[:, 0, :], Cbf[:, 0, :]), (None, qs[:, 1, :], Cbf[:, 1, :])]
                    rds += [qs, Cbf]
                pN = psN.next()
                n = len(items)
                S.mm([(pN[:], a, b, i == 0, i == n - 1) for i, (_, a, b) in enumerate(items)], reads=rds, writes=[pN])
                r = rr.next()
                S.op("act", lambda be: be.activation(out=r[:, 6:7], in_=pN[:, 256:257], func=AF.Abs), reads=[pN], writes=[r])
                S.op("dve", lambda be: be.tensor_tensor(r[:, 0:1], r[:, 6:7], Ecol[:, ch, h:h + 1], op=ALU.max),
                     reads=[r, Ecol], writes=[r])
                S.op("dve", lambda be: be.reciprocal(r[:, 1:2], r[:, 0:1]), reads=[r], writes=[r])
                hh = hr.next()
                S.op("act", lambda be: be.activation(out=hh[:], in_=pN[:, 0:256], func=AF.Copy, scale=r[:, 1:2]),
                     reads=[pN, r], writes=[hh])
                jk = jr.next()
                S.op("act", lambda be: be.activation(out=jk[:], in_=hh[:], func=AF.Square, accum_out=r[:, 2:3]),
                     reads=[hh, r], writes=[jk, r])
                S.op("dve", lambda be: be.tensor_scalar(r[:, 3:4], r[:, 2:3], 1.0 / 256.0, EPS, op0=ALU.mult, op1=ALU.add),
                     reads=[r], writes=[r])
                S.op("act", lambda be: be.activation(out=r[:, 4:5], in_=r[:, 3:4], func=AF.Sqrt), reads=[r], writes=[r])
                S.op("dve", lambda be: be.reciprocal(r[:, 5:6], r[:, 4:5]), reads=[r], writes=[r])
                S.op("dve", lambda be: be.scalar_tensor_tensor(out=hh[:], in0=hh[:], scalar=r[:, 5:6], in1=ng[:, hs],
                                                               op0=ALU.mult, op1=ALU.mult), reads=[hh, r, ng], writes=[hh])
                S.op("pool", lambda be: be.tensor_tensor(hh[:], hh[:], ot[:, ch, :], op=ALU.mult), reads=[hh, ot], writes=[hh])
                yk = yr.next()
                S.op("pool", lambda be: be.tensor_tensor(yk[:], hh[:], zt[:, ch, :], op=ALU.mult), reads=[hh, zt], writes=[yk])
                pT = psT.next()
                for db in range(2):
                    S.transpose(pT[:, db, :], yk[:, db * 128:(db + 1) * 128], idt[:], reads=[yk, idt], writes=[pT])
                ys = ysr.next()
                S.op("act", lambda be: be.copy(out=ys[:], in_=pT[:]), reads=[pT], writes=[ys])
                S.dma("sp", c.yT[1].ap[hs, csl].rearrange("(k p) n -> p k n", p=128), ys[:], reads=[ys],
                      writes=[c.yT[1].o((2 * h + k, ch // 4)) for k in range(2)])
                if ch == NCH - 1:
                    continue
                kw = kwr.next()
                S.op("act", lambda be: be.activation(out=kw[:, 0:1], in_=Bcol[:, ch, h:h + 1], func=AF.Exp,
                                                     bias=negmu[:, h, ch + 1:ch + 2]), reads=[Bcol, negmu], writes=[kw])
                pK = psT.next()
                for db in range(2):
                    S.transpose(pK[:, db, :], kT[:, db, csl], idt[:], reads=[kT, idt], writes=[pK])
                kk = kkr.next()
                S.op("dve", lambda be: be.tensor_scalar(kk[:], pK[:].rearrange("p a b -> p (a b)"), kw[:, 0:1], None, op0=ALU.mult),
                     reads=[pK, kw], writes=[kk])
                for db in range(2):
                    S.mm([(psC[db][:], kk[:, db * 128:(db + 1) * 128], va[:, ch, :], True, True)], reads=[kk, va], writes=[psC[db]])
                    if ch == 0:
                        S.op("dve", lambda be: be.tensor_copy(C32[:, db, :], psC[db][:]), reads=[psC[db]], writes=[C32])
                    else:
                        S.op("dve", lambda be: be.scalar_tensor_tensor(out=C32[:, db, :], in0=C32[:, db, :], scalar=dec[:, h, ch:ch + 1],
                                                                       in1=psC[db][:], op0=ALU.mult, op1=ALU.add),
                             reads=[C32, dec, psC[db]], writes=[C32])
                S.op("act", lambda be: be.copy(out=Cbf[:], in_=C32[:]), reads=[C32], writes=[Cbf])


TWO_PI = 2.0 * math.pi


def _sincos(S, out_t, ang_src, th, off, kt):
    if isinstance(th, float):
        S.op("dve", lambda be: be.tensor_scalar(out_t[0], ang_src[0], th, off, op0=ALU.mult, op1=ALU.add),
             reads=ang_src[1], writes=[out_t[1]])
    else:
        S.op("act", lambda be: be.activation(out=out_t[0], in_=ang_src[0], func=AF.Identity, scale=th, bias=off),
             reads=ang_src[1], writes=[out_t[1]])
    S.op("dve", lambda be: be.tensor_copy(kt[0], out_t[0]), reads=[out_t[1]], writes=[kt[1]])
    S.op("pool", lambda be: be.tensor_tensor(out_t[0], out_t[0], kt[0], op=ALU.subtract), reads=[out_t[1], kt[1]], writes=[out_t[1]])
    S.op("act", lambda be: be.activation(out=out_t[0], in_=out_t[0], func=AF.Sin, scale=TWO_PI * (1.0 - 1e-6)),
         reads=[out_t[1]], writes=[out_t[1]])


def phase_s5(S, c, l, L):
    w = c.w
    SEG = min(L, 1024)
    NSEG = L // SEG
    NCS = SEG // 512
    OFF_S = 0.0
    OFF_C = 0.25
    with Scope(S) as st:
        BBpad = sb(S, st, "s5_BB", [128, NG, 128], BF16)
        BBsw = sb(S, st, "s5_BBs", [128, NG, 128], BF16)
        CCpad = sb(S, st, "s5_CC", [128, NG, 128], BF16)
        r2 = sb(S, st, "s5_r2", [128, NG], F32)
        th = sb(S, st, "s5_th", [128, NG], F32)
        offs = sb(S, st, "s5_offs", [128, 2], F32)
        dsk = sb(S, st, "s5_dsk", [128, 8], F32)
        bgl = sb(S, st, "s5_bg", [128, 8], F32)
        S.dma("sp", dsk[:], w["s5_d"].ap[l].rearrange("(b p) -> p b", p=128), writes=[dsk], slow=True)
        S.dma("sp", bgl[:], w["s5_b_glu"].ap[l].rearrange("(b p) -> p b", p=128), writes=[bgl], slow=True)
        S.op("pool", lambda be: be.memset(offs[0:64, 0:1], OFF_S), writes=[offs])
        S.op("pool", lambda be: be.memset(offs[64:128, 0:1], OFF_S + 0.5), reads=[offs], writes=[offs])
        S.op("pool", lambda be: be.memset(offs[:, 1:2], OFF_C), reads=[offs], writes=[offs])
        with Scope(S) as pp:
            def t3(name):
                return sb(S, pp, name, [128, 8, 64], F32)
            lre, lim, dt, er, cs, sn, wr, wi, t1, t2, Br, Bi, Bbr, Bbi = [t3(f"s5p{i}") for i in range(14)]
            mg = sb(S, pp, "s5_mg", [128, 8], F32)
            dt8 = sb(S, pp, "s5_dt8", [128, 8], F32)
            m2 = sb(S, pp, "s5_m2", [128, 8, 128], F32)
            S.dma("sp", mg[:], c.maskg.ap[:, :], writes=[mg])
            S.dma("sp", m2[:], c.mask2.ap[:, :, :], writes=[m2])
            hre, him, hdt = w["s5_lam_re"].h, w["s5_lam_im"].h, w["s5_log_dt"].h
            for g8 in range(8):
                ps_ = slice(g8 * 16, (g8 + 1) * 16)
                S.dma("sp", lre[ps_, :, :], bass.AP(tensor=hre, offset=l * 4096 + g8 * 64, ap=[[0, 16], [512, 8], [1, 64]]), writes=[lre], slow=True)
                S.dma("sp", lim[ps_, :, :], bass.AP(tensor=him, offset=l * 4096 + g8 * 64, ap=[[0, 16], [512, 8], [1, 64]]), writes=[lim], slow=True)
                S.dma("sp", dt8[ps_, :], bass.AP(tensor=hdt, offset=l * 64 + g8, ap=[[0, 16], [8, 8]]), writes=[dt8], slow=True)
                for blk in range(8):
                    S.dma("sp", Br[ps_, blk, :], w["s5_b_re"].ap[l, blk * 8 + g8].rearrange("p c -> c p"), writes=[Br], slow=True)
                    S.dma("sp", Bi[ps_, blk, :], w["s5_b_im"].ap[l, blk * 8 + g8].rearrange("p c -> c p"), writes=[Bi], slow=True)

            def V(e, fn, rd, wr_):
                S.op(e, fn, reads=rd, writes=wr_)
            V("dve", lambda be: be.tensor_scalar(lre[:], lre[:], -1e-4, None, op0=ALU.min), [lre], [lre])
            V("act", lambda be: be.activation(out=dt8[:], in_=dt8[:], func=AF.Exp), [dt8], [dt8])
            V("dve", lambda be: be.tensor_copy(dt[:], dt8[:].unsqueeze(2).to_broadcast([128, 8, 64])), [dt8], [dt])
            V("dve", lambda be: be.tensor_tensor(t1[:], lre[:], dt[:], op=ALU.mult), [lre, dt], [t1])
            V("act", lambda be: be.activation(out=er[:], in_=t1[:], func=AF.Exp), [t1], [er])
            V("dve", lambda be: be.tensor_tensor(t2[:], lim[:], dt[:], op=ALU.mult), [lim, dt], [t2])
            kA = sb(S, pp, "s5_kA", [128, 8, 64], mybir.dt.int32)
            _sincos(S, (cs[:], cs), (t2[:], [t2]), 1.0 / TWO_PI, OFF_C, (kA[:], kA))
            _sincos(S, (sn[:], sn), (t2[:], [t2]), 1.0 / TWO_PI, OFF_S, (kA[:], kA))
            V("dve", lambda be: be.tensor_tensor(cs[:], cs[:], er[:], op=ALU.mult), [cs, er], [cs])
            V("dve", lambda be: be.tensor_tensor(sn[:], sn[:], er[:], op=ALU.mult), [sn, er], [sn])
            V("dve", lambda be: be.tensor_scalar(cs[:], cs[:], -1.0, None, op0=ALU.add), [cs], [cs])
            V("dve", lambda be: be.tensor_tensor(t1[:], lre[:], lre[:], op=ALU.mult), [lre], [t1])
            V("dve", lambda be: be.tensor_tensor(t2[:], lim[:], lim[:], op=ALU.mult), [lim], [t2])
            V("dve", lambda be: be.tensor_tensor(t1[:], t1[:], t2[:], op=ALU.add), [t1, t2], [t1])
            V("dve", lambda be: be.reciprocal(t1[:], t1[:]), [t1], [t1])
            V("dve", lambda be: be.tensor_tensor(wr[:], cs[:], lre[:], op=ALU.mult), [cs, lre], [wr])
            V("dve", lambda be: be.tensor_tensor(t2[:], sn[:], lim[:], op=ALU.mult), [sn, lim], [t2])
            V("dve", lambda be: be.tensor_tensor(wr[:], wr[:], t2[:], op=ALU.add), [wr, t2], [wr])
            V("dve", lambda be: be.tensor_tensor(wr[:], wr[:], t1[:], op=ALU.mult), [wr, t1], [wr])
            V("dve", lambda be: be.tensor_tensor(wi[:], sn[:], lre[:], op=ALU.mult), [sn, lre], [wi])
            V("dve", lambda be: be.tensor_tensor(t2[:], cs[:], lim[:], op=ALU.mult), [cs, lim], [t2])
            V("dve", lambda be: be.tensor_tensor(wi[:], wi[:], t2[:], op=ALU.subtract), [wi, t2], [wi])
            V("dve", lambda be: be.tensor_tensor(wi[:], wi[:], t1[:], op=ALU.mult), [wi, t1], [wi])
            V("dve", lambda be: be.tensor_tensor(Bbr[:], wr[:], Br[:], op=ALU.mult), [wr, Br], [Bbr])
            V("dve", lambda be: be.tensor_tensor(t2[:], wi[:], Bi[:], op=ALU.mult), [wi, Bi], [t2])
            V("dve", lambda be: be.tensor_tensor(Bbr[:], Bbr[:], t2[:], op=ALU.subtract), [Bbr, t2], [Bbr])
            V("dve", lambda be: be.tensor_tensor(Bbi[:], wr[:], Bi[:], op=ALU.mult), [wr, Bi], [Bbi])
            V("dve", lambda be: be.tensor_tensor(t2[:], wi[:], Br[:], op=ALU.mult), [wi, Br], [t2])
            V("dve", lambda be: be.tensor_tensor(Bbi[:], Bbi[:], t2[:], op=ALU.add), [Bbi, t2], [Bbi])
            mgb = mg[:].unsqueeze(1).unsqueeze(3).to_broadcast([128, 8, 8, 64])
            for dst, lo, hi in ((BBpad, Bbr, Bbi), (BBsw, Bbi, Bbr)):
                for half, src in ((0, lo), (1, hi)):
                    dv = dst[:, :, half * 64:(half + 1) * 64].rearrange("p (blk g) q -> p blk g q", g=8)
                    sv = src[:].unsqueeze(2).to_broadcast([128, 8, 8, 64])
                    V("dve", lambda be: be.tensor_tensor(dv, sv, mgb, op=ALU.mult), [src, mg], [dst])
            lb = sb(S, pp, "s5_lb", [128, NG], F32)
            S.dma("sp", lb[0:64, :], w["s5_lam_re"].ap[l].rearrange("g p -> p g"), writes=[lb], slow=True)
            S.dma("sp", lb[64:128, :], w["s5_lam_re"].ap[l].rearrange("g p -> p g"), writes=[lb], slow=True)
            S.dma("sp", th[0:64, :], w["s5_lam_im"].ap[l].rearrange("g p -> p g"), writes=[th], slow=True)
            S.dma("sp", th[64:128, :], w["s5_lam_im"].ap[l].rearrange("g p -> p g"), writes=[th], slow=True)
            dtb = sb(S, pp, "s5_dtb", [128, NG], F32)
            S.dma("sp", dtb[:], w["s5_log_dt"].ap[l:l + 1, :].partition_broadcast(128), writes=[dtb])
            V("act", lambda be: be.activation(out=dtb[:], in_=dtb[:], func=AF.Exp), [dtb], [dtb])
            V("dve", lambda be: be.tensor_scalar(lb[:], lb[:], -1e-4, None, op0=ALU.min), [lb], [lb])
            V("dve", lambda be: be.tensor_tensor(lb[:], lb[:], dtb[:], op=ALU.mult), [lb, dtb], [lb])
            V("act", lambda be: be.activation(out=r2[:], in_=lb[:], func=AF.Exp), [lb], [r2])
            V("dve", lambda be: be.tensor_tensor(th[:], th[:], dtb[:], op=ALU.mult), [th, dtb], [th])
            kB = sb(S, pp, "s5_kB", [128, NG], mybir.dt.int32)
            V("dve", lambda be: be.tensor_scalar(th[:], th[:], 1.0 / TWO_PI, None, op0=ALU.mult), [th], [th])
            V("dve", lambda be: be.tensor_copy(kB[:], th[:]), [th], [kB])
            V("dve", lambda be: be.tensor_tensor(th[:], th[:], kB[:], op=ALU.subtract), [th, kB], [th])
            Cc = sb(S, pp, "s5_Cc", [128, 8, 128], F32)
            S.dma("sp", Cc[:, :, 0:64], w["s5_c_re"].ap[l].rearrange("(blk g8) co p -> (g8 co) blk p", g8=8), writes=[Cc])
            S.dma("sp", Cc[:, :, 64:128], w["s5_c_im"].ap[l].rearrange("(blk g8) co p -> (g8 co) blk p", g8=8), writes=[Cc])
            V("dve", lambda be: be.tensor_scalar(Cc[:, :, 64:128], Cc[:, :, 64:128], -1.0, None, op0=ALU.mult), [Cc], [Cc])
            idf = sb(S, pp, "s5_idf", [128, 128], F32)
            S.dma("sp", idf[:], c.identf.ap[:, :], writes=[idf])
            pcr = ps_ring(S, pp, "s5_pc", [128, 128], F32, 2)
            for blk in range(8):
                pc = pcr.next()
                S.transpose(pc[:], Cc[:, blk, :], idf[:], reads=[Cc, idf], writes=[pc])
                V("dve", lambda be: be.tensor_tensor(CCpad[:, blk * 8:(blk + 1) * 8, :], pc[:].unsqueeze(1).to_broadcast([128, 8, 128]),
                                                     m2[:], op=ALU.mult), [pc, m2], [CCpad])

        with Scope(S) as ms:
            trs = []
            for sg in range(NSEG):
                tr_ = sb(S, ms, f"s5_tr{sg}", [128, SEG], F32)
                S.dma("sp", tr_[:], c.trow.ap[0:1, sg * SEG:(sg + 1) * SEG].partition_broadcast(128), writes=[tr_])
                trs.append(tr_)

            def rg(name, dt_=F32):
                return sb_ring(S, ms, name, [128, SEG], dt_, 2)
            COSr, SINr, BUr, BSr, Vr, VSr, T2r, T3r = [rg(f"s5m{i}") for i in range(8)]
            Sgr = rg("s5_sg", BF16)
            kir = rg("s5_ki", mybir.dt.int32)
            ur = sb_ring(S, ms, "s5_u", [128, L], BF16, 2)
            car = sb(S, ms, "s5_car", [128, 8, 2], F32)
            yvr = sb_ring(S, ms, "s5_yv", [128, 512], F32, 2)
            tgr = sb_ring(S, ms, "s5_tg", [128, 512], F32, 2)
            ygr = sb_ring(S, ms, "s5_yg", [128, 512], BF16, 2)
            pbu = ps_ring(S, ms, "s5_pb", [128, 512], F32, 2)
            pbs = ps_ring(S, ms, "s5_pbs", [128, 512], F32, 2)
            pyr = ps_ring(S, ms, "s5_py", [128, 512], F32, 4)
            for blk in range(8):
                ut = ur.next()
                S.dma("sp", ut[:], c.s5uT.ap[blk * 128:(blk + 1) * 128, :], reads=[c.s5uT.o((blk, tc)) for tc in range(L // 512)], writes=[ut])
                for sg in range(NSEG):
                    pys = [pyr.next() for _ in range(NCS)]
                    for g8 in range(8):
                        g = blk * 8 + g8
                        COS, SIN, BU, BS, Vt, VS, T2, T3, Sg = [r_.next() for r_ in (COSr, SINr, BUr, BSr, Vr, VSr, T2r, T3r, Sgr)]
                        kt_ = kir.next()
                        _sincos(S, (COS[:], COS), (trs[sg][:], [trs[sg], th, offs]), th[:, g:g + 1], offs[:, 1:2], (kt_[:], kt_))
                        kt_ = kir.next()
                        _sincos(S, (SIN[:], SIN), (trs[sg][:], [trs[sg], th, offs]), th[:, g:g + 1], offs[:, 0:1], (kt_[:], kt_))
                        for cs_ in range(NCS):
                            fsl = slice(cs_ * 512, (cs_ + 1) * 512)
                            tsl = slice(sg * SEG + cs_ * 512, sg * SEG + (cs_ + 1) * 512)
                            p1, p2 = pbu.next(), pbs.next()
                            S.mm([(p1[:], BBpad[:, g, :], ut[:, tsl], True, True)], reads=[BBpad, ut], writes=[p1])
                            S.mm([(p2[:], BBsw[:, g, :], ut[:, tsl], True, True)], reads=[BBsw, ut], writes=[p2])
                            S.op("dve", lambda be: be.tensor_tensor(Vt[:, fsl], COS[:, fsl], p1[:], op=ALU.mult), reads=[COS, p1], writes=[Vt])
                            S.op("dve", lambda be: be.tensor_tensor(T2[:, fsl], SIN[:, fsl], p2[:], op=ALU.mult), reads=[SIN, p2], writes=[T2])
                            S.op("dve", lambda be: be.tensor_tensor(VS[:, fsl], COS[:, fsl], p2[:], op=ALU.mult), reads=[COS, p2], writes=[VS])
                            S.op("dve", lambda be: be.tensor_tensor(T3[:, fsl], SIN[:, fsl], p1[:], op=ALU.mult), reads=[SIN, p1], writes=[T3])
                        S.op("pool", lambda be: be.tensor_tensor(Vt[:], Vt[:], T2[:], op=ALU.add), reads=[Vt, T2], writes=[Vt])
                        S.op("pool", lambda be: be.tensor_tensor(VS[:], VS[:], T3[:], op=ALU.subtract), reads=[VS, T3], writes=[VS])
                        dec_ = r2[:, g:g + 1].to_broadcast([128, SEG])
                        i0 = 0.0 if sg == 0 else car[:, g8, 0:1]
                        i1 = 0.0 if sg == 0 else car[:, g8, 1:2]
                        S.op("dve", lambda be: be.tensor_tensor_scan(out=BU[:], data0=dec_, data1=Vt[:], initial=i0, op0=ALU.mult, op1=ALU.add),
                             reads=[r2, Vt, car], writes=[BU])
                        S.op("dve", lambda be: be.tensor_tensor_scan(out=BS[:], data0=dec_, data1=VS[:], initial=i1, op0=ALU.mult, op1=ALU.add),
                             reads=[r2, VS, car], writes=[BS])
                        if sg < NSEG - 1:
                            S.op("act", lambda be: be.copy(out=car[:, g8, 0:1], in_=BU[:, SEG - 1:SEG]), reads=[BU, car], writes=[car])
                            S.op("act", lambda be: be.copy(out=car[:, g8, 1:2], in_=BS[:, SEG - 1:SEG]), reads=[BS, car], writes=[car])
                        S.op("dve", lambda be: be.tensor_tensor(Vt[:], COS[:], BU[:], op=ALU.mult), reads=[COS, BU], writes=[Vt])
                        S.op("pool", lambda be: be.tensor_tensor(VS[:], SIN[:], BS[:], op=ALU.mult), reads=[SIN, BS], writes=[VS])
                        S.op("pool", lambda be: be.tensor_tensor(Sg[:], Vt[:], VS[:], op=ALU.subtract), reads=[Vt, VS], writes=[Sg])
                        for cs_ in range(NCS):
                            fsl = slice(cs_ * 512, (cs_ + 1) * 512)
                            S.mm([(pys[cs_][:], CCpad[:, g, :], Sg[:, fsl], g8 == 0, g8 == 7)], reads=[CCpad, Sg], writes=[pys[cs_]])
                    for cs_ in range(NCS):
                        tsl = slice(sg * SEG + cs_ * 512, sg * SEG + (cs_ + 1) * 512)
                        py = pys[cs_]
                        yv, tg, yg = yvr.next(), tgr.next(), ygr.next()
                        S.op("dve", lambda be: be.scalar_tensor_tensor(out=yv[:], in0=ut[:, tsl], scalar=dsk[:, blk:blk + 1], in1=py[:],
                                                                       op0=ALU.mult, op1=ALU.add), reads=[ut, dsk, py], writes=[yv])
                        S.op("pool", lambda be: be.tensor_tensor(tg[:], yv[:], yv[:], op=ALU.mult), reads=[yv], writes=[tg])
                        S.op("pool", lambda be: be.tensor_scalar(tg[:], tg[:], 0.044715, 1.0, op0=ALU.mult, op1=ALU.add), reads=[tg], writes=[tg])
                        S.op("pool", lambda be: be.tensor_tensor(tg[:], tg[:], yv[:], op=ALU.mult), reads=[tg, yv], writes=[tg])
                        S.op("act", lambda be: be.activation(out=tg[:], in_=tg[:], func=AF.Sigmoid, scale=2.0 * math.sqrt(2.0 / math.pi)),
                             reads=[tg], writes=[tg])
                        S.op("dve", lambda be: be.tensor_tensor(yg[:], yv[:], tg[:], op=ALU.mult), reads=[yv, tg], writes=[yg])
                        S.dma("sp", c.s5yT.ap[blk * 128:(blk + 1) * 128, tsl], yg[:], reads=[yg], writes=[c.s5yT.o((blk, tsl.start // 512))])

        with Scope(S) as gs:
            wg = sb(S, gs, "s5_wg", [128, 8, DB], BF16)
            for q in range(2):
                S.dma("pool", wg[:, :, q * 512:(q + 1) * 512],
                      w["s5_w_glu"].ap[l, :, q * 512:(q + 1) * 512].rearrange("(k p) n -> p k n", p=128), writes=[wg])
            ygr2 = sb_ring(S, gs, "s5_y2", [128, 8, 512], BF16, 2)
            zr2 = sb_ring(S, gs, "s5_z2", [128, 8, 512], BF16, 2)
            sgr = sb_ring(S, gs, "s5_sgm", [128, 512], F32, 2)
            outr = sb_ring(S, gs, "s5_o2", [128, 8, 512], BF16, 2)
            pgr = ps_ring(S, gs, "s5_pg", [128, 512], F32, 4)
            for tc in range(L // 512):
                tsl = slice(tc * 512, (tc + 1) * 512)
                yt, zt, ob = ygr2.next(), zr2.next(), outr.next()
                S.dma("sp", yt[:], c.s5yT.ap[:, tsl].rearrange("(k p) n -> p k n", p=128), reads=[c.s5yT.o((k, tc)) for k in range(8)], writes=[yt])
                S.dma("sp", zt[:], c.s5zT.ap[:, tsl].rearrange("(k p) n -> p k n", p=128), reads=[c.s5zT.o((k, tc)) for k in range(8)], writes=[zt])
                for j in range(8):
                    pg = pgr.next()
                    S.mm([(pg[:], wg[:, kc, j * 128:(j + 1) * 128], yt[:, kc, :], kc == 0, kc == 7) for kc in range(8)],
                         reads=[wg, yt], writes=[pg])
                    sg_ = sgr.next()
                    S.op("act", lambda be: be.activation(out=sg_[:], in_=pg[:], func=AF.Sigmoid, bias=bgl[:, j:j + 1]),
                         reads=[pg, bgl], writes=[sg_])
                    S.op("dve", lambda be: be.tensor_tensor(sg_[:], sg_[:], yt[:, j, :], op=ALU.mult), reads=[sg_, yt], writes=[sg_])
                    S.op("pool", lambda be: be.tensor_tensor(ob[:, j, :], sg_[:], zt[:, j, :], op=ALU.mult), reads=[sg_, zt], writes=[ob])
                S.dma("sp", c.yT[0].ap[:, tsl].rearrange("(k p) n -> p k n", p=128), ob[:], reads=[ob],
                      writes=[c.yT[0].o((k, tc)) for k in range(8)])


def build(L=4096, depth=4, debug=False, upto="all"):
    nc = bass.Bass("TRN2", target_bir_lowering=False)
    c = declare(nc, L, depth, debug)
    with ExitStack() as top:
        S = Sched(nc, top)
        for l in range(depth):
            xin = c.x if l == 0 else c.xs[(l - 1) % 2]
            xout = c.out if l == depth - 1 else c.xs[l % 2]
            with Scope(S) as st:
                hT = sb(S, st, "hT", [128, 16, L], BF16)
                phase_norm_T(S, c, xin, c.w["g_pre"].ap[l:l + 1, :], hT, L)
                if "inproj" in upto or upto == "all":
                    phase_inproj(S, c, l, hT, L)
            if "s5" in upto or upto == "all":
                phase_s5(S, c, l, L)
            if "ml" in upto or upto == "all":
                phase_ml(S, c, l, L)
            if "da" in upto or upto == "all":
                phase_da(S, c, l, L)
            if "xa" in upto or upto == "all":
                phase_xa(S, c, l, L)
            if "merge" in upto or upto == "all":
                phase_merge(S, c, l, L)
                phase_out(S, c, l, L, xin, xout)
        outs = [o for o in c.out.objs.values()]
        if debug:
            for d in [c.s5uT, c.s5zT, c.mlqT, c.mlkT, c.mlv, c.mlo, c.mlz, c.mlif, c.daqT, c.dakT, c.dav, c.daz,
                      c.xaqT, c.xaz, c.gT, c.mT] + c.yT + c.xs:
                outs += list(d.objs.values())
        S.final_wait("sp", outs)
        print("instructions:", S.n_inst)
    return nc, c


def make_consts(L):
    bf = ml_dtypes.bfloat16
    inv = 1.0 / (10000.0 ** (np.arange(0, 64, 2, dtype=np.float32) / 64.0))
    ang = np.arange(L, dtype=np.float32)[:, None] * inv[None, :]
    cos, sin = np.cos(ang).T, np.sin(ang).T
    c64 = np.concatenate([cos, cos], 0)
    s64 = np.concatenate([-sin, sin], 0)
    kq = (np.arange(128)[None, :] >= np.arange(128)[:, None]).astype(np.float32)
    return {
        "c_ident": np.eye(128, dtype=np.float32).astype(bf),
        "c_identf": np.eye(128, dtype=np.float32),
        "c_trow": np.arange(L, dtype=np.float32)[None, :].copy(),
        "c_ropec": np.concatenate([c64, c64], 0).astype(bf),
        "c_ropes": np.concatenate([s64, s64], 0).astype(bf),
        "c_maskkq": kq.astype(bf),
        "c_maskg": (np.arange(128)[:, None] // 16 == np.arange(8)[None, :]).astype(np.float32),
        "c_mask2": np.broadcast_to((np.arange(8)[:, None] == (np.arange(128)[None, :] // 16)).astype(np.float32)[None], (128, 8, 128)).copy(),
    }


SEQ_FULL = 4096
DEPTH_FULL = 4


def kernel(**inputs):
    L, depth = SEQ_FULL, DEPTH_FULL
    nc, _ = build(L=L, depth=depth, debug=False)
    consts = make_consts(L)
    shared = {k: np.ascontiguousarray(np.asarray(v, dtype=np.float32)) for k, v in inputs.items() if k not in ("x", "mem")}
    x = np.asarray(inputs["x"], dtype=np.float32)
    mem = np.asarray(inputs["mem"], dtype=np.float32)
    in_maps = []
    for b in range(x.shape[0]):
        m = dict(shared)
        m["x"] = np.ascontiguousarray(x[b])
        m["mem"] = np.ascontiguousarray(mem[b])
        m.update(consts)
        in_maps.append(m)
    res = run_bass_kernel_spmd(nc, in_maps, core_ids=list(range(len(in_maps))))
    return np.stack([np.asarray(r["out"], dtype=np.float32) for r in res.results], axis=0)
```

```python
import math
from contextlib import ExitStack

import numpy as np
import ml_dtypes
import concourse.bass as bass
import concourse.mybir as mybir
from concourse.bass_utils import run_bass_kernel_spmd

F32 = mybir.dt.float32
BF16 = mybir.dt.bfloat16
AF = mybir.ActivationFunctionType
ALU = mybir.AluOpType
AX = mybir.AxisListType

D = 2048
DB = 1024
MEM = 256
NG = 64
D_IN = 21512
EPS = 1e-6
import os
SAME_ENGINE_SYNC = os.environ.get("MK_SES", "1") == "1"

C_S5U, C_S5Z = 0, 1024
C_MLQ, C_MLK, C_MLV, C_MLO, C_MLZ = 2048, 3072, 4096, 5120, 6144
C_MLIF = 7168
C_DAQ, C_DAK, C_DAV, C_DAZ = 7176, 8200, 9224, 10248
C_XAQ, C_XAZ = 11272, 12296
C_GATE = 13320


class Obj:
    __slots__ = ("lw", "rd", "name")

    def __init__(self, name=""):
        self.lw = None
        self.rd = {}
        self.name = name


class Tile:
    def __init__(self, h, name):
        self.h = h
        self.o = Obj(name)

    def __getitem__(self, k):
        return self.h[k]


class Ring:
    def __init__(self, tiles):
        self.tiles = tiles
        self.i = 0

    def next(self):
        t = self.tiles[self.i]
        self.i = (self.i + 1) % len(self.tiles)
        return t


class DT:
    def __init__(self, nc, name, shape, dtype, kind="Internal"):
        self.h = nc.dram_tensor(name, list(shape), dtype, kind=kind)
        self.ap = self.h.ap()
        self.objs = {}
        self.name = name

    def o(self, key=0):
        if key not in self.objs:
            self.objs[key] = Obj(f"{self.name}:{key}")
        return self.objs[key]


class Sched:
    def __init__(self, nc, stack, n_sp=40, n_pool=8, n_act=4):
        self.nc = nc
        self.eng = {"pe": nc.tensor, "act": nc.scalar, "dve": nc.vector, "pool": nc.gpsimd, "sp": nc.sync}
        self.semobj = {}
        for e in ("pe", "act", "dve", "pool"):
            self.semobj[e] = stack.enter_context(nc.semaphore("s_" + e))
        self.tick = {e: 0 for e in ("pe", "act", "dve", "pool")}
        self.waited = {e: {} for e in self.eng}
        self.dq = {"sp": n_sp, "pool": n_pool, "act": n_act}
        self.dnext = {q: 0 for q in self.dq}
        self.dcnt = {}
        for q, n in self.dq.items():
            for i in range(n):
                self.semobj[(q, i)] = stack.enter_context(nc.semaphore(f"d_{q}{i}"))
                self.dcnt[(q, i)] = 0
        self.n_inst = 0
        self.released = {}

    def _deps(self, reads, writes):
        deps = []
        for t in reads:
            o = t.o if isinstance(t, Tile) else t
            if o.lw is not None:
                deps.append(o.lw)
        for t in writes:
            o = t.o if isinstance(t, Tile) else t
            if o.lw is not None:
                deps.append(o.lw)
            deps.extend(o.rd.items())
        return deps

    def _wait(self, e, deps):
        w = self.waited[e]
        need = {}
        for sk, val in deps:
            if w.get(sk, 0) < val and need.get(sk, 0) < val:
                need[sk] = val
        for sk, val in need.items():
            w[sk] = val
            self.eng[e].wait_ge(self.semobj[sk], val)
            self.n_inst += 1

    def _mark(self, tok, reads, writes):
        for t in writes:
            o = t.o if isinstance(t, Tile) else t
            o.lw = tok
            o.rd = {}
        for t in reads:
            o = t.o if isinstance(t, Tile) else t
            if o.rd.get(tok[0], 0) < tok[1]:
                o.rd[tok[0]] = tok[1]

    def op(self, e, fn, reads=(), writes=()):
        deps = self._deps(reads, writes)
        if e == "pe" or not SAME_ENGINE_SYNC:
            deps = [d for d in deps if d[0] != e]
        self._wait(e, deps)
        self.tick[e] += 1
        fn(self.eng[e]).then_inc(self.semobj[e], 1)
        self.n_inst += 1
        self._mark((e, self.tick[e]), reads, writes)

    def mm(self, items, reads=(), writes=(), skip=False):
        deps = [d for d in self._deps(reads, writes) if d[0] != "pe"]
        self._wait("pe", deps)
        n = len(items)
        for i, (out, lhsT, rhs, st, sp) in enumerate(items):
            if skip:
                ins = self.nc.tensor.matmul(out, lhsT, rhs, start=st, stop=sp, skip_group_check=True)
            else:
                ins = self.nc.tensor.matmul(out, lhsT, rhs, start=st, stop=sp)
            self.n_inst += 1
            if i == n - 1:
                self.tick["pe"] += 1
                ins.then_inc(self.semobj["pe"], 1)
        self._mark(("pe", self.tick["pe"]), reads, writes)

    def transpose(self, out, in_, ident, reads=(), writes=()):
        self.op("pe", lambda be: be.transpose(out, in_, ident), reads=reads, writes=writes)

    def dma(self, q, out, in_, reads=(), writes=(), slow=False):
        i = self.dnext[q]
        self.dnext[q] = (i + 1) % self.dq[q]
        sk = (q, i)
        prev = self.dcnt[sk]
        deps = self._deps(reads, writes)
        if q in self.tick and not SAME_ENGINE_SYNC:
            deps = [d for d in deps if d[0] != q]
        if prev > 0:
            deps.append((sk, prev))
        self._wait(q, deps)
        self.dcnt[sk] = prev + 16
        if slow:
            self.eng[q].dma_start(out=out, in_=in_, allow_slow_non_contiguous=True).then_inc(self.semobj[sk], 16)
        else:
            self.eng[q].dma_start(out=out, in_=in_).then_inc(self.semobj[sk], 16)
        self.n_inst += 1
        self._mark((sk, prev + 16), reads, writes)

    def final_wait(self, e, objs):
        deps = []
        for o in objs:
            if o.lw is not None:
                deps.append(o.lw)
        self._wait(e, deps)


class Scope(ExitStack):
    def __init__(self, S):
        super().__init__()
        self.S = S
        self.tiles = []

    def __exit__(self, *a):
        rel = self.S.released
        for t in self.tiles:
            o = t.o
            if o.lw is not None and rel.get(o.lw[0], 0) < o.lw[1]:
                rel[o.lw[0]] = o.lw[1]
            for k, v in o.rd.items():
                if rel.get(k, 0) < v:
                    rel[k] = v
        return super().__exit__(*a)


_UID = [0]


def _uname(name):
    _UID[0] += 1
    return f"{name}_{_UID[0]}"


def _new_tile(S, stack, h, name):
    t = Tile(h, name)
    t.o.rd = dict(S.released)
    stack.tiles.append(t)
    return t


def sb(S, stack, name, shape, dtype):
    return _new_tile(S, stack, stack.enter_context(S.nc.sbuf_tensor(_uname(name), list(shape), dtype)), name)


def ps(S, stack, name, shape, dtype=F32):
    return _new_tile(S, stack, stack.enter_context(S.nc.psum_tensor(_uname(name), list(shape), dtype)), name)


def sb_ring(S, stack, name, shape, dtype, n):
    return Ring([sb(S, stack, f"{name}{i}", shape, dtype) for i in range(n)])


def ps_ring(S, stack, name, shape, dtype, n):
    return Ring([ps(S, stack, f"{name}{i}", shape, dtype) for i in range(n)])


class Ctx:
    pass


def declare(nc, L, depth, debug):
    c = Ctx()
    c.L, c.depth = L, depth
    kin = "ExternalInput"
    c.x = DT(nc, "x", [L, D], F32, kin)
    c.mem = DT(nc, "mem", [MEM, D], F32, kin)
    shapes = dict(
        g_pre=[depth, D], w_in=[depth, D, D_IN], s5_lam_re=[depth, NG, 64], s5_lam_im=[depth, NG, 64],
        s5_log_dt=[depth, NG], s5_b_re=[depth, NG, 64, 16], s5_b_im=[depth, NG, 64, 16],
        s5_c_re=[depth, NG, 16, 64], s5_c_im=[depth, NG, 16, 64], s5_d=[depth, DB],
        s5_w_glu=[depth, DB, DB], s5_b_glu=[depth, DB], ml_conv_w=[depth, 4, 2 * DB], ml_conv_b=[depth, 2 * DB],
        ml_b_i=[depth, 4], ml_b_f=[depth, 4], ml_norm_g=[depth, DB], da_lq1=[depth, 64], da_lk1=[depth, 64],
        da_lq2=[depth, 64], da_lk2=[depth, 64], da_subln_g=[depth, 128], g_mem=[depth, D],
        xa_w_kv=[depth, D, 2 * DB], w_branch=[depth, 4, DB, D], w_out=[depth, D, D], g_post=[depth, D])
    c.w = {k: DT(nc, k, v, F32, kin) for k, v in shapes.items()}
    c.ident = DT(nc, "c_ident", [128, 128], BF16, kin)
    c.identf = DT(nc, "c_identf", [128, 128], F32, kin)
    c.trow = DT(nc, "c_trow", [1, L], F32, kin)
    c.ropec = DT(nc, "c_ropec", [128, L], BF16, kin)
    c.ropes = DT(nc, "c_ropes", [128, L], BF16, kin)
    c.maskkq = DT(nc, "c_maskkq", [128, 128], BF16, kin)
    c.maskg = DT(nc, "c_maskg", [128, 8], F32, kin)
    c.mask2 = DT(nc, "c_mask2", [128, 8, 128], F32, kin)
    c.out = DT(nc, "out", [L, D], F32, "ExternalOutput")
    sk = "ExternalOutput" if debug else "Internal"
    c.dbg = debug
    c.xs = [DT(nc, f"xs{i}", [L, D], F32, sk) for i in range(2)]
    c.s5uT = DT(nc, "s5uT", [DB, L], BF16, sk)
    c.s5zT = DT(nc, "s5zT", [DB, L], BF16, sk)
    c.mlqT = DT(nc, "mlqT", [DB, L], BF16, sk)
    c.mlkT = DT(nc, "mlkT", [DB, L], BF16, sk)
    c.mlv = DT(nc, "mlv", [L, DB], BF16, sk)
    c.mlo = DT(nc, "mlo", [L, DB], BF16, sk)
    c.mlz = DT(nc, "mlz", [L, DB], BF16, sk)
    c.mlif = DT(nc, "mlif", [8, L], F32, sk)
    c.daqT = DT(nc, "daqT", [DB, L], BF16, sk)
    c.dakT = DT(nc, "dakT", [DB, L], BF16, sk)
    c.dav = DT(nc, "dav", [L, DB], BF16, sk)
    c.daz = DT(nc, "daz", [L, DB], BF16, sk)
    c.xaqT = DT(nc, "xaqT", [DB, L], BF16, sk)
    c.xaz = DT(nc, "xaz", [L, DB], BF16, sk)
    c.gT = DT(nc, "gT", [4 * D, L], BF16, sk)
    c.yT = [DT(nc, f"yT{b}", [DB, L], BF16, sk) for b in range(4)]
    c.mT = DT(nc, "mT", [D, L], BF16, sk)
    c.mlg = DT(nc, "mlg", [3, 4, L + 1], F32, "Internal")
    c.s5yT = DT(nc, "s5yT", [DB, L], BF16, sk)
    return c


def bcast_rows(ap_row, nparts):
    return ap_row.partition_broadcast(nparts)


def phase_norm_T(S, c, src, g_row_ap, hT, L, rows=None, tag="a"):
    nc = S.nc
    rows = L if rows is None else rows
    with Scope(S) as st:
        gb = sb(S, st, tag + "_gb", [128, D], F32)
        S.dma("sp", gb[:], bcast_rows(g_row_ap, 128), writes=[gb])
        idt = sb(S, st, tag + "_id", [128, 128], BF16)
        S.dma("sp", idt[:], c.ident.ap[:, :], writes=[idt])
        xr = sb_ring(S, st, tag + "_x", [128, D], F32, 2)
        jr = sb_ring(S, st, tag + "_j", [128, D], F32, 1)
        hr = sb_ring(S, st, tag + "_h", [128, D], BF16, 2)
        ssr = sb_ring(S, st, tag + "_ss", [128, 4], F32, 2)
        pr = ps_ring(S, st, tag + "_p", [128, 512], BF16, 4)
        for t in range(rows // 128):
            xt = xr.next()
            S.dma("sp", xt[:], src.ap[t * 128:(t + 1) * 128, :], reads=[src.o(t)], writes=[xt])
            jk = jr.next()
            ss = ssr.next()
            S.op("act", lambda be: be.activation(out=jk[:], in_=xt[:], func=AF.Square, accum_out=ss[:, 0:1]),
                 reads=[xt], writes=[jk, ss])
            S.op("dve", lambda be: be.tensor_scalar(ss[:, 1:2], ss[:, 0:1], 1.0 / D, EPS, op0=ALU.mult, op1=ALU.add),
                 reads=[ss], writes=[ss])
            S.op("act", lambda be: be.activation(out=ss[:, 2:3], in_=ss[:, 1:2], func=AF.Sqrt), reads=[ss], writes=[ss])
            S.op("dve", lambda be: be.reciprocal(ss[:, 3:4], ss[:, 2:3]), reads=[ss], writes=[ss])
            ht = hr.next()
            S.op("dve", lambda be: be.scalar_tensor_tensor(out=ht[:], in0=xt[:], scalar=ss[:, 3:4], in1=gb[:],
                                                           op0=ALU.mult, op1=ALU.mult),
                 reads=[xt, ss, gb], writes=[ht])
            for q in range(4):
                pt = pr.next()
                for j in range(4):
                    kc = q * 4 + j
                    S.transpose(pt[:, j * 128:(j + 1) * 128], ht[:, kc * 128:(kc + 1) * 128], idt[:],
                                reads=[ht, idt], writes=[pt])
                eng = "act" if q % 2 else "dve"
                dst = hT[:, q * 4:(q + 1) * 4, t * 128:(t + 1) * 128]
                srcp = pt[:].rearrange("p (j n) -> p j n", j=4)
                if eng == "act":
                    S.op("act", lambda be: be.copy(out=dst, in_=srcp), reads=[pt], writes=[hT])
                else:
                    S.op("dve", lambda be: be.tensor_copy(dst, srcp), reads=[pt], writes=[hT])


def phase_inproj(S, c, l, hT, L):
    nc = S.nc
    w_in = c.w["w_in"].ap
    NT, NC = L // 128, L // 512
    with Scope(S) as st:
        wr = sb_ring(S, st, "ip_w", [128, 16, 512], BF16, 2)
        wsw = sb(S, st, "ip_wsw", [128, 16, 512], BF16)
        pr = ps_ring(S, st, "ip_p", [128, 512], F32, 6)
        sbf = sb_ring(S, st, "ip_sb", [128, 512], BF16, 4)
        sf32 = sb_ring(S, st, "ip_sf", [128, 512], F32, 2)
        pre = sb_ring(S, st, "ip_pre", [128, 3 + 512], F32, 2)
        acc = sb_ring(S, st, "ip_acc", [128, 512], F32, 2)
        rcr = sb_ring(S, st, "ip_rc", [128, 512], BF16, 2)
        rsr = sb_ring(S, st, "ip_rs", [128, 512], BF16, 2)
        cw = sb(S, st, "ip_cw", [128, 4, 16], F32)
        cb = sb(S, st, "ip_cb", [128, 16], F32)
        for j in range(4):
            S.dma("sp", cw[:, j, :], c.w["ml_conv_w"].ap[l, j].rearrange("(b p) -> p b", p=128), writes=[cw], slow=True)
        S.dma("sp", cb[:], c.w["ml_conv_b"].ap[l].rearrange("(b p) -> p b", p=128), writes=[cb], slow=True)

        def load_w(c0, n):
            wc = wr.next()
            S.dma("pool", wc[:, :, :n], w_in[l, :, c0:c0 + n].rearrange("(k p) n -> p k n", p=128), writes=[wc])
            return wc

        def tok_major(c0, dst, func):
            for cc in range(0, DB, 512):
                wc = load_w(c0 + cc, 512)
                for t in range(NT):
                    p = pr.next()
                    S.mm([(p[:], hT[:, kc, t * 128:(t + 1) * 128], wc[:, kc, :], kc == 0, kc == 15) for kc in range(16)],
                         reads=[hT, wc], writes=[p])
                    s = sbf.next()
                    if func is None:
                        S.op("dve", lambda be: be.tensor_copy(s[:], p[:]), reads=[p], writes=[s])
                    else:
                        S.op("act", lambda be: be.activation(out=s[:], in_=p[:], func=func), reads=[p], writes=[s])
                    S.dma("sp", dst.ap[t * 128:(t + 1) * 128, cc:cc + 512], s[:], reads=[s], writes=[dst.o(t)])

        def feat_major(c0, ncols, dst, row0, kind, cbase=0):
            for cc in range(0, ncols, 512):
                n = min(512, ncols - cc)
                wc = load_w(c0 + cc, n)
                if kind == "rope":
                    srcv = wc[:, :, :n].rearrange("p k (b h d) -> p k b h d", h=2, d=32)
                    dstv = wsw[:, :, :n].rearrange("p k (b h d) -> p k b h d", h=2, d=32)
                    for kc in range(16):
                        S.op("act" if kc % 2 else "dve",
                             (lambda be: be.copy(out=dstv[:, kc, :, 0, :], in_=srcv[:, kc, :, 1, :])) if kc % 2 else
                             (lambda be: be.tensor_copy(dstv[:, kc, :, 0, :], srcv[:, kc, :, 1, :])), reads=[wc], writes=[wsw])
                        S.op("act" if kc % 2 else "dve",
                             (lambda be: be.copy(out=dstv[:, kc, :, 1, :], in_=srcv[:, kc, :, 0, :])) if kc % 2 else
                             (lambda be: be.tensor_copy(dstv[:, kc, :, 1, :], srcv[:, kc, :, 0, :])), reads=[wc], writes=[wsw])
                for j in range(n // 128):
                    blk = (cc // 128) + j
                    prev = None
                    for tc in range(NC):
                        tsl = slice(tc * 512, (tc + 1) * 512)
                        p = pr.next()
                        S.mm([(p[:], wc[:, kc, j * 128:(j + 1) * 128], hT[:, kc, tsl], kc == 0, kc == 15)
                              for kc in range(16)], reads=[hT, wc], writes=[p])
                        s = sbf.next()
                        if kind == "copy":
                            S.op("dve", lambda be: be.tensor_copy(s[:], p[:]), reads=[p], writes=[s])
                        elif kind in ("silu", "sigmoid"):
                            f = AF.Silu if kind == "silu" else AF.Sigmoid
                            S.op("act", lambda be: be.activation(out=s[:], in_=p[:], func=f), reads=[p], writes=[s])
                        elif kind in ("conv", "convk"):
                            cblk = cbase + blk
                            pt = pre.next()
                            if prev is None:
                                S.op("pool", lambda be: be.memset(pt[:, 0:3], 0.0), writes=[pt])
                            else:
                                pv = prev
                                S.op("pool", lambda be: be.tensor_copy(pt[:, 0:3], pv[:, 512:515]), reads=[pv], writes=[pt])
                            S.op("act", lambda be: be.copy(out=pt[:, 3:515], in_=p[:]), reads=[p], writes=[pt])
                            a = acc.next()
                            S.op("dve", lambda be: be.tensor_scalar(a[:], pt[:, 3:515], cw[:, 3, cblk:cblk + 1], cb[:, cblk:cblk + 1],
                                                                   op0=ALU.mult, op1=ALU.add), reads=[pt, cw, cb], writes=[a])
                            for tap in range(3):
                                S.op("dve", lambda be: be.scalar_tensor_tensor(out=a[:], in0=pt[:, tap:tap + 512],
                                                                               scalar=cw[:, tap, cblk:cblk + 1], in1=a[:],
                                                                               op0=ALU.mult, op1=ALU.add),
                                     reads=[pt, cw, a], writes=[a])
                            if kind == "conv":
                                S.op("act", lambda be: be.activation(out=s[:], in_=a[:], func=AF.Silu), reads=[a], writes=[s])
                            else:
                                a2 = sf32.next()
                                S.op("act", lambda be: be.activation(out=a2[:], in_=a[:], func=AF.Silu), reads=[a], writes=[a2])
                                S.op("pool", lambda be: be.tensor_scalar(s[:], a2[:], 0.0625, None, op0=ALU.mult),
                                     reads=[a2], writes=[s])
                            prev = pt
                        elif kind == "rope":
                            p2 = pr.next()
                            S.mm([(p2[:], wsw[:, kc, j * 128:(j + 1) * 128], hT[:, kc, tsl], kc == 0, kc == 15)
                                  for kc in range(16)], reads=[hT, wsw], writes=[p2])
                            a = acc.next()
                            a2 = sf32.next()
                            rc, rs = rcr.next(), rsr.next()
                            S.dma("sp", rc[:], c.ropec.ap[:, tsl], writes=[rc])
                            S.dma("sp", rs[:], c.ropes.ap[:, tsl], writes=[rs])
                            S.op("dve", lambda be: be.tensor_tensor(a[:], p[:], rc[:], op=ALU.mult), reads=[p, rc], writes=[a])
                            S.op("dve", lambda be: be.tensor_tensor(a2[:], p2[:], rs[:], op=ALU.mult), reads=[p2, rs], writes=[a2])
                            S.op("pool", lambda be: be.tensor_tensor(s[:], a[:], a2[:], op=ALU.add), reads=[a, a2], writes=[s])
                        r0 = row0 + blk * 128
                        S.dma("sp", dst.ap[r0:r0 + 128, tsl], s[:], reads=[s], writes=[dst.o((r0 // 128, tc))])

        feat_major(C_S5U, DB, c.s5uT, 0, "copy")
        feat_major(C_S5Z, DB, c.s5zT, 0, "silu")
        feat_major(C_MLQ, DB, c.mlqT, 0, "conv", cbase=0)
        feat_major(C_MLK, DB, c.mlkT, 0, "convk", cbase=8)
        tok_major(C_MLV, c.mlv, None)
        tok_major(C_MLO, c.mlo, AF.Sigmoid)
        tok_major(C_MLZ, c.mlz, AF.Silu)
        wc = load_w(C_MLIF, 8)
        for tc in range(NC):
            tsl = slice(tc * 512, (tc + 1) * 512)
            p = pr.next()
            S.mm([(p[0:8, :], wc[:, kc, 0:8], hT[:, kc, tsl], kc == 0, kc == 15) for kc in range(16)],
                 reads=[hT, wc], writes=[p])
            a = acc.next()
            S.op("dve", lambda be: be.tensor_copy(a[0:8, :], p[0:8, :]), reads=[p], writes=[a])
            S.dma("sp", c.mlif.ap[:, tsl], a[0:8, :], reads=[a], writes=[c.mlif.o(tc)])
        feat_major(C_DAQ, DB, c.daqT, 0, "rope")
        feat_major(C_DAK, DB, c.dakT, 0, "rope")
        tok_major(C_DAV, c.dav, None)
        tok_major(C_DAZ, c.daz, AF.Silu)
        feat_major(C_XAQ, DB, c.xaqT, 0, "copy")
        tok_major(C_XAZ, c.xaz, AF.Silu)
        feat_major(C_GATE, 4 * D, c.gT, 0, "sigmoid")


def emit_yT(S, ytoks, dst, row0, nblk, tsl, key_tc, idt, ptr, ysbr):
    ysb = ysbr.next()
    for blk in range(nblk):
        pt = ptr.next()
        for qt in range(4):
            S.transpose(pt[:, qt * 128:(qt + 1) * 128], ytoks[qt][:, blk * 128:(blk + 1) * 128], idt[:],
                        reads=[ytoks[qt], idt], writes=[pt])
        if blk % 2:
            S.op("act", lambda be: be.copy(out=ysb[:, blk, :], in_=pt[:]), reads=[pt], writes=[ysb])
        else:
            S.op("dve", lambda be: be.tensor_copy(ysb[:, blk, :], pt[:]), reads=[pt], writes=[ysb])
    S.dma("sp", dst.ap[row0:row0 + nblk * 128, tsl].rearrange("(k p) n -> p k n", p=128), ysb[:, 0:nblk, :],
          reads=[ysb], writes=[dst.o((r, key_tc)) for r in range(row0 // 128, row0 // 128 + nblk)])


def phase_merge(S, c, l, L, wo):
    NC = L // 512
    wb = c.w["w_branch"].ap
    with Scope(S) as st:
        wqr = sb_ring(S, st, "mg_w", [128, 32, 512], BF16, 2)

        def load_wq(dq):
            t = wqr.next()
            for b in range(4):
                S.dma("pool", t[:, b * 8:(b + 1) * 8, :],
                      wb[l, b, :, dq * 512:(dq + 1) * 512].rearrange("(k p) n -> p k n", p=128), writes=[t])
            return t
        wq_next = load_wq(0)
        yr = sb_ring(S, st, "mg_y", [128, 32, 512], BF16, 2)
        gr = sb_ring(S, st, "mg_g", [128, 4, 512], BF16, 2)
        tr = sb_ring(S, st, "mg_t", [128, 4, 512], F32, 2)
        ar = sb_ring(S, st, "mg_a", [128, 2, 512], F32, 2)
        orr = sb_ring(S, st, "mg_o", [128, 512], BF16, 2)
        pr = ps_ring(S, st, "mg_p", [128, 512], F32, 8)
        gview = c.gT.ap.rearrange("(b r) n -> r b n", b=4)
        for dq in range(4):
            wq = wq_next
            if dq < 3:
                wq_next = load_wq(dq + 1)
            for tc in range(NC):
                tsl = slice(tc * 512, (tc + 1) * 512)
                yt = yr.next()
                for b in range(4):
                    S.dma("sp", yt[:, b * 8:(b + 1) * 8, :], c.yT[b].ap[:, tsl].rearrange("(k p) n -> p k n", p=128),
                          reads=[c.yT[b].o((r, tc)) for r in range(8)], writes=[yt])
                for j in range(4):
                    r0 = dq * 512 + j * 128
                    gt = gr.next()
                    S.dma("sp", gt[:], gview[r0:r0 + 128, :, tsl],
                          reads=[c.gT.o(((b * D + r0) // 128, tc)) for b in range(4)], writes=[gt])
                    tt = tr.next()
                    for b in range(4):
                        p = pr.next()
                        S.mm([(p[:], wq[:, b * 8 + kc, j * 128:(j + 1) * 128], yt[:, b * 8 + kc, :], kc == 0, kc == 7)
                              for kc in range(8)], reads=[wq, yt], writes=[p])
                        S.op("dve", lambda be: be.tensor_tensor(tt[:, b, :], p[:], gt[:, b, :], op=ALU.mult),
                             reads=[p, gt], writes=[tt])
                    a = ar.next()
                    S.op("dve", lambda be: be.tensor_tensor(a[:], tt[:, 0:2, :], tt[:, 2:4, :], op=ALU.add), reads=[tt], writes=[a])
                    o = orr.next()
                    S.op("dve", lambda be: be.tensor_tensor(o[:], a[:, 0, :], a[:, 1, :], op=ALU.add), reads=[a], writes=[o])
                    S.dma("sp", c.mT.ap[r0:r0 + 128, tsl], o[:], reads=[o], writes=[c.mT.o((r0 // 128, tc))])


def phase_out(S, c, l, L, xin, xout, wo):
    NT = L // 128
    with Scope(S) as st:
        wo = sb(S, st, "po_w", [128, 16, D], BF16)
        for q in range(4):
            S.dma("pool", wo[:, :, q * 512:(q + 1) * 512],
                  c.w["w_out"].ap[l, :, q * 512:(q + 1) * 512].rearrange("(k p) n -> p k n", p=128), writes=[wo])
        gp = sb(S, st, "po_g", [128, D], F32)
        S.dma("sp", gp[:], c.w["g_post"].ap[l:l + 1, :].partition_broadcast(128), writes=[gp])
        mr = sb_ring(S, st, "po_m", [128, 16, 128], BF16, 2)
        xr = sb_ring(S, st, "po_x", [128, D], F32, 2)
        orr = sb_ring(S, st, "po_o", [128, D], F32, 2)
        jr = sb_ring(S, st, "po_j", [128, 512], F32, 2)
        ssr = sb_ring(S, st, "po_ss", [128, 8], F32, 2)
        pr = ps_ring(S, st, "po_p", [128, 512], F32, 8)
        for t in range(NT):
            mt = mr.next()
            S.dma("sp", mt[:], c.mT.ap[:, t * 128:(t + 1) * 128].rearrange("(k p) n -> p k n", p=128),
                  reads=[c.mT.o((r, t // 4)) for r in range(16)], writes=[mt])
            xt = xr.next()
            S.dma("sp", xt[:], xin.ap[t * 128:(t + 1) * 128, :], reads=[xin.o(t)], writes=[xt])
            ss = ssr.next()
            pp = []
            for n in range(4):
                p = pr.next()
                pp.append(p)
                S.mm([(p[:], mt[:, kc, :], wo[:, kc, n * 512:(n + 1) * 512], kc == 0, kc == 15) for kc in range(16)],
                     reads=[mt, wo], writes=[p])
                jk = jr.next()
                S.op("act", lambda be: be.activation(out=jk[:], in_=p[:], func=AF.Square, accum_out=ss[:, n:n + 1]),
                     reads=[p], writes=[jk, ss])
            S.op("dve", lambda be: be.reduce_sum(out=ss[:, 4:5], in_=ss[:, 0:4], axis=AX.X), reads=[ss], writes=[ss])
            S.op("dve", lambda be: be.tensor_scalar(ss[:, 5:6], ss[:, 4:5], 1.0 / D, EPS, op0=ALU.mult, op1=ALU.add),
                 reads=[ss], writes=[ss])
            S.op("act", lambda be: be.activation(out=ss[:, 6:7], in_=ss[:, 5:6], func=AF.Sqrt), reads=[ss], writes=[ss])
            S.op("dve", lambda be: be.reciprocal(ss[:, 7:8], ss[:, 6:7]), reads=[ss], writes=[ss])
            ot = orr.next()
            for n in range(4):
                nsl = slice(n * 512, (n + 1) * 512)
                p = pp[n]
                S.op("dve", lambda be: be.scalar_tensor_tensor(out=ot[:, nsl], in0=p[:], scalar=ss[:, 7:8], in1=gp[:, nsl],
                                                               op0=ALU.mult, op1=ALU.mult), reads=[p, ss, gp], writes=[ot])
            S.op("pool", lambda be: be.tensor_tensor(ot[:], ot[:], xt[:], op=ALU.add), reads=[ot, xt], writes=[ot])
            S.dma("sp", xout.ap[t * 128:(t + 1) * 128, :], ot[:], reads=[ot], writes=[xout.o(t)])


def phase_xa(S, c, l, L):
    NC = L // 512
    wkv = c.w["xa_w_kv"].ap
    with Scope(S) as st:
        idt = sb(S, st, "xa_id", [128, 128], BF16)
        S.dma("sp", idt[:], c.ident.ap[:, :], writes=[idt])
        kT = sb(S, st, "xa_kT", [128, 8, MEM], BF16)
        vaug = sb(S, st, "xa_v", [128, 2, 4, 257], BF16)
        S.op("pool", lambda be: be.memset(vaug[:, :, :, 256:257], 1.0), writes=[vaug])
        with Scope(S) as st2:
            memT = sb(S, st2, "xa_memT", [128, 16, MEM], BF16)
            phase_norm_T(S, c, c.mem, c.w["g_mem"].ap[l:l + 1, :], memT, MEM, rows=MEM, tag="xn")
            wr = sb_ring(S, st2, "xa_w", [128, 16, 512], BF16, 2)
            pr = ps_ring(S, st2, "xa_pp", [128, 512], F32, 2)
            for q in range(4):
                wc = wr.next()
                S.dma("pool", wc[:], wkv[l, :, q * 512:(q + 1) * 512].rearrange("(k p) n -> p k n", p=128), writes=[wc])
                if q < 2:
                    for j in range(4):
                        p = pr.next()
                        S.mm([(p[:, 0:MEM], wc[:, kc, j * 128:(j + 1) * 128], memT[:, kc, :], kc == 0, kc == 15)
                              for kc in range(16)], reads=[wc, memT], writes=[p])
                        S.op("dve", lambda be: be.tensor_copy(kT[:, q * 4 + j, :], p[:, 0:MEM]), reads=[p], writes=[kT])
                else:
                    for mt in range(2):
                        p = pr.next()
                        S.mm([(p[:], memT[:, kc, mt * 128:(mt + 1) * 128], wc[:, kc, :], kc == 0, kc == 15)
                              for kc in range(16)], reads=[wc, memT], writes=[p])
                        h0 = (q - 2) * 2
                        S.op("dve", lambda be: be.tensor_copy(vaug[:, mt, h0:h0 + 2, 0:256],
                                                              p[:].rearrange("p (h d) -> p h d", h=2)),
                             reads=[p], writes=[vaug])
        qr = sb_ring(S, st, "xa_q", [128, 8, 512], BF16, 2)
        zr = sb_ring(S, st, "xa_z", [128, 4, DB], BF16, 2)
        ptr_ = sb_ring(S, st, "xa_pT", [128, 512], BF16, 4)
        yts = [sb_ring(S, st, f"xa_y{i}", [128, DB], BF16, 2) for i in range(4)]
        rr = sb_ring(S, st, "xa_r", [128, 2], F32, 4)
        ysbr = sb_ring(S, st, "xa_ysb", [128, 8, 512], BF16, 2)
        psr = ps_ring(S, st, "xa_ps", [128, 512], F32, 3)
        por = ps_ring(S, st, "xa_po", [128, 257], F32, 3)
        ptr2 = ps_ring(S, st, "xa_pt", [128, 512], BF16, 2)
        for tc in range(NC):
            tsl = slice(tc * 512, (tc + 1) * 512)
            qt_ = qr.next()
            S.dma("sp", qt_[:], c.xaqT.ap[:, tsl].rearrange("(k p) n -> p k n", p=128),
                  reads=[c.xaqT.o((r, tc)) for r in range(8)], writes=[qt_])
            zt = zr.next()
            S.dma("sp", zt[:], c.xaz.ap[tsl, :].rearrange("(t p) d -> p t d", p=128),
                  reads=[c.xaz.o(tc * 4 + i) for i in range(4)], writes=[zt])
            ytoks = [yts[i].next() for i in range(4)]
            for h in range(4):
                pTs = []
                for mt in range(2):
                    p = psr.next()
                    S.mm([(p[:], kT[:, h * 2 + db, mt * 128:(mt + 1) * 128], qt_[:, h * 2 + db, :], db == 0, db == 1)
                          for db in range(2)], reads=[kT, qt_], writes=[p])
                    pT = ptr_.next()
                    S.op("act", lambda be: be.activation(out=pT[:], in_=p[:], func=AF.Exp, scale=1.0 / 16.0),
                         reads=[p], writes=[pT])
                    pTs.append(pT)
                for qi in range(4):
                    po = por.next()
                    S.mm([(po[:], pTs[mt][:, qi * 128:(qi + 1) * 128], vaug[:, mt, h, :], mt == 0, mt == 1)
                          for mt in range(2)], reads=pTs + [vaug], writes=[po])
                    r = rr.next()
                    S.op("dve", lambda be: be.reciprocal(r[:, 0:1], po[:, 256:257]), reads=[po], writes=[r])
                    yk = ytoks[qi]
                    S.op("dve", lambda be: be.scalar_tensor_tensor(out=yk[:, h * 256:(h + 1) * 256], in0=po[:, 0:256],
                                                                   scalar=r[:, 0:1], in1=zt[:, qi, h * 256:(h + 1) * 256],
                                                                   op0=ALU.mult, op1=ALU.mult),
                         reads=[po, r, zt], writes=[yk])
            emit_yT(S, ytoks, c.yT[3], 0, 8, tsl, tc, idt, ptr2, ysbr)


def phase_da(S, c, l, L):
    NC, NT = L // 512, L // 128
    lam_init = 0.8 - 0.6 * math.exp(-0.3 * l)
    w = c.w
    with Scope(S) as st:
        idt = sb(S, st, "da_id", [128, 128], BF16)
        S.dma("sp", idt[:], c.ident.ap[:, :], writes=[idt])
        mk = sb(S, st, "da_mk", [128, 128], BF16)
        S.dma("sp", mk[:], c.maskkq.ap[:, :], writes=[mk])
        lt = sb(S, st, "da_lt", [128, 4, 64], F32)
        for i, nm in enumerate(("da_lq1", "da_lk1", "da_lq2", "da_lk2")):
            S.dma("sp", lt[:, i, :], w[nm].ap[l:l + 1, :].partition_broadcast(128), writes=[lt])
        lp = sb(S, st, "da_lp", [128, 2, 64], F32)
        lv = sb(S, st, "da_lv", [128, 8], F32)
        S.op("dve", lambda be: be.tensor_tensor(lp[:, 0, :], lt[:, 0, :], lt[:, 1, :], op=ALU.mult), reads=[lt], writes=[lp])
        S.op("dve", lambda be: be.tensor_tensor(lp[:, 1, :], lt[:, 2, :], lt[:, 3, :], op=ALU.mult), reads=[lt, lp], writes=[lp])
        S.op("dve", lambda be: be.reduce_sum(out=lv[:, 0:2], in_=lp[:], axis=AX.X), reads=[lp], writes=[lv])
        S.op("act", lambda be: be.activation(out=lv[:, 2:4], in_=lv[:, 0:2], func=AF.Exp), reads=[lv], writes=[lv])
        S.op("dve", lambda be: be.tensor_tensor(lv[:, 4:5], lv[:, 3:4], lv[:, 2:3], op=ALU.subtract), reads=[lv], writes=[lv])
        S.op("dve", lambda be: be.tensor_scalar(lv[:, 5:6], lv[:, 4:5], -lam_init, None, op0=ALU.add), reads=[lv], writes=[lv])
        sg = sb(S, st, "da_sg", [128, 128], F32)
        S.dma("sp", sg[:], w["da_subln_g"].ap[l:l + 1, :].partition_broadcast(128), writes=[sg])
        S.op("dve", lambda be: be.tensor_scalar(sg[:], sg[:], 1.0 - lam_init, None, op0=ALU.mult), reads=[sg], writes=[sg])

        kr = sb_ring(S, st, "da_k", [128, L], BF16, 2)
        vr = sb_ring(S, st, "da_v", [128, NT, 129], BF16, 2)
        qr = sb_ring(S, st, "da_q", [128, 512], BF16, 2)
        zr = sb_ring(S, st, "da_z", [128, 4, 128], BF16, 2)
        pTr = sb_ring(S, st, "da_pT", [128, 512], BF16, 4)
        yts = [sb_ring(S, st, f"da_y{i}", [128, 128], BF16, 2) for i in range(4)]
        ar = sb_ring(S, st, "da_a", [128, 128], F32, 2)
        dr = sb_ring(S, st, "da_d", [128, 128], F32, 2)
        jr = sb_ring(S, st, "da_j", [128, 128], F32, 2)
        rr = sb_ring(S, st, "da_r", [128, 8], F32, 4)
        ysbr = sb_ring(S, st, "da_ysb", [128, 1, 512], BF16, 2)
        psr = ps_ring(S, st, "da_ps", [128, 512], F32, 3)
        accs = [ps(S, st, f"da_acc{i}", [128, 3, 129], F32) for i in range(3)]
        ptr2 = ps_ring(S, st, "da_pt", [128, 512], BF16, 1)

        def acc(comp, qt):
            i = comp * 4 + qt
            return accs[i // 3], i % 3

        for h in range(8):
            kt_ = kr.next()
            S.dma("sp", kt_[:], c.dakT.ap[h * 128:(h + 1) * 128, :], reads=[c.dakT.o((h, tc)) for tc in range(NC)], writes=[kt_])
            vt = vr.next()
            S.dma("sp", vt[:, :, 0:128], c.dav.ap[:, h * 128:(h + 1) * 128].rearrange("(t p) d -> p t d", p=128),
                  reads=[c.dav.o(t) for t in range(NT)], writes=[vt])
            S.op("pool", lambda be: be.memset(vt[:, :, 128:129], 1.0), writes=[vt])
            for tc in range(NC):
                tsl = slice(tc * 512, (tc + 1) * 512)
                qt_ = qr.next()
                S.dma("sp", qt_[:], c.daqT.ap[h * 128:(h + 1) * 128, tsl], reads=[c.daqT.o((h, tc))], writes=[qt_])
                zt = zr.next()
                S.dma("sp", zt[:], c.daz.ap[tsl, h * 128:(h + 1) * 128].rearrange("(t p) d -> p t d", p=128),
                      reads=[c.daz.o(tc * 4 + i) for i in range(4)], writes=[zt])
                nk = 4 * tc + 4
                for a_t in accs:
                    S.op("dve", lambda be: be.memset(a_t[:], 0.0), writes=[a_t])
                steps = [(kt, comp) for kt in range(nk) for comp in range(2)]

                def emit_st(i):
                    kt, comp = steps[i]
                    dq = kt - 4 * tc
                    q0 = max(dq, 0) * 128
                    csl = slice(comp * 64, (comp + 1) * 64)
                    p = psr.next()
                    S.mm([(p[:, q0:512], kt_[csl, kt * 128:(kt + 1) * 128], qt_[csl, q0:512], True, True)],
                         reads=[kt_, qt_], writes=[p])
                    pT = pTr.next()
                    S.op("act", lambda be: be.activation(out=pT[:, q0:512], in_=p[:, q0:512], func=AF.Exp, scale=0.125),
                         reads=[p], writes=[pT])
                    if dq >= 0:
                        S.op("dve", lambda be: be.tensor_tensor(pT[:, q0:q0 + 128], pT[:, q0:q0 + 128], mk[:], op=ALU.mult),
                             reads=[pT, mk], writes=[pT])
                    return pT

                LOOK = 2
                pts = {}
                for i in range(min(LOOK, len(steps))):
                    pts[i] = emit_st(i)
                for i in range(len(steps)):
                    if i + LOOK < len(steps):
                        pts[i + LOOK] = emit_st(i + LOOK)
                    kt, comp = steps[i]
                    dq = kt - 4 * tc
                    pT = pts.pop(i)
                    for qi in range(max(dq, 0), 4):
                        a_t, a_i = acc(comp, qi)
                        S.mm([(a_t[:, a_i, :], pT[:, qi * 128:(qi + 1) * 128], vt[:, kt, :], False, False)],
                             reads=[pT, vt], writes=[a_t], skip=True)
                ytoks = []
                for qi in range(4):
                    a0, i0 = acc(0, qi)
                    a1, i1 = acc(1, qi)
                    r = rr.next()
                    S.op("dve", lambda be: be.reciprocal(r[:, 0:1], a0[:, i0, 128:129]), reads=[a0], writes=[r])
                    S.op("dve", lambda be: be.reciprocal(r[:, 1:2], a1[:, i1, 128:129]), reads=[a1, r], writes=[r])
                    S.op("dve", lambda be: be.tensor_tensor(r[:, 2:3], r[:, 1:2], lv[:, 5:6], op=ALU.mult), reads=[r, lv], writes=[r])
                    a = ar.next()
                    S.op("act", lambda be: be.activation(out=a[:], in_=a0[:, i0, 0:128], func=AF.Copy, scale=r[:, 0:1]),
                         reads=[a0, r], writes=[a])
                    d = dr.next()
                    S.op("dve", lambda be: be.scalar_tensor_tensor(out=d[:], in0=a1[:, i1, 0:128], scalar=r[:, 2:3], in1=a[:],
                                                                   op0=ALU.mult, op1=ALU.add), reads=[a1, r, a], writes=[d])
                    jk = jr.next()
                    S.op("act", lambda be: be.activation(out=jk[:], in_=d[:], func=AF.Square, accum_out=r[:, 3:4]),
                         reads=[d, r], writes=[jk, r])
                    S.op("dve", lambda be: be.tensor_scalar(r[:, 4:5], r[:, 3:4], 1.0 / 128.0, EPS, op0=ALU.mult, op1=ALU.add),
                         reads=[r], writes=[r])
                    S.op("act", lambda be: be.activation(out=r[:, 5:6], in_=r[:, 4:5], func=AF.Sqrt), reads=[r], writes=[r])
                    S.op("dve", lambda be: be.reciprocal(r[:, 6:7], r[:, 5:6]), reads=[r], writes=[r])
                    S.op("dve", lambda be: be.scalar_tensor_tensor(out=d[:], in0=d[:], scalar=r[:, 6:7], in1=sg[:],
                                                                   op0=ALU.mult, op1=ALU.mult), reads=[d, r, sg], writes=[d])
                    yk = yts[qi].next()
                    S.op("pool", lambda be: be.tensor_tensor(yk[:], d[:], zt[:, qi, :], op=ALU.mult), reads=[d, zt], writes=[yk])
                    ytoks.append(yk)
                emit_yT(S, ytoks, c.yT[2], h * 128, 1, tsl, tc, idt, ptr2, ysbr)


def phase_ml(S, c, l, L):
    NCH = L // 128
    w = c.w
    mlg = c.mlg
    with Scope(S) as st:
        Bcol = sb(S, st, "ml_Bcol", [128, NCH, 4], F32)
        Ecol = sb(S, st, "ml_Ecol", [128, NCH, 4], F32)
        mu = sb(S, st, "ml_mu", [128, 4, NCH + 1], F32)
        negmu = sb(S, st, "ml_nmu", [128, 4, NCH + 1], F32)
        dec = sb(S, st, "ml_dec", [128, 4, NCH], F32)
        with Scope(S) as g:
            ig = sb(S, g, "mlg_i", [4, L], F32)
            fg = sb(S, g, "mlg_f", [4, L], F32)
            Ft = sb(S, g, "mlg_F", [4, L], F32)
            Gt = sb(S, g, "mlg_G", [4, L], F32)
            ones = sb(S, g, "mlg_1", [4, L], F32)
            bb = sb(S, g, "mlg_b", [4, 4], F32)
            S.dma("sp", ig[:], c.mlif.ap[0:4, :], reads=[c.mlif.o(tc) for tc in range(L // 512)], writes=[ig])
            S.dma("sp", fg[:], c.mlif.ap[4:8, :], reads=[c.mlif.o(tc) for tc in range(L // 512)], writes=[fg])
            S.dma("sp", bb[:, 0:1], w["ml_b_i"].ap[l].rearrange("(h o) -> h o", o=1), writes=[bb], slow=True)
            S.dma("sp", bb[:, 1:2], w["ml_b_f"].ap[l].rearrange("(h o) -> h o", o=1), writes=[bb], slow=True)
            S.op("dve", lambda be: be.tensor_scalar(bb[:, 2:3], bb[:, 1:2], -1.0, None, op0=ALU.mult), reads=[bb], writes=[bb])
            S.op("pool", lambda be: be.memset(ones[:], 1.0), writes=[ones])
            S.op("pool", lambda be: be.memset(bb[:, 3:4], 0.0), reads=[bb], writes=[bb])
            S.op("act", lambda be: be.activation(out=fg[:], in_=fg[:], func=AF.Exp, scale=-1.0, bias=bb[:, 2:3]),
                 reads=[fg, bb], writes=[fg])
            S.op("act", lambda be: be.activation(out=fg[:], in_=fg[:], func=AF.Ln, bias=1.0), reads=[fg], writes=[fg])
            S.op("dve", lambda be: be.tensor_scalar(fg[:], fg[:], -1.0, None, op0=ALU.mult), reads=[fg], writes=[fg])
            S.op("dve", lambda be: be.tensor_tensor_scan(out=Ft[:], data0=ones[:], data1=fg[:], initial=0.0,
                                                         op0=ALU.mult, op1=ALU.add), reads=[ones, fg], writes=[Ft])
            S.op("dve", lambda be: be.scalar_tensor_tensor(out=ig[:], in0=ig[:], scalar=bb[:, 0:1], in1=Ft[:],
                                                           op0=ALU.add, op1=ALU.subtract), reads=[ig, bb, Ft], writes=[ig])
            S.op("dve", lambda be: be.tensor_tensor_scan(out=Gt[:], data0=ones[:], data1=ig[:], initial=0.0,
                                                         op0=ALU.mult, op1=ALU.max), reads=[ones, ig], writes=[Gt])
            S.op("dve", lambda be: be.tensor_tensor(fg[:], Ft[:], Gt[:], op=ALU.add), reads=[Ft, Gt, fg], writes=[fg])
            S.op("act", lambda be: be.activation(out=fg[:], in_=fg[:], func=AF.Exp, scale=-1.0), reads=[fg], writes=[fg])
            S.dma("sp", mlg.ap[0, :, 0:L], ig[:], reads=[ig], writes=[mlg.o(0)])
            S.dma("sp", mlg.ap[1, :, 0:L], fg[:], reads=[fg], writes=[mlg.o(1)])
            S.dma("sp", mlg.ap[2, :, 1:L + 1], Gt[:], reads=[Gt], writes=[mlg.o(2)])
            S.dma("sp", mlg.ap[2, :, 0:1], bb[:, 3:4], reads=[bb], writes=[mlg.o(3)], slow=True)
            for h in range(4):
                S.dma("sp", Bcol[:, :, h], mlg.ap[0, h, 0:L].rearrange("(c t) -> t c", t=128), reads=[mlg.o(0)], writes=[Bcol], slow=True)
                S.dma("sp", Ecol[:, :, h], mlg.ap[1, h, 0:L].rearrange("(c t) -> t c", t=128), reads=[mlg.o(1)], writes=[Ecol], slow=True)
                S.dma("sp", mu[:, h, :], mlg.ap[2, h:h + 1, 0:L + 1:128].partition_broadcast(128),
                      reads=[mlg.o(2), mlg.o(3)], writes=[mu], slow=True)
        S.op("dve", lambda be: be.tensor_scalar(negmu[:], mu[:], -1.0, None, op0=ALU.mult), reads=[mu], writes=[negmu])
        S.op("dve", lambda be: be.tensor_tensor(dec[:], mu[:, :, 0:NCH], mu[:, :, 1:NCH + 1], op=ALU.subtract), reads=[mu], writes=[dec])
        S.op("act", lambda be: be.activation(out=dec[:], in_=dec[:], func=AF.Exp), reads=[dec], writes=[dec])

        idt = sb(S, st, "ml_id", [128, 128], BF16)
        S.dma("sp", idt[:], c.ident.ap[:, :], writes=[idt])
        mk = sb(S, st, "ml_mk", [128, 128], BF16)
        S.dma("sp", mk[:], c.maskkq.ap[:, :], writes=[mk])
        ng = sb(S, st, "ml_ng", [128, DB], F32)
        S.dma("sp", ng[:], w["ml_norm_g"].ap[l:l + 1, :].partition_broadcast(128), writes=[ng])
        qT = sb(S, st, "ml_qT", [128, 2, L], BF16)
        kT = sb(S, st, "ml_kT", [128, 2, L], BF16)
        va = sb(S, st, "ml_va", [128, NCH, 257], BF16)
        ot = sb(S, st, "ml_o", [128, NCH, 256], BF16)
        zt = sb(S, st, "ml_z", [128, NCH, 256], BF16)
        C32 = sb(S, st, "ml_C32", [128, 2, 257], F32)
        Cbf = sb(S, st, "ml_Cbf", [128, 2, 257], BF16)
        gbr = sb_ring(S, st, "ml_gb", [128, 128], F32, 3)
        ptr_ = sb_ring(S, st, "ml_pt", [128, 128], F32, 2)
        ptmr = sb_ring(S, st, "ml_ptm", [128, 128], F32, 2)
        str_ = sb_ring(S, st, "ml_st", [128, 128], BF16, 2)
        scr = sb_ring(S, st, "ml_sc", [128, 128], F32, 2)
        qsr = sb_ring(S, st, "ml_qs", [128, 2, 128], BF16, 2)
        rr = sb_ring(S, st, "ml_r", [128, 8], F32, 3)
        hr = sb_ring(S, st, "ml_h", [128, 256], F32, 2)
        jr = sb_ring(S, st, "ml_j", [128, 256], F32, 1)
        yr = sb_ring(S, st, "ml_y", [128, 256], BF16, 2)
        ysr = sb_ring(S, st, "ml_ys", [128, 2, 128], BF16, 2)
        kwr = sb_ring(S, st, "ml_kw", [128, 2], F32, 2)
        kkr = sb_ring(S, st, "ml_kk", [128, 256], BF16, 2)
        psS = ps_ring(S, st, "ml_pS", [128, 128], F32, 2)
        psN = ps_ring(S, st, "ml_pN", [128, 257], F32, 2)
        psT = ps_ring(S, st, "ml_pT", [128, 2, 128], BF16, 2)
        psC = [ps(S, st, f"ml_pC{i}", [128, 257], F32) for i in range(2)]
        for h in range(4):
            hs = slice(h * 256, (h + 1) * 256)
            S.dma("sp", qT[:], c.mlqT.ap[hs, :].rearrange("(k p) n -> p k n", p=128),
                  reads=[c.mlqT.o((2 * h + k, tc)) for k in range(2) for tc in range(L // 512)], writes=[qT])
            S.dma("sp", kT[:], c.mlkT.ap[hs, :].rearrange("(k p) n -> p k n", p=128),
                  reads=[c.mlkT.o((2 * h + k, tc)) for k in range(2) for tc in range(L // 512)], writes=[kT])
            S.dma("sp", va[:, :, 0:256], c.mlv.ap[:, hs].rearrange("(c p) d -> p c d", p=128),
                  reads=[c.mlv.o(t) for t in range(NCH)], writes=[va])
            S.op("pool", lambda be: be.memset(va[:, :, 256:257], 1.0), writes=[va])
            S.dma("sp", ot[:], c.mlo.ap[:, hs].rearrange("(c p) d -> p c d", p=128), reads=[c.mlo.o(t) for t in range(NCH)], writes=[ot])
            S.dma("sp", zt[:], c.mlz.ap[:, hs].rearrange("(c p) d -> p c d", p=128), reads=[c.mlz.o(t) for t in range(NCH)], writes=[zt])
            for ch in range(NCH):
                csl = slice(ch * 128, (ch + 1) * 128)
                gb = gbr.next()
                S.dma("sp", gb[:], mlg.ap[2, h:h + 1, 1 + ch * 128:1 + (ch + 1) * 128].partition_broadcast(128),
                      reads=[mlg.o(2)], writes=[gb])
                pS = psS.next()
                S.mm([(pS[:], kT[:, db, csl], qT[:, db, csl], db == 0, db == 1) for db in range(2)], reads=[kT, qT], writes=[pS])
                pt = ptr_.next()
                S.op("act", lambda be: be.activation(out=pt[:], in_=gb[:], func=AF.Exp, scale=-1.0, bias=Bcol[:, ch, h:h + 1]),
                     reads=[gb, Bcol], writes=[pt])
                ptm = ptmr.next()
                S.op("pool", lambda be: be.tensor_tensor(ptm[:], pt[:], mk[:], op=ALU.mult), reads=[pt, mk], writes=[ptm])
                stt = str_.next()
                S.op("dve", lambda be: be.tensor_tensor(stt[:], pS[:], ptm[:], op=ALU.mult), reads=[pS, ptm], writes=[stt])
                items = [(None, stt[:], va[:, ch, :])]
                rds = [stt, va]
                if ch > 0:
                    sc = scr.next()
                    S.op("act", lambda be: be.activation(out=sc[:], in_=gb[:], func=AF.Exp, scale=-1.0, bias=mu[:, h, ch:ch + 1]),
                         reads=[gb, mu], writes=[sc])
                    qs = qsr.next()
                    for db in range(2):
                        S.op("pool", lambda be: be.tensor_tensor(qs[:, db, :], qT[:, db, csl], sc[:], op=ALU.mult),
                             reads=[qT, sc], writes=[qs])
                    items += [(None, qs[:, 0, :], Cbf[:, 0, :]), (None, qs[:, 1, :], Cbf[:, 1, :])]
                    rds += [qs, Cbf]
                pN = psN.next()
                n = len(items)
                S.mm([(pN[:], a, b, i == 0, i == n - 1) for i, (_, a, b) in enumerate(items)], reads=rds, writes=[pN])
                r = rr.next()
                S.op("act", lambda be: be.activation(out=r[:, 6:7], in_=pN[:, 256:257], func=AF.Abs), reads=[pN], writes=[r])
                S.op("dve", lambda be: be.tensor_tensor(r[:, 0:1], r[:, 6:7], Ecol[:, ch, h:h + 1], op=ALU.max),
                     reads=[r, Ecol], writes=[r])
                S.op("dve", lambda be: be.reciprocal(r[:, 1:2], r[:, 0:1]), reads=[r], writes=[r])
                hh = hr.next()
                S.op("act", lambda be: be.activation(out=hh[:], in_=pN[:, 0:256], func=AF.Copy, scale=r[:, 1:2]),
                     reads=[pN, r], writes=[hh])
                jk = jr.next()
                S.op("act", lambda be: be.activation(out=jk[:], in_=hh[:], func=AF.Square, accum_out=r[:, 2:3]),
                     reads=[hh, r], writes=[jk, r])
                S.op("dve", lambda be: be.tensor_scalar(r[:, 3:4], r[:, 2:3], 1.0 / 256.0, EPS, op0=ALU.mult, op1=ALU.add),
                     reads=[r], writes=[r])
                S.op("act", lambda be: be.activation(out=r[:, 4:5], in_=r[:, 3:4], func=AF.Sqrt), reads=[r], writes=[r])
                S.op("dve", lambda be: be.reciprocal(r[:, 5:6], r[:, 4:5]), reads=[r], writes=[r])
                S.op("dve", lambda be: be.scalar_tensor_tensor(out=hh[:], in0=hh[:], scalar=r[:, 5:6], in1=ng[:, hs],
                                                               op0=ALU.mult, op1=ALU.mult), reads=[hh, r, ng], writes=[hh])
                S.op("pool", lambda be: be.tensor_tensor(hh[:], hh[:], ot[:, ch, :], op=ALU.mult), reads=[hh, ot], writes=[hh])
                yk = yr.next()
                S.op("pool", lambda be: be.tensor_tensor(yk[:], hh[:], zt[:, ch, :], op=ALU.mult), reads=[hh, zt], writes=[yk])
                pT = psT.next()
                for db in range(2):
                    S.transpose(pT[:, db, :], yk[:, db * 128:(db + 1) * 128], idt[:], reads=[yk, idt], writes=[pT])
                ys = ysr.next()
                S.op("act", lambda be: be.copy(out=ys[:], in_=pT[:]), reads=[pT], writes=[ys])
                S.dma("sp", c.yT[1].ap[hs, csl].rearrange("(k p) n -> p k n", p=128), ys[:], reads=[ys],
                      writes=[c.yT[1].o((2 * h + k, ch // 4)) for k in range(2)])
                if ch == NCH - 1:
                    continue
                kw = kwr.next()
                S.op("act", lambda be: be.activation(out=kw[:, 0:1], in_=Bcol[:, ch, h:h + 1], func=AF.Exp,
                                                     bias=negmu[:, h, ch + 1:ch + 2]), reads=[Bcol, negmu], writes=[kw])
                pK = psT.next()
                for db in range(2):
                    S.transpose(pK[:, db, :], kT[:, db, csl], idt[:], reads=[kT, idt], writes=[pK])
                kk = kkr.next()
                S.op("dve", lambda be: be.tensor_scalar(kk[:], pK[:].rearrange("p a b -> p (a b)"), kw[:, 0:1], None, op0=ALU.mult),
                     reads=[pK, kw], writes=[kk])
                for db in range(2):
                    S.mm([(psC[db][:], kk[:, db * 128:(db + 1) * 128], va[:, ch, :], True, True)], reads=[kk, va], writes=[psC[db]])
                    if ch == 0:
                        S.op("dve", lambda be: be.tensor_copy(C32[:, db, :], psC[db][:]), reads=[psC[db]], writes=[C32])
                    else:
                        S.op("dve", lambda be: be.scalar_tensor_tensor(out=C32[:, db, :], in0=C32[:, db, :], scalar=dec[:, h, ch:ch + 1],
                                                                       in1=psC[db][:], op0=ALU.mult, op1=ALU.add),
                             reads=[C32, dec, psC[db]], writes=[C32])
                S.op("act", lambda be: be.copy(out=Cbf[:], in_=C32[:]), reads=[C32], writes=[Cbf])


TWO_PI = 2.0 * math.pi


def _sincos(S, out_t, ang_src, th, off, kt):
    if isinstance(th, float):
        S.op("dve", lambda be: be.tensor_scalar(out_t[0], ang_src[0], th, off, op0=ALU.mult, op1=ALU.add),
             reads=ang_src[1], writes=[out_t[1]])
    else:
        S.op("act", lambda be: be.activation(out=out_t[0], in_=ang_src[0], func=AF.Identity, scale=th, bias=off),
             reads=ang_src[1], writes=[out_t[1]])
    S.op("dve", lambda be: be.tensor_copy(kt[0], out_t[0]), reads=[out_t[1]], writes=[kt[1]])
    S.op("dve", lambda be: be.tensor_tensor(out_t[0], out_t[0], kt[0], op=ALU.subtract), reads=[out_t[1], kt[1]], writes=[out_t[1]])
    S.op("act", lambda be: be.activation(out=out_t[0], in_=out_t[0], func=AF.Sin, scale=TWO_PI * (1.0 - 1e-6)),
         reads=[out_t[1]], writes=[out_t[1]])


def phase_s5(S, c, l, L):
    w = c.w
    SEG = min(L, 1024)
    NSEG = L // SEG
    NCS = SEG // 512
    OFF_S = 0.0
    OFF_C = 0.25
    with Scope(S) as st:
        BBpad = sb(S, st, "s5_BB", [128, NG, 128], BF16)
        BBsw = sb(S, st, "s5_BBs", [128, NG, 128], BF16)
        CCpad = sb(S, st, "s5_CC", [128, NG, 128], BF16)
        r2 = sb(S, st, "s5_r2", [128, NG], F32)
        th = sb(S, st, "s5_th", [128, NG], F32)
        offs = sb(S, st, "s5_offs", [128, 2], F32)
        dsk = sb(S, st, "s5_dsk", [128, 8], F32)
        bgl = sb(S, st, "s5_bg", [128, 8], F32)
        S.dma("sp", dsk[:], w["s5_d"].ap[l].rearrange("(b p) -> p b", p=128), writes=[dsk], slow=True)
        S.dma("sp", bgl[:], w["s5_b_glu"].ap[l].rearrange("(b p) -> p b", p=128), writes=[bgl], slow=True)
        S.op("pool", lambda be: be.memset(offs[0:64, 0:1], OFF_S), writes=[offs])
        S.op("pool", lambda be: be.memset(offs[64:128, 0:1], OFF_S + 0.5), reads=[offs], writes=[offs])
        S.op("pool", lambda be: be.memset(offs[:, 1:2], OFF_C), reads=[offs], writes=[offs])
        with Scope(S) as pp:
            def t3(name):
                return sb(S, pp, name, [128, 8, 64], F32)
            lre, lim, dt, er, cs, sn, wr, wi, t1, t2, Br, Bi, Bbr, Bbi = [t3(f"s5p{i}") for i in range(14)]
            mg = sb(S, pp, "s5_mg", [128, 8], F32)
            dt8 = sb(S, pp, "s5_dt8", [128, 8], F32)
            m2 = sb(S, pp, "s5_m2", [128, 8, 128], F32)
            S.dma("sp", mg[:], c.maskg.ap[:, :], writes=[mg])
            S.dma("sp", m2[:], c.mask2.ap[:, :, :], writes=[m2])
            hre, him, hdt = w["s5_lam_re"].h, w["s5_lam_im"].h, w["s5_log_dt"].h
            for g8 in range(8):
                ps_ = slice(g8 * 16, (g8 + 1) * 16)
                S.dma("sp", lre[ps_, :, :], bass.AP(tensor=hre, offset=l * 4096 + g8 * 64, ap=[[0, 16], [512, 8], [1, 64]]), writes=[lre], slow=True)
                S.dma("sp", lim[ps_, :, :], bass.AP(tensor=him, offset=l * 4096 + g8 * 64, ap=[[0, 16], [512, 8], [1, 64]]), writes=[lim], slow=True)
                S.dma("sp", dt8[ps_, :], bass.AP(tensor=hdt, offset=l * 64 + g8, ap=[[0, 16], [8, 8]]), writes=[dt8], slow=True)
                for blk in range(8):
                    S.dma("sp", Br[ps_, blk, :], w["s5_b_re"].ap[l, blk * 8 + g8].rearrange("p c -> c p"), writes=[Br], slow=True)
                    S.dma("sp", Bi[ps_, blk, :], w["s5_b_im"].ap[l, blk * 8 + g8].rearrange("p c -> c p"), writes=[Bi], slow=True)

            def V(e, fn, rd, wr_):
                S.op(e, fn, reads=rd, writes=wr_)
            V("dve", lambda be: be.tensor_scalar(lre[:], lre[:], -1e-4, None, op0=ALU.min), [lre], [lre])
            V("act", lambda be: be.activation(out=dt8[:], in_=dt8[:], func=AF.Exp), [dt8], [dt8])
            V("dve", lambda be: be.tensor_copy(dt[:], dt8[:].unsqueeze(2).to_broadcast([128, 8, 64])), [dt8], [dt])
            V("dve", lambda be: be.tensor_tensor(t1[:], lre[:], dt[:], op=ALU.mult), [lre, dt], [t1])
            V("act", lambda be: be.activation(out=er[:], in_=t1[:], func=AF.Exp), [t1], [er])
            V("dve", lambda be: be.tensor_tensor(t2[:], lim[:], dt[:], op=ALU.mult), [lim, dt], [t2])
            kA = sb(S, pp, "s5_kA", [128, 8, 64], mybir.dt.int32)
            _sincos(S, (cs[:], cs), (t2[:], [t2]), 1.0 / TWO_PI, OFF_C, (kA[:], kA))
            _sincos(S, (sn[:], sn), (t2[:], [t2]), 1.0 / TWO_PI, OFF_S, (kA[:], kA))
            V("dve", lambda be: be.tensor_tensor(cs[:], cs[:], er[:], op=ALU.mult), [cs, er], [cs])
            V("dve", lambda be: be.tensor_tensor(sn[:], sn[:], er[:], op=ALU.mult), [sn, er], [sn])
            V("dve", lambda be: be.tensor_scalar(cs[:], cs[:], -1.0, None, op0=ALU.add), [cs], [cs])
            V("dve", lambda be: be.tensor_tensor(t1[:], lre[:], lre[:], op=ALU.mult), [lre], [t1])
            V("dve", lambda be: be.tensor_tensor(t2[:], lim[:], lim[:], op=ALU.mult), [lim], [t2])
            V("dve", lambda be: be.tensor_tensor(t1[:], t1[:], t2[:], op=ALU.add), [t1, t2], [t1])
            V("dve", lambda be: be.reciprocal(t1[:], t1[:]), [t1], [t1])
            V("dve", lambda be: be.tensor_tensor(wr[:], cs[:], lre[:], op=ALU.mult), [cs, lre], [wr])
            V("dve", lambda be: be.tensor_tensor(t2[:], sn[:], lim[:], op=ALU.mult), [sn, lim], [t2])
            V("dve", lambda be: be.tensor_tensor(wr[:], wr[:], t2[:], op=ALU.add), [wr, t2], [wr])
            V("dve", lambda be: be.tensor_tensor(wr[:], wr[:], t1[:], op=ALU.mult), [wr, t1], [wr])
            V("dve", lambda be: be.tensor_tensor(wi[:], sn[:], lre[:], op=ALU.mult), [sn, lre], [wi])
            V("dve", lambda be: be.tensor_tensor(t2[:], cs[:], lim[:], op=ALU.mult), [cs, lim], [t2])
            V("dve", lambda be: be.tensor_tensor(wi[:], wi[:], t2[:], op=ALU.subtract), [wi, t2], [wi])
            V("dve", lambda be: be.tensor_tensor(wi[:], wi[:], t1[:], op=ALU.mult), [wi, t1], [wi])
            V("dve", lambda be: be.tensor_tensor(Bbr[:], wr[:], Br[:], op=ALU.mult), [wr, Br], [Bbr])
            V("dve", lambda be: be.tensor_tensor(t2[:], wi[:], Bi[:], op=ALU.mult), [wi, Bi], [t2])
            V("dve", lambda be: be.tensor_tensor(Bbr[:], Bbr[:], t2[:], op=ALU.subtract), [Bbr, t2], [Bbr])
            V("dve", lambda be: be.tensor_tensor(Bbi[:], wr[:], Bi[:], op=ALU.mult), [wr, Bi], [Bbi])
            V("dve", lambda be: be.tensor_tensor(t2[:], wi[:], Br[:], op=ALU.mult), [wi, Br], [t2])
            V("dve", lambda be: be.tensor_tensor(Bbi[:], Bbi[:], t2[:], op=ALU.add), [Bbi, t2], [Bbi])
            mgb = mg[:].unsqueeze(1).unsqueeze(3).to_broadcast([128, 8, 8, 64])
            for dst, lo, hi in ((BBpad, Bbr, Bbi), (BBsw, Bbi, Bbr)):
                for half, src in ((0, lo), (1, hi)):
                    dv = dst[:, :, half * 64:(half + 1) * 64].rearrange("p (blk g) q -> p blk g q", g=8)
                    sv = src[:].unsqueeze(2).to_broadcast([128, 8, 8, 64])
                    V("dve", lambda be: be.tensor_tensor(dv, sv, mgb, op=ALU.mult), [src, mg], [dst])
            lb = sb(S, pp, "s5_lb", [128, NG], F32)
            S.dma("sp", lb[0:64, :], w["s5_lam_re"].ap[l].rearrange("g p -> p g"), writes=[lb], slow=True)
            S.dma("sp", lb[64:128, :], w["s5_lam_re"].ap[l].rearrange("g p -> p g"), writes=[lb], slow=True)
            S.dma("sp", th[0:64, :], w["s5_lam_im"].ap[l].rearrange("g p -> p g"), writes=[th], slow=True)
            S.dma("sp", th[64:128, :], w["s5_lam_im"].ap[l].rearrange("g p -> p g"), writes=[th], slow=True)
            dtb = sb(S, pp, "s5_dtb", [128, NG], F32)
            S.dma("sp", dtb[:], w["s5_log_dt"].ap[l:l + 1, :].partition_broadcast(128), writes=[dtb])
            V("act", lambda be: be.activation(out=dtb[:], in_=dtb[:], func=AF.Exp), [dtb], [dtb])
            V("dve", lambda be: be.tensor_scalar(lb[:], lb[:], -1e-4, None, op0=ALU.min), [lb], [lb])
            V("dve", lambda be: be.tensor_tensor(lb[:], lb[:], dtb[:], op=ALU.mult), [lb, dtb], [lb])
            V("act", lambda be: be.activation(out=r2[:], in_=lb[:], func=AF.Exp), [lb], [r2])
            V("dve", lambda be: be.tensor_tensor(th[:], th[:], dtb[:], op=ALU.mult), [th, dtb], [th])
            kB = sb(S, pp, "s5_kB", [128, NG], mybir.dt.int32)
            V("dve", lambda be: be.tensor_scalar(th[:], th[:], 1.0 / TWO_PI, None, op0=ALU.mult), [th], [th])
            V("dve", lambda be: be.tensor_copy(kB[:], th[:]), [th], [kB])
            V("dve", lambda be: be.tensor_tensor(th[:], th[:], kB[:], op=ALU.subtract), [th, kB], [th])
            Cc = sb(S, pp, "s5_Cc", [128, 8, 128], F32)
            S.dma("sp", Cc[:, :, 0:64], w["s5_c_re"].ap[l].rearrange("(blk g8) co p -> (g8 co) blk p", g8=8), writes=[Cc])
            S.dma("sp", Cc[:, :, 64:128], w["s5_c_im"].ap[l].rearrange("(blk g8) co p -> (g8 co) blk p", g8=8), writes=[Cc])
            V("dve", lambda be: be.tensor_scalar(Cc[:, :, 64:128], Cc[:, :, 64:128], -1.0, None, op0=ALU.mult), [Cc], [Cc])
            idf = sb(S, pp, "s5_idf", [128, 128], F32)
            S.dma("sp", idf[:], c.identf.ap[:, :], writes=[idf])
            pcr = ps_ring(S, pp, "s5_pc", [128, 128], F32, 2)
            for blk in range(8):
                pc = pcr.next()
                S.transpose(pc[:], Cc[:, blk, :], idf[:], reads=[Cc, idf], writes=[pc])
                V("dve", lambda be: be.tensor_tensor(CCpad[:, blk * 8:(blk + 1) * 8, :], pc[:].unsqueeze(1).to_broadcast([128, 8, 128]),
                                                     m2[:], op=ALU.mult), [pc, m2], [CCpad])

        with Scope(S) as ms:
            trs = []
            for sg in range(NSEG):
                tr_ = sb(S, ms, f"s5_tr{sg}", [128, SEG], F32)
                S.dma("sp", tr_[:], c.trow.ap[0:1, sg * SEG:(sg + 1) * SEG].partition_broadcast(128), writes=[tr_])
                trs.append(tr_)

            def rg(name, n, dt_=F32):
                return sb_ring(S, ms, name, [128, SEG], dt_, n)
            COSr, SINr = rg("s5mC", 4), rg("s5mS", 4)
            Vr, VSr = rg("s5mV", 3), rg("s5mVS", 3)
            T2r, T3r = rg("s5mT2", 2), rg("s5mT3", 2)
            BUr, BSr = rg("s5mBU", 2), rg("s5mBS", 2)
            Sgr = rg("s5_sg", 2, BF16)
            kir = rg("s5_ki", 2, mybir.dt.int32)
            ur = sb_ring(S, ms, "s5_u", [128, L], BF16, 2)
            car = sb(S, ms, "s5_car", [128, 8, 2], F32)
            yvr = sb_ring(S, ms, "s5_yv", [128, 512], F32, 2)
            tgr = sb_ring(S, ms, "s5_tg", [128, 512], F32, 2)
            ygr = sb_ring(S, ms, "s5_yg", [128, 512], BF16, 2)
            pbu = ps_ring(S, ms, "s5_pb", [128, 512], F32, 2)
            pbs = ps_ring(S, ms, "s5_pbs", [128, 512], F32, 2)
            pyr = ps_ring(S, ms, "s5_py", [128, 512], F32, 4)
            items = [(blk, sg, g8) for blk in range(8) for sg in range(NSEG) for g8 in range(8)]
            uts, pysd, stt_ = {}, {}, {}

            def get_ut(blk):
                if blk not in uts:
                    ut = ur.next()
                    S.dma("sp", ut[:], c.s5uT.ap[blk * 128:(blk + 1) * 128, :],
                          reads=[c.s5uT.o((blk, tc)) for tc in range(L // 512)], writes=[ut])
                    uts[blk] = ut
                return uts[blk]

            def stageA(i):
                blk, sg, g8 = items[i]
                g = blk * 8 + g8
                COS, SIN = COSr.next(), SINr.next()
                kt_ = kir.next()
                _sincos(S, (COS[:], COS), (trs[sg][:], [trs[sg], th, offs]), th[:, g:g + 1], offs[:, 1:2], (kt_[:], kt_))
                kt_ = kir.next()
                _sincos(S, (SIN[:], SIN), (trs[sg][:], [trs[sg], th, offs]), th[:, g:g + 1], offs[:, 0:1], (kt_[:], kt_))
                stt_[i] = dict(COS=COS, SIN=SIN)

            def stageB1(i):
                blk, sg, g8 = items[i]
                g = blk * 8 + g8
                ut = get_ut(blk)
                d = stt_[i]
                COS, SIN = d["COS"], d["SIN"]
                Vt, VS, T2, T3 = Vr.next(), VSr.next(), T2r.next(), T3r.next()
                for cs_ in range(NCS):
                    fsl = slice(cs_ * 512, (cs_ + 1) * 512)
                    tsl = slice(sg * SEG + cs_ * 512, sg * SEG + (cs_ + 1) * 512)
                    p1, p2 = pbu.next(), pbs.next()
                    S.mm([(p1[:], BBpad[:, g, :], ut[:, tsl], True, True)], reads=[BBpad, ut], writes=[p1])
                    S.mm([(p2[:], BBsw[:, g, :], ut[:, tsl], True, True)], reads=[BBsw, ut], writes=[p2])
                    S.op("dve", lambda be: be.tensor_tensor(Vt[:, fsl], COS[:, fsl], p1[:], op=ALU.mult), reads=[COS, p1], writes=[Vt])
                    S.op("dve", lambda be: be.tensor_tensor(T2[:, fsl], SIN[:, fsl], p2[:], op=ALU.mult), reads=[SIN, p2], writes=[T2])
                    S.op("dve", lambda be: be.tensor_tensor(VS[:, fsl], COS[:, fsl], p2[:], op=ALU.mult), reads=[COS, p2], writes=[VS])
                    S.op("dve", lambda be: be.tensor_tensor(T3[:, fsl], SIN[:, fsl], p1[:], op=ALU.mult), reads=[SIN, p1], writes=[T3])
                S.op("dve", lambda be: be.tensor_tensor(Vt[:], Vt[:], T2[:], op=ALU.add), reads=[Vt, T2], writes=[Vt])
                S.op("dve", lambda be: be.tensor_tensor(VS[:], VS[:], T3[:], op=ALU.subtract), reads=[VS, T3], writes=[VS])
                d.update(Vt=Vt, VS=VS)

            def stageB2(i):
                blk, sg, g8 = items[i]
                g = blk * 8 + g8
                d = stt_[i]
                Vt, VS = d["Vt"], d["VS"]
                BU, BS = BUr.next(), BSr.next()
                dec_ = r2[:, g:g + 1].to_broadcast([128, SEG])
                i0 = 0.0 if sg == 0 else car[:, g8, 0:1]
                i1 = 0.0 if sg == 0 else car[:, g8, 1:2]
                S.op("dve", lambda be: be.tensor_tensor_scan(out=BU[:], data0=dec_, data1=Vt[:], initial=i0, op0=ALU.mult, op1=ALU.add),
                     reads=[r2, Vt, car], writes=[BU])
                S.op("dve", lambda be: be.tensor_tensor_scan(out=BS[:], data0=dec_, data1=VS[:], initial=i1, op0=ALU.mult, op1=ALU.add),
                     reads=[r2, VS, car], writes=[BS])
                if sg < NSEG - 1:
                    S.op("act", lambda be: be.copy(out=car[:, g8, 0:1], in_=BU[:, SEG - 1:SEG]), reads=[BU, car], writes=[car])
                    S.op("act", lambda be: be.copy(out=car[:, g8, 1:2], in_=BS[:, SEG - 1:SEG]), reads=[BS, car], writes=[car])
                d.update(BU=BU, BS=BS)

            def stageC(i):
                blk, sg, g8 = items[i]
                g = blk * 8 + g8
                d = stt_.pop(i)
                COS, SIN, Vt, VS, BU, BS = d["COS"], d["SIN"], d["Vt"], d["VS"], d["BU"], d["BS"]
                Sg = Sgr.next()
                S.op("dve", lambda be: be.tensor_tensor(Vt[:], COS[:], BU[:], op=ALU.mult), reads=[COS, BU], writes=[Vt])
                S.op("dve", lambda be: be.tensor_tensor(VS[:], SIN[:], BS[:], op=ALU.mult), reads=[SIN, BS], writes=[VS])
                S.op("dve", lambda be: be.tensor_tensor(Sg[:], Vt[:], VS[:], op=ALU.subtract), reads=[Vt, VS], writes=[Sg])
                if (blk, sg) not in pysd:
                    pysd[(blk, sg)] = [pyr.next() for _ in range(NCS)]
                pys = pysd[(blk, sg)]
                for cs_ in range(NCS):
                    fsl = slice(cs_ * 512, (cs_ + 1) * 512)
                    S.mm([(pys[cs_][:], CCpad[:, g, :], Sg[:, fsl], g8 == 0, g8 == 7)], reads=[CCpad, Sg], writes=[pys[cs_]])
                if g8 != 7:
                    return
                ut = uts[blk]
                for cs_ in range(NCS):
                    tsl = slice(sg * SEG + cs_ * 512, sg * SEG + (cs_ + 1) * 512)
                    py = pys[cs_]
                    yv, tg, yg = yvr.next(), tgr.next(), ygr.next()
                    S.op("dve", lambda be: be.scalar_tensor_tensor(out=yv[:], in0=ut[:, tsl], scalar=dsk[:, blk:blk + 1], in1=py[:],
                                                                   op0=ALU.mult, op1=ALU.add), reads=[ut, dsk, py], writes=[yv])
                    S.op("dve", lambda be: be.tensor_tensor(tg[:], yv[:], yv[:], op=ALU.mult), reads=[yv], writes=[tg])
                    S.op("dve", lambda be: be.tensor_scalar(tg[:], tg[:], 0.044715, 1.0, op0=ALU.mult, op1=ALU.add), reads=[tg], writes=[tg])
                    S.op("dve", lambda be: be.tensor_tensor(tg[:], tg[:], yv[:], op=ALU.mult), reads=[tg, yv], writes=[tg])
                    S.op("act", lambda be: be.activation(out=tg[:], in_=tg[:], func=AF.Sigmoid, scale=2.0 * math.sqrt(2.0 / math.pi)),
                         reads=[tg], writes=[tg])
                    S.op("dve", lambda be: be.tensor_tensor(yg[:], yv[:], tg[:], op=ALU.mult), reads=[yv, tg], writes=[yg])
                    S.dma("sp", c.s5yT.ap[blk * 128:(blk + 1) * 128, tsl], yg[:], reads=[yg], writes=[c.s5yT.o((blk, tsl.start // 512))])

            n_it = len(items)
            for step in range(n_it + 3):
                if step < n_it:
                    stageA(step)
                if 0 <= step - 1 < n_it:
                    stageB1(step - 1)
                if 0 <= step - 3 < n_it:
                    stageC(step - 3)
                if 0 <= step - 2 < n_it:
                    stageB2(step - 2)

        with Scope(S) as gs:
            wg = sb(S, gs, "s5_wg", [128, 8, DB], BF16)
            for q in range(2):
                S.dma("pool", wg[:, :, q * 512:(q + 1) * 512],
                      w["s5_w_glu"].ap[l, :, q * 512:(q + 1) * 512].rearrange("(k p) n -> p k n", p=128), writes=[wg])
            ygr2 = sb_ring(S, gs, "s5_y2", [128, 8, 512], BF16, 2)
            zr2 = sb_ring(S, gs, "s5_z2", [128, 8, 512], BF16, 2)
            sgr = sb_ring(S, gs, "s5_sgm", [128, 512], F32, 2)
            outr = sb_ring(S, gs, "s5_o2", [128, 8, 512], BF16, 2)
            pgr = ps_ring(S, gs, "s5_pg", [128, 512], F32, 4)
            for tc in range(L // 512):
                tsl = slice(tc * 512, (tc + 1) * 512)
                yt, zt, ob = ygr2.next(), zr2.next(), outr.next()
                S.dma("sp", yt[:], c.s5yT.ap[:, tsl].rearrange("(k p) n -> p k n", p=128), reads=[c.s5yT.o((k, tc)) for k in range(8)], writes=[yt])
                S.dma("sp", zt[:], c.s5zT.ap[:, tsl].rearrange("(k p) n -> p k n", p=128), reads=[c.s5zT.o((k, tc)) for k in range(8)], writes=[zt])
                for j in range(8):
                    pg = pgr.next()
                    S.mm([(pg[:], wg[:, kc, j * 128:(j + 1) * 128], yt[:, kc, :], kc == 0, kc == 7) for kc in range(8)],
                         reads=[wg, yt], writes=[pg])
                    sg_ = sgr.next()
                    S.op("act", lambda be: be.activation(out=sg_[:], in_=pg[:], func=AF.Sigmoid, bias=bgl[:, j:j + 1]),
                         reads=[pg, bgl], writes=[sg_])
                    S.op("dve", lambda be: be.tensor_tensor(sg_[:], sg_[:], yt[:, j, :], op=ALU.mult), reads=[sg_, yt], writes=[sg_])
                    S.op("pool", lambda be: be.tensor_tensor(ob[:, j, :], sg_[:], zt[:, j, :], op=ALU.mult), reads=[sg_, zt], writes=[ob])
                S.dma("sp", c.yT[0].ap[:, tsl].rearrange("(k p) n -> p k n", p=128), ob[:], reads=[ob],
                      writes=[c.yT[0].o((k, tc)) for k in range(8)])


def build(L=4096, depth=4, debug=False, upto="all"):
    nc = bass.Bass("TRN2", target_bir_lowering=False)
    c = declare(nc, L, depth, debug)
    with ExitStack() as top:
        S = Sched(nc, top)
        for l in range(depth):
            xin = c.x if l == 0 else c.xs[(l - 1) % 2]
            xout = c.out if l == depth - 1 else c.xs[l % 2]
            with Scope(S) as st:
                hT = sb(S, st, "hT", [128, 16, L], BF16)
                phase_norm_T(S, c, xin, c.w["g_pre"].ap[l:l + 1, :], hT, L)
                if "inproj" in upto or upto == "all":
                    phase_inproj(S, c, l, hT, L)
            if "s5" in upto or upto == "all":
                phase_s5(S, c, l, L)
            if "ml" in upto or upto == "all":
                phase_ml(S, c, l, L)
            if "da" in upto or upto == "all":
                phase_da(S, c, l, L)
            if "xa" in upto or upto == "all":
                phase_xa(S, c, l, L)
            if "merge" in upto or upto == "all":
                phase_merge(S, c, l, L, None)
                phase_out(S, c, l, L, xin, xout, None)
        outs = [o for o in c.out.objs.values()]
        if debug:
            for d in [c.s5uT, c.s5zT, c.mlqT, c.mlkT, c.mlv, c.mlo, c.mlz, c.mlif, c.daqT, c.dakT, c.dav, c.daz,
                      c.xaqT, c.xaz, c.gT, c.mT] + c.yT + c.xs:
                outs += list(d.objs.values())
        S.final_wait("sp", outs)
        print("instructions:", S.n_inst)
    return nc, c


def make_consts(L):
    bf = ml_dtypes.bfloat16
    inv = 1.0 / (10000.0 ** (np.arange(0, 64, 2, dtype=np.float32) / 64.0))
    ang = np.arange(L, dtype=np.float32)[:, None] * inv[None, :]
    cos, sin = np.cos(ang).T, np.sin(ang).T
    c64 = np.concatenate([cos, cos], 0)
    s64 = np.concatenate([-sin, sin], 0)
    kq = (np.arange(128)[None, :] >= np.arange(128)[:, None]).astype(np.float32)
    return {
        "c_ident": np.eye(128, dtype=np.float32).astype(bf),
        "c_identf": np.eye(128, dtype=np.float32),
        "c_trow": np.arange(L, dtype=np.float32)[None, :].copy(),
        "c_ropec": np.concatenate([c64, c64], 0).astype(bf),
        "c_ropes": np.concatenate([s64, s64], 0).astype(bf),
        "c_maskkq": kq.astype(bf),
        "c_maskg": (np.arange(128)[:, None] // 16 == np.arange(8)[None, :]).astype(np.float32),
        "c_mask2": np.broadcast_to((np.arange(8)[:, None] == (np.arange(128)[None, :] // 16)).astype(np.float32)[None], (128, 8, 128)).copy(),
    }


SEQ_FULL = 4096
DEPTH_FULL = 4


def kernel(**inputs):
    L, depth = SEQ_FULL, DEPTH_FULL
    nc, _ = build(L=L, depth=depth, debug=False)
    consts = make_consts(L)
    shared = {k: np.ascontiguousarray(np.asarray(v, dtype=np.float32)) for k, v in inputs.items() if k not in ("x", "mem")}
    x = np.asarray(inputs["x"], dtype=np.float32)
    mem = np.asarray(inputs["mem"], dtype=np.float32)
    in_maps = []
    for b in range(x.shape[0]):
        m = dict(shared)
        m["x"] = np.ascontiguousarray(x[b])
        m["mem"] = np.ascontiguousarray(mem[b])
        m.update(consts)
        in_maps.append(m)
    res = run_bass_kernel_spmd(nc, in_maps, core_ids=list(range(len(in_maps))))
    return np.stack([np.asarray(r["out"], dtype=np.float32) for r in res.results], axis=0)
```

```python
import math
from contextlib import ExitStack

import numpy as np
import ml_dtypes
import concourse.bass as bass
import concourse.mybir as mybir
from concourse.bass_utils import run_bass_kernel_spmd

F32 = mybir.dt.float32
BF16 = mybir.dt.bfloat16
AF = mybir.ActivationFunctionType
ALU = mybir.AluOpType
AX = mybir.AxisListType

D = 2048
DB = 1024
MEM = 256
NG = 64
D_IN = 21512
EPS = 1e-6
import os
SAME_ENGINE_SYNC = os.environ.get("MK_SES", "1") == "1"

C_S5U, C_S5Z = 0, 1024
C_MLQ, C_MLK, C_MLV, C_MLO, C_MLZ = 2048, 3072, 4096, 5120, 6144
C_MLIF = 7168
C_DAQ, C_DAK, C_DAV, C_DAZ = 7176, 8200, 9224, 10248
C_XAQ, C_XAZ = 11272, 12296
C_GATE = 13320


class Obj:
    __slots__ = ("lw", "rd", "name")

    def __init__(self, name=""):
        self.lw = None
        self.rd = {}
        self.name = name


class Tile:
    def __init__(self, h, name):
        self.h = h
        self.o = Obj(name)

    def __getitem__(self, k):
        return self.h[k]


class Ring:
    def __init__(self, tiles):
        self.tiles = tiles
        self.i = 0

    def next(self):
        t = self.tiles[self.i]
        self.i = (self.i + 1) % len(self.tiles)
        return t


class DT:
    def __init__(self, nc, name, shape, dtype, kind="Internal"):
        self.h = nc.dram_tensor(name, list(shape), dtype, kind=kind)
        self.ap = self.h.ap()
        self.objs = {}
        self.name = name

    def o(self, key=0):
        if key not in self.objs:
            self.objs[key] = Obj(f"{self.name}:{key}")
        return self.objs[key]


class Sched:
    def __init__(self, nc, stack, n_sp=40, n_pool=8, n_act=4):
        self.nc = nc
        self.eng = {"pe": nc.tensor, "act": nc.scalar, "dve": nc.vector, "pool": nc.gpsimd, "sp": nc.sync}
        self.semobj = {}
        for e in ("pe", "act", "dve", "pool"):
            self.semobj[e] = stack.enter_context(nc.semaphore("s_" + e))
        self.tick = {e: 0 for e in ("pe", "act", "dve", "pool")}
        self.waited = {e: {} for e in self.eng}
        self.dq = {"sp": n_sp, "pool": n_pool, "act": n_act}
        self.dnext = {q: 0 for q in self.dq}
        self.dcnt = {}
        for q, n in self.dq.items():
            for i in range(n):
                self.semobj[(q, i)] = stack.enter_context(nc.semaphore(f"d_{q}{i}"))
                self.dcnt[(q, i)] = 0
        self.n_inst = 0
        self.released = {}

    def _deps(self, reads, writes):
        deps = []
        for t in reads:
            o = t.o if isinstance(t, Tile) else t
            if o.lw is not None:
                deps.append(o.lw)
        for t in writes:
            o = t.o if isinstance(t, Tile) else t
            if o.lw is not None:
                deps.append(o.lw)
            deps.extend(o.rd.items())
        return deps

    def _wait(self, e, deps):
        w = self.waited[e]
        need = {}
        for sk, val in deps:
            if w.get(sk, 0) < val and need.get(sk, 0) < val:
                need[sk] = val
        for sk, val in need.items():
            w[sk] = val
            self.eng[e].wait_ge(self.semobj[sk], val)
            self.n_inst += 1

    def _mark(self, tok, reads, writes):
        for t in writes:
            o = t.o if isinstance(t, Tile) else t
            o.lw = tok
            o.rd = {}
        for t in reads:
            o = t.o if isinstance(t, Tile) else t
            if o.rd.get(tok[0], 0) < tok[1]:
                o.rd[tok[0]] = tok[1]

    def op(self, e, fn, reads=(), writes=()):
        deps = self._deps(reads, writes)
        if e == "pe" or not SAME_ENGINE_SYNC:
            deps = [d for d in deps if d[0] != e]
        self._wait(e, deps)
        self.tick[e] += 1
        fn(self.eng[e]).then_inc(self.semobj[e], 1)
        self.n_inst += 1
        self._mark((e, self.tick[e]), reads, writes)

    def mm(self, items, reads=(), writes=(), skip=False):
        deps = [d for d in self._deps(reads, writes) if d[0] != "pe"]
        self._wait("pe", deps)
        n = len(items)
        for i, (out, lhsT, rhs, st, sp) in enumerate(items):
            if skip:
                ins = self.nc.tensor.matmul(out, lhsT, rhs, start=st, stop=sp, skip_group_check=True)
            else:
                ins = self.nc.tensor.matmul(out, lhsT, rhs, start=st, stop=sp)
            self.n_inst += 1
            if i == n - 1:
                self.tick["pe"] += 1
                ins.then_inc(self.semobj["pe"], 1)
        self._mark(("pe", self.tick["pe"]), reads, writes)

    def transpose(self, out, in_, ident, reads=(), writes=()):
        self.op("pe", lambda be: be.transpose(out, in_, ident), reads=reads, writes=writes)

    def dma(self, q, out, in_, reads=(), writes=(), slow=False):
        i = self.dnext[q]
        self.dnext[q] = (i + 1) % self.dq[q]
        sk = (q, i)
        prev = self.dcnt[sk]
        deps = self._deps(reads, writes)
        if q in self.tick and not SAME_ENGINE_SYNC:
            deps = [d for d in deps if d[0] != q]
        if prev > 0:
            deps.append((sk, prev))
        self._wait(q, deps)
        self.dcnt[sk] = prev + 16
        if slow:
            self.eng[q].dma_start(out=out, in_=in_, allow_slow_non_contiguous=True).then_inc(self.semobj[sk], 16)
        else:
            self.eng[q].dma_start(out=out, in_=in_).then_inc(self.semobj[sk], 16)
        self.n_inst += 1
        self._mark((sk, prev + 16), reads, writes)

    def final_wait(self, e, objs):
        deps = []
        for o in objs:
            if o.lw is not None:
                deps.append(o.lw)
        self._wait(e, deps)


class Scope(ExitStack):
    def __init__(self, S):
        super().__init__()
        self.S = S
        self.tiles = []

    def __exit__(self, *a):
        rel = self.S.released
        for t in self.tiles:
            o = t.o
            if o.lw is not None and rel.get(o.lw[0], 0) < o.lw[1]:
                rel[o.lw[0]] = o.lw[1]
            for k, v in o.rd.items():
                if rel.get(k, 0) < v:
                    rel[k] = v
        return super().__exit__(*a)


_UID = [0]


def _uname(name):
    _UID[0] += 1
    return f"{name}_{_UID[0]}"


def _new_tile(S, stack, h, name):
    t = Tile(h, name)
    t.o.rd = dict(S.released)
    stack.tiles.append(t)
    return t


def sb(S, stack, name, shape, dtype):
    return _new_tile(S, stack, stack.enter_context(S.nc.sbuf_tensor(_uname(name), list(shape), dtype)), name)


def ps(S, stack, name, shape, dtype=F32):
    return _new_tile(S, stack, stack.enter_context(S.nc.psum_tensor(_uname(name), list(shape), dtype)), name)


def sb_ring(S, stack, name, shape, dtype, n):
    return Ring([sb(S, stack, f"{name}{i}", shape, dtype) for i in range(n)])


def ps_ring(S, stack, name, shape, dtype, n):
    return Ring([ps(S, stack, f"{name}{i}", shape, dtype) for i in range(n)])


class Ctx:
    pass


def declare(nc, L, depth, debug):
    c = Ctx()
    c.L, c.depth = L, depth
    kin = "ExternalInput"
    c.x = DT(nc, "x", [L, D], F32, kin)
    c.mem = DT(nc, "mem", [MEM, D], F32, kin)
    shapes = dict(
        g_pre=[depth, D], w_in=[depth, D, D_IN], s5_lam_re=[depth, NG, 64], s5_lam_im=[depth, NG, 64],
        s5_log_dt=[depth, NG], s5_b_re=[depth, NG, 64, 16], s5_b_im=[depth, NG, 64, 16],
        s5_c_re=[depth, NG, 16, 64], s5_c_im=[depth, NG, 16, 64], s5_d=[depth, DB],
        s5_w_glu=[depth, DB, DB], s5_b_glu=[depth, DB], ml_conv_w=[depth, 4, 2 * DB], ml_conv_b=[depth, 2 * DB],
        ml_b_i=[depth, 4], ml_b_f=[depth, 4], ml_norm_g=[depth, DB], da_lq1=[depth, 64], da_lk1=[depth, 64],
        da_lq2=[depth, 64], da_lk2=[depth, 64], da_subln_g=[depth, 128], g_mem=[depth, D],
        xa_w_kv=[depth, D, 2 * DB], w_branch=[depth, 4, DB, D], w_out=[depth, D, D], g_post=[depth, D])
    c.w = {k: DT(nc, k, v, F32, kin) for k, v in shapes.items()}
    c.ident = DT(nc, "c_ident", [128, 128], BF16, kin)
    c.identf = DT(nc, "c_identf", [128, 128], F32, kin)
    c.trow = DT(nc, "c_trow", [1, L], F32, kin)
    c.ropec = DT(nc, "c_ropec", [128, L], BF16, kin)
    c.ropes = DT(nc, "c_ropes", [128, L], BF16, kin)
    c.maskkq = DT(nc, "c_maskkq", [128, 128], BF16, kin)
    c.maskg = DT(nc, "c_maskg", [128, 8], F32, kin)
    c.mask2 = DT(nc, "c_mask2", [128, 8, 128], F32, kin)
    c.out = DT(nc, "out", [L, D], F32, "ExternalOutput")
    sk = "ExternalOutput" if debug else "Internal"
    c.dbg = debug
    c.xs = [DT(nc, f"xs{i}", [L, D], F32, sk) for i in range(2)]
    c.s5uT = DT(nc, "s5uT", [DB, L], BF16, sk)
    c.s5zT = DT(nc, "s5zT", [DB, L], BF16, sk)
    c.mlqT = DT(nc, "mlqT", [DB, L], BF16, sk)
    c.mlkT = DT(nc, "mlkT", [DB, L], BF16, sk)
    c.mlv = DT(nc, "mlv", [L, DB], BF16, sk)
    c.mlo = DT(nc, "mlo", [L, DB], BF16, sk)
    c.mlz = DT(nc, "mlz", [L, DB], BF16, sk)
    c.mlif = DT(nc, "mlif", [8, L], F32, sk)
    c.daqT = DT(nc, "daqT", [DB, L], BF16, sk)
    c.dakT = DT(nc, "dakT", [DB, L], BF16, sk)
    c.dav = DT(nc, "dav", [L, DB], BF16, sk)
    c.daz = DT(nc, "daz", [L, DB], BF16, sk)
    c.xaqT = DT(nc, "xaqT", [DB, L], BF16, sk)
    c.xaz = DT(nc, "xaz", [L, DB], BF16, sk)
    c.gT = DT(nc, "gT", [4 * D, L], BF16, sk)
    c.yT = [DT(nc, f"yT{b}", [DB, L], BF16, sk) for b in range(4)]
    c.mT = DT(nc, "mT", [D, L], BF16, sk)
    c.mlg = DT(nc, "mlg", [3, 4, L + 1], F32, "Internal")
    c.s5yT = DT(nc, "s5yT", [DB, L], BF16, sk)
    return c


def bcast_rows(ap_row, nparts):
    return ap_row.partition_broadcast(nparts)


def chunk_objs(S, stack, n):
    objs = []
    for i in range(n):
        o = Obj(f"chunk{i}")
        o.rd = dict(S.released)
        h = Tile(None, "chunk")
        h.o = o
        stack.tiles.append(h)
        objs.append(o)
    return objs


def phase_norm_T(S, c, src, g_row_ap, hT, L, rows=None, tag="a", hobjs=None):
    nc = S.nc
    rows = L if rows is None else rows
    with Scope(S) as st:
        gb = sb(S, st, tag + "_gb", [128, D], F32)
        S.dma("sp", gb[:], bcast_rows(g_row_ap, 128), writes=[gb])
        idt = sb(S, st, tag + "_id", [128, 128], BF16)
        S.dma("sp", idt[:], c.ident.ap[:, :], writes=[idt])
        xr = sb_ring(S, st, tag + "_x", [128, D], F32, 2)
        jr = sb_ring(S, st, tag + "_j", [128, D], F32, 1)
        hr = sb_ring(S, st, tag + "_h", [128, D], BF16, 2)
        ssr = sb_ring(S, st, tag + "_ss", [128, 4], F32, 2)
        pr = ps_ring(S, st, tag + "_p", [128, 512], BF16, 4)
        for t in range(rows // 128):
            xt = xr.next()
            S.dma("sp", xt[:], src.ap[t * 128:(t + 1) * 128, :], reads=[src.o(t)], writes=[xt])
            jk = jr.next()
            ss = ssr.next()
            S.op("act", lambda be: be.activation(out=jk[:], in_=xt[:], func=AF.Square, accum_out=ss[:, 0:1]),
                 reads=[xt], writes=[jk, ss])
            S.op("dve", lambda be: be.tensor_scalar(ss[:, 1:2], ss[:, 0:1], 1.0 / D, EPS, op0=ALU.mult, op1=ALU.add),
                 reads=[ss], writes=[ss])
            S.op("act", lambda be: be.activation(out=ss[:, 2:3], in_=ss[:, 1:2], func=AF.Sqrt), reads=[ss], writes=[ss])
            S.op("dve", lambda be: be.reciprocal(ss[:, 3:4], ss[:, 2:3]), reads=[ss], writes=[ss])
            ht = hr.next()
            S.op("dve", lambda be: be.scalar_tensor_tensor(out=ht[:], in0=xt[:], scalar=ss[:, 3:4], in1=gb[:],
                                                           op0=ALU.mult, op1=ALU.mult),
                 reads=[xt, ss, gb], writes=[ht])
            for q in range(4):
                pt = pr.next()
                for j in range(4):
                    kc = q * 4 + j
                    S.transpose(pt[:, j * 128:(j + 1) * 128], ht[:, kc * 128:(kc + 1) * 128], idt[:],
                                reads=[ht, idt], writes=[pt])
                eng = "act" if q % 2 else "dve"
                dst = hT[:, q * 4:(q + 1) * 4, t * 128:(t + 1) * 128]
                srcp = pt[:].rearrange("p (j n) -> p j n", j=4)
                wo_ = [hobjs[t // 4]] if hobjs is not None else [hT]
                if eng == "act":
                    S.op("act", lambda be: be.copy(out=dst, in_=srcp), reads=[pt], writes=wo_)
                else:
                    S.op("dve", lambda be: be.tensor_copy(dst, srcp), reads=[pt], writes=wo_)


def phase_inproj(S, c, l, hT, L, hobjs):
    nc = S.nc
    w_in = c.w["w_in"].ap
    NT, NC = L // 128, L // 512
    with Scope(S) as st:
        wr = sb_ring(S, st, "ip_w", [128, 16, 512], BF16, 2)
        wsw = sb(S, st, "ip_wsw", [128, 16, 512], BF16)
        pr = ps_ring(S, st, "ip_p", [128, 512], F32, 6)
        sbf = sb_ring(S, st, "ip_sb", [128, 512], BF16, 4)
        sf32 = sb_ring(S, st, "ip_sf", [128, 512], F32, 2)
        pre = sb_ring(S, st, "ip_pre", [128, 3 + 512], F32, 2)
        acc = sb_ring(S, st, "ip_acc", [128, 512], F32, 2)
        rcr = sb_ring(S, st, "ip_rc", [128, 512], BF16, 2)
        rsr = sb_ring(S, st, "ip_rs", [128, 512], BF16, 2)
        cw = sb(S, st, "ip_cw", [128, 4, 16], F32)
        cb = sb(S, st, "ip_cb", [128, 16], F32)
        for j in range(4):
            S.dma("sp", cw[:, j, :], c.w["ml_conv_w"].ap[l, j].rearrange("(b p) -> p b", p=128), writes=[cw], slow=True)
        S.dma("sp", cb[:], c.w["ml_conv_b"].ap[l].rearrange("(b p) -> p b", p=128), writes=[cb], slow=True)

        def load_w(c0, n):
            wc = wr.next()
            S.dma("pool", wc[:, :, :n], w_in[l, :, c0:c0 + n].rearrange("(k p) n -> p k n", p=128), writes=[wc])
            return wc

        def tok_major(c0, dst, func):
            for cc in range(0, DB, 512):
                wc = load_w(c0 + cc, 512)
                for t in range(NT):
                    p = pr.next()
                    S.mm([(p[:], hT[:, kc, t * 128:(t + 1) * 128], wc[:, kc, :], kc == 0, kc == 15) for kc in range(16)],
                         reads=[hobjs[t // 4], wc], writes=[p])
                    s = sbf.next()
                    if func is None:
                        S.op("dve", lambda be: be.tensor_copy(s[:], p[:]), reads=[p], writes=[s])
                    else:
                        S.op("act", lambda be: be.activation(out=s[:], in_=p[:], func=func), reads=[p], writes=[s])
                    S.dma("sp", dst.ap[t * 128:(t + 1) * 128, cc:cc + 512], s[:], reads=[s], writes=[dst.o(t)])

        def feat_major(c0, ncols, dst, row0, kind, cbase=0):
            for cc in range(0, ncols, 512):
                n = min(512, ncols - cc)
                wc = load_w(c0 + cc, n)
                if kind == "rope":
                    srcv = wc[:, :, :n].rearrange("p k (b h d) -> p k b h d", h=2, d=32)
                    dstv = wsw[:, :, :n].rearrange("p k (b h d) -> p k b h d", h=2, d=32)
                    for kc in range(16):
                        S.op("act" if kc % 2 else "dve",
                             (lambda be: be.copy(out=dstv[:, kc, :, 0, :], in_=srcv[:, kc, :, 1, :])) if kc % 2 else
                             (lambda be: be.tensor_copy(dstv[:, kc, :, 0, :], srcv[:, kc, :, 1, :])), reads=[wc], writes=[wsw])
                        S.op("act" if kc % 2 else "dve",
                             (lambda be: be.copy(out=dstv[:, kc, :, 1, :], in_=srcv[:, kc, :, 0, :])) if kc % 2 else
                             (lambda be: be.tensor_copy(dstv[:, kc, :, 1, :], srcv[:, kc, :, 0, :])), reads=[wc], writes=[wsw])
                for j in range(n // 128):
                    blk = (cc // 128) + j
                    prev = None
                    for tc in range(NC):
                        tsl = slice(tc * 512, (tc + 1) * 512)
                        p = pr.next()
                        S.mm([(p[:], wc[:, kc, j * 128:(j + 1) * 128], hT[:, kc, tsl], kc == 0, kc == 15)
                              for kc in range(16)], reads=[hobjs[tc], wc], writes=[p])
                        s = sbf.next()
                        if kind == "copy":
                            S.op("dve", lambda be: be.tensor_copy(s[:], p[:]), reads=[p], writes=[s])
                        elif kind in ("silu", "sigmoid"):
                            f = AF.Silu if kind == "silu" else AF.Sigmoid
                            S.op("act", lambda be: be.activation(out=s[:], in_=p[:], func=f), reads=[p], writes=[s])
                        elif kind in ("conv", "convk"):
                            cblk = cbase + blk
                            pt = pre.next()
                            if prev is None:
                                S.op("dve", lambda be: be.memset(pt[:, 0:3], 0.0), writes=[pt])
                            else:
                                pv = prev
                                S.op("act", lambda be: be.copy(out=pt[:, 0:3], in_=pv[:, 512:515]), reads=[pv], writes=[pt])
                            S.op("act", lambda be: be.copy(out=pt[:, 3:515], in_=p[:]), reads=[p], writes=[pt])
                            a = acc.next()
                            S.op("dve", lambda be: be.tensor_scalar(a[:], pt[:, 3:515], cw[:, 3, cblk:cblk + 1], cb[:, cblk:cblk + 1],
                                                                   op0=ALU.mult, op1=ALU.add), reads=[pt, cw, cb], writes=[a])
                            for tap in range(3):
                                S.op("dve", lambda be: be.scalar_tensor_tensor(out=a[:], in0=pt[:, tap:tap + 512],
                                                                               scalar=cw[:, tap, cblk:cblk + 1], in1=a[:],
                                                                               op0=ALU.mult, op1=ALU.add),
                                     reads=[pt, cw, a], writes=[a])
                            if kind == "conv":
                                S.op("act", lambda be: be.activation(out=s[:], in_=a[:], func=AF.Silu), reads=[a], writes=[s])
                            else:
                                a2 = sf32.next()
                                S.op("act", lambda be: be.activation(out=a2[:], in_=a[:], func=AF.Silu), reads=[a], writes=[a2])
                                S.op("dve", lambda be: be.tensor_scalar(s[:], a2[:], 0.0625, None, op0=ALU.mult),
                                     reads=[a2], writes=[s])
                            prev = pt
                        elif kind == "rope":
                            p2 = pr.next()
                            S.mm([(p2[:], wsw[:, kc, j * 128:(j + 1) * 128], hT[:, kc, tsl], kc == 0, kc == 15)
                                  for kc in range(16)], reads=[hobjs[tc], wsw], writes=[p2])
                            a = acc.next()
                            a2 = sf32.next()
                            rc, rs = rcr.next(), rsr.next()
                            S.dma("sp", rc[:], c.ropec.ap[:, tsl], writes=[rc])
                            S.dma("sp", rs[:], c.ropes.ap[:, tsl], writes=[rs])
                            S.op("dve", lambda be: be.tensor_tensor(a[:], p[:], rc[:], op=ALU.mult), reads=[p, rc], writes=[a])
                            S.op("dve", lambda be: be.tensor_tensor(a2[:], p2[:], rs[:], op=ALU.mult), reads=[p2, rs], writes=[a2])
                            S.op("dve", lambda be: be.tensor_tensor(s[:], a[:], a2[:], op=ALU.add), reads=[a, a2], writes=[s])
                        r0 = row0 + blk * 128
                        S.dma("sp", dst.ap[r0:r0 + 128, tsl], s[:], reads=[s], writes=[dst.o((r0 // 128, tc))])

        feat_major(C_S5U, DB, c.s5uT, 0, "copy")
        feat_major(C_S5Z, DB, c.s5zT, 0, "silu")
        feat_major(C_MLQ, DB, c.mlqT, 0, "conv", cbase=0)
        feat_major(C_MLK, DB, c.mlkT, 0, "convk", cbase=8)
        tok_major(C_MLV, c.mlv, None)
        tok_major(C_MLO, c.mlo, AF.Sigmoid)
        tok_major(C_MLZ, c.mlz, AF.Silu)
        wc = load_w(C_MLIF, 8)
        for tc in range(NC):
            tsl = slice(tc * 512, (tc + 1) * 512)
            p = pr.next()
            S.mm([(p[0:8, :], wc[:, kc, 0:8], hT[:, kc, tsl], kc == 0, kc == 15) for kc in range(16)],
                 reads=[hobjs[tc], wc], writes=[p])
            a = acc.next()
            S.op("dve", lambda be: be.tensor_copy(a[0:8, :], p[0:8, :]), reads=[p], writes=[a])
            S.dma("sp", c.mlif.ap[:, tsl], a[0:8, :], reads=[a], writes=[c.mlif.o(tc)])
        feat_major(C_DAQ, DB, c.daqT, 0, "rope")
        feat_major(C_DAK, DB, c.dakT, 0, "rope")
        tok_major(C_DAV, c.dav, None)
        tok_major(C_DAZ, c.daz, AF.Silu)
        feat_major(C_XAQ, DB, c.xaqT, 0, "copy")
        tok_major(C_XAZ, c.xaz, AF.Silu)
        feat_major(C_GATE, 4 * D, c.gT, 0, "sigmoid")


def emit_yT(S, ytoks, dst, row0, nblk, tsl, key_tc, idt, ptr, ysbr):
    ysb = ysbr.next()
    for blk in range(nblk):
        pt = ptr.next()
        for qt in range(4):
            S.transpose(pt[:, qt * 128:(qt + 1) * 128], ytoks[qt][:, blk * 128:(blk + 1) * 128], idt[:],
                        reads=[ytoks[qt], idt], writes=[pt])
        if blk % 2:
            S.op("act", lambda be: be.copy(out=ysb[:, blk, :], in_=pt[:]), reads=[pt], writes=[ysb])
        else:
            S.op("dve", lambda be: be.tensor_copy(ysb[:, blk, :], pt[:]), reads=[pt], writes=[ysb])
    S.dma("sp", dst.ap[row0:row0 + nblk * 128, tsl].rearrange("(k p) n -> p k n", p=128), ysb[:, 0:nblk, :],
          reads=[ysb], writes=[dst.o((r, key_tc)) for r in range(row0 // 128, row0 // 128 + nblk)])


def phase_merge(S, c, l, L, wo):
    NC = L // 512
    wb = c.w["w_branch"].ap
    with Scope(S) as st:
        wqr = sb_ring(S, st, "mg_w", [128, 32, 512], BF16, 2)

        def load_wq(dq):
            t = wqr.next()
            for b in range(4):
                S.dma("pool", t[:, b * 8:(b + 1) * 8, :],
                      wb[l, b, :, dq * 512:(dq + 1) * 512].rearrange("(k p) n -> p k n", p=128), writes=[t])
            return t
        wq_next = load_wq(0)
        yr = sb_ring(S, st, "mg_y", [128, 32, 512], BF16, 2)
        gr = sb_ring(S, st, "mg_g", [128, 4, 512], BF16, 2)
        tr = sb_ring(S, st, "mg_t", [128, 4, 512], F32, 2)
        ar = sb_ring(S, st, "mg_a", [128, 2, 512], F32, 2)
        orr = sb_ring(S, st, "mg_o", [128, 512], BF16, 2)
        pr = ps_ring(S, st, "mg_p", [128, 512], F32, 8)
        gview = c.gT.ap.rearrange("(b r) n -> r b n", b=4)
        for dq in range(4):
            wq = wq_next
            if dq < 3:
                wq_next = load_wq(dq + 1)
            for tc in range(NC):
                tsl = slice(tc * 512, (tc + 1) * 512)
                yt = yr.next()
                for b in range(4):
                    S.dma("sp", yt[:, b * 8:(b + 1) * 8, :], c.yT[b].ap[:, tsl].rearrange("(k p) n -> p k n", p=128),
                          reads=[c.yT[b].o((r, tc)) for r in range(8)], writes=[yt])
                for j in range(4):
                    r0 = dq * 512 + j * 128
                    gt = gr.next()
                    S.dma("sp", gt[:], gview[r0:r0 + 128, :, tsl],
                          reads=[c.gT.o(((b * D + r0) // 128, tc)) for b in range(4)], writes=[gt])
                    tt = tr.next()
                    for b in range(4):
                        p = pr.next()
                        S.mm([(p[:], wq[:, b * 8 + kc, j * 128:(j + 1) * 128], yt[:, b * 8 + kc, :], kc == 0, kc == 7)
                              for kc in range(8)], reads=[wq, yt], writes=[p])
                        S.op("dve", lambda be: be.tensor_tensor(tt[:, b, :], p[:], gt[:, b, :], op=ALU.mult),
                             reads=[p, gt], writes=[tt])
                    a = ar.next()
                    S.op("dve", lambda be: be.tensor_tensor(a[:], tt[:, 0:2, :], tt[:, 2:4, :], op=ALU.add), reads=[tt], writes=[a])
                    o = orr.next()
                    S.op("dve", lambda be: be.tensor_tensor(o[:], a[:, 0, :], a[:, 1, :], op=ALU.add), reads=[a], writes=[o])
                    S.dma("sp", c.mT.ap[r0:r0 + 128, tsl], o[:], reads=[o], writes=[c.mT.o((r0 // 128, tc))])


def phase_out(S, c, l, L, xin, xout, wo):
    NT = L // 128
    with Scope(S) as st:
        wo = sb(S, st, "po_w", [128, 16, D], BF16)
        for q in range(4):
            S.dma("pool", wo[:, :, q * 512:(q + 1) * 512],
                  c.w["w_out"].ap[l, :, q * 512:(q + 1) * 512].rearrange("(k p) n -> p k n", p=128), writes=[wo])
        gp = sb(S, st, "po_g", [128, D], F32)
        S.dma("sp", gp[:], c.w["g_post"].ap[l:l + 1, :].partition_broadcast(128), writes=[gp])
        mr = sb_ring(S, st, "po_m", [128, 16, 512], BF16, 2)
        xr = sb_ring(S, st, "po_x", [128, D], F32, 2)
        orr = sb_ring(S, st, "po_o", [128, D], F32, 2)
        jr = sb_ring(S, st, "po_j", [128, 512], F32, 2)
        ssr = sb_ring(S, st, "po_ss", [128, 8], F32, 2)
        pr = ps_ring(S, st, "po_p", [128, 512], F32, 8)
        for t in range(NT):
            if t % 4 == 0:
                mt = mr.next()
                S.dma("sp", mt[:], c.mT.ap[:, t * 128:(t + 4) * 128].rearrange("(k p) n -> p k n", p=128),
                      reads=[c.mT.o((r, t // 4)) for r in range(16)], writes=[mt])
            msl = slice((t % 4) * 128, (t % 4 + 1) * 128)
            xt = xr.next()
            S.dma("sp", xt[:], xin.ap[t * 128:(t + 1) * 128, :], reads=[xin.o(t)], writes=[xt])
            ss = ssr.next()
            pp = []
            for n in range(4):
                p = pr.next()
                pp.append(p)
                S.mm([(p[:], mt[:, kc, msl], wo[:, kc, n * 512:(n + 1) * 512], kc == 0, kc == 15) for kc in range(16)],
                     reads=[mt, wo], writes=[p])
                jk = jr.next()
                S.op("act", lambda be: be.activation(out=jk[:], in_=p[:], func=AF.Square, accum_out=ss[:, n:n + 1]),
                     reads=[p], writes=[jk, ss])
            S.op("dve", lambda be: be.reduce_sum(out=ss[:, 4:5], in_=ss[:, 0:4], axis=AX.X), reads=[ss], writes=[ss])
            S.op("dve", lambda be: be.tensor_scalar(ss[:, 5:6], ss[:, 4:5], 1.0 / D, EPS, op0=ALU.mult, op1=ALU.add),
                 reads=[ss], writes=[ss])
            S.op("act", lambda be: be.activation(out=ss[:, 6:7], in_=ss[:, 5:6], func=AF.Sqrt), reads=[ss], writes=[ss])
            S.op("dve", lambda be: be.reciprocal(ss[:, 7:8], ss[:, 6:7]), reads=[ss], writes=[ss])
            ot = orr.next()
            for n in range(4):
                nsl = slice(n * 512, (n + 1) * 512)
                p = pp[n]
                S.op("dve", lambda be: be.scalar_tensor_tensor(out=ot[:, nsl], in0=p[:], scalar=ss[:, 7:8], in1=gp[:, nsl],
                                                               op0=ALU.mult, op1=ALU.mult), reads=[p, ss, gp], writes=[ot])
            S.op("dve", lambda be: be.tensor_tensor(ot[:], ot[:], xt[:], op=ALU.add), reads=[ot, xt], writes=[ot])
            S.dma("sp", xout.ap[t * 128:(t + 1) * 128, :], ot[:], reads=[ot], writes=[xout.o(t)])


def phase_xa(S, c, l, L):
    NC = L // 512
    wkv = c.w["xa_w_kv"].ap
    with Scope(S) as st:
        idt = sb(S, st, "xa_id", [128, 128], BF16)
        S.dma("sp", idt[:], c.ident.ap[:, :], writes=[idt])
        kT = sb(S, st, "xa_kT", [128, 8, MEM], BF16)
        vaug = sb(S, st, "xa_v", [128, 2, 4, 257], BF16)
        S.op("pool", lambda be: be.memset(vaug[:, :, :, 256:257], 1.0), writes=[vaug])
        with Scope(S) as st2:
            memT = sb(S, st2, "xa_memT", [128, 16, MEM], BF16)
            phase_norm_T(S, c, c.mem, c.w["g_mem"].ap[l:l + 1, :], memT, MEM, rows=MEM, tag="xn")
            wr = sb_ring(S, st2, "xa_w", [128, 16, 512], BF16, 2)
            pr = ps_ring(S, st2, "xa_pp", [128, 512], F32, 2)
            for q in range(4):
                wc = wr.next()
                S.dma("pool", wc[:], wkv[l, :, q * 512:(q + 1) * 512].rearrange("(k p) n -> p k n", p=128), writes=[wc])
                if q < 2:
                    for j in range(4):
                        p = pr.next()
                        S.mm([(p[:, 0:MEM], wc[:, kc, j * 128:(j + 1) * 128], memT[:, kc, :], kc == 0, kc == 15)
                              for kc in range(16)], reads=[wc, memT], writes=[p])
                        S.op("dve", lambda be: be.tensor_copy(kT[:, q * 4 + j, :], p[:, 0:MEM]), reads=[p], writes=[kT])
                else:
                    for mt in range(2):
                        p = pr.next()
                        S.mm([(p[:], memT[:, kc, mt * 128:(mt + 1) * 128], wc[:, kc, :], kc == 0, kc == 15)
                              for kc in range(16)], reads=[wc, memT], writes=[p])
                        h0 = (q - 2) * 2
                        S.op("dve", lambda be: be.tensor_copy(vaug[:, mt, h0:h0 + 2, 0:256],
                                                              p[:].rearrange("p (h d) -> p h d", h=2)),
                             reads=[p], writes=[vaug])
        qr = sb_ring(S, st, "xa_q", [128, 8, 512], BF16, 2)
        zr = sb_ring(S, st, "xa_z", [128, 4, DB], BF16, 2)
        ptr_ = sb_ring(S, st, "xa_pT", [128, 512], BF16, 4)
        yts = [sb_ring(S, st, f"xa_y{i}", [128, DB], BF16, 2) for i in range(4)]
        rr = sb_ring(S, st, "xa_r", [128, 2], F32, 4)
        ysbr = sb_ring(S, st, "xa_ysb", [128, 8, 512], BF16, 2)
        psr = ps_ring(S, st, "xa_ps", [128, 512], F32, 3)
        por = ps_ring(S, st, "xa_po", [128, 257], F32, 3)
        ptr2 = ps_ring(S, st, "xa_pt", [128, 512], BF16, 2)
        for tc in range(NC):
            tsl = slice(tc * 512, (tc + 1) * 512)
            qt_ = qr.next()
            S.dma("sp", qt_[:], c.xaqT.ap[:, tsl].rearrange("(k p) n -> p k n", p=128),
                  reads=[c.xaqT.o((r, tc)) for r in range(8)], writes=[qt_])
            zt = zr.next()
            S.dma("sp", zt[:], c.xaz.ap[tsl, :].rearrange("(t p) d -> p t d", p=128),
                  reads=[c.xaz.o(tc * 4 + i) for i in range(4)], writes=[zt])
            ytoks = [yts[i].next() for i in range(4)]
            for h in range(4):
                pTs = []
                for mt in range(2):
                    p = psr.next()
                    S.mm([(p[:], kT[:, h * 2 + db, mt * 128:(mt + 1) * 128], qt_[:, h * 2 + db, :], db == 0, db == 1)
                          for db in range(2)], reads=[kT, qt_], writes=[p])
                    pT = ptr_.next()
                    S.op("act", lambda be: be.activation(out=pT[:], in_=p[:], func=AF.Exp, scale=1.0 / 16.0),
                         reads=[p], writes=[pT])
                    pTs.append(pT)
                for qi in range(4):
                    po = por.next()
                    S.mm([(po[:], pTs[mt][:, qi * 128:(qi + 1) * 128], vaug[:, mt, h, :], mt == 0, mt == 1)
                          for mt in range(2)], reads=pTs + [vaug], writes=[po])
                    r = rr.next()
                    S.op("dve", lambda be: be.reciprocal(r[:, 0:1], po[:, 256:257]), reads=[po], writes=[r])
                    yk = ytoks[qi]
                    S.op("dve", lambda be: be.scalar_tensor_tensor(out=yk[:, h * 256:(h + 1) * 256], in0=po[:, 0:256],
                                                                   scalar=r[:, 0:1], in1=zt[:, qi, h * 256:(h + 1) * 256],
                                                                   op0=ALU.mult, op1=ALU.mult),
                         reads=[po, r, zt], writes=[yk])
            emit_yT(S, ytoks, c.yT[3], 0, 8, tsl, tc, idt, ptr2, ysbr)


def phase_da(S, c, l, L):
    NC, NT = L // 512, L // 128
    lam_init = 0.8 - 0.6 * math.exp(-0.3 * l)
    w = c.w
    with Scope(S) as st:
        idt = sb(S, st, "da_id", [128, 128], BF16)
        S.dma("sp", idt[:], c.ident.ap[:, :], writes=[idt])
        mk = sb(S, st, "da_mk", [128, 128], BF16)
        S.dma("sp", mk[:], c.maskkq.ap[:, :], writes=[mk])
        lt = sb(S, st, "da_lt", [128, 4, 64], F32)
        for i, nm in enumerate(("da_lq1", "da_lk1", "da_lq2", "da_lk2")):
            S.dma("sp", lt[:, i, :], w[nm].ap[l:l + 1, :].partition_broadcast(128), writes=[lt])
        lp = sb(S, st, "da_lp", [128, 2, 64], F32)
        lv = sb(S, st, "da_lv", [128, 8], F32)
        S.op("dve", lambda be: be.tensor_tensor(lp[:, 0, :], lt[:, 0, :], lt[:, 1, :], op=ALU.mult), reads=[lt], writes=[lp])
        S.op("dve", lambda be: be.tensor_tensor(lp[:, 1, :], lt[:, 2, :], lt[:, 3, :], op=ALU.mult), reads=[lt, lp], writes=[lp])
        S.op("dve", lambda be: be.reduce_sum(out=lv[:, 0:2], in_=lp[:], axis=AX.X), reads=[lp], writes=[lv])
        S.op("act", lambda be: be.activation(out=lv[:, 2:4], in_=lv[:, 0:2], func=AF.Exp), reads=[lv], writes=[lv])
        S.op("dve", lambda be: be.tensor_tensor(lv[:, 4:5], lv[:, 3:4], lv[:, 2:3], op=ALU.subtract), reads=[lv], writes=[lv])
        S.op("dve", lambda be: be.tensor_scalar(lv[:, 5:6], lv[:, 4:5], -lam_init, None, op0=ALU.add), reads=[lv], writes=[lv])
        sg = sb(S, st, "da_sg", [128, 128], F32)
        S.dma("sp", sg[:], w["da_subln_g"].ap[l:l + 1, :].partition_broadcast(128), writes=[sg])
        S.op("dve", lambda be: be.tensor_scalar(sg[:], sg[:], 1.0 - lam_init, None, op0=ALU.mult), reads=[sg], writes=[sg])

        kr = sb_ring(S, st, "da_k", [128, L], BF16, 2)
        vr = sb_ring(S, st, "da_v", [128, NT, 129], BF16, 2)
        qr = sb_ring(S, st, "da_q", [128, 512], BF16, 2)
        zr = sb_ring(S, st, "da_z", [128, 4, 128], BF16, 2)
        pTr = sb_ring(S, st, "da_pT", [128, 512], BF16, 4)
        yts = [sb_ring(S, st, f"da_y{i}", [128, 128], BF16, 2) for i in range(4)]
        ar = sb_ring(S, st, "da_a", [128, 128], F32, 2)
        dr = sb_ring(S, st, "da_d", [128, 128], F32, 2)
        jr = sb_ring(S, st, "da_j", [128, 128], F32, 2)
        rr = sb_ring(S, st, "da_r", [128, 8], F32, 4)
        ysbr = sb_ring(S, st, "da_ysb", [128, 1, 512], BF16, 2)
        psr = ps_ring(S, st, "da_ps", [128, 512], F32, 3)
        accs = [ps(S, st, f"da_acc{i}", [128, 3, 129], F32) for i in range(3)]
        ptr2 = ps_ring(S, st, "da_pt", [128, 512], BF16, 1)

        def acc(comp, qt):
            i = comp * 4 + qt
            return accs[i // 3], i % 3
        asbr = sb_ring(S, st, "da_asb", [128, 9, 129], F32, 2)
        pending = []

        for h in range(8):
            kt_ = kr.next()
            S.dma("sp", kt_[:], c.dakT.ap[h * 128:(h + 1) * 128, :], reads=[c.dakT.o((h, tc)) for tc in range(NC)], writes=[kt_])
            vt = vr.next()
            S.dma("sp", vt[:, :, 0:128], c.dav.ap[:, h * 128:(h + 1) * 128].rearrange("(t p) d -> p t d", p=128),
                  reads=[c.dav.o(t) for t in range(NT)], writes=[vt])
            S.op("pool", lambda be: be.memset(vt[:, :, 128:129], 1.0), writes=[vt])
            for tc in range(NC):
                tsl = slice(tc * 512, (tc + 1) * 512)
                qt_ = qr.next()
                S.dma("sp", qt_[:], c.daqT.ap[h * 128:(h + 1) * 128, tsl], reads=[c.daqT.o((h, tc))], writes=[qt_])
                zt = zr.next()
                S.dma("sp", zt[:], c.daz.ap[tsl, h * 128:(h + 1) * 128].rearrange("(t p) d -> p t d", p=128),
                      reads=[c.daz.o(tc * 4 + i) for i in range(4)], writes=[zt])
                nk = 4 * tc + 4
                for a_t in accs:
                    S.op("dve", lambda be: be.memset(a_t[:], 0.0), writes=[a_t])
                steps = [(kt, comp) for kt in range(nk) for comp in range(2)]

                def emit_st(i):
                    kt, comp = steps[i]
                    dq = kt - 4 * tc
                    q0 = max(dq, 0) * 128
                    csl = slice(comp * 64, (comp + 1) * 64)
                    p = psr.next()
                    S.mm([(p[:, q0:512], kt_[csl, kt * 128:(kt + 1) * 128], qt_[csl, q0:512], True, True)],
                         reads=[kt_, qt_], writes=[p])
                    pT = pTr.next()
                    S.op("act", lambda be: be.activation(out=pT[:, q0:512], in_=p[:, q0:512], func=AF.Exp, scale=0.125),
                         reads=[p], writes=[pT])
                    if dq >= 0:
                        S.op("dve", lambda be: be.tensor_tensor(pT[:, q0:q0 + 128], pT[:, q0:q0 + 128], mk[:], op=ALU.mult),
                             reads=[pT, mk], writes=[pT])
                    return pT

                LOOK = 2
                pts = {}
                for i in range(min(LOOK, len(steps))):
                    pts[i] = emit_st(i)
                for i in range(len(steps)):
                    if i == min(3, len(steps) - 1):
                        while pending:
                            pending.pop(0)()
                    if i + LOOK < len(steps):
                        pts[i + LOOK] = emit_st(i + LOOK)
                    kt, comp = steps[i]
                    dq = kt - 4 * tc
                    pT = pts.pop(i)
                    for qi in range(max(dq, 0), 4):
                        a_t, a_i = acc(comp, qi)
                        S.mm([(a_t[:, a_i, :], pT[:, qi * 128:(qi + 1) * 128], vt[:, kt, :], False, False)],
                             reads=[pT, vt], writes=[a_t], skip=True)
                asb = asbr.next()
                for i_t, a_t in enumerate(accs):
                    if i_t % 2:
                        S.op("act", lambda be: be.copy(out=asb[:, i_t * 3:(i_t + 1) * 3, :], in_=a_t[:]), reads=[a_t], writes=[asb])
                    else:
                        S.op("dve", lambda be: be.tensor_copy(asb[:, i_t * 3:(i_t + 1) * 3, :], a_t[:]), reads=[a_t], writes=[asb])

                def epilogue(asb=asb, zt=zt, h=h, tc=tc, tsl=tsl):
                    ytoks = []
                    for qi in range(4):
                        i0, i1 = qi, 4 + qi
                        r = rr.next()
                        S.op("dve", lambda be: be.reciprocal(r[:, 0:1], asb[:, i0, 128:129]), reads=[asb], writes=[r])
                        S.op("dve", lambda be: be.reciprocal(r[:, 1:2], asb[:, i1, 128:129]), reads=[asb, r], writes=[r])
                        S.op("dve", lambda be: be.tensor_tensor(r[:, 2:3], r[:, 1:2], lv[:, 5:6], op=ALU.mult), reads=[r, lv], writes=[r])
                        a = ar.next()
                        S.op("act", lambda be: be.activation(out=a[:], in_=asb[:, i0, 0:128], func=AF.Copy, scale=r[:, 0:1]),
                             reads=[asb, r], writes=[a])
                        d = dr.next()
                        S.op("dve", lambda be: be.scalar_tensor_tensor(out=d[:], in0=asb[:, i1, 0:128], scalar=r[:, 2:3], in1=a[:],
                                                                       op0=ALU.mult, op1=ALU.add), reads=[asb, r, a], writes=[d])
                        jk = jr.next()
                        S.op("act", lambda be: be.activation(out=jk[:], in_=d[:], func=AF.Square, accum_out=r[:, 3:4]),
                             reads=[d, r], writes=[jk, r])
                        S.op("dve", lambda be: be.tensor_scalar(r[:, 4:5], r[:, 3:4], 1.0 / 128.0, EPS, op0=ALU.mult, op1=ALU.add),
                             reads=[r], writes=[r])
                        S.op("act", lambda be: be.activation(out=r[:, 5:6], in_=r[:, 4:5], func=AF.Sqrt), reads=[r], writes=[r])
                        S.op("dve", lambda be: be.reciprocal(r[:, 6:7], r[:, 5:6]), reads=[r], writes=[r])
                        S.op("dve", lambda be: be.scalar_tensor_tensor(out=d[:], in0=d[:], scalar=r[:, 6:7], in1=sg[:],
                                                                       op0=ALU.mult, op1=ALU.mult), reads=[d, r, sg], writes=[d])
                        yk = yts[qi].next()
                        S.op("dve", lambda be: be.tensor_tensor(yk[:], d[:], zt[:, qi, :], op=ALU.mult), reads=[d, zt], writes=[yk])
                        ytoks.append(yk)
                    emit_yT(S, ytoks, c.yT[2], h * 128, 1, tsl, tc, idt, ptr2, ysbr)
                pending.append(epilogue)
        while pending:
            pending.pop(0)()


def phase_ml(S, c, l, L):
    NCH = L // 128
    w = c.w
    mlg = c.mlg
    with Scope(S) as st:
        Bcol = sb(S, st, "ml_Bcol", [128, NCH, 4], F32)
        Ecol = sb(S, st, "ml_Ecol", [128, NCH, 4], F32)
        mu = sb(S, st, "ml_mu", [128, 4, NCH + 1], F32)
        negmu = sb(S, st, "ml_nmu", [128, 4, NCH + 1], F32)
        dec = sb(S, st, "ml_dec", [128, 4, NCH], F32)
        with Scope(S) as g:
            ig = sb(S, g, "mlg_i", [4, L], F32)
            fg = sb(S, g, "mlg_f", [4, L], F32)
            Ft = sb(S, g, "mlg_F", [4, L], F32)
            Gt = sb(S, g, "mlg_G", [4, L], F32)
            ones = sb(S, g, "mlg_1", [4, L], F32)
            bb = sb(S, g, "mlg_b", [4, 4], F32)
            S.dma("sp", ig[:], c.mlif.ap[0:4, :], reads=[c.mlif.o(tc) for tc in range(L // 512)], writes=[ig])
            S.dma("sp", fg[:], c.mlif.ap[4:8, :], reads=[c.mlif.o(tc) for tc in range(L // 512)], writes=[fg])
            S.dma("sp", bb[:, 0:1], w["ml_b_i"].ap[l].rearrange("(h o) -> h o", o=1), writes=[bb], slow=True)
            S.dma("sp", bb[:, 1:2], w["ml_b_f"].ap[l].rearrange("(h o) -> h o", o=1), writes=[bb], slow=True)
            S.op("dve", lambda be: be.tensor_scalar(bb[:, 2:3], bb[:, 1:2], -1.0, None, op0=ALU.mult), reads=[bb], writes=[bb])
            S.op("pool", lambda be: be.memset(ones[:], 1.0), writes=[ones])
            S.op("pool", lambda be: be.memset(bb[:, 3:4], 0.0), reads=[bb], writes=[bb])
            S.op("act", lambda be: be.activation(out=fg[:], in_=fg[:], func=AF.Exp, scale=-1.0, bias=bb[:, 2:3]),
                 reads=[fg, bb], writes=[fg])
            S.op("act", lambda be: be.activation(out=fg[:], in_=fg[:], func=AF.Ln, bias=1.0), reads=[fg], writes=[fg])
            S.op("dve", lambda be: be.tensor_scalar(fg[:], fg[:], -1.0, None, op0=ALU.mult), reads=[fg], writes=[fg])
            S.op("dve", lambda be: be.tensor_tensor_scan(out=Ft[:], data0=ones[:], data1=fg[:], initial=0.0,
                                                         op0=ALU.mult, op1=ALU.add), reads=[ones, fg], writes=[Ft])
            S.op("dve", lambda be: be.scalar_tensor_tensor(out=ig[:], in0=ig[:], scalar=bb[:, 0:1], in1=Ft[:],
                                                           op0=ALU.add, op1=ALU.subtract), reads=[ig, bb, Ft], writes=[ig])
            S.op("dve", lambda be: be.tensor_tensor_scan(out=Gt[:], data0=ones[:], data1=ig[:], initial=0.0,
                                                         op0=ALU.mult, op1=ALU.max), reads=[ones, ig], writes=[Gt])
            S.op("dve", lambda be: be.tensor_tensor(fg[:], Ft[:], Gt[:], op=ALU.add), reads=[Ft, Gt, fg], writes=[fg])
            S.op("act", lambda be: be.activation(out=fg[:], in_=fg[:], func=AF.Exp, scale=-1.0), reads=[fg], writes=[fg])
            S.dma("sp", mlg.ap[0, :, 0:L], ig[:], reads=[ig], writes=[mlg.o(0)])
            S.dma("sp", mlg.ap[1, :, 0:L], fg[:], reads=[fg], writes=[mlg.o(1)])
            S.dma("sp", mlg.ap[2, :, 1:L + 1], Gt[:], reads=[Gt], writes=[mlg.o(2)])
            S.dma("sp", mlg.ap[2, :, 0:1], bb[:, 3:4], reads=[bb], writes=[mlg.o(3)], slow=True)
            for h in range(4):
                S.dma("sp", Bcol[:, :, h], mlg.ap[0, h, 0:L].rearrange("(c t) -> t c", t=128), reads=[mlg.o(0)], writes=[Bcol], slow=True)
                S.dma("sp", Ecol[:, :, h], mlg.ap[1, h, 0:L].rearrange("(c t) -> t c", t=128), reads=[mlg.o(1)], writes=[Ecol], slow=True)
                S.dma("sp", mu[:, h, :], mlg.ap[2, h:h + 1, 0:L + 1:128].partition_broadcast(128),
                      reads=[mlg.o(2), mlg.o(3)], writes=[mu], slow=True)
        S.op("dve", lambda be: be.tensor_scalar(negmu[:], mu[:], -1.0, None, op0=ALU.mult), reads=[mu], writes=[negmu])
        S.op("dve", lambda be: be.tensor_tensor(dec[:], mu[:, :, 0:NCH], mu[:, :, 1:NCH + 1], op=ALU.subtract), reads=[mu], writes=[dec])
        S.op("act", lambda be: be.activation(out=dec[:], in_=dec[:], func=AF.Exp), reads=[dec], writes=[dec])

        idt = sb(S, st, "ml_id", [128, 128], BF16)
        S.dma("sp", idt[:], c.ident.ap[:, :], writes=[idt])
        mk = sb(S, st, "ml_mk", [128, 128], BF16)
        S.dma("sp", mk[:], c.maskkq.ap[:, :], writes=[mk])
        ng = sb(S, st, "ml_ng", [128, DB], F32)
        S.dma("sp", ng[:], w["ml_norm_g"].ap[l:l + 1, :].partition_broadcast(128), writes=[ng])
        qT = sb(S, st, "ml_qT", [128, 2, L], BF16)
        kT = sb(S, st, "ml_kT", [128, 2, L], BF16)
        va = sb(S, st, "ml_va", [128, NCH, 257], BF16)
        ot = sb(S, st, "ml_o", [128, NCH, 256], BF16)
        zt = sb(S, st, "ml_z", [128, NCH, 256], BF16)
        C32 = sb(S, st, "ml_C32", [128, 2, 257], F32)
        Cbf = sb(S, st, "ml_Cbf", [128, 2, 257], BF16)
        gbr = sb_ring(S, st, "ml_gb", [128, 128], F32, 3)
        ptr_ = sb_ring(S, st, "ml_pt", [128, 128], F32, 2)
        ptmr = sb_ring(S, st, "ml_ptm", [128, 128], F32, 2)
        str_ = sb_ring(S, st, "ml_st", [128, 128], BF16, 2)
        scr = sb_ring(S, st, "ml_sc", [128, 128], F32, 2)
        qsr = sb_ring(S, st, "ml_qs", [128, 2, 128], BF16, 2)
        rr = sb_ring(S, st, "ml_r", [128, 8], F32, 3)
        hr = sb_ring(S, st, "ml_h", [128, 256], F32, 2)
        jr = sb_ring(S, st, "ml_j", [128, 256], F32, 1)
        yr = sb_ring(S, st, "ml_y", [128, 256], BF16, 2)
        ysr = sb_ring(S, st, "ml_ys", [128, 2, 128], BF16, 2)
        kwr = sb_ring(S, st, "ml_kw", [128, 2], F32, 2)
        kkr = sb_ring(S, st, "ml_kk", [128, 256], BF16, 2)
        psS = ps_ring(S, st, "ml_pS", [128, 128], F32, 2)
        psN = ps_ring(S, st, "ml_pN", [128, 257], F32, 2)
        psT = ps_ring(S, st, "ml_pT", [128, 2, 128], BF16, 2)
        psC = [ps(S, st, f"ml_pC{i}", [128, 257], F32) for i in range(2)]
        for h in range(4):
            hs = slice(h * 256, (h + 1) * 256)
            S.dma("sp", qT[:], c.mlqT.ap[hs, :].rearrange("(k p) n -> p k n", p=128),
                  reads=[c.mlqT.o((2 * h + k, tc)) for k in range(2) for tc in range(L // 512)], writes=[qT])
            S.dma("sp", kT[:], c.mlkT.ap[hs, :].rearrange("(k p) n -> p k n", p=128),
                  reads=[c.mlkT.o((2 * h + k, tc)) for k in range(2) for tc in range(L // 512)], writes=[kT])
            S.dma("sp", va[:, :, 0:256], c.mlv.ap[:, hs].rearrange("(c p) d -> p c d", p=128),
                  reads=[c.mlv.o(t) for t in range(NCH)], writes=[va])
            S.op("pool", lambda be: be.memset(va[:, :, 256:257], 1.0), writes=[va])
            S.dma("sp", ot[:], c.mlo.ap[:, hs].rearrange("(c p) d -> p c d", p=128), reads=[c.mlo.o(t) for t in range(NCH)], writes=[ot])
            S.dma("sp", zt[:], c.mlz.ap[:, hs].rearrange("(c p) d -> p c d", p=128), reads=[c.mlz.o(t) for t in range(NCH)], writes=[zt])
            for ch in range(NCH):
                csl = slice(ch * 128, (ch + 1) * 128)
                gb = gbr.next()
                S.dma("sp", gb[:], mlg.ap[2, h:h + 1, 1 + ch * 128:1 + (ch + 1) * 128].partition_broadcast(128),
                      reads=[mlg.o(2)], writes=[gb])
                pS = psS.next()
                S.mm([(pS[:], kT[:, db, csl], qT[:, db, csl], db == 0, db == 1) for db in range(2)], reads=[kT, qT], writes=[pS])
                pt = ptr_.next()
                S.op("act", lambda be: be.activation(out=pt[:], in_=gb[:], func=AF.Exp, scale=-1.0, bias=Bcol[:, ch, h:h + 1]),
                     reads=[gb, Bcol], writes=[pt])
                ptm = ptmr.next()
                S.op("dve", lambda be: be.tensor_tensor(ptm[:], pt[:], mk[:], op=ALU.mult), reads=[pt, mk], writes=[ptm])
                stt = str_.next()
                S.op("dve", lambda be: be.tensor_tensor(stt[:], pS[:], ptm[:], op=ALU.mult), reads=[pS, ptm], writes=[stt])
                items = [(None, stt[:], va[:, ch, :])]
                rds = [stt, va]
                if ch > 0:
                    sc = scr.next()
                    S.op("act", lambda be: be.activation(out=sc[:], in_=gb[:], func=AF.Exp, scale=-1.0, bias=mu[:, h, ch:ch + 1]),
                         reads=[gb, mu], writes=[sc])
                    qs = qsr.next()
                    for db in range(2):
                        S.op("dve", lambda be: be.tensor_tensor(qs[:, db, :], qT[:, db, csl], sc[:], op=ALU.mult),
                             reads=[qT, sc], writes=[qs])
                    items += [(None, qs[:, 0, :], Cbf[:, 0, :]), (None, qs[:, 1, :], Cbf[:, 1, :])]
                    rds += [qs, Cbf]
                pN = psN.next()
                n = len(items)
                S.mm([(pN[:], a, b, i == 0, i == n - 1) for i, (_, a, b) in enumerate(items)], reads=rds, writes=[pN])
                r = rr.next()
                S.op("act", lambda be: be.activation(out=r[:, 6:7], in_=pN[:, 256:257], func=AF.Abs), reads=[pN], writes=[r])
                S.op("dve", lambda be: be.tensor_tensor(r[:, 0:1], r[:, 6:7], Ecol[:, ch, h:h + 1], op=ALU.max),
                     reads=[r, Ecol], writes=[r])
                S.op("dve", lambda be: be.reciprocal(r[:, 1:2], r[:, 0:1]), reads=[r], writes=[r])
                hh = hr.next()
                S.op("act", lambda be: be.activation(out=hh[:], in_=pN[:, 0:256], func=AF.Copy, scale=r[:, 1:2]),
                     reads=[pN, r], writes=[hh])
                jk = jr.next()
                S.op("act", lambda be: be.activation(out=jk[:], in_=hh[:], func=AF.Square, accum_out=r[:, 2:3]),
                     reads=[hh, r], writes=[jk, r])
                S.op("dve", lambda be: be.tensor_scalar(r[:, 3:4], r[:, 2:3], 1.0 / 256.0, EPS, op0=ALU.mult, op1=ALU.add),
                     reads=[r], writes=[r])
                S.op("act", lambda be: be.activation(out=r[:, 4:5], in_=r[:, 3:4], func=AF.Sqrt), reads=[r], writes=[r])
                S.op("dve", lambda be: be.reciprocal(r[:, 5:6], r[:, 4:5]), reads=[r], writes=[r])
                S.op("dve", lambda be: be.scalar_tensor_tensor(out=hh[:], in0=hh[:], scalar=r[:, 5:6], in1=ng[:, hs],
                                                               op0=ALU.mult, op1=ALU.mult), reads=[hh, r, ng], writes=[hh])
                S.op("dve", lambda be: be.tensor_tensor(hh[:], hh[:], ot[:, ch, :], op=ALU.mult), reads=[hh, ot], writes=[hh])
                yk = yr.next()
                S.op("dve", lambda be: be.tensor_tensor(yk[:], hh[:], zt[:, ch, :], op=ALU.mult), reads=[hh, zt], writes=[yk])
                pT = psT.next()
                for db in range(2):
                    S.transpose(pT[:, db, :], yk[:, db * 128:(db + 1) * 128], idt[:], reads=[yk, idt], writes=[pT])
                ys = ysr.next()
                S.op("act", lambda be: be.copy(out=ys[:], in_=pT[:]), reads=[pT], writes=[ys])
                S.dma("sp", c.yT[1].ap[hs, csl].rearrange("(k p) n -> p k n", p=128), ys[:], reads=[ys],
                      writes=[c.yT[1].o((2 * h + k, ch // 4)) for k in range(2)])
                if ch == NCH - 1:
                    continue
                kw = kwr.next()
                S.op("act", lambda be: be.activation(out=kw[:, 0:1], in_=Bcol[:, ch, h:h + 1], func=AF.Exp,
                                                     bias=negmu[:, h, ch + 1:ch + 2]), reads=[Bcol, negmu], writes=[kw])
                pK = psT.next()
                for db in range(2):
                    S.transpose(pK[:, db, :], kT[:, db, csl], idt[:], reads=[kT, idt], writes=[pK])
                kk = kkr.next()
                S.op("dve", lambda be: be.tensor_scalar(kk[:], pK[:].rearrange("p a b -> p (a b)"), kw[:, 0:1], None, op0=ALU.mult),
                     reads=[pK, kw], writes=[kk])
                for db in range(2):
                    S.mm([(psC[db][:], kk[:, db * 128:(db + 1) * 128], va[:, ch, :], True, True)], reads=[kk, va], writes=[psC[db]])
                    if ch == 0:
                        S.op("dve", lambda be: be.tensor_copy(C32[:, db, :], psC[db][:]), reads=[psC[db]], writes=[C32])
                    else:
                        S.op("dve", lambda be: be.scalar_tensor_tensor(out=C32[:, db, :], in0=C32[:, db, :], scalar=dec[:, h, ch:ch + 1],
                                                                       in1=psC[db][:], op0=ALU.mult, op1=ALU.add),
                             reads=[C32, dec, psC[db]], writes=[C32])
                S.op("act", lambda be: be.copy(out=Cbf[:], in_=C32[:]), reads=[C32], writes=[Cbf])


TWO_PI = 2.0 * math.pi


def _sincos(S, out_t, ang_src, th, off, kt, scr=None):
    y = out_t if scr is None else scr
    if isinstance(th, float):
        S.op("dve", lambda be: be.tensor_scalar(y[0], ang_src[0], th, off, op0=ALU.mult, op1=ALU.add),
             reads=ang_src[1], writes=[y[1]])
    else:
        S.op("act", lambda be: be.activation(out=y[0], in_=ang_src[0], func=AF.Identity, scale=th, bias=off),
             reads=ang_src[1], writes=[y[1]])
    S.op("dve", lambda be: be.tensor_copy(kt[0], y[0]), reads=[y[1]], writes=[kt[1]])
    S.op("dve", lambda be: be.tensor_tensor(y[0], y[0], kt[0], op=ALU.subtract), reads=[y[1], kt[1]], writes=[y[1]])
    S.op("act", lambda be: be.activation(out=out_t[0], in_=y[0], func=AF.Sin, scale=TWO_PI * (1.0 - 1e-6)),
         reads=[y[1]], writes=[out_t[1]])


def phase_s5(S, c, l, L):
    w = c.w
    SEG = min(L, 1024)
    NSEG = L // SEG
    NCS = SEG // 512
    OFF_S = 0.0
    OFF_C = 0.25
    with Scope(S) as st:
        BBpad = sb(S, st, "s5_BB", [128, NG, 128], BF16)
        BBsw = sb(S, st, "s5_BBs", [128, NG, 128], BF16)
        CCpad = sb(S, st, "s5_CC", [128, NG, 128], BF16)
        r2 = sb(S, st, "s5_r2", [128, NG], F32)
        th = sb(S, st, "s5_th", [128, NG], F32)
        offs = sb(S, st, "s5_offs", [128, 2], F32)
        dsk = sb(S, st, "s5_dsk", [128, 8], F32)
        bgl = sb(S, st, "s5_bg", [128, 8], F32)
        S.dma("sp", dsk[:], w["s5_d"].ap[l].rearrange("(b p) -> p b", p=128), writes=[dsk], slow=True)
        S.dma("sp", bgl[:], w["s5_b_glu"].ap[l].rearrange("(b p) -> p b", p=128), writes=[bgl], slow=True)
        S.op("pool", lambda be: be.memset(offs[0:64, 0:1], OFF_S), writes=[offs])
        S.op("pool", lambda be: be.memset(offs[64:128, 0:1], OFF_S + 0.5), reads=[offs], writes=[offs])
        S.op("pool", lambda be: be.memset(offs[:, 1:2], OFF_C), reads=[offs], writes=[offs])
        with Scope(S) as pp:
            def t3(name):
                return sb(S, pp, name, [128, 8, 64], F32)
            lre, lim, dt, er, cs, sn, wr, wi, t1, t2, Br, Bi, Bbr, Bbi = [t3(f"s5p{i}") for i in range(14)]
            mg = sb(S, pp, "s5_mg", [128, 8], F32)
            dt8 = sb(S, pp, "s5_dt8", [128, 8], F32)
            m2 = sb(S, pp, "s5_m2", [128, 8, 128], F32)
            S.dma("sp", mg[:], c.maskg.ap[:, :], writes=[mg])
            S.dma("sp", m2[:], c.mask2.ap[:, :, :], writes=[m2])
            hre, him, hdt = w["s5_lam_re"].h, w["s5_lam_im"].h, w["s5_log_dt"].h
            for g8 in range(8):
                ps_ = slice(g8 * 16, (g8 + 1) * 16)
                S.dma("sp", lre[ps_, :, :], bass.AP(tensor=hre, offset=l * 4096 + g8 * 64, ap=[[0, 16], [512, 8], [1, 64]]), writes=[lre], slow=True)
                S.dma("sp", lim[ps_, :, :], bass.AP(tensor=him, offset=l * 4096 + g8 * 64, ap=[[0, 16], [512, 8], [1, 64]]), writes=[lim], slow=True)
                S.dma("sp", dt8[ps_, :], bass.AP(tensor=hdt, offset=l * 64 + g8, ap=[[0, 16], [8, 8]]), writes=[dt8], slow=True)
                for blk in range(8):
                    S.dma("sp", Br[ps_, blk, :], w["s5_b_re"].ap[l, blk * 8 + g8].rearrange("p c -> c p"), writes=[Br], slow=True)
                    S.dma("sp", Bi[ps_, blk, :], w["s5_b_im"].ap[l, blk * 8 + g8].rearrange("p c -> c p"), writes=[Bi], slow=True)

            def V(e, fn, rd, wr_):
                S.op(e, fn, reads=rd, writes=wr_)
            V("dve", lambda be: be.tensor_scalar(lre[:], lre[:], -1e-4, None, op0=ALU.min), [lre], [lre])
            V("act", lambda be: be.activation(out=dt8[:], in_=dt8[:], func=AF.Exp), [dt8], [dt8])
            V("dve", lambda be: be.tensor_copy(dt[:], dt8[:].unsqueeze(2).to_broadcast([128, 8, 64])), [dt8], [dt])
            V("dve", lambda be: be.tensor_tensor(t1[:], lre[:], dt[:], op=ALU.mult), [lre, dt], [t1])
            V("act", lambda be: be.activation(out=er[:], in_=t1[:], func=AF.Exp), [t1], [er])
            V("dve", lambda be: be.tensor_tensor(t2[:], lim[:], dt[:], op=ALU.mult), [lim, dt], [t2])
            kA = sb(S, pp, "s5_kA", [128, 8, 64], mybir.dt.int32)
            _sincos(S, (cs[:], cs), (t2[:], [t2]), 1.0 / TWO_PI, OFF_C, (kA[:], kA))
            _sincos(S, (sn[:], sn), (t2[:], [t2]), 1.0 / TWO_PI, OFF_S, (kA[:], kA))
            V("dve", lambda be: be.tensor_tensor(cs[:], cs[:], er[:], op=ALU.mult), [cs, er], [cs])
            V("dve", lambda be: be.tensor_tensor(sn[:], sn[:], er[:], op=ALU.mult), [sn, er], [sn])
            V("dve", lambda be: be.tensor_scalar(cs[:], cs[:], -1.0, None, op0=ALU.add), [cs], [cs])
            V("dve", lambda be: be.tensor_tensor(t1[:], lre[:], lre[:], op=ALU.mult), [lre], [t1])
            V("dve", lambda be: be.tensor_tensor(t2[:], lim[:], lim[:], op=ALU.mult), [lim], [t2])
            V("dve", lambda be: be.tensor_tensor(t1[:], t1[:], t2[:], op=ALU.add), [t1, t2], [t1])
            V("dve", lambda be: be.reciprocal(t1[:], t1[:]), [t1], [t1])
            V("dve", lambda be: be.tensor_tensor(wr[:], cs[:], lre[:], op=ALU.mult), [cs, lre], [wr])
            V("dve", lambda be: be.tensor_tensor(t2[:], sn[:], lim[:], op=ALU.mult), [sn, lim], [t2])
            V("dve", lambda be: be.tensor_tensor(wr[:], wr[:], t2[:], op=ALU.add), [wr, t2], [wr])
            V("dve", lambda be: be.tensor_tensor(wr[:], wr[:], t1[:], op=ALU.mult), [wr, t1], [wr])
            V("dve", lambda be: be.tensor_tensor(wi[:], sn[:], lre[:], op=ALU.mult), [sn, lre], [wi])
            V("dve", lambda be: be.tensor_tensor(t2[:], cs[:], lim[:], op=ALU.mult), [cs, lim], [t2])
            V("dve", lambda be: be.tensor_tensor(wi[:], wi[:], t2[:], op=ALU.subtract), [wi, t2], [wi])
            V("dve", lambda be: be.tensor_tensor(wi[:], wi[:], t1[:], op=ALU.mult), [wi, t1], [wi])
            V("dve", lambda be: be.tensor_tensor(Bbr[:], wr[:], Br[:], op=ALU.mult), [wr, Br], [Bbr])
            V("dve", lambda be: be.tensor_tensor(t2[:], wi[:], Bi[:], op=ALU.mult), [wi, Bi], [t2])
            V("dve", lambda be: be.tensor_tensor(Bbr[:], Bbr[:], t2[:], op=ALU.subtract), [Bbr, t2], [Bbr])
            V("dve", lambda be: be.tensor_tensor(Bbi[:], wr[:], Bi[:], op=ALU.mult), [wr, Bi], [Bbi])
            V("dve", lambda be: be.tensor_tensor(t2[:], wi[:], Br[:], op=ALU.mult), [wi, Br], [t2])
            V("dve", lambda be: be.tensor_tensor(Bbi[:], Bbi[:], t2[:], op=ALU.add), [Bbi, t2], [Bbi])
            mgb = mg[:].unsqueeze(1).unsqueeze(3).to_broadcast([128, 8, 8, 64])
            for dst, lo, hi in ((BBpad, Bbr, Bbi), (BBsw, Bbi, Bbr)):
                for half, src in ((0, lo), (1, hi)):
                    dv = dst[:, :, half * 64:(half + 1) * 64].rearrange("p (blk g) q -> p blk g q", g=8)
                    sv = src[:].unsqueeze(2).to_broadcast([128, 8, 8, 64])
                    V("dve", lambda be: be.tensor_tensor(dv, sv, mgb, op=ALU.mult), [src, mg], [dst])
            lb = sb(S, pp, "s5_lb", [128, NG], F32)
            S.dma("sp", lb[0:64, :], w["s5_lam_re"].ap[l].rearrange("g p -> p g"), writes=[lb], slow=True)
            S.dma("sp", lb[64:128, :], w["s5_lam_re"].ap[l].rearrange("g p -> p g"), writes=[lb], slow=True)
            S.dma("sp", th[0:64, :], w["s5_lam_im"].ap[l].rearrange("g p -> p g"), writes=[th], slow=True)
            S.dma("sp", th[64:128, :], w["s5_lam_im"].ap[l].rearrange("g p -> p g"), writes=[th], slow=True)
            dtb = sb(S, pp, "s5_dtb", [128, NG], F32)
            S.dma("sp", dtb[:], w["s5_log_dt"].ap[l:l + 1, :].partition_broadcast(128), writes=[dtb])
            V("act", lambda be: be.activation(out=dtb[:], in_=dtb[:], func=AF.Exp), [dtb], [dtb])
            V("dve", lambda be: be.tensor_scalar(lb[:], lb[:], -1e-4, None, op0=ALU.min), [lb], [lb])
            V("dve", lambda be: be.tensor_tensor(lb[:], lb[:], dtb[:], op=ALU.mult), [lb, dtb], [lb])
            V("act", lambda be: be.activation(out=r2[:], in_=lb[:], func=AF.Exp), [lb], [r2])
            V("dve", lambda be: be.tensor_tensor(th[:], th[:], dtb[:], op=ALU.mult), [th, dtb], [th])
            kB = sb(S, pp, "s5_kB", [128, NG], mybir.dt.int32)
            V("dve", lambda be: be.tensor_scalar(th[:], th[:], 1.0 / TWO_PI, None, op0=ALU.mult), [th], [th])
            V("dve", lambda be: be.tensor_copy(kB[:], th[:]), [th], [kB])
            V("dve", lambda be: be.tensor_tensor(th[:], th[:], kB[:], op=ALU.subtract), [th, kB], [th])
            Cc = sb(S, pp, "s5_Cc", [128, 8, 128], F32)
            S.dma("sp", Cc[:, :, 0:64], w["s5_c_re"].ap[l].rearrange("(blk g8) co p -> (g8 co) blk p", g8=8), writes=[Cc])
            S.dma("sp", Cc[:, :, 64:128], w["s5_c_im"].ap[l].rearrange("(blk g8) co p -> (g8 co) blk p", g8=8), writes=[Cc])
            V("dve", lambda be: be.tensor_scalar(Cc[:, :, 64:128], Cc[:, :, 64:128], -1.0, None, op0=ALU.mult), [Cc], [Cc])
            idf = sb(S, pp, "s5_idf", [128, 128], F32)
            S.dma("sp", idf[:], c.identf.ap[:, :], writes=[idf])
            pcr = ps_ring(S, pp, "s5_pc", [128, 128], F32, 2)
            for blk in range(8):
                pc = pcr.next()
                S.transpose(pc[:], Cc[:, blk, :], idf[:], reads=[Cc, idf], writes=[pc])
                V("dve", lambda be: be.tensor_tensor(CCpad[:, blk * 8:(blk + 1) * 8, :], pc[:].unsqueeze(1).to_broadcast([128, 8, 128]),
                                                     m2[:], op=ALU.mult), [pc, m2], [CCpad])

        with Scope(S) as ms:
            trs = []
            for sg in range(NSEG):
                tr_ = sb(S, ms, f"s5_tr{sg}", [128, SEG], F32)
                S.dma("sp", tr_[:], c.trow.ap[0:1, sg * SEG:(sg + 1) * SEG].partition_broadcast(128), writes=[tr_])
                trs.append(tr_)

            def rg(name, n, dt_=F32):
                return sb_ring(S, ms, name, [128, SEG], dt_, n)
            COSr, SINr = rg("s5mC", 4, BF16), rg("s5mS", 4, BF16)
            Vr, VSr = rg("s5mV", 3, BF16), rg("s5mVS", 3, BF16)
            T2r, T3r = rg("s5mT2", 2, BF16), rg("s5mT3", 2, BF16)
            BUr, BSr = rg("s5mBU", 2, BF16), rg("s5mBS", 2, BF16)
            yfr = rg("s5_yf", 2)
            Sgr = rg("s5_sg", 2, BF16)
            kir = rg("s5_ki", 2, mybir.dt.int32)
            ur = sb_ring(S, ms, "s5_u", [128, L], BF16, 2)
            car = sb(S, ms, "s5_car", [128, 8, 2], F32)
            yvr = sb_ring(S, ms, "s5_yv", [128, 512], F32, 2)
            tgr = sb_ring(S, ms, "s5_tg", [128, 512], F32, 2)
            ygr = sb_ring(S, ms, "s5_yg", [128, 512], BF16, 2)
            pbu = ps_ring(S, ms, "s5_pb", [128, 512], F32, 2)
            pbs = ps_ring(S, ms, "s5_pbs", [128, 512], F32, 2)
            pyr = ps_ring(S, ms, "s5_py", [128, 512], F32, 4)
            items = [(blk, sg, g8) for blk in range(8) for sg in range(NSEG) for g8 in range(8)]
            uts, pysd, stt_ = {}, {}, {}

            def get_ut(blk):
                if blk not in uts:
                    ut = ur.next()
                    S.dma("sp", ut[:], c.s5uT.ap[blk * 128:(blk + 1) * 128, :],
                          reads=[c.s5uT.o((blk, tc)) for tc in range(L // 512)], writes=[ut])
                    uts[blk] = ut
                return uts[blk]

            def stageA(i):
                blk, sg, g8 = items[i]
                g = blk * 8 + g8
                COS, SIN = COSr.next(), SINr.next()
                kt_, yf = kir.next(), yfr.next()
                _sincos(S, (COS[:], COS), (trs[sg][:], [trs[sg], th, offs]), th[:, g:g + 1], offs[:, 1:2], (kt_[:], kt_), (yf[:], yf))
                kt_, yf = kir.next(), yfr.next()
                _sincos(S, (SIN[:], SIN), (trs[sg][:], [trs[sg], th, offs]), th[:, g:g + 1], offs[:, 0:1], (kt_[:], kt_), (yf[:], yf))
                stt_[i] = dict(COS=COS, SIN=SIN)

            def stageB1(i):
                blk, sg, g8 = items[i]
                g = blk * 8 + g8
                ut = get_ut(blk)
                d = stt_[i]
                COS, SIN = d["COS"], d["SIN"]
                Vt, VS, T2, T3 = Vr.next(), VSr.next(), T2r.next(), T3r.next()
                for cs_ in range(NCS):
                    fsl = slice(cs_ * 512, (cs_ + 1) * 512)
                    tsl = slice(sg * SEG + cs_ * 512, sg * SEG + (cs_ + 1) * 512)
                    p1, p2 = pbu.next(), pbs.next()
                    S.mm([(p1[:], BBpad[:, g, :], ut[:, tsl], True, True)], reads=[BBpad, ut], writes=[p1])
                    S.mm([(p2[:], BBsw[:, g, :], ut[:, tsl], True, True)], reads=[BBsw, ut], writes=[p2])
                    S.op("dve", lambda be: be.tensor_tensor(Vt[:, fsl], COS[:, fsl], p1[:], op=ALU.mult), reads=[COS, p1], writes=[Vt])
                    S.op("dve", lambda be: be.tensor_tensor(T2[:, fsl], SIN[:, fsl], p2[:], op=ALU.mult), reads=[SIN, p2], writes=[T2])
                    S.op("dve", lambda be: be.tensor_tensor(VS[:, fsl], COS[:, fsl], p2[:], op=ALU.mult), reads=[COS, p2], writes=[VS])
                    S.op("dve", lambda be: be.tensor_tensor(T3[:, fsl], SIN[:, fsl], p1[:], op=ALU.mult), reads=[SIN, p1], writes=[T3])
                S.op("dve", lambda be: be.tensor_tensor(Vt[:], Vt[:], T2[:], op=ALU.add), reads=[Vt, T2], writes=[Vt])
                S.op("dve", lambda be: be.tensor_tensor(VS[:], VS[:], T3[:], op=ALU.subtract), reads=[VS, T3], writes=[VS])
                d.update(Vt=Vt, VS=VS)

            def stageB2(i):
                blk, sg, g8 = items[i]
                g = blk * 8 + g8
                d = stt_[i]
                Vt, VS = d["Vt"], d["VS"]
                BU, BS = BUr.next(), BSr.next()
                dec_ = r2[:, g:g + 1].to_broadcast([128, SEG])
                i0 = 0.0 if sg == 0 else car[:, g8, 0:1]
                i1 = 0.0 if sg == 0 else car[:, g8, 1:2]
                S.op("dve", lambda be: be.tensor_tensor_scan(out=BU[:], data0=dec_, data1=Vt[:], initial=i0, op0=ALU.mult, op1=ALU.add),
                     reads=[r2, Vt, car], writes=[BU])
                S.op("dve", lambda be: be.tensor_tensor_scan(out=BS[:], data0=dec_, data1=VS[:], initial=i1, op0=ALU.mult, op1=ALU.add),
                     reads=[r2, VS, car], writes=[BS])
                if sg < NSEG - 1:
                    S.op("act", lambda be: be.copy(out=car[:, g8, 0:1], in_=BU[:, SEG - 1:SEG]), reads=[BU, car], writes=[car])
                    S.op("act", lambda be: be.copy(out=car[:, g8, 1:2], in_=BS[:, SEG - 1:SEG]), reads=[BS, car], writes=[car])
                d.update(BU=BU, BS=BS)

            def stageC(i):
                blk, sg, g8 = items[i]
                g = blk * 8 + g8
                d = stt_.pop(i)
                COS, SIN, Vt, VS, BU, BS = d["COS"], d["SIN"], d["Vt"], d["VS"], d["BU"], d["BS"]
                Sg = Sgr.next()
                S.op("dve", lambda be: be.tensor_tensor(Vt[:], COS[:], BU[:], op=ALU.mult), reads=[COS, BU], writes=[Vt])
                S.op("dve", lambda be: be.tensor_tensor(VS[:], SIN[:], BS[:], op=ALU.mult), reads=[SIN, BS], writes=[VS])
                S.op("dve", lambda be: be.tensor_tensor(Sg[:], Vt[:], VS[:], op=ALU.subtract), reads=[Vt, VS], writes=[Sg])
                if (blk, sg) not in pysd:
                    pysd[(blk, sg)] = [pyr.next() for _ in range(NCS)]
                pys = pysd[(blk, sg)]
                for cs_ in range(NCS):
                    fsl = slice(cs_ * 512, (cs_ + 1) * 512)
                    S.mm([(pys[cs_][:], CCpad[:, g, :], Sg[:, fsl], g8 == 0, g8 == 7)], reads=[CCpad, Sg], writes=[pys[cs_]])
                if g8 != 7:
                    return
                ut = uts[blk]
                for cs_ in range(NCS):
                    tsl = slice(sg * SEG + cs_ * 512, sg * SEG + (cs_ + 1) * 512)
                    py = pys[cs_]
                    yv, tg, yg = yvr.next(), tgr.next(), ygr.next()
                    S.op("dve", lambda be: be.scalar_tensor_tensor(out=yv[:], in0=ut[:, tsl], scalar=dsk[:, blk:blk + 1], in1=py[:],
                                                                   op0=ALU.mult, op1=ALU.add), reads=[ut, dsk, py], writes=[yv])
                    S.op("dve", lambda be: be.tensor_tensor(tg[:], yv[:], yv[:], op=ALU.mult), reads=[yv], writes=[tg])
                    S.op("dve", lambda be: be.tensor_scalar(tg[:], tg[:], 0.044715, 1.0, op0=ALU.mult, op1=ALU.add), reads=[tg], writes=[tg])
                    S.op("dve", lambda be: be.tensor_tensor(tg[:], tg[:], yv[:], op=ALU.mult), reads=[tg, yv], writes=[tg])
                    S.op("act", lambda be: be.activation(out=tg[:], in_=tg[:], func=AF.Sigmoid, scale=2.0 * math.sqrt(2.0 / math.pi)),
                         reads=[tg], writes=[tg])
                    S.op("dve", lambda be: be.tensor_tensor(yg[:], yv[:], tg[:], op=ALU.mult), reads=[yv, tg], writes=[yg])
                    S.dma("sp", c.s5yT.ap[blk * 128:(blk + 1) * 128, tsl], yg[:], reads=[yg], writes=[c.s5yT.o((blk, tsl.start // 512))])

            n_it = len(items)
            for step in range(n_it + 3):
                if step < n_it:
                    stageA(step)
                if 0 <= step - 1 < n_it:
                    stageB1(step - 1)
                if 0 <= step - 3 < n_it:
                    stageC(step - 3)
                if 0 <= step - 2 < n_it:
                    stageB2(step - 2)

        with Scope(S) as gs:
            wg = sb(S, gs, "s5_wg", [128, 8, DB], BF16)
            for q in range(2):
                S.dma("pool", wg[:, :, q * 512:(q + 1) * 512],
                      w["s5_w_glu"].ap[l, :, q * 512:(q + 1) * 512].rearrange("(k p) n -> p k n", p=128), writes=[wg])
            ygr2 = sb_ring(S, gs, "s5_y2", [128, 8, 512], BF16, 2)
            zr2 = sb_ring(S, gs, "s5_z2", [128, 8, 512], BF16, 2)
            sgr = sb_ring(S, gs, "s5_sgm", [128, 512], F32, 2)
            outr = sb_ring(S, gs, "s5_o2", [128, 8, 512], BF16, 2)
            pgr = ps_ring(S, gs, "s5_pg", [128, 512], F32, 4)
            for tc in range(L // 512):
                tsl = slice(tc * 512, (tc + 1) * 512)
                yt, zt, ob = ygr2.next(), zr2.next(), outr.next()
                S.dma("sp", yt[:], c.s5yT.ap[:, tsl].rearrange("(k p) n -> p k n", p=128), reads=[c.s5yT.o((k, tc)) for k in range(8)], writes=[yt])
                S.dma("sp", zt[:], c.s5zT.ap[:, tsl].rearrange("(k p) n -> p k n", p=128), reads=[c.s5zT.o((k, tc)) for k in range(8)], writes=[zt])
                for j in range(8):
                    pg = pgr.next()
                    S.mm([(pg[:], wg[:, kc, j * 128:(j + 1) * 128], yt[:, kc, :], kc == 0, kc == 7) for kc in range(8)],
                         reads=[wg, yt], writes=[pg])
                    sg_ = sgr.next()
                    S.op("act", lambda be: be.activation(out=sg_[:], in_=pg[:], func=AF.Sigmoid, bias=bgl[:, j:j + 1]),
                         reads=[pg, bgl], writes=[sg_])
                    S.op("dve", lambda be: be.tensor_tensor(sg_[:], sg_[:], yt[:, j, :], op=ALU.mult), reads=[sg_, yt], writes=[sg_])
                    S.op("dve", lambda be: be.tensor_tensor(ob[:, j, :], sg_[:], zt[:, j, :], op=ALU.mult), reads=[sg_, zt], writes=[ob])
                S.dma("sp", c.yT[0].ap[:, tsl].rearrange("(k p) n -> p k n", p=128), ob[:], reads=[ob],
                      writes=[c.yT[0].o((k, tc)) for k in range(8)])


def build(L=4096, depth=4, debug=False, upto="all"):
    nc = bass.Bass("TRN2", target_bir_lowering=False)
    c = declare(nc, L, depth, debug)
    with ExitStack() as top:
        S = Sched(nc, top)
        for l in range(depth):
            xin = c.x if l == 0 else c.xs[(l - 1) % 2]
            xout = c.out if l == depth - 1 else c.xs[l % 2]
            with Scope(S) as st:
                hT = sb(S, st, "hT", [128, 16, L], BF16)
                hobjs = chunk_objs(S, st, L // 512)
                phase_norm_T(S, c, xin, c.w["g_pre"].ap[l:l + 1, :], hT, L, hobjs=hobjs)
                if "inproj" in upto or upto == "all":
                    phase_inproj(S, c, l, hT, L, hobjs)
            if "s5" in upto or upto == "all":
                phase_s5(S, c, l, L)
            if "ml" in upto or upto == "all":
                phase_ml(S, c, l, L)
            if "da" in upto or upto == "all":
                phase_da(S, c, l, L)
            if "xa" in upto or upto == "all":
                phase_xa(S, c, l, L)
            if "merge" in upto or upto == "all":
                phase_merge(S, c, l, L, None)
                phase_out(S, c, l, L, xin, xout, None)
        outs = [o for o in c.out.objs.values()]
        if debug:
            for d in [c.s5uT, c.s5zT, c.mlqT, c.mlkT, c.mlv, c.mlo, c.mlz, c.mlif, c.daqT, c.dakT, c.dav, c.daz,
                      c.xaqT, c.xaz, c.gT, c.mT] + c.yT + c.xs:
                outs += list(d.objs.values())
        S.final_wait("sp", outs)
        print("instructions:", S.n_inst)
    return nc, c


def make_consts(L):
    bf = ml_dtypes.bfloat16
    inv = 1.0 / (10000.0 ** (np.arange(0, 64, 2, dtype=np.float32) / 64.0))
    ang = np.arange(L, dtype=np.float32)[:, None] * inv[None, :]
    cos, sin = np.cos(ang).T, np.sin(ang).T
    c64 = np.concatenate([cos, cos], 0)
    s64 = np.concatenate([-sin, sin], 0)
    kq = (np.arange(128)[None, :] >= np.arange(128)[:, None]).astype(np.float32)
    return {
        "c_ident": np.eye(128, dtype=np.float32).astype(bf),
        "c_identf": np.eye(128, dtype=np.float32),
        "c_trow": np.arange(L, dtype=np.float32)[None, :].copy(),
        "c_ropec": np.concatenate([c64, c64], 0).astype(bf),
        "c_ropes": np.concatenate([s64, s64], 0).astype(bf),
        "c_maskkq": kq.astype(bf),
        "c_maskg": (np.arange(128)[:, None] // 16 == np.arange(8)[None, :]).astype(np.float32),
        "c_mask2": np.broadcast_to((np.arange(8)[:, None] == (np.arange(128)[None, :] // 16)).astype(np.float32)[None], (128, 8, 128)).copy(),
    }


SEQ_FULL = 4096
DEPTH_FULL = 4


def kernel(**inputs):
    L, depth = SEQ_FULL, DEPTH_FULL
    nc, _ = build(L=L, depth=depth, debug=False)
    consts = make_consts(L)
    shared = {k: np.ascontiguousarray(np.asarray(v, dtype=np.float32)) for k, v in inputs.items() if k not in ("x", "mem")}
    x = np.asarray(inputs["x"], dtype=np.float32)
    mem = np.asarray(inputs["mem"], dtype=np.float32)
    in_maps = []
    for b in range(x.shape[0]):
        m = dict(shared)
        m["x"] = np.ascontiguousarray(x[b])
        m["mem"] = np.ascontiguousarray(mem[b])
        m.update(consts)
        in_maps.append(m)
    res = run_bass_kernel_spmd(nc, in_maps, core_ids=list(range(len(in_maps))))
    return np.stack([np.asarray(r["out"], dtype=np.float32) for r in res.results], axis=0)
```

```python
import math
from contextlib import ExitStack

import numpy as np
import ml_dtypes
import concourse.bass as bass
import concourse.mybir as mybir
from concourse.bass_utils import run_bass_kernel_spmd

F32 = mybir.dt.float32
BF16 = mybir.dt.bfloat16
AF = mybir.ActivationFunctionType
ALU = mybir.AluOpType
AX = mybir.AxisListType

D = 2048
DB = 1024
MEM = 256
NG = 64
D_IN = 21512
EPS = 1e-6
import os
SAME_ENGINE_SYNC = os.environ.get("MK_SES", "1") == "1"

C_S5U, C_S5Z = 0, 1024
C_MLQ, C_MLK, C_MLV, C_MLO, C_MLZ = 2048, 3072, 4096, 5120, 6144
C_MLIF = 7168
C_DAQ, C_DAK, C_DAV, C_DAZ = 7176, 8200, 9224, 10248
C_XAQ, C_XAZ = 11272, 12296
C_GATE = 13320


class Obj:
    __slots__ = ("lw", "rd", "name")

    def __init__(self, name=""):
        self.lw = None
        self.rd = {}
        self.name = name


class Tile:
    def __init__(self, h, name):
        self.h = h
        self.o = Obj(name)

    def __getitem__(self, k):
        return self.h[k]


class Ring:
    def __init__(self, tiles):
        self.tiles = tiles
        self.i = 0

    def next(self):
        t = self.tiles[self.i]
        self.i = (self.i + 1) % len(self.tiles)
        return t


class DT:
    def __init__(self, nc, name, shape, dtype, kind="Internal"):
        self.h = nc.dram_tensor(name, list(shape), dtype, kind=kind)
        self.ap = self.h.ap()
        self.objs = {}
        self.name = name

    def o(self, key=0):
        if key not in self.objs:
            self.objs[key] = Obj(f"{self.name}:{key}")
        return self.objs[key]


class Sched:
    def __init__(self, nc, stack, n_sp=40, n_pool=8, n_act=4):
        self.nc = nc
        self.eng = {"pe": nc.tensor, "act": nc.scalar, "dve": nc.vector, "pool": nc.gpsimd, "sp": nc.sync}
        self.semobj = {}
        for e in ("pe", "act", "dve", "pool"):
            self.semobj[e] = stack.enter_context(nc.semaphore("s_" + e))
        self.tick = {e: 0 for e in ("pe", "act", "dve", "pool")}
        self.waited = {e: {} for e in self.eng}
        self.dq = {"sp": n_sp, "pool": n_pool, "act": n_act}
        self.dnext = {q: 0 for q in self.dq}
        self.dcnt = {}
        for q, n in self.dq.items():
            for i in range(n):
                self.semobj[(q, i)] = stack.enter_context(nc.semaphore(f"d_{q}{i}"))
                self.dcnt[(q, i)] = 0
        self.n_inst = 0
        self.released = {}

    def _deps(self, reads, writes):
        deps = []
        for t in reads:
            o = t.o if isinstance(t, Tile) else t
            if o.lw is not None:
                deps.append(o.lw)
        for t in writes:
            o = t.o if isinstance(t, Tile) else t
            if o.lw is not None:
                deps.append(o.lw)
            deps.extend(o.rd.items())
        return deps

    def _wait(self, e, deps):
        w = self.waited[e]
        need = {}
        for sk, val in deps:
            if w.get(sk, 0) < val and need.get(sk, 0) < val:
                need[sk] = val
        for sk, val in need.items():
            w[sk] = val
            self.eng[e].wait_ge(self.semobj[sk], val)
            self.n_inst += 1

    def _mark(self, tok, reads, writes):
        for t in writes:
            o = t.o if isinstance(t, Tile) else t
            o.lw = tok
            o.rd = {}
        for t in reads:
            o = t.o if isinstance(t, Tile) else t
            if o.rd.get(tok[0], 0) < tok[1]:
                o.rd[tok[0]] = tok[1]

    def op(self, e, fn, reads=(), writes=()):
        deps = self._deps(reads, writes)
        if e == "pe" or not SAME_ENGINE_SYNC:
            deps = [d for d in deps if d[0] != e]
        self._wait(e, deps)
        self.tick[e] += 1
        fn(self.eng[e]).then_inc(self.semobj[e], 1)
        self.n_inst += 1
        self._mark((e, self.tick[e]), reads, writes)

    def mm(self, items, reads=(), writes=(), skip=False):
        deps = [d for d in self._deps(reads, writes) if d[0] != "pe"]
        self._wait("pe", deps)
        n = len(items)
        for i, (out, lhsT, rhs, st, sp) in enumerate(items):
            if skip:
                ins = self.nc.tensor.matmul(out, lhsT, rhs, start=st, stop=sp, skip_group_check=True)
            else:
                ins = self.nc.tensor.matmul(out, lhsT, rhs, start=st, stop=sp)
            self.n_inst += 1
            if i == n - 1:
                self.tick["pe"] += 1
                ins.then_inc(self.semobj["pe"], 1)
        self._mark(("pe", self.tick["pe"]), reads, writes)

    def transpose(self, out, in_, ident, reads=(), writes=()):
        self.op("pe", lambda be: be.transpose(out, in_, ident), reads=reads, writes=writes)

    def dma(self, q, out, in_, reads=(), writes=(), slow=False):
        i = self.dnext[q]
        self.dnext[q] = (i + 1) % self.dq[q]
        sk = (q, i)
        prev = self.dcnt[sk]
        deps = self._deps(reads, writes)
        if q in self.tick and not SAME_ENGINE_SYNC:
            deps = [d for d in deps if d[0] != q]
        if prev > 0:
            deps.append((sk, prev))
        self._wait(q, deps)
        self.dcnt[sk] = prev + 16
        if slow:
            self.eng[q].dma_start(out=out, in_=in_, allow_slow_non_contiguous=True).then_inc(self.semobj[sk], 16)
        else:
            self.eng[q].dma_start(out=out, in_=in_).then_inc(self.semobj[sk], 16)
        self.n_inst += 1
        self._mark((sk, prev + 16), reads, writes)

    def final_wait(self, e, objs):
        deps = []
        for o in objs:
            if o.lw is not None:
                deps.append(o.lw)
        self._wait(e, deps)


class Scope(ExitStack):
    def __init__(self, S):
        super().__init__()
        self.S = S
        self.tiles = []

    def __exit__(self, *a):
        rel = self.S.released
        for t in self.tiles:
            o = t.o
            if o.lw is not None and rel.get(o.lw[0], 0) < o.lw[1]:
                rel[o.lw[0]] = o.lw[1]
            for k, v in o.rd.items():
                if rel.get(k, 0) < v:
                    rel[k] = v
        return super().__exit__(*a)


_UID = [0]


def _uname(name):
    _UID[0] += 1
    return f"{name}_{_UID[0]}"


def _new_tile(S, stack, h, name):
    t = Tile(h, name)
    t.o.rd = dict(S.released)
    stack.tiles.append(t)
    return t


def sb(S, stack, name, shape, dtype):
    return _new_tile(S, stack, stack.enter_context(S.nc.sbuf_tensor(_uname(name), list(shape), dtype)), name)


def ps(S, stack, name, shape, dtype=F32):
    return _new_tile(S, stack, stack.enter_context(S.nc.psum_tensor(_uname(name), list(shape), dtype)), name)


def sb_ring(S, stack, name, shape, dtype, n):
    return Ring([sb(S, stack, f"{name}{i}", shape, dtype) for i in range(n)])


def ps_ring(S, stack, name, shape, dtype, n):
    return Ring([ps(S, stack, f"{name}{i}", shape, dtype) for i in range(n)])


class Ctx:
    pass


def declare(nc, L, depth, debug):
    c = Ctx()
    c.L, c.depth = L, depth
    kin = "ExternalInput"
    c.x = DT(nc, "x", [L, D], F32, kin)
    c.mem = DT(nc, "mem", [MEM, D], F32, kin)
    shapes = dict(
        g_pre=[depth, D], w_in=[depth, D, D_IN], s5_lam_re=[depth, NG, 64], s5_lam_im=[depth, NG, 64],
        s5_log_dt=[depth, NG], s5_b_re=[depth, NG, 64, 16], s5_b_im=[depth, NG, 64, 16],
        s5_c_re=[depth, NG, 16, 64], s5_c_im=[depth, NG, 16, 64], s5_d=[depth, DB],
        s5_w_glu=[depth, DB, DB], s5_b_glu=[depth, DB], ml_conv_w=[depth, 4, 2 * DB], ml_conv_b=[depth, 2 * DB],
        ml_b_i=[depth, 4], ml_b_f=[depth, 4], ml_norm_g=[depth, DB], da_lq1=[depth, 64], da_lk1=[depth, 64],
        da_lq2=[depth, 64], da_lk2=[depth, 64], da_subln_g=[depth, 128], g_mem=[depth, D],
        xa_w_kv=[depth, D, 2 * DB], w_branch=[depth, 4, DB, D], w_out=[depth, D, D], g_post=[depth, D])
    c.w = {k: DT(nc, k, v, F32, kin) for k, v in shapes.items()}
    c.ident = DT(nc, "c_ident", [128, 128], BF16, kin)
    c.identf = DT(nc, "c_identf", [128, 128], F32, kin)
    c.trow = DT(nc, "c_trow", [1, L], F32, kin)
    c.ropec = DT(nc, "c_ropec", [128, L], BF16, kin)
    c.ropes = DT(nc, "c_ropes", [128, L], BF16, kin)
    c.maskkq = DT(nc, "c_maskkq", [128, 128], BF16, kin)
    c.maskg = DT(nc, "c_maskg", [128, 8], F32, kin)
    c.mask2 = DT(nc, "c_mask2", [128, 8, 128], F32, kin)
    c.out = DT(nc, "out", [L, D], F32, "ExternalOutput")
    sk = "ExternalOutput" if debug else "Internal"
    c.dbg = debug
    c.xs = [DT(nc, f"xs{i}", [L, D], F32, sk) for i in range(2)]
    c.s5uT = DT(nc, "s5uT", [DB, L], BF16, sk)
    c.s5zT = DT(nc, "s5zT", [DB, L], BF16, sk)
    c.mlqT = DT(nc, "mlqT", [DB, L], BF16, sk)
    c.mlkT = DT(nc, "mlkT", [DB, L], BF16, sk)
    c.mlv = DT(nc, "mlv", [L, DB], BF16, sk)
    c.mlo = DT(nc, "mlo", [L, DB], BF16, sk)
    c.mlz = DT(nc, "mlz", [L, DB], BF16, sk)
    c.mlif = DT(nc, "mlif", [8, L], F32, sk)
    c.daqT = DT(nc, "daqT", [DB, L], BF16, sk)
    c.dakT = DT(nc, "dakT", [DB, L], BF16, sk)
    c.dav = DT(nc, "dav", [L, DB], BF16, sk)
    c.daz = DT(nc, "daz", [L, DB], BF16, sk)
    c.xaqT = DT(nc, "xaqT", [DB, L], BF16, sk)
    c.xaz = DT(nc, "xaz", [L, DB], BF16, sk)
    c.gT = DT(nc, "gT", [4 * D, L], BF16, sk)
    c.yT = [DT(nc, f"yT{b}", [DB, L], BF16, sk) for b in range(4)]
    c.mT = DT(nc, "mT", [D, L], BF16, sk)
    c.mlg = DT(nc, "mlg", [3, 4, L + 1], F32, "Internal")
    c.s5yT = DT(nc, "s5yT", [DB, L], BF16, sk)
    return c


def bcast_rows(ap_row, nparts):
    return ap_row.partition_broadcast(nparts)


def chunk_objs(S, stack, n):
    objs = []
    for i in range(n):
        o = Obj(f"chunk{i}")
        o.rd = dict(S.released)
        h = Tile(None, "chunk")
        h.o = o
        stack.tiles.append(h)
        objs.append(o)
    return objs


def phase_norm_T(S, c, src, g_row_ap, hT, L, rows=None, tag="a", hobjs=None):
    nc = S.nc
    rows = L if rows is None else rows
    with Scope(S) as st:
        gb = sb(S, st, tag + "_gb", [128, D], F32)
        S.dma("sp", gb[:], bcast_rows(g_row_ap, 128), writes=[gb])
        idt = sb(S, st, tag + "_id", [128, 128], BF16)
        S.dma("sp", idt[:], c.ident.ap[:, :], writes=[idt])
        xr = sb_ring(S, st, tag + "_x", [128, D], F32, 2)
        jr = sb_ring(S, st, tag + "_j", [128, D], F32, 1)
        hr = sb_ring(S, st, tag + "_h", [128, D], BF16, 2)
        ssr = sb_ring(S, st, tag + "_ss", [128, 4], F32, 2)
        pr = ps_ring(S, st, tag + "_p", [128, 512], BF16, 4)
        for t in range(rows // 128):
            xt = xr.next()
            S.dma("sp", xt[:], src.ap[t * 128:(t + 1) * 128, :], reads=[src.o(t)], writes=[xt])
            jk = jr.next()
            ss = ssr.next()
            S.op("act", lambda be: be.activation(out=jk[:], in_=xt[:], func=AF.Square, accum_out=ss[:, 0:1]),
                 reads=[xt], writes=[jk, ss])
            S.op("dve", lambda be: be.tensor_scalar(ss[:, 1:2], ss[:, 0:1], 1.0 / D, EPS, op0=ALU.mult, op1=ALU.add),
                 reads=[ss], writes=[ss])
            S.op("act", lambda be: be.activation(out=ss[:, 2:3], in_=ss[:, 1:2], func=AF.Sqrt), reads=[ss], writes=[ss])
            S.op("dve", lambda be: be.reciprocal(ss[:, 3:4], ss[:, 2:3]), reads=[ss], writes=[ss])
            ht = hr.next()
            S.op("dve", lambda be: be.scalar_tensor_tensor(out=ht[:], in0=xt[:], scalar=ss[:, 3:4], in1=gb[:],
                                                           op0=ALU.mult, op1=ALU.mult),
                 reads=[xt, ss, gb], writes=[ht])
            for q in range(4):
                pt = pr.next()
                for j in range(4):
                    kc = q * 4 + j
                    S.transpose(pt[:, j * 128:(j + 1) * 128], ht[:, kc * 128:(kc + 1) * 128], idt[:],
                                reads=[ht, idt], writes=[pt])
                eng = "act" if q % 2 else "dve"
                dst = hT[:, q * 4:(q + 1) * 4, t * 128:(t + 1) * 128]
                srcp = pt[:].rearrange("p (j n) -> p j n", j=4)
                wo_ = [hobjs[t // 4]] if hobjs is not None else [hT]
                if eng == "act":
                    S.op("act", lambda be: be.copy(out=dst, in_=srcp), reads=[pt], writes=wo_)
                else:
                    S.op("dve", lambda be: be.tensor_copy(dst, srcp), reads=[pt], writes=wo_)


def phase_inproj(S, c, l, hT, L, hobjs):
    nc = S.nc
    w_in = c.w["w_in"].ap
    NT, NC = L // 128, L // 512
    with Scope(S) as st:
        wr = sb_ring(S, st, "ip_w", [128, 16, 512], BF16, 2)
        wsw = sb(S, st, "ip_wsw", [128, 16, 512], BF16)
        pr = ps_ring(S, st, "ip_p", [128, 512], F32, 6)
        sbf = sb_ring(S, st, "ip_sb", [128, 512], BF16, 4)
        sf32 = sb_ring(S, st, "ip_sf", [128, 512], F32, 2)
        pre = sb_ring(S, st, "ip_pre", [128, 3 + 512], F32, 2)
        acc = sb_ring(S, st, "ip_acc", [128, 512], F32, 2)
        rcr = sb_ring(S, st, "ip_rc", [128, 512], BF16, 2)
        rsr = sb_ring(S, st, "ip_rs", [128, 512], BF16, 2)
        cw = sb(S, st, "ip_cw", [128, 4, 16], F32)
        cb = sb(S, st, "ip_cb", [128, 16], F32)
        for j in range(4):
            S.dma("sp", cw[:, j, :], c.w["ml_conv_w"].ap[l, j].rearrange("(b p) -> p b", p=128), writes=[cw], slow=True)
        S.dma("sp", cb[:], c.w["ml_conv_b"].ap[l].rearrange("(b p) -> p b", p=128), writes=[cb], slow=True)

        def load_w(c0, n):
            wc = wr.next()
            S.dma("pool", wc[:, :, :n], w_in[l, :, c0:c0 + n].rearrange("(k p) n -> p k n", p=128), writes=[wc])
            return wc

        def tok_major(c0, dst, func):
            for cc in range(0, DB, 512):
                wc = load_w(c0 + cc, 512)
                for t in range(NT):
                    p = pr.next()
                    S.mm([(p[:], hT[:, kc, t * 128:(t + 1) * 128], wc[:, kc, :], kc == 0, kc == 15) for kc in range(16)],
                         reads=[hobjs[t // 4], wc], writes=[p])
                    s = sbf.next()
                    if func is None:
                        S.op("dve", lambda be: be.tensor_copy(s[:], p[:]), reads=[p], writes=[s])
                    else:
                        S.op("act", lambda be: be.activation(out=s[:], in_=p[:], func=func), reads=[p], writes=[s])
                    S.dma("sp", dst.ap[t * 128:(t + 1) * 128, cc:cc + 512], s[:], reads=[s], writes=[dst.o(t)])

        def feat_major(c0, ncols, dst, row0, kind, cbase=0):
            for cc in range(0, ncols, 512):
                n = min(512, ncols - cc)
                wc = load_w(c0 + cc, n)
                if kind == "rope":
                    srcv = wc[:, :, :n].rearrange("p k (b h d) -> p k b h d", h=2, d=32)
                    dstv = wsw[:, :, :n].rearrange("p k (b h d) -> p k b h d", h=2, d=32)
                    for kc in range(16):
                        S.op("act" if kc % 2 else "dve",
                             (lambda be: be.copy(out=dstv[:, kc, :, 0, :], in_=srcv[:, kc, :, 1, :])) if kc % 2 else
                             (lambda be: be.tensor_copy(dstv[:, kc, :, 0, :], srcv[:, kc, :, 1, :])), reads=[wc], writes=[wsw])
                        S.op("act" if kc % 2 else "dve",
                             (lambda be: be.copy(out=dstv[:, kc, :, 1, :], in_=srcv[:, kc, :, 0, :])) if kc % 2 else
                             (lambda be: be.tensor_copy(dstv[:, kc, :, 1, :], srcv[:, kc, :, 0, :])), reads=[wc], writes=[wsw])
                for j in range(n // 128):
                    blk = (cc // 128) + j
                    prev = None
                    for tc in range(NC):
                        tsl = slice(tc * 512, (tc + 1) * 512)
                        p = pr.next()
                        S.mm([(p[:], wc[:, kc, j * 128:(j + 1) * 128], hT[:, kc, tsl], kc == 0, kc == 15)
                              for kc in range(16)], reads=[hobjs[tc], wc], writes=[p])
                        s = sbf.next()
                        if kind == "copy":
                            S.op("dve", lambda be: be.tensor_copy(s[:], p[:]), reads=[p], writes=[s])
                        elif kind in ("silu", "sigmoid"):
                            f = AF.Silu if kind == "silu" else AF.Sigmoid
                            S.op("act", lambda be: be.activation(out=s[:], in_=p[:], func=f), reads=[p], writes=[s])
                        elif kind in ("conv", "convk"):
                            cblk = cbase + blk
                            pt = pre.next()
                            if prev is None:
                                S.op("dve", lambda be: be.memset(pt[:, 0:3], 0.0), writes=[pt])
                            else:
                                pv = prev
                                S.op("act", lambda be: be.copy(out=pt[:, 0:3], in_=pv[:, 512:515]), reads=[pv], writes=[pt])
                            S.op("act", lambda be: be.copy(out=pt[:, 3:515], in_=p[:]), reads=[p], writes=[pt])
                            a = acc.next()
                            S.op("dve", lambda be: be.tensor_scalar(a[:], pt[:, 3:515], cw[:, 3, cblk:cblk + 1], cb[:, cblk:cblk + 1],
                                                                   op0=ALU.mult, op1=ALU.add), reads=[pt, cw, cb], writes=[a])
                            for tap in range(3):
                                S.op("dve", lambda be: be.scalar_tensor_tensor(out=a[:], in0=pt[:, tap:tap + 512],
                                                                               scalar=cw[:, tap, cblk:cblk + 1], in1=a[:],
                                                                               op0=ALU.mult, op1=ALU.add),
                                     reads=[pt, cw, a], writes=[a])
                            if kind == "conv":
                                S.op("act", lambda be: be.activation(out=s[:], in_=a[:], func=AF.Silu), reads=[a], writes=[s])
                            else:
                                a2 = sf32.next()
                                S.op("act", lambda be: be.activation(out=a2[:], in_=a[:], func=AF.Silu), reads=[a], writes=[a2])
                                S.op("dve", lambda be: be.tensor_scalar(s[:], a2[:], 0.0625, None, op0=ALU.mult),
                                     reads=[a2], writes=[s])
                            prev = pt
                        elif kind == "rope":
                            p2 = pr.next()
                            S.mm([(p2[:], wsw[:, kc, j * 128:(j + 1) * 128], hT[:, kc, tsl], kc == 0, kc == 15)
                                  for kc in range(16)], reads=[hobjs[tc], wsw], writes=[p2])
                            a = acc.next()
                            a2 = sf32.next()
                            rc, rs = rcr.next(), rsr.next()
                            S.dma("sp", rc[:], c.ropec.ap[:, tsl], writes=[rc])
                            S.dma("sp", rs[:], c.ropes.ap[:, tsl], writes=[rs])
                            S.op("dve", lambda be: be.tensor_tensor(a[:], p[:], rc[:], op=ALU.mult), reads=[p, rc], writes=[a])
                            S.op("dve", lambda be: be.tensor_tensor(a2[:], p2[:], rs[:], op=ALU.mult), reads=[p2, rs], writes=[a2])
                            S.op("dve", lambda be: be.tensor_tensor(s[:], a[:], a2[:], op=ALU.add), reads=[a, a2], writes=[s])
                        r0 = row0 + blk * 128
                        S.dma("sp", dst.ap[r0:r0 + 128, tsl], s[:], reads=[s], writes=[dst.o((r0 // 128, tc))])

        feat_major(C_S5U, DB, c.s5uT, 0, "copy")
        feat_major(C_S5Z, DB, c.s5zT, 0, "silu")
        feat_major(C_MLQ, DB, c.mlqT, 0, "conv", cbase=0)
        feat_major(C_MLK, DB, c.mlkT, 0, "convk", cbase=8)
        tok_major(C_MLV, c.mlv, None)
        tok_major(C_MLO, c.mlo, AF.Sigmoid)
        tok_major(C_MLZ, c.mlz, AF.Silu)
        wc = load_w(C_MLIF, 8)
        for tc in range(NC):
            tsl = slice(tc * 512, (tc + 1) * 512)
            p = pr.next()
            S.mm([(p[0:8, :], wc[:, kc, 0:8], hT[:, kc, tsl], kc == 0, kc == 15) for kc in range(16)],
                 reads=[hobjs[tc], wc], writes=[p])
            a = acc.next()
            S.op("dve", lambda be: be.tensor_copy(a[0:8, :], p[0:8, :]), reads=[p], writes=[a])
            S.dma("sp", c.mlif.ap[:, tsl], a[0:8, :], reads=[a], writes=[c.mlif.o(tc)])
        feat_major(C_DAQ, DB, c.daqT, 0, "rope")
        feat_major(C_DAK, DB, c.dakT, 0, "rope")
        tok_major(C_DAV, c.dav, None)
        tok_major(C_DAZ, c.daz, AF.Silu)
        feat_major(C_XAQ, DB, c.xaqT, 0, "copy")
        tok_major(C_XAZ, c.xaz, AF.Silu)
        feat_major(C_GATE, 4 * D, c.gT, 0, "sigmoid")


def emit_yT(S, ytoks, dst, row0, nblk, tsl, key_tc, idt, ptr, ysbr):
    ysb = ysbr.next()
    for blk in range(nblk):
        pt = ptr.next()
        for qt in range(4):
            S.transpose(pt[:, qt * 128:(qt + 1) * 128], ytoks[qt][:, blk * 128:(blk + 1) * 128], idt[:],
                        reads=[ytoks[qt], idt], writes=[pt])
        if blk % 2:
            S.op("act", lambda be: be.copy(out=ysb[:, blk, :], in_=pt[:]), reads=[pt], writes=[ysb])
        else:
            S.op("dve", lambda be: be.tensor_copy(ysb[:, blk, :], pt[:]), reads=[pt], writes=[ysb])
    S.dma("sp", dst.ap[row0:row0 + nblk * 128, tsl].rearrange("(k p) n -> p k n", p=128), ysb[:, 0:nblk, :],
          reads=[ysb], writes=[dst.o((r, key_tc)) for r in range(row0 // 128, row0 // 128 + nblk)])


def phase_merge(S, c, l, L, wo):
    NC = L // 512
    wb = c.w["w_branch"].ap
    with Scope(S) as st:
        wqr = sb_ring(S, st, "mg_w", [128, 32, 512], BF16, 2)

        def load_wq(dq):
            t = wqr.next()
            for b in range(4):
                S.dma("pool", t[:, b * 8:(b + 1) * 8, :],
                      wb[l, b, :, dq * 512:(dq + 1) * 512].rearrange("(k p) n -> p k n", p=128), writes=[t])
            return t
        wq_next = load_wq(0)
        yr = sb_ring(S, st, "mg_y", [128, 32, 512], BF16, 2)
        gr = sb_ring(S, st, "mg_g", [128, 4, 512], BF16, 2)
        tr = sb_ring(S, st, "mg_t", [128, 4, 512], F32, 2)
        ar = sb_ring(S, st, "mg_a", [128, 2, 512], F32, 2)
        orr = sb_ring(S, st, "mg_o", [128, 512], BF16, 2)
        pr = ps_ring(S, st, "mg_p", [128, 512], F32, 8)
        gview = c.gT.ap.rearrange("(b r) n -> r b n", b=4)
        for dq in range(4):
            wq = wq_next
            if dq < 3:
                wq_next = load_wq(dq + 1)
            for tc in range(NC):
                tsl = slice(tc * 512, (tc + 1) * 512)
                yt = yr.next()
                for b in range(4):
                    S.dma("sp", yt[:, b * 8:(b + 1) * 8, :], c.yT[b].ap[:, tsl].rearrange("(k p) n -> p k n", p=128),
                          reads=[c.yT[b].o((r, tc)) for r in range(8)], writes=[yt])
                for j in range(4):
                    r0 = dq * 512 + j * 128
                    gt = gr.next()
                    S.dma("sp", gt[:], gview[r0:r0 + 128, :, tsl],
                          reads=[c.gT.o(((b * D + r0) // 128, tc)) for b in range(4)], writes=[gt])
                    tt = tr.next()
                    for b in range(4):
                        p = pr.next()
                        S.mm([(p[:], wq[:, b * 8 + kc, j * 128:(j + 1) * 128], yt[:, b * 8 + kc, :], kc == 0, kc == 7)
                              for kc in range(8)], reads=[wq, yt], writes=[p])
                        S.op("dve", lambda be: be.tensor_tensor(tt[:, b, :], p[:], gt[:, b, :], op=ALU.mult),
                             reads=[p, gt], writes=[tt])
                    a = ar.next()
                    S.op("dve", lambda be: be.tensor_tensor(a[:], tt[:, 0:2, :], tt[:, 2:4, :], op=ALU.add), reads=[tt], writes=[a])
                    o = orr.next()
                    S.op("dve", lambda be: be.tensor_tensor(o[:], a[:, 0, :], a[:, 1, :], op=ALU.add), reads=[a], writes=[o])
                    S.dma("sp", c.mT.ap[r0:r0 + 128, tsl], o[:], reads=[o], writes=[c.mT.o((r0 // 128, tc))])


def phase_out(S, c, l, L, xin, xout, wo):
    NT = L // 128
    with Scope(S) as st:
        wo = sb(S, st, "po_w", [128, 16, D], BF16)
        for q in range(4):
            S.dma("pool", wo[:, :, q * 512:(q + 1) * 512],
                  c.w["w_out"].ap[l, :, q * 512:(q + 1) * 512].rearrange("(k p) n -> p k n", p=128), writes=[wo])
        gp = sb(S, st, "po_g", [128, D], F32)
        S.dma("sp", gp[:], c.w["g_post"].ap[l:l + 1, :].partition_broadcast(128), writes=[gp])
        mr = sb_ring(S, st, "po_m", [128, 16, 512], BF16, 2)
        xr = sb_ring(S, st, "po_x", [128, D], F32, 2)
        orr = sb_ring(S, st, "po_o", [128, D], F32, 2)
        jr = sb_ring(S, st, "po_j", [128, 512], F32, 2)
        ssr = sb_ring(S, st, "po_ss", [128, 8], F32, 2)
        pr = ps_ring(S, st, "po_p", [128, 512], F32, 8)
        for t in range(NT):
            if t % 4 == 0:
                mt = mr.next()
                S.dma("sp", mt[:], c.mT.ap[:, t * 128:(t + 4) * 128].rearrange("(k p) n -> p k n", p=128),
                      reads=[c.mT.o((r, t // 4)) for r in range(16)], writes=[mt])
            msl = slice((t % 4) * 128, (t % 4 + 1) * 128)
            xt = xr.next()
            S.dma("sp", xt[:], xin.ap[t * 128:(t + 1) * 128, :], reads=[xin.o(t)], writes=[xt])
            ss = ssr.next()
            pp = []
            for n in range(4):
                p = pr.next()
                pp.append(p)
                S.mm([(p[:], mt[:, kc, msl], wo[:, kc, n * 512:(n + 1) * 512], kc == 0, kc == 15) for kc in range(16)],
                     reads=[mt, wo], writes=[p])
                jk = jr.next()
                S.op("act", lambda be: be.activation(out=jk[:], in_=p[:], func=AF.Square, accum_out=ss[:, n:n + 1]),
                     reads=[p], writes=[jk, ss])
            S.op("dve", lambda be: be.reduce_sum(out=ss[:, 4:5], in_=ss[:, 0:4], axis=AX.X), reads=[ss], writes=[ss])
            S.op("dve", lambda be: be.tensor_scalar(ss[:, 5:6], ss[:, 4:5], 1.0 / D, EPS, op0=ALU.mult, op1=ALU.add),
                 reads=[ss], writes=[ss])
            S.op("act", lambda be: be.activation(out=ss[:, 6:7], in_=ss[:, 5:6], func=AF.Sqrt), reads=[ss], writes=[ss])
            S.op("dve", lambda be: be.reciprocal(ss[:, 7:8], ss[:, 6:7]), reads=[ss], writes=[ss])
            ot = orr.next()
            for n in range(4):
                nsl = slice(n * 512, (n + 1) * 512)
                p = pp[n]
                S.op("dve", lambda be: be.scalar_tensor_tensor(out=ot[:, nsl], in0=p[:], scalar=ss[:, 7:8], in1=gp[:, nsl],
                                                               op0=ALU.mult, op1=ALU.mult), reads=[p, ss, gp], writes=[ot])
            S.op("dve", lambda be: be.tensor_tensor(ot[:], ot[:], xt[:], op=ALU.add), reads=[ot, xt], writes=[ot])
            S.dma("sp", xout.ap[t * 128:(t + 1) * 128, :], ot[:], reads=[ot], writes=[xout.o(t)])


def phase_xa(S, c, l, L):
    NC = L // 512
    wkv = c.w["xa_w_kv"].ap
    with Scope(S) as st:
        idt = sb(S, st, "xa_id", [128, 128], BF16)
        S.dma("sp", idt[:], c.ident.ap[:, :], writes=[idt])
        kT = sb(S, st, "xa_kT", [128, 8, MEM], BF16)
        vaug = sb(S, st, "xa_v", [128, 2, 4, 257], BF16)
        S.op("pool", lambda be: be.memset(vaug[:, :, :, 256:257], 1.0), writes=[vaug])
        with Scope(S) as st2:
            memT = sb(S, st2, "xa_memT", [128, 16, MEM], BF16)
            phase_norm_T(S, c, c.mem, c.w["g_mem"].ap[l:l + 1, :], memT, MEM, rows=MEM, tag="xn")
            wr = sb_ring(S, st2, "xa_w", [128, 16, 512], BF16, 2)
            pr = ps_ring(S, st2, "xa_pp", [128, 512], F32, 2)
            for q in range(4):
                wc = wr.next()
                S.dma("pool", wc[:], wkv[l, :, q * 512:(q + 1) * 512].rearrange("(k p) n -> p k n", p=128), writes=[wc])
                if q < 2:
                    for j in range(4):
                        p = pr.next()
                        S.mm([(p[:, 0:MEM], wc[:, kc, j * 128:(j + 1) * 128], memT[:, kc, :], kc == 0, kc == 15)
                              for kc in range(16)], reads=[wc, memT], writes=[p])
                        S.op("dve", lambda be: be.tensor_copy(kT[:, q * 4 + j, :], p[:, 0:MEM]), reads=[p], writes=[kT])
                else:
                    for mt in range(2):
                        p = pr.next()
                        S.mm([(p[:], memT[:, kc, mt * 128:(mt + 1) * 128], wc[:, kc, :], kc == 0, kc == 15)
                              for kc in range(16)], reads=[wc, memT], writes=[p])
                        h0 = (q - 2) * 2
                        S.op("dve", lambda be: be.tensor_copy(vaug[:, mt, h0:h0 + 2, 0:256],
                                                              p[:].rearrange("p (h d) -> p h d", h=2)),
                             reads=[p], writes=[vaug])
        qr = sb_ring(S, st, "xa_q", [128, 8, 512], BF16, 2)
        zr = sb_ring(S, st, "xa_z", [128, 4, DB], BF16, 2)
        ptr_ = sb_ring(S, st, "xa_pT", [128, 512], BF16, 4)
        yts = [sb_ring(S, st, f"xa_y{i}", [128, DB], BF16, 2) for i in range(4)]
        rr = sb_ring(S, st, "xa_r", [128, 2], F32, 4)
        ysbr = sb_ring(S, st, "xa_ysb", [128, 8, 512], BF16, 2)
        psr = ps_ring(S, st, "xa_ps", [128, 512], F32, 3)
        por = ps_ring(S, st, "xa_po", [128, 257], F32, 3)
        ptr2 = ps_ring(S, st, "xa_pt", [128, 512], BF16, 2)
        for tc in range(NC):
            tsl = slice(tc * 512, (tc + 1) * 512)
            qt_ = qr.next()
            S.dma("sp", qt_[:], c.xaqT.ap[:, tsl].rearrange("(k p) n -> p k n", p=128),
                  reads=[c.xaqT.o((r, tc)) for r in range(8)], writes=[qt_])
            zt = zr.next()
            S.dma("sp", zt[:], c.xaz.ap[tsl, :].rearrange("(t p) d -> p t d", p=128),
                  reads=[c.xaz.o(tc * 4 + i) for i in range(4)], writes=[zt])
            ytoks = [yts[i].next() for i in range(4)]
            for h in range(4):
                pTs = []
                for mt in range(2):
                    p = psr.next()
                    S.mm([(p[:], kT[:, h * 2 + db, mt * 128:(mt + 1) * 128], qt_[:, h * 2 + db, :], db == 0, db == 1)
                          for db in range(2)], reads=[kT, qt_], writes=[p])
                    pT = ptr_.next()
                    S.op("act", lambda be: be.activation(out=pT[:], in_=p[:], func=AF.Exp, scale=1.0 / 16.0),
                         reads=[p], writes=[pT])
                    pTs.append(pT)
                for qi in range(4):
                    po = por.next()
                    S.mm([(po[:], pTs[mt][:, qi * 128:(qi + 1) * 128], vaug[:, mt, h, :], mt == 0, mt == 1)
                          for mt in range(2)], reads=pTs + [vaug], writes=[po])
                    r = rr.next()
                    S.op("dve", lambda be: be.reciprocal(r[:, 0:1], po[:, 256:257]), reads=[po], writes=[r])
                    yk = ytoks[qi]
                    S.op("dve", lambda be: be.scalar_tensor_tensor(out=yk[:, h * 256:(h + 1) * 256], in0=po[:, 0:256],
                                                                   scalar=r[:, 0:1], in1=zt[:, qi, h * 256:(h + 1) * 256],
                                                                   op0=ALU.mult, op1=ALU.mult),
                         reads=[po, r, zt], writes=[yk])
            emit_yT(S, ytoks, c.yT[3], 0, 8, tsl, tc, idt, ptr2, ysbr)


def phase_da(S, c, l, L):
    NC, NT = L // 512, L // 128
    lam_init = 0.8 - 0.6 * math.exp(-0.3 * l)
    w = c.w
    with Scope(S) as st:
        idt = sb(S, st, "da_id", [128, 128], BF16)
        S.dma("sp", idt[:], c.ident.ap[:, :], writes=[idt])
        mk = sb(S, st, "da_mk", [128, 128], BF16)
        S.dma("sp", mk[:], c.maskkq.ap[:, :], writes=[mk])
        lt = sb(S, st, "da_lt", [128, 4, 64], F32)
        for i, nm in enumerate(("da_lq1", "da_lk1", "da_lq2", "da_lk2")):
            S.dma("sp", lt[:, i, :], w[nm].ap[l:l + 1, :].partition_broadcast(128), writes=[lt])
        lp = sb(S, st, "da_lp", [128, 2, 64], F32)
        lv = sb(S, st, "da_lv", [128, 8], F32)
        S.op("dve", lambda be: be.tensor_tensor(lp[:, 0, :], lt[:, 0, :], lt[:, 1, :], op=ALU.mult), reads=[lt], writes=[lp])
        S.op("dve", lambda be: be.tensor_tensor(lp[:, 1, :], lt[:, 2, :], lt[:, 3, :], op=ALU.mult), reads=[lt, lp], writes=[lp])
        S.op("dve", lambda be: be.reduce_sum(out=lv[:, 0:2], in_=lp[:], axis=AX.X), reads=[lp], writes=[lv])
        S.op("act", lambda be: be.activation(out=lv[:, 2:4], in_=lv[:, 0:2], func=AF.Exp), reads=[lv], writes=[lv])
        S.op("dve", lambda be: be.tensor_tensor(lv[:, 4:5], lv[:, 3:4], lv[:, 2:3], op=ALU.subtract), reads=[lv], writes=[lv])
        S.op("dve", lambda be: be.tensor_scalar(lv[:, 5:6], lv[:, 4:5], -lam_init, None, op0=ALU.add), reads=[lv], writes=[lv])
        sg = sb(S, st, "da_sg", [128, 128], F32)
        S.dma("sp", sg[:], w["da_subln_g"].ap[l:l + 1, :].partition_broadcast(128), writes=[sg])
        S.op("dve", lambda be: be.tensor_scalar(sg[:], sg[:], 1.0 - lam_init, None, op0=ALU.mult), reads=[sg], writes=[sg])

        kr = sb_ring(S, st, "da_k", [128, L], BF16, 2)
        vr = sb_ring(S, st, "da_v", [128, NT, 129], BF16, 2)
        qr = sb_ring(S, st, "da_q", [128, 512], BF16, 2)
        zr = sb_ring(S, st, "da_z", [128, 4, 128], BF16, 2)
        pTr = sb_ring(S, st, "da_pT", [128, 512], BF16, 6)
        yts = [sb_ring(S, st, f"da_y{i}", [128, 128], BF16, 2) for i in range(4)]
        ar = sb_ring(S, st, "da_a", [128, 128], F32, 2)
        dr = sb_ring(S, st, "da_d", [128, 128], F32, 8)
        jr = sb_ring(S, st, "da_j", [128, 128], F32, 2)
        rr = sb_ring(S, st, "da_r", [128, 16], F32, 3)
        ysbr = sb_ring(S, st, "da_ysb", [128, 1, 512], BF16, 2)
        psr = ps_ring(S, st, "da_ps", [128, 512], F32, 4)
        accs = [ps(S, st, f"da_acc{i}", [128, 3, 129], F32) for i in range(3)]
        ptr2 = ps_ring(S, st, "da_pt", [128, 512], BF16, 1)

        def acc(comp, qt):
            i = comp * 4 + qt
            return accs[i // 3], i % 3
        asbr = sb_ring(S, st, "da_asb", [128, 9, 129], F32, 2)
        pending = []

        for h in range(8):
            kt_ = kr.next()
            S.dma("sp", kt_[:], c.dakT.ap[h * 128:(h + 1) * 128, :], reads=[c.dakT.o((h, tc)) for tc in range(NC)], writes=[kt_])
            vt = vr.next()
            S.dma("sp", vt[:, :, 0:128], c.dav.ap[:, h * 128:(h + 1) * 128].rearrange("(t p) d -> p t d", p=128),
                  reads=[c.dav.o(t) for t in range(NT)], writes=[vt])
            S.op("pool", lambda be: be.memset(vt[:, :, 128:129], 1.0), writes=[vt])
            for tc in range(NC):
                tsl = slice(tc * 512, (tc + 1) * 512)
                qt_ = qr.next()
                S.dma("sp", qt_[:], c.daqT.ap[h * 128:(h + 1) * 128, tsl], reads=[c.daqT.o((h, tc))], writes=[qt_])
                zt = zr.next()
                S.dma("sp", zt[:], c.daz.ap[tsl, h * 128:(h + 1) * 128].rearrange("(t p) d -> p t d", p=128),
                      reads=[c.daz.o(tc * 4 + i) for i in range(4)], writes=[zt])
                nk = 4 * tc + 4
                for a_t in accs:
                    S.op("dve", lambda be: be.memset(a_t[:], 0.0), writes=[a_t])
                steps = [(kt, comp) for kt in range(nk) for comp in range(2)]

                def emit_st(i):
                    kt, comp = steps[i]
                    dq = kt - 4 * tc
                    q0 = max(dq, 0) * 128
                    csl = slice(comp * 64, (comp + 1) * 64)
                    p = psr.next()
                    S.mm([(p[:, q0:512], kt_[csl, kt * 128:(kt + 1) * 128], qt_[csl, q0:512], True, True)],
                         reads=[kt_, qt_], writes=[p])
                    pT = pTr.next()
                    S.op("act", lambda be: be.activation(out=pT[:, q0:512], in_=p[:, q0:512], func=AF.Exp, scale=0.125),
                         reads=[p], writes=[pT])
                    if dq >= 0:
                        S.op("dve", lambda be: be.tensor_tensor(pT[:, q0:q0 + 128], pT[:, q0:q0 + 128], mk[:], op=ALU.mult),
                             reads=[pT, mk], writes=[pT])
                    return pT

                LOOK = 3
                pts = {}
                for i in range(min(LOOK, len(steps))):
                    pts[i] = emit_st(i)
                for i in range(len(steps)):
                    if i == min(3, len(steps) - 1):
                        while pending:
                            pending.pop(0)()
                    if i + LOOK < len(steps):
                        pts[i + LOOK] = emit_st(i + LOOK)
                    kt, comp = steps[i]
                    dq = kt - 4 * tc
                    pT = pts.pop(i)
                    for qi in range(max(dq, 0), 4):
                        a_t, a_i = acc(comp, qi)
                        S.mm([(a_t[:, a_i, :], pT[:, qi * 128:(qi + 1) * 128], vt[:, kt, :], False, False)],
                             reads=[pT, vt], writes=[a_t], skip=True)
                asb = asbr.next()
                for i_t, a_t in enumerate(accs):
                    if i_t % 2:
                        S.op("act", lambda be: be.copy(out=asb[:, i_t * 3:(i_t + 1) * 3, :], in_=a_t[:]), reads=[a_t], writes=[asb])
                    else:
                        S.op("dve", lambda be: be.tensor_copy(asb[:, i_t * 3:(i_t + 1) * 3, :], a_t[:]), reads=[a_t], writes=[asb])

                def epilogue(asb=asb, zt=zt, h=h, tc=tc, tsl=tsl):
                    r = rr.next()
                    ds = []
                    for qi in range(4):
                        i0, i1 = qi, 4 + qi
                        S.op("dve", lambda be: be.reciprocal(r[:, 8 + qi:9 + qi], asb[:, i0, 128:129]), reads=[asb, r], writes=[r])
                        S.op("dve", lambda be: be.reciprocal(r[:, 12 + qi:13 + qi], asb[:, i1, 128:129]), reads=[asb, r], writes=[r])
                        S.op("dve", lambda be: be.tensor_tensor(r[:, 12 + qi:13 + qi], r[:, 12 + qi:13 + qi], lv[:, 5:6], op=ALU.mult),
                             reads=[r, lv], writes=[r])
                        a = ar.next()
                        S.op("dve", lambda be: be.tensor_scalar(a[:], asb[:, i0, 0:128], r[:, 8 + qi:9 + qi], None, op0=ALU.mult),
                             reads=[asb, r], writes=[a])
                        d = dr.next()
                        S.op("dve", lambda be: be.scalar_tensor_tensor(out=d[:], in0=asb[:, i1, 0:128], scalar=r[:, 12 + qi:13 + qi], in1=a[:],
                                                                       op0=ALU.mult, op1=ALU.add), reads=[asb, r, a], writes=[d])
                        jk = jr.next()
                        S.op("dve", lambda be: be.scalar_tensor_tensor(out=jk[:], in0=d[:], scalar=1.0, in1=d[:], op0=ALU.mult, op1=ALU.mult,
                                                                       accum_out=r[:, qi:qi + 1]), reads=[d, r], writes=[jk, r])
                        ds.append(d)
                    S.op("dve", lambda be: be.tensor_scalar(r[:, 4:8], r[:, 0:4], 1.0 / 128.0, EPS, op0=ALU.mult, op1=ALU.add),
                         reads=[r], writes=[r])
                    S.op("act", lambda be: be.activation(out=r[:, 4:8], in_=r[:, 4:8], func=AF.Sqrt), reads=[r], writes=[r])
                    S.op("dve", lambda be: be.reciprocal(r[:, 4:8], r[:, 4:8]), reads=[r], writes=[r])
                    ytoks = []
                    for qi in range(4):
                        d = ds[qi]
                        S.op("dve", lambda be: be.scalar_tensor_tensor(out=d[:], in0=d[:], scalar=r[:, 4 + qi:5 + qi], in1=sg[:],
                                                                       op0=ALU.mult, op1=ALU.mult), reads=[d, r, sg], writes=[d])
                        yk = yts[qi].next()
                        S.op("dve", lambda be: be.tensor_tensor(yk[:], d[:], zt[:, qi, :], op=ALU.mult), reads=[d, zt], writes=[yk])
                        ytoks.append(yk)
                    emit_yT(S, ytoks, c.yT[2], h * 128, 1, tsl, tc, idt, ptr2, ysbr)
                pending.append(epilogue)
        while pending:
            pending.pop(0)()


def phase_ml(S, c, l, L):
    NCH = L // 128
    w = c.w
    mlg = c.mlg
    with Scope(S) as st:
        Bcol = sb(S, st, "ml_Bcol", [128, NCH, 4], F32)
        Ecol = sb(S, st, "ml_Ecol", [128, NCH, 4], F32)
        mu = sb(S, st, "ml_mu", [128, 4, NCH + 1], F32)
        negmu = sb(S, st, "ml_nmu", [128, 4, NCH + 1], F32)
        dec = sb(S, st, "ml_dec", [128, 4, NCH], F32)
        with Scope(S) as g:
            ig = sb(S, g, "mlg_i", [4, L], F32)
            fg = sb(S, g, "mlg_f", [4, L], F32)
            Ft = sb(S, g, "mlg_F", [4, L], F32)
            Gt = sb(S, g, "mlg_G", [4, L], F32)
            ones = sb(S, g, "mlg_1", [4, L], F32)
            bb = sb(S, g, "mlg_b", [4, 4], F32)
            S.dma("sp", ig[:], c.mlif.ap[0:4, :], reads=[c.mlif.o(tc) for tc in range(L // 512)], writes=[ig])
            S.dma("sp", fg[:], c.mlif.ap[4:8, :], reads=[c.mlif.o(tc) for tc in range(L // 512)], writes=[fg])
            S.dma("sp", bb[:, 0:1], w["ml_b_i"].ap[l].rearrange("(h o) -> h o", o=1), writes=[bb], slow=True)
            S.dma("sp", bb[:, 1:2], w["ml_b_f"].ap[l].rearrange("(h o) -> h o", o=1), writes=[bb], slow=True)
            S.op("dve", lambda be: be.tensor_scalar(bb[:, 2:3], bb[:, 1:2], -1.0, None, op0=ALU.mult), reads=[bb], writes=[bb])
            S.op("pool", lambda be: be.memset(ones[:], 1.0), writes=[ones])
            S.op("pool", lambda be: be.memset(bb[:, 3:4], 0.0), reads=[bb], writes=[bb])
            S.op("act", lambda be: be.activation(out=fg[:], in_=fg[:], func=AF.Exp, scale=-1.0, bias=bb[:, 2:3]),
                 reads=[fg, bb], writes=[fg])
            S.op("act", lambda be: be.activation(out=fg[:], in_=fg[:], func=AF.Ln, bias=1.0), reads=[fg], writes=[fg])
            S.op("dve", lambda be: be.tensor_scalar(fg[:], fg[:], -1.0, None, op0=ALU.mult), reads=[fg], writes=[fg])
            S.op("dve", lambda be: be.tensor_tensor_scan(out=Ft[:], data0=ones[:], data1=fg[:], initial=0.0,
                                                         op0=ALU.mult, op1=ALU.add), reads=[ones, fg], writes=[Ft])
            S.op("dve", lambda be: be.scalar_tensor_tensor(out=ig[:], in0=ig[:], scalar=bb[:, 0:1], in1=Ft[:],
                                                           op0=ALU.add, op1=ALU.subtract), reads=[ig, bb, Ft], writes=[ig])
            S.op("dve", lambda be: be.tensor_tensor_scan(out=Gt[:], data0=ones[:], data1=ig[:], initial=0.0,
                                                         op0=ALU.mult, op1=ALU.max), reads=[ones, ig], writes=[Gt])
            S.op("dve", lambda be: be.tensor_tensor(fg[:], Ft[:], Gt[:], op=ALU.add), reads=[Ft, Gt, fg], writes=[fg])
            S.op("act", lambda be: be.activation(out=fg[:], in_=fg[:], func=AF.Exp, scale=-1.0), reads=[fg], writes=[fg])
            S.dma("sp", mlg.ap[0, :, 0:L], ig[:], reads=[ig], writes=[mlg.o(0)])
            S.dma("sp", mlg.ap[1, :, 0:L], fg[:], reads=[fg], writes=[mlg.o(1)])
            S.dma("sp", mlg.ap[2, :, 1:L + 1], Gt[:], reads=[Gt], writes=[mlg.o(2)])
            S.dma("sp", mlg.ap[2, :, 0:1], bb[:, 3:4], reads=[bb], writes=[mlg.o(3)], slow=True)
            for h in range(4):
                S.dma("sp", Bcol[:, :, h], mlg.ap[0, h, 0:L].rearrange("(c t) -> t c", t=128), reads=[mlg.o(0)], writes=[Bcol], slow=True)
                S.dma("sp", Ecol[:, :, h], mlg.ap[1, h, 0:L].rearrange("(c t) -> t c", t=128), reads=[mlg.o(1)], writes=[Ecol], slow=True)
                S.dma("sp", mu[:, h, :], mlg.ap[2, h:h + 1, 0:L + 1:128].partition_broadcast(128),
                      reads=[mlg.o(2), mlg.o(3)], writes=[mu], slow=True)
        S.op("dve", lambda be: be.tensor_scalar(negmu[:], mu[:], -1.0, None, op0=ALU.mult), reads=[mu], writes=[negmu])
        S.op("dve", lambda be: be.tensor_tensor(dec[:], mu[:, :, 0:NCH], mu[:, :, 1:NCH + 1], op=ALU.subtract), reads=[mu], writes=[dec])
        S.op("act", lambda be: be.activation(out=dec[:], in_=dec[:], func=AF.Exp), reads=[dec], writes=[dec])

        idt = sb(S, st, "ml_id", [128, 128], BF16)
        S.dma("sp", idt[:], c.ident.ap[:, :], writes=[idt])
        mk = sb(S, st, "ml_mk", [128, 128], BF16)
        S.dma("sp", mk[:], c.maskkq.ap[:, :], writes=[mk])
        ng = sb(S, st, "ml_ng", [128, DB], F32)
        S.dma("sp", ng[:], w["ml_norm_g"].ap[l:l + 1, :].partition_broadcast(128), writes=[ng])
        hsets = []
        for par in range(2):
            hsets.append(dict(
                qT=sb(S, st, f"ml_qT{par}", [128, 2, L], BF16), kT=sb(S, st, f"ml_kT{par}", [128, 2, L], BF16),
                va=sb(S, st, f"ml_va{par}", [128, NCH, 257], BF16), ot=sb(S, st, f"ml_o{par}", [128, NCH, 256], BF16),
                zt=sb(S, st, f"ml_z{par}", [128, NCH, 256], BF16), C32=sb(S, st, f"ml_C32{par}", [128, 2, 257], F32),
                Cbf=sb(S, st, f"ml_Cbf{par}", [128, 2, 257], BF16)))
        gbr = sb_ring(S, st, "ml_gb", [128, 128], F32, 3)
        ptr_ = sb_ring(S, st, "ml_pt", [128, 128], F32, 2)
        ptmr = sb_ring(S, st, "ml_ptm", [128, 128], F32, 2)
        str_ = sb_ring(S, st, "ml_st", [128, 128], BF16, 2)
        scr = sb_ring(S, st, "ml_sc", [128, 128], F32, 2)
        qsr = sb_ring(S, st, "ml_qs", [128, 2, 128], BF16, 2)
        rr = sb_ring(S, st, "ml_r", [128, 8], F32, 3)
        hr = sb_ring(S, st, "ml_h", [128, 256], F32, 2)
        jr = sb_ring(S, st, "ml_j", [128, 256], F32, 1)
        yr = sb_ring(S, st, "ml_y", [128, 256], BF16, 2)
        ysr = sb_ring(S, st, "ml_ys", [128, 2, 128], BF16, 2)
        kwr = sb_ring(S, st, "ml_kw", [128, 2], F32, 2)
        kkr = sb_ring(S, st, "ml_kk", [128, 256], BF16, 2)
        psS = ps_ring(S, st, "ml_pS", [128, 128], F32, 2)
        psN = ps_ring(S, st, "ml_pN", [128, 257], F32, 2)
        psT = ps_ring(S, st, "ml_pT", [128, 2, 128], BF16, 2)
        psC = [ps(S, st, f"ml_pC{i}", [128, 257], F32) for i in range(2)]
        def load_head(h):
            hs = slice(h * 256, (h + 1) * 256)
            d_ = hsets[h % 2]
            qT, kT, va, ot, zt = d_["qT"], d_["kT"], d_["va"], d_["ot"], d_["zt"]
            S.dma("sp", qT[:], c.mlqT.ap[hs, :].rearrange("(k p) n -> p k n", p=128),
                  reads=[c.mlqT.o((2 * h + k, tc)) for k in range(2) for tc in range(L // 512)], writes=[qT])
            S.dma("sp", kT[:], c.mlkT.ap[hs, :].rearrange("(k p) n -> p k n", p=128),
                  reads=[c.mlkT.o((2 * h + k, tc)) for k in range(2) for tc in range(L // 512)], writes=[kT])
            S.dma("sp", va[:, :, 0:256], c.mlv.ap[:, hs].rearrange("(c p) d -> p c d", p=128),
                  reads=[c.mlv.o(t) for t in range(NCH)], writes=[va])
            S.op("pool", lambda be: be.memset(va[:, :, 256:257], 1.0), writes=[va])
            S.dma("sp", ot[:], c.mlo.ap[:, hs].rearrange("(c p) d -> p c d", p=128), reads=[c.mlo.o(t) for t in range(NCH)], writes=[ot])
            S.dma("sp", zt[:], c.mlz.ap[:, hs].rearrange("(c p) d -> p c d", p=128), reads=[c.mlz.o(t) for t in range(NCH)], writes=[zt])

        def step(h, ch):
            if True:
                hs = slice(h * 256, (h + 1) * 256)
                d_ = hsets[h % 2]
                qT, kT, va, ot, zt, C32, Cbf = d_["qT"], d_["kT"], d_["va"], d_["ot"], d_["zt"], d_["C32"], d_["Cbf"]
                csl = slice(ch * 128, (ch + 1) * 128)
                gb = gbr.next()
                S.dma("pool", gb[:], mlg.ap[2, h:h + 1, 1 + ch * 128:1 + (ch + 1) * 128].partition_broadcast(128),
                      reads=[mlg.o(2)], writes=[gb])
                pS = psS.next()
                S.mm([(pS[:], kT[:, db, csl], qT[:, db, csl], db == 0, db == 1) for db in range(2)], reads=[kT, qT], writes=[pS])
                pt = ptr_.next()
                S.op("act", lambda be: be.activation(out=pt[:], in_=gb[:], func=AF.Exp, scale=-1.0, bias=Bcol[:, ch, h:h + 1]),
                     reads=[gb, Bcol], writes=[pt])
                ptm = ptmr.next()
                S.op("dve", lambda be: be.tensor_tensor(ptm[:], pt[:], mk[:], op=ALU.mult), reads=[pt, mk], writes=[ptm])
                stt = str_.next()
                S.op("dve", lambda be: be.tensor_tensor(stt[:], pS[:], ptm[:], op=ALU.mult), reads=[pS, ptm], writes=[stt])
                items = [(None, stt[:], va[:, ch, :])]
                rds = [stt, va]
                if ch > 0:
                    sc = scr.next()
                    S.op("act", lambda be: be.activation(out=sc[:], in_=gb[:], func=AF.Exp, scale=-1.0, bias=mu[:, h, ch:ch + 1]),
                         reads=[gb, mu], writes=[sc])
                    qs = qsr.next()
                    for db in range(2):
                        S.op("dve", lambda be: be.tensor_tensor(qs[:, db, :], qT[:, db, csl], sc[:], op=ALU.mult),
                             reads=[qT, sc], writes=[qs])
                    items += [(None, qs[:, 0, :], Cbf[:, 0, :]), (None, qs[:, 1, :], Cbf[:, 1, :])]
                    rds += [qs, Cbf]
                pN = psN.next()
                n = len(items)
                S.mm([(pN[:], a, b, i == 0, i == n - 1) for i, (_, a, b) in enumerate(items)], reads=rds, writes=[pN])
                r = rr.next()
                S.op("act", lambda be: be.activation(out=r[:, 6:7], in_=pN[:, 256:257], func=AF.Abs), reads=[pN], writes=[r])
                S.op("dve", lambda be: be.tensor_tensor(r[:, 0:1], r[:, 6:7], Ecol[:, ch, h:h + 1], op=ALU.max),
                     reads=[r, Ecol], writes=[r])
                S.op("dve", lambda be: be.reciprocal(r[:, 1:2], r[:, 0:1]), reads=[r], writes=[r])
                hh = hr.next()
                S.op("act", lambda be: be.activation(out=hh[:], in_=pN[:, 0:256], func=AF.Copy, scale=r[:, 1:2]),
                     reads=[pN, r], writes=[hh])
                jk = jr.next()
                S.op("act", lambda be: be.activation(out=jk[:], in_=hh[:], func=AF.Square, accum_out=r[:, 2:3]),
                     reads=[hh, r], writes=[jk, r])
                S.op("dve", lambda be: be.tensor_scalar(r[:, 3:4], r[:, 2:3], 1.0 / 256.0, EPS, op0=ALU.mult, op1=ALU.add),
                     reads=[r], writes=[r])
                S.op("act", lambda be: be.activation(out=r[:, 4:5], in_=r[:, 3:4], func=AF.Sqrt), reads=[r], writes=[r])
                S.op("dve", lambda be: be.reciprocal(r[:, 5:6], r[:, 4:5]), reads=[r], writes=[r])
                S.op("dve", lambda be: be.scalar_tensor_tensor(out=hh[:], in0=hh[:], scalar=r[:, 5:6], in1=ng[:, hs],
                                                               op0=ALU.mult, op1=ALU.mult), reads=[hh, r, ng], writes=[hh])
                S.op("dve", lambda be: be.tensor_tensor(hh[:], hh[:], ot[:, ch, :], op=ALU.mult), reads=[hh, ot], writes=[hh])
                yk = yr.next()
                S.op("dve", lambda be: be.tensor_tensor(yk[:], hh[:], zt[:, ch, :], op=ALU.mult), reads=[hh, zt], writes=[yk])
                pT = psT.next()
                for db in range(2):
                    S.transpose(pT[:, db, :], yk[:, db * 128:(db + 1) * 128], idt[:], reads=[yk, idt], writes=[pT])
                ys = ysr.next()
                S.op("act", lambda be: be.copy(out=ys[:], in_=pT[:]), reads=[pT], writes=[ys])
                S.dma("sp", c.yT[1].ap[hs, csl].rearrange("(k p) n -> p k n", p=128), ys[:], reads=[ys],
                      writes=[c.yT[1].o((2 * h + k, ch // 4)) for k in range(2)])
                if ch == NCH - 1:
                    return
                kw = kwr.next()
                S.op("act", lambda be: be.activation(out=kw[:, 0:1], in_=Bcol[:, ch, h:h + 1], func=AF.Exp,
                                                     bias=negmu[:, h, ch + 1:ch + 2]), reads=[Bcol, negmu], writes=[kw])
                pK = psT.next()
                for db in range(2):
                    S.transpose(pK[:, db, :], kT[:, db, csl], idt[:], reads=[kT, idt], writes=[pK])
                kk = kkr.next()
                S.op("dve", lambda be: be.tensor_scalar(kk[:], pK[:].rearrange("p a b -> p (a b)"), kw[:, 0:1], None, op0=ALU.mult),
                     reads=[pK, kw], writes=[kk])
                for db in range(2):
                    S.mm([(psC[db][:], kk[:, db * 128:(db + 1) * 128], va[:, ch, :], True, True)], reads=[kk, va], writes=[psC[db]])
                    if ch == 0:
                        S.op("dve", lambda be: be.tensor_copy(C32[:, db, :], psC[db][:]), reads=[psC[db]], writes=[C32])
                    else:
                        S.op("dve", lambda be: be.scalar_tensor_tensor(out=C32[:, db, :], in0=C32[:, db, :], scalar=dec[:, h, ch:ch + 1],
                                                                       in1=psC[db][:], op0=ALU.mult, op1=ALU.add),
                             reads=[C32, dec, psC[db]], writes=[C32])
                S.op("act", lambda be: be.copy(out=Cbf[:], in_=C32[:]), reads=[C32], writes=[Cbf])

        for hp in (0, 2):
            load_head(hp)
            load_head(hp + 1)
            for ch in range(NCH):
                step(hp, ch)
                step(hp + 1, ch)


TWO_PI = 2.0 * math.pi


def _sincos(S, out_t, ang_src, th, off, kt, scr=None):
    y = out_t if scr is None else scr
    if isinstance(th, float):
        S.op("dve", lambda be: be.tensor_scalar(y[0], ang_src[0], th, off, op0=ALU.mult, op1=ALU.add),
             reads=ang_src[1], writes=[y[1]])
    else:
        S.op("act", lambda be: be.activation(out=y[0], in_=ang_src[0], func=AF.Identity, scale=th, bias=off),
             reads=ang_src[1], writes=[y[1]])
    S.op("dve", lambda be: be.tensor_copy(kt[0], y[0]), reads=[y[1]], writes=[kt[1]])
    S.op("dve", lambda be: be.tensor_tensor(y[0], y[0], kt[0], op=ALU.subtract), reads=[y[1], kt[1]], writes=[y[1]])
    S.op("act", lambda be: be.activation(out=out_t[0], in_=y[0], func=AF.Sin, scale=TWO_PI * (1.0 - 1e-6)),
         reads=[y[1]], writes=[out_t[1]])


def phase_s5(S, c, l, L):
    w = c.w
    SEG = min(L, 1024)
    NSEG = L // SEG
    NCS = SEG // 512
    OFF_S = 0.0
    OFF_C = 0.25
    with Scope(S) as st:
        BBpad = sb(S, st, "s5_BB", [128, NG, 128], BF16)
        BBsw = sb(S, st, "s5_BBs", [128, NG, 128], BF16)
        CCpad = sb(S, st, "s5_CC", [128, NG, 128], BF16)
        r2 = sb(S, st, "s5_r2", [128, NG], F32)
        th = sb(S, st, "s5_th", [128, NG], F32)
        offs = sb(S, st, "s5_offs", [128, 2], F32)
        dsk = sb(S, st, "s5_dsk", [128, 8], F32)
        bgl = sb(S, st, "s5_bg", [128, 8], F32)
        S.dma("sp", dsk[:], w["s5_d"].ap[l].rearrange("(b p) -> p b", p=128), writes=[dsk], slow=True)
        S.dma("sp", bgl[:], w["s5_b_glu"].ap[l].rearrange("(b p) -> p b", p=128), writes=[bgl], slow=True)
        S.op("pool", lambda be: be.memset(offs[0:64, 0:1], OFF_S), writes=[offs])
        S.op("pool", lambda be: be.memset(offs[64:128, 0:1], OFF_S + 0.5), reads=[offs], writes=[offs])
        S.op("pool", lambda be: be.memset(offs[:, 1:2], OFF_C), reads=[offs], writes=[offs])
        with Scope(S) as pp:
            def t3(name):
                return sb(S, pp, name, [128, 8, 64], F32)
            lre, lim, dt, er, cs, sn, wr, wi, t1, t2, Br, Bi, Bbr, Bbi = [t3(f"s5p{i}") for i in range(14)]
            mg = sb(S, pp, "s5_mg", [128, 8], F32)
            dt8 = sb(S, pp, "s5_dt8", [128, 8], F32)
            m2 = sb(S, pp, "s5_m2", [128, 8, 128], F32)
            S.dma("sp", mg[:], c.maskg.ap[:, :], writes=[mg])
            S.dma("sp", m2[:], c.mask2.ap[:, :, :], writes=[m2])
            hre, him, hdt = w["s5_lam_re"].h, w["s5_lam_im"].h, w["s5_log_dt"].h
            for g8 in range(8):
                ps_ = slice(g8 * 16, (g8 + 1) * 16)
                S.dma("sp", lre[ps_, :, :], bass.AP(tensor=hre, offset=l * 4096 + g8 * 64, ap=[[0, 16], [512, 8], [1, 64]]), writes=[lre], slow=True)
                S.dma("sp", lim[ps_, :, :], bass.AP(tensor=him, offset=l * 4096 + g8 * 64, ap=[[0, 16], [512, 8], [1, 64]]), writes=[lim], slow=True)
                S.dma("sp", dt8[ps_, :], bass.AP(tensor=hdt, offset=l * 64 + g8, ap=[[0, 16], [8, 8]]), writes=[dt8], slow=True)
                for blk in range(8):
                    S.dma("sp", Br[ps_, blk, :], w["s5_b_re"].ap[l, blk * 8 + g8].rearrange("p c -> c p"), writes=[Br], slow=True)
                    S.dma("sp", Bi[ps_, blk, :], w["s5_b_im"].ap[l, blk * 8 + g8].rearrange("p c -> c p"), writes=[Bi], slow=True)

            def V(e, fn, rd, wr_):
                S.op(e, fn, reads=rd, writes=wr_)
            V("dve", lambda be: be.tensor_scalar(lre[:], lre[:], -1e-4, None, op0=ALU.min), [lre], [lre])
            V("act", lambda be: be.activation(out=dt8[:], in_=dt8[:], func=AF.Exp), [dt8], [dt8])
            V("dve", lambda be: be.tensor_copy(dt[:], dt8[:].unsqueeze(2).to_broadcast([128, 8, 64])), [dt8], [dt])
            V("dve", lambda be: be.tensor_tensor(t1[:], lre[:], dt[:], op=ALU.mult), [lre, dt], [t1])
            V("act", lambda be: be.activation(out=er[:], in_=t1[:], func=AF.Exp), [t1], [er])
            V("dve", lambda be: be.tensor_tensor(t2[:], lim[:], dt[:], op=ALU.mult), [lim, dt], [t2])
            kA = sb(S, pp, "s5_kA", [128, 8, 64], mybir.dt.int32)
            _sincos(S, (cs[:], cs), (t2[:], [t2]), 1.0 / TWO_PI, OFF_C, (kA[:], kA))
            _sincos(S, (sn[:], sn), (t2[:], [t2]), 1.0 / TWO_PI, OFF_S, (kA[:], kA))
            V("dve", lambda be: be.tensor_tensor(cs[:], cs[:], er[:], op=ALU.mult), [cs, er], [cs])
            V("dve", lambda be: be.tensor_tensor(sn[:], sn[:], er[:], op=ALU.mult), [sn, er], [sn])
            V("dve", lambda be: be.tensor_scalar(cs[:], cs[:], -1.0, None, op0=ALU.add), [cs], [cs])
            V("dve", lambda be: be.tensor_tensor(t1[:], lre[:], lre[:], op=ALU.mult), [lre], [t1])
            V("dve", lambda be: be.tensor_tensor(t2[:], lim[:], lim[:], op=ALU.mult), [lim], [t2])
            V("dve", lambda be: be.tensor_tensor(t1[:], t1[:], t2[:], op=ALU.add), [t1, t2], [t1])
            V("dve", lambda be: be.reciprocal(t1[:], t1[:]), [t1], [t1])
            V("dve", lambda be: be.tensor_tensor(wr[:], cs[:], lre[:], op=ALU.mult), [cs, lre], [wr])
            V("dve", lambda be: be.tensor_tensor(t2[:], sn[:], lim[:], op=ALU.mult), [sn, lim], [t2])
            V("dve", lambda be: be.tensor_tensor(wr[:], wr[:], t2[:], op=ALU.add), [wr, t2], [wr])
            V("dve", lambda be: be.tensor_tensor(wr[:], wr[:], t1[:], op=ALU.mult), [wr, t1], [wr])
            V("dve", lambda be: be.tensor_tensor(wi[:], sn[:], lre[:], op=ALU.mult), [sn, lre], [wi])
            V("dve", lambda be: be.tensor_tensor(t2[:], cs[:], lim[:], op=ALU.mult), [cs, lim], [t2])
            V("dve", lambda be: be.tensor_tensor(wi[:], wi[:], t2[:], op=ALU.subtract), [wi, t2], [wi])
            V("dve", lambda be: be.tensor_tensor(wi[:], wi[:], t1[:], op=ALU.mult), [wi, t1], [wi])
            V("dve", lambda be: be.tensor_tensor(Bbr[:], wr[:], Br[:], op=ALU.mult), [wr, Br], [Bbr])
            V("dve", lambda be: be.tensor_tensor(t2[:], wi[:], Bi[:], op=ALU.mult), [wi, Bi], [t2])
            V("dve", lambda be: be.tensor_tensor(Bbr[:], Bbr[:], t2[:], op=ALU.subtract), [Bbr, t2], [Bbr])
            V("dve", lambda be: be.tensor_tensor(Bbi[:], wr[:], Bi[:], op=ALU.mult), [wr, Bi], [Bbi])
            V("dve", lambda be: be.tensor_tensor(t2[:], wi[:], Br[:], op=ALU.mult), [wi, Br], [t2])
            V("dve", lambda be: be.tensor_tensor(Bbi[:], Bbi[:], t2[:], op=ALU.add), [Bbi, t2], [Bbi])
            mgb = mg[:].unsqueeze(1).unsqueeze(3).to_broadcast([128, 8, 8, 64])
            for dst, lo, hi in ((BBpad, Bbr, Bbi), (BBsw, Bbi, Bbr)):
                for half, src in ((0, lo), (1, hi)):
                    dv = dst[:, :, half * 64:(half + 1) * 64].rearrange("p (blk g) q -> p blk g q", g=8)
                    sv = src[:].unsqueeze(2).to_broadcast([128, 8, 8, 64])
                    V("dve", lambda be: be.tensor_tensor(dv, sv, mgb, op=ALU.mult), [src, mg], [dst])
            lb = sb(S, pp, "s5_lb", [128, NG], F32)
            S.dma("sp", lb[0:64, :], w["s5_lam_re"].ap[l].rearrange("g p -> p g"), writes=[lb], slow=True)
            S.dma("sp", lb[64:128, :], w["s5_lam_re"].ap[l].rearrange("g p -> p g"), writes=[lb], slow=True)
            S.dma("sp", th[0:64, :], w["s5_lam_im"].ap[l].rearrange("g p -> p g"), writes=[th], slow=True)
            S.dma("sp", th[64:128, :], w["s5_lam_im"].ap[l].rearrange("g p -> p g"), writes=[th], slow=True)
            dtb = sb(S, pp, "s5_dtb", [128, NG], F32)
            S.dma("sp", dtb[:], w["s5_log_dt"].ap[l:l + 1, :].partition_broadcast(128), writes=[dtb])
            V("act", lambda be: be.activation(out=dtb[:], in_=dtb[:], func=AF.Exp), [dtb], [dtb])
            V("dve", lambda be: be.tensor_scalar(lb[:], lb[:], -1e-4, None, op0=ALU.min), [lb], [lb])
            V("dve", lambda be: be.tensor_tensor(lb[:], lb[:], dtb[:], op=ALU.mult), [lb, dtb], [lb])
            V("act", lambda be: be.activation(out=r2[:], in_=lb[:], func=AF.Exp), [lb], [r2])
            V("dve", lambda be: be.tensor_tensor(th[:], th[:], dtb[:], op=ALU.mult), [th, dtb], [th])
            kB = sb(S, pp, "s5_kB", [128, NG], mybir.dt.int32)
            V("dve", lambda be: be.tensor_scalar(th[:], th[:], 1.0 / TWO_PI, None, op0=ALU.mult), [th], [th])
            V("dve", lambda be: be.tensor_copy(kB[:], th[:]), [th], [kB])
            V("dve", lambda be: be.tensor_tensor(th[:], th[:], kB[:], op=ALU.subtract), [th, kB], [th])
            Cc = sb(S, pp, "s5_Cc", [128, 8, 128], F32)
            S.dma("sp", Cc[:, :, 0:64], w["s5_c_re"].ap[l].rearrange("(blk g8) co p -> (g8 co) blk p", g8=8), writes=[Cc])
            S.dma("sp", Cc[:, :, 64:128], w["s5_c_im"].ap[l].rearrange("(blk g8) co p -> (g8 co) blk p", g8=8), writes=[Cc])
            V("dve", lambda be: be.tensor_scalar(Cc[:, :, 64:128], Cc[:, :, 64:128], -1.0, None, op0=ALU.mult), [Cc], [Cc])
            idf = sb(S, pp, "s5_idf", [128, 128], F32)
            S.dma("sp", idf[:], c.identf.ap[:, :], writes=[idf])
            pcr = ps_ring(S, pp, "s5_pc", [128, 128], F32, 2)
            for blk in range(8):
                pc = pcr.next()
                S.transpose(pc[:], Cc[:, blk, :], idf[:], reads=[Cc, idf], writes=[pc])
                V("dve", lambda be: be.tensor_tensor(CCpad[:, blk * 8:(blk + 1) * 8, :], pc[:].unsqueeze(1).to_broadcast([128, 8, 128]),
                                                     m2[:], op=ALU.mult), [pc, m2], [CCpad])

        with Scope(S) as ms:
            trs = []
            for sg in range(NSEG):
                tr_ = sb(S, ms, f"s5_tr{sg}", [128, SEG], F32)
                S.dma("sp", tr_[:], c.trow.ap[0:1, sg * SEG:(sg + 1) * SEG].partition_broadcast(128), writes=[tr_])
                trs.append(tr_)

            def rg(name, n, dt_=F32):
                return sb_ring(S, ms, name, [128, SEG], dt_, n)
            COSr, SINr = rg("s5mC", 4, BF16), rg("s5mS", 4, BF16)
            Vr, VSr = rg("s5mV", 3, BF16), rg("s5mVS", 3, BF16)
            T2r, T3r = rg("s5mT2", 2, BF16), rg("s5mT3", 2, BF16)
            BUr, BSr = rg("s5mBU", 2, BF16), rg("s5mBS", 2, BF16)
            yfr = rg("s5_yf", 2)
            Sgr = rg("s5_sg", 2, BF16)
            kir = rg("s5_ki", 2, mybir.dt.int32)
            ur = sb_ring(S, ms, "s5_u", [128, L], BF16, 2)
            car = sb(S, ms, "s5_car", [128, 8, 2], F32)
            yvr = sb_ring(S, ms, "s5_yv", [128, 512], F32, 2)
            tgr = sb_ring(S, ms, "s5_tg", [128, 512], F32, 2)
            ygr = sb_ring(S, ms, "s5_yg", [128, 512], BF16, 2)
            pbu = ps_ring(S, ms, "s5_pb", [128, 512], F32, 2)
            pbs = ps_ring(S, ms, "s5_pbs", [128, 512], F32, 2)
            pyr = ps_ring(S, ms, "s5_py", [128, 512], F32, 4)
            items = [(blk, sg, g8) for blk in range(8) for sg in range(NSEG) for g8 in range(8)]
            uts, pysd, stt_ = {}, {}, {}

            def get_ut(blk):
                if blk not in uts:
                    ut = ur.next()
                    S.dma("sp", ut[:], c.s5uT.ap[blk * 128:(blk + 1) * 128, :],
                          reads=[c.s5uT.o((blk, tc)) for tc in range(L // 512)], writes=[ut])
                    uts[blk] = ut
                return uts[blk]

            def stageA(i):
                blk, sg, g8 = items[i]
                g = blk * 8 + g8
                COS, SIN = COSr.next(), SINr.next()
                kt_, yf = kir.next(), yfr.next()
                _sincos(S, (COS[:], COS), (trs[sg][:], [trs[sg], th, offs]), th[:, g:g + 1], offs[:, 1:2], (kt_[:], kt_), (yf[:], yf))
                kt_, yf = kir.next(), yfr.next()
                _sincos(S, (SIN[:], SIN), (trs[sg][:], [trs[sg], th, offs]), th[:, g:g + 1], offs[:, 0:1], (kt_[:], kt_), (yf[:], yf))
                stt_[i] = dict(COS=COS, SIN=SIN)

            def stageB1(i):
                blk, sg, g8 = items[i]
                g = blk * 8 + g8
                ut = get_ut(blk)
                d = stt_[i]
                COS, SIN = d["COS"], d["SIN"]
                Vt, VS, T2, T3 = Vr.next(), VSr.next(), T2r.next(), T3r.next()
                for cs_ in range(NCS):
                    fsl = slice(cs_ * 512, (cs_ + 1) * 512)
                    tsl = slice(sg * SEG + cs_ * 512, sg * SEG + (cs_ + 1) * 512)
                    p1, p2 = pbu.next(), pbs.next()
                    S.mm([(p1[:], BBpad[:, g, :], ut[:, tsl], True, True)], reads=[BBpad, ut], writes=[p1])
                    S.mm([(p2[:], BBsw[:, g, :], ut[:, tsl], True, True)], reads=[BBsw, ut], writes=[p2])
                    S.op("dve", lambda be: be.tensor_tensor(Vt[:, fsl], COS[:, fsl], p1[:], op=ALU.mult), reads=[COS, p1], writes=[Vt])
                    S.op("dve", lambda be: be.tensor_tensor(T2[:, fsl], SIN[:, fsl], p2[:], op=ALU.mult), reads=[SIN, p2], writes=[T2])
                    S.op("dve", lambda be: be.tensor_tensor(VS[:, fsl], COS[:, fsl], p2[:], op=ALU.mult), reads=[COS, p2], writes=[VS])
                    S.op("dve", lambda be: be.tensor_tensor(T3[:, fsl], SIN[:, fsl], p1[:], op=ALU.mult), reads=[SIN, p1], writes=[T3])
                S.op("dve", lambda be: be.tensor_tensor(Vt[:], Vt[:], T2[:], op=ALU.add), reads=[Vt, T2], writes=[Vt])
                S.op("dve", lambda be: be.tensor_tensor(VS[:], VS[:], T3[:], op=ALU.subtract), reads=[VS, T3], writes=[VS])
                d.update(Vt=Vt, VS=VS)

            def stageB2(i):
                blk, sg, g8 = items[i]
                g = blk * 8 + g8
                d = stt_[i]
                Vt, VS = d["Vt"], d["VS"]
                BU, BS = BUr.next(), BSr.next()
                dec_ = r2[:, g:g + 1].to_broadcast([128, SEG])
                i0 = 0.0 if sg == 0 else car[:, g8, 0:1]
                i1 = 0.0 if sg == 0 else car[:, g8, 1:2]
                S.op("dve", lambda be: be.tensor_tensor_scan(out=BU[:], data0=dec_, data1=Vt[:], initial=i0, op0=ALU.mult, op1=ALU.add),
                     reads=[r2, Vt, car], writes=[BU])
                S.op("dve", lambda be: be.tensor_tensor_scan(out=BS[:], data0=dec_, data1=VS[:], initial=i1, op0=ALU.mult, op1=ALU.add),
                     reads=[r2, VS, car], writes=[BS])
                if sg < NSEG - 1:
                    S.op("act", lambda be: be.copy(out=car[:, g8, 0:1], in_=BU[:, SEG - 1:SEG]), reads=[BU, car], writes=[car])
                    S.op("act", lambda be: be.copy(out=car[:, g8, 1:2], in_=BS[:, SEG - 1:SEG]), reads=[BS, car], writes=[car])
                d.update(BU=BU, BS=BS)

            def stageC(i):
                blk, sg, g8 = items[i]
                g = blk * 8 + g8
                d = stt_.pop(i)
                COS, SIN, Vt, VS, BU, BS = d["COS"], d["SIN"], d["Vt"], d["VS"], d["BU"], d["BS"]
                Sg = Sgr.next()
                S.op("dve", lambda be: be.tensor_tensor(Vt[:], COS[:], BU[:], op=ALU.mult), reads=[COS, BU], writes=[Vt])
                S.op("dve", lambda be: be.tensor_tensor(VS[:], SIN[:], BS[:], op=ALU.mult), reads=[SIN, BS], writes=[VS])
                S.op("dve", lambda be: be.tensor_tensor(Sg[:], Vt[:], VS[:], op=ALU.subtract), reads=[Vt, VS], writes=[Sg])
                if (blk, sg) not in pysd:
                    pysd[(blk, sg)] = [pyr.next() for _ in range(NCS)]
                pys = pysd[(blk, sg)]
                for cs_ in range(NCS):
                    fsl = slice(cs_ * 512, (cs_ + 1) * 512)
                    S.mm([(pys[cs_][:], CCpad[:, g, :], Sg[:, fsl], g8 == 0, g8 == 7)], reads=[CCpad, Sg], writes=[pys[cs_]])
                if g8 != 7:
                    return
                ut = uts[blk]
                for cs_ in range(NCS):
                    tsl = slice(sg * SEG + cs_ * 512, sg * SEG + (cs_ + 1) * 512)
                    py = pys[cs_]
                    yv, tg, yg = yvr.next(), tgr.next(), ygr.next()
                    S.op("dve", lambda be: be.scalar_tensor_tensor(out=yv[:], in0=ut[:, tsl], scalar=dsk[:, blk:blk + 1], in1=py[:],
                                                                   op0=ALU.mult, op1=ALU.add), reads=[ut, dsk, py], writes=[yv])
                    S.op("dve", lambda be: be.tensor_tensor(tg[:], yv[:], yv[:], op=ALU.mult), reads=[yv], writes=[tg])
                    S.op("dve", lambda be: be.tensor_scalar(tg[:], tg[:], 0.044715, 1.0, op0=ALU.mult, op1=ALU.add), reads=[tg], writes=[tg])
                    S.op("dve", lambda be: be.tensor_tensor(tg[:], tg[:], yv[:], op=ALU.mult), reads=[tg, yv], writes=[tg])
                    S.op("act", lambda be: be.activation(out=tg[:], in_=tg[:], func=AF.Sigmoid, scale=2.0 * math.sqrt(2.0 / math.pi)),
                         reads=[tg], writes=[tg])
                    S.op("dve", lambda be: be.tensor_tensor(yg[:], yv[:], tg[:], op=ALU.mult), reads=[yv, tg], writes=[yg])
                    S.dma("sp", c.s5yT.ap[blk * 128:(blk + 1) * 128, tsl], yg[:], reads=[yg], writes=[c.s5yT.o((blk, tsl.start // 512))])

            n_it = len(items)
            for step in range(n_it + 3):
                if step < n_it:
                    stageA(step)
                if 0 <= step - 1 < n_it:
                    stageB1(step - 1)
                if 0 <= step - 3 < n_it:
                    stageC(step - 3)
                if 0 <= step - 2 < n_it:
                    stageB2(step - 2)

        with Scope(S) as gs:
            wg = sb(S, gs, "s5_wg", [128, 8, DB], BF16)
            for q in range(2):
                S.dma("pool", wg[:, :, q * 512:(q + 1) * 512],
                      w["s5_w_glu"].ap[l, :, q * 512:(q + 1) * 512].rearrange("(k p) n -> p k n", p=128), writes=[wg])
            ygr2 = sb_ring(S, gs, "s5_y2", [128, 8, 512], BF16, 2)
            zr2 = sb_ring(S, gs, "s5_z2", [128, 8, 512], BF16, 2)
            sgr = sb_ring(S, gs, "s5_sgm", [128, 512], F32, 2)
            outr = sb_ring(S, gs, "s5_o2", [128, 8, 512], BF16, 2)
            pgr = ps_ring(S, gs, "s5_pg", [128, 512], F32, 4)
            for tc in range(L // 512):
                tsl = slice(tc * 512, (tc + 1) * 512)
                yt, zt, ob = ygr2.next(), zr2.next(), outr.next()
                S.dma("sp", yt[:], c.s5yT.ap[:, tsl].rearrange("(k p) n -> p k n", p=128), reads=[c.s5yT.o((k, tc)) for k in range(8)], writes=[yt])
                S.dma("sp", zt[:], c.s5zT.ap[:, tsl].rearrange("(k p) n -> p k n", p=128), reads=[c.s5zT.o((k, tc)) for k in range(8)], writes=[zt])
                for j in range(8):
                    pg = pgr.next()
                    S.mm([(pg[:], wg[:, kc, j * 128:(j + 1) * 128], yt[:, kc, :], kc == 0, kc == 7) for kc in range(8)],
                         reads=[wg, yt], writes=[pg])
                    sg_ = sgr.next()
                    S.op("act", lambda be: be.activation(out=sg_[:], in_=pg[:], func=AF.Sigmoid, bias=bgl[:, j:j + 1]),
                         reads=[pg, bgl], writes=[sg_])
                    S.op("dve", lambda be: be.tensor_tensor(sg_[:], sg_[:], yt[:, j, :], op=ALU.mult), reads=[sg_, yt], writes=[sg_])
                    S.op("dve", lambda be: be.tensor_tensor(ob[:, j, :], sg_[:], zt[:, j, :], op=ALU.mult), reads=[sg_, zt], writes=[ob])
                S.dma("sp", c.yT[0].ap[:, tsl].rearrange("(k p) n -> p k n", p=128), ob[:], reads=[ob],
                      writes=[c.yT[0].o((k, tc)) for k in range(8)])


def build(L=4096, depth=4, debug=False, upto="all"):
    nc = bass.Bass("TRN2", target_bir_lowering=False)
    c = declare(nc, L, depth, debug)
    with ExitStack() as top:
        S = Sched(nc, top)
        for l in range(depth):
            xin = c.x if l == 0 else c.xs[(l - 1) % 2]
            xout = c.out if l == depth - 1 else c.xs[l % 2]
            with Scope(S) as st:
                hT = sb(S, st, "hT", [128, 16, L], BF16)
                hobjs = chunk_objs(S, st, L // 512)
                phase_norm_T(S, c, xin, c.w["g_pre"].ap[l:l + 1, :], hT, L, hobjs=hobjs)
                if "inproj" in upto or upto == "all":
                    phase_inproj(S, c, l, hT, L, hobjs)
            if "s5" in upto or upto == "all":
                phase_s5(S, c, l, L)
            if "ml" in upto or upto == "all":
                phase_ml(S, c, l, L)
            if "da" in upto or upto == "all":
                phase_da(S, c, l, L)
            if "xa" in upto or upto == "all":
                phase_xa(S, c, l, L)
            if "merge" in upto or upto == "all":
                phase_merge(S, c, l, L, None)
                phase_out(S, c, l, L, xin, xout, None)
        outs = [o for o in c.out.objs.values()]
        if debug:
            for d in [c.s5uT, c.s5zT, c.mlqT, c.mlkT, c.mlv, c.mlo, c.mlz, c.mlif, c.daqT, c.dakT, c.dav, c.daz,
                      c.xaqT, c.xaz, c.gT, c.mT] + c.yT + c.xs:
                outs += list(d.objs.values())
        S.final_wait("sp", outs)
        print("instructions:", S.n_inst)
    return nc, c


def make_consts(L):
    bf = ml_dtypes.bfloat16
    inv = 1.0 / (10000.0 ** (np.arange(0, 64, 2, dtype=np.float32) / 64.0))
    ang = np.arange(L, dtype=np.float32)[:, None] * inv[None, :]
    cos, sin = np.cos(ang).T, np.sin(ang).T
    c64 = np.concatenate([cos, cos], 0)
    s64 = np.concatenate([-sin, sin], 0)
    kq = (np.arange(128)[None, :] >= np.arange(128)[:, None]).astype(np.float32)
    return {
        "c_ident": np.eye(128, dtype=np.float32).astype(bf),
        "c_identf": np.eye(128, dtype=np.float32),
        "c_trow": np.arange(L, dtype=np.float32)[None, :].copy(),
        "c_ropec": np.concatenate([c64, c64], 0).astype(bf),
        "c_ropes": np.concatenate([s64, s64], 0).astype(bf),
        "c_maskkq": kq.astype(bf),
        "c_maskg": (np.arange(128)[:, None] // 16 == np.arange(8)[None, :]).astype(np.float32),
        "c_mask2": np.broadcast_to((np.arange(8)[:, None] == (np.arange(128)[None, :] // 16)).astype(np.float32)[None], (128, 8, 128)).copy(),
    }


SEQ_FULL = 4096
DEPTH_FULL = 4


def kernel(**inputs):
    L, depth = SEQ_FULL, DEPTH_FULL
    nc, _ = build(L=L, depth=depth, debug=False)
    consts = make_consts(L)
    shared = {k: np.ascontiguousarray(np.asarray(v, dtype=np.float32)) for k, v in inputs.items() if k not in ("x", "mem")}
    x = np.asarray(inputs["x"], dtype=np.float32)
    mem = np.asarray(inputs["mem"], dtype=np.float32)
    in_maps = []
    for b in range(x.shape[0]):
        m = dict(shared)
        m["x"] = np.ascontiguousarray(x[b])
        m["mem"] = np.ascontiguousarray(mem[b])
        m.update(consts)
        in_maps.append(m)
    res = run_bass_kernel_spmd(nc, in_maps, core_ids=list(range(len(in_maps))))
    return np.stack([np.asarray(r["out"], dtype=np.float32) for r in res.results], axis=0)
```
